# Optimizing a Trainium2 kernel written in Bass

```python
import math
import jax
import jax.numpy as jnp
from jax import lax
import numpy as np

D_MODEL = 2048
BATCH = 8
SEQ = 2048
DEPTH = 4

N_MIXERS = 3
N_LAYERS_GDN = (DEPTH + 2) // 3
N_LAYERS_SSM = (DEPTH + 1) // 3
N_LAYERS_DSA = DEPTH // 3

DN_ALPHA = (2.0 * DEPTH) ** 0.25
DN_BETA = (8.0 * DEPTH) ** -0.25
LN_EPS = 1e-5
RMS_EPS = 1e-6
NEG_INF = -1e30

GDN_HEAD_DIM = 128
GDN_HEADS = D_MODEL // GDN_HEAD_DIM
GDN_QK = GDN_HEADS * GDN_HEAD_DIM
GDN_V = GDN_HEADS * GDN_HEAD_DIM
GDN_CONV = 4
GDN_CONV_CH = 2 * GDN_QK + GDN_V
GDN_IN = 2 * GDN_QK + 2 * GDN_V + 2 * GDN_HEADS
GDN_CHUNK = 64

SSM_WIDTH = D_MODEL
SSM_GROUP = 16
SSM_GROUPS = SSM_WIDTH // SSM_GROUP
SSM_STATE = 64

ATT_HEAD_DIM = 128
ATT_HEADS = D_MODEL // ATT_HEAD_DIM
IDX_HEADS = 16
IDX_DIM = 128
TOPK_MAX = 256
Q_BLOCK = 128
DSA_IN = ATT_HEADS * ATT_HEAD_DIM + 2 * ATT_HEAD_DIM + IDX_HEADS * IDX_DIM + IDX_DIM + IDX_HEADS

MOE_GROUPS = 4
MOE_EXPERTS_PER_GROUP = 8
MOE_EXPERTS = MOE_GROUPS * MOE_EXPERTS_PER_GROUP
MOE_TOPK = 2
MOE_FF = D_MODEL // 4
MOE_BLOCK = 256

kernel_name = 'hybrid_gdn_s5_dsa_hmoe_deepnorm'


def layer_norm(x, g, b):
    xf = x.astype(jnp.float32)
    mu = jnp.mean(xf, axis=-1, keepdims=True)
    var = jnp.mean(jnp.square(xf - mu), axis=-1, keepdims=True)
    return ((xf - mu) * lax.rsqrt(var + LN_EPS) * g.astype(jnp.float32) + b.astype(jnp.float32)).astype(x.dtype)


def l2_normalize(x):
    return x * lax.rsqrt(jnp.sum(jnp.square(x), axis=-1, keepdims=True) + RMS_EPS)


def causal_depthwise_conv(x, w):
    width, ch = w.shape
    return lax.conv_general_dilated(x, w[:, None, :].astype(x.dtype), window_strides=(1,),
                                    padding=[(width - 1, 0)],
                                    dimension_numbers=('NWC', 'WIO', 'NWC'),
                                    feature_group_count=ch)


def gated_delta_rule_chunked(q, k, v, g, beta):
    bsz, seq, heads, dk = q.shape
    dv = v.shape[-1]
    c = GDN_CHUNK
    n = seq // c

    def to_chunks(a):
        a = a.reshape((bsz, n, c, heads) + a.shape[3:])
        return jnp.moveaxis(a, 3, 1)

    q = to_chunks(q * dk ** -0.5)
    k = to_chunks(k)
    v = to_chunks(v)
    g = to_chunks(g)
    beta = to_chunks(beta)
    g_cum = jnp.cumsum(g, axis=-1)
    lower = jnp.tril(jnp.ones((c, c), dtype=bool))
    strict = jnp.tril(jnp.ones((c, c), dtype=bool), -1)
    diff = g_cum[..., :, None] - g_cum[..., None, :]
    decay = jnp.where(lower, jnp.exp(jnp.where(lower, diff, 0.0)), 0.0)
    k_beta = k * beta[..., None]
    kk = jnp.einsum('bhnid,bhnjd->bhnij', k_beta, k)
    a_mat = jnp.where(strict, kk * decay, 0.0) + jnp.eye(c, dtype=jnp.float32)
    rhs = jnp.concatenate([v * beta[..., None], k_beta * jnp.exp(g_cum)[..., None]], axis=-1)
    sol = lax.linalg.triangular_solve(a_mat, rhs, left_side=True, lower=True)
    u, w = sol[..., :dv], sol[..., dv:]
    attn = jnp.where(lower, jnp.einsum('bhnid,bhnjd->bhnij', q, k) * decay, 0.0)
    q_dec = q * jnp.exp(g_cum)[..., None]
    k_dec = k * jnp.exp(g_cum[..., -1:] - g_cum)[..., None]
    chunk_dec = jnp.exp(g_cum[..., -1])
    xs = tuple(jnp.moveaxis(t, 2, 0) for t in (u, w, attn, q_dec, k_dec, chunk_dec))

    def step(state, inp):
        u_c, w_c, a_c, qd_c, kd_c, cd_c = inp
        v_new = u_c - jnp.einsum('bhcd,bhde->bhce', w_c, state)
        o_c = jnp.einsum('bhcd,bhde->bhce', qd_c, state) + jnp.einsum('bhcj,bhje->bhce', a_c, v_new)
        state = state * cd_c[..., None, None] + jnp.einsum('bhcd,bhce->bhde', kd_c, v_new)
        return state, o_c

    state0 = jnp.zeros((bsz, heads, dk, dv), jnp.float32)
    _, o = lax.scan(step, state0, xs)
    return jnp.transpose(o, (1, 0, 3, 2, 4)).reshape(bsz, seq, heads, dv)


def gated_deltanet_mixer(x, w_in, conv_w, a_log, dt_bias, norm_g, w_out):
    bsz, seq, _ = x.shape
    proj = x @ w_in
    qkv = proj[..., :GDN_CONV_CH]
    z = proj[..., GDN_CONV_CH:GDN_CONV_CH + GDN_V]
    a = proj[..., GDN_CONV_CH + GDN_V:GDN_CONV_CH + GDN_V + GDN_HEADS]
    b = proj[..., GDN_CONV_CH + GDN_V + GDN_HEADS:]
    qkv = jax.nn.silu(causal_depthwise_conv(qkv, conv_w)).astype(jnp.float32)
    q = l2_normalize(qkv[..., :GDN_QK].reshape(bsz, seq, GDN_HEADS, GDN_HEAD_DIM))
    k = l2_normalize(qkv[..., GDN_QK:2 * GDN_QK].reshape(bsz, seq, GDN_HEADS, GDN_HEAD_DIM))
    v = qkv[..., 2 * GDN_QK:].reshape(bsz, seq, GDN_HEADS, GDN_HEAD_DIM)
    g = -jnp.exp(a_log.astype(jnp.float32)) * jax.nn.softplus(a.astype(jnp.float32) + dt_bias.astype(jnp.float32))
    beta = jax.nn.sigmoid(b.astype(jnp.float32))
    o = gated_delta_rule_chunked(q, k, v, g, beta)
    o = o * lax.rsqrt(jnp.mean(jnp.square(o), axis=-1, keepdims=True) + RMS_EPS) * norm_g.astype(jnp.float32)
    o = o * jax.nn.silu(z.astype(jnp.float32).reshape(bsz, seq, GDN_HEADS, GDN_HEAD_DIM))
    return o.reshape(bsz, seq, GDN_V).astype(x.dtype) @ w_out


def s5_mixer(x, w_in, b_re, b_im, c_re, c_im, a_re, a_im, log_dt, d_skip, w_glu, b_glu, w_out):
    bsz, seq, _ = x.shape
    u = (x @ w_in).astype(jnp.float32)
    ug = u.reshape(bsz, seq, SSM_GROUPS, SSM_GROUP)
    f32 = jnp.float32
    a_re = a_re.astype(f32)
    a_im = a_im.astype(f32)
    b_re = b_re.astype(f32)
    b_im = b_im.astype(f32)
    c_re = c_re.astype(f32)
    c_im = c_im.astype(f32)
    dt = jnp.exp(log_dt.astype(f32))[:, None]
    mag = jnp.exp(a_re * dt)
    ang = a_im * dt
    lb_re = mag * jnp.cos(ang)
    lb_im = mag * jnp.sin(ang)
    den = jnp.square(a_re) + jnp.square(a_im)
    f_re = ((lb_re - 1.0) * a_re + lb_im * a_im) / den
    f_im = (lb_im * a_re - (lb_re - 1.0) * a_im) / den
    bb_re = f_re[..., None] * b_re - f_im[..., None] * b_im
    bb_im = f_re[..., None] * b_im + f_im[..., None] * b_re

    def combine(e1, e2):
        a1r, a1i, s1r, s1i = e1
        a2r, a2i, s2r, s2i = e2
        return (a2r * a1r - a2i * a1i, a2r * a1i + a2i * a1r,
                a2r * s1r - a2i * s1i + s2r, a2r * s1i + a2i * s1r + s2i)

    def per_sequence(u_seq):
        bu_re = jnp.einsum('tgc,gpc->tgp', u_seq, bb_re)
        bu_im = jnp.einsum('tgc,gpc->tgp', u_seq, bb_im)
        ar = jnp.broadcast_to(lb_re, bu_re.shape)
        ai = jnp.broadcast_to(lb_im, bu_re.shape)
        _, _, s_re, s_im = lax.associative_scan(combine, (ar, ai, bu_re, bu_im), axis=0)
        return jnp.einsum('tgp,gcp->tgc', s_re, c_re) - jnp.einsum('tgp,gcp->tgc', s_im, c_im)

    y = lax.map(per_sequence, ug).reshape(bsz, seq, SSM_WIDTH)
    y = jax.nn.gelu(y + d_skip.astype(f32) * u).astype(x.dtype)
    y = y * jax.nn.sigmoid(y @ w_glu + b_glu)
    return y @ w_out


def dsa_mixer(x, w_in, w_out):
    bsz, seq, _ = x.shape
    proj = x @ w_in
    o0 = ATT_HEADS * ATT_HEAD_DIM
    o1 = o0 + ATT_HEAD_DIM
    o2 = o1 + ATT_HEAD_DIM
    o3 = o2 + IDX_HEADS * IDX_DIM
    o4 = o3 + IDX_DIM
    q = proj[..., :o0].reshape(bsz, seq, ATT_HEADS, ATT_HEAD_DIM)
    k = proj[..., o0:o1]
    v = proj[..., o1:o2]
    qi = proj[..., o2:o3].reshape(bsz, seq, IDX_HEADS, IDX_DIM)
    ki = proj[..., o3:o4]
    wi = proj[..., o4:]
    n_sel = min(TOPK_MAX, seq // 4)
    nb = seq // Q_BLOCK
    slopes = 2.0 ** (-8.0 * jnp.arange(1, ATT_HEADS + 1, dtype=jnp.float32) / ATT_HEADS)
    key_pos = jnp.arange(seq, dtype=jnp.int32)

    def blocks(a):
        return jnp.swapaxes(a.reshape((bsz, nb, Q_BLOCK) + a.shape[2:]), 0, 1)

    starts = jnp.arange(nb, dtype=jnp.int32) * Q_BLOCK

    def attend_block(inp):
        q_b, qi_b, wi_b, start = inp
        t_pos = start + jnp.arange(Q_BLOCK, dtype=jnp.int32)
        dots = jnp.einsum('bqhd,bsd->bqhs', qi_b, ki).astype(jnp.float32) * IDX_DIM ** -0.5
        score = jnp.einsum('bqhs,bqh->bqs', jax.nn.relu(dots), wi_b.astype(jnp.float32) * IDX_HEADS ** -0.5)
        causal = key_pos[None, :] <= t_pos[:, None]
        score = jnp.where(causal[None], score, NEG_INF)
        _, idx = lax.top_k(score, n_sel)
        k_sel = jax.vmap(lambda kk, ii: kk[ii])(k, idx)
        v_sel = jax.vmap(lambda vv, ii: vv[ii])(v, idx)
        logits = jnp.einsum('bqhd,bqkd->bhqk', q_b, k_sel).astype(jnp.float32) * ATT_HEAD_DIM ** -0.5
        dist = (t_pos[None, :, None] - idx).astype(jnp.float32)
        logits = logits - slopes[None, :, None, None] * dist[:, None]
        valid = (idx <= t_pos[None, :, None])[:, None]
        p = jax.nn.softmax(jnp.where(valid, logits, NEG_INF), axis=-1)
        return jnp.einsum('bhqk,bqkd->bqhd', p.astype(v_sel.dtype), v_sel)

    o = lax.map(attend_block, (blocks(q), blocks(qi), blocks(wi), starts))
    o = jnp.swapaxes(o, 0, 1).reshape(bsz, seq, ATT_HEADS * ATT_HEAD_DIM)
    return o @ w_out


def routed_experts(xt, expert_id, gate, w_gate, w_up, w_down):
    n_tok, d = xt.shape
    n_exp = w_gate.shape[0]
    top = expert_id.shape[1]
    n_asg = n_tok * top
    flat_e = expert_id.reshape(-1).astype(jnp.int32)
    flat_tok = jnp.repeat(jnp.arange(n_tok, dtype=jnp.int32), top)
    flat_gate = gate.reshape(-1)
    order = jnp.argsort(flat_e)
    e_s = flat_e[order]
    tok_s = flat_tok[order]
    gate_s = flat_gate[order]
    counts = jax.ops.segment_sum(jnp.ones_like(flat_e), flat_e, num_segments=n_exp)
    padded = (counts + MOE_BLOCK - 1) // MOE_BLOCK * MOE_BLOCK
    pad_end = jnp.cumsum(padded)
    pad_start = pad_end - padded
    cnt_start = jnp.cumsum(counts) - counts
    dest = pad_start[e_s] + (jnp.arange(n_asg, dtype=jnp.int32) - cnt_start[e_s])
    n_blocks = (n_asg + n_exp * (MOE_BLOCK - 1) + MOE_BLOCK - 1) // MOE_BLOCK
    cap = n_blocks * MOE_BLOCK
    slot_tok = jnp.full((cap,), n_tok, jnp.int32).at[dest].set(tok_s)
    slot_gate = jnp.zeros((cap,), jnp.float32).at[dest].set(gate_s)
    block_start = jnp.arange(n_blocks, dtype=jnp.int32) * MOE_BLOCK
    block_expert = jnp.minimum(jnp.searchsorted(pad_end, block_start, side='right'), n_exp - 1)
    x_pad = jnp.concatenate([xt, jnp.zeros((1, d), xt.dtype)], axis=0)

    def expert_block(inp):
        tok_b, e = inp
        xb = x_pad[tok_b]
        h = jax.nn.silu(xb @ w_gate[e]) * (xb @ w_up[e])
        return h @ w_down[e]

    y_slots = lax.map(expert_block, (slot_tok.reshape(n_blocks, MOE_BLOCK), block_expert))
    y_slots = y_slots.reshape(cap, d) * slot_gate[:, None].astype(xt.dtype)
    return jnp.zeros((n_tok + 1, d), xt.dtype).at[slot_tok].add(y_slots)[:n_tok]


def hierarchical_moe(x, rg_w, rg_b, re_w, re_b, w_gate, w_up, w_down):
    bsz, seq, d = x.shape
    xt = x.reshape(-1, d)
    n_tok = xt.shape[0]
    p_group = jax.nn.softmax((xt @ rg_w).astype(jnp.float32) + rg_b.astype(jnp.float32), axis=-1)
    p_g, g_idx = lax.top_k(p_group, 1)
    e_logits = ((xt @ re_w).astype(jnp.float32) + re_b.astype(jnp.float32)).reshape(n_tok, MOE_GROUPS, MOE_EXPERTS_PER_GROUP)
    gi = jnp.broadcast_to(g_idx[:, :, None], (n_tok, 1, MOE_EXPERTS_PER_GROUP))
    e_sel = jnp.take_along_axis(e_logits, gi, axis=1)[:, 0]
    p_e = jax.nn.softmax(e_sel, axis=-1)
    top_p, top_i = lax.top_k(p_e, MOE_TOPK)
    gate = p_g * top_p / jnp.sum(top_p, axis=-1, keepdims=True)
    expert_id = g_idx * MOE_EXPERTS_PER_GROUP + top_i
    y = routed_experts(xt, expert_id, gate, w_gate, w_up, w_down)
    return y.reshape(bsz, seq, d)


def setup_inputs(seed: int = 0) -> dict:
    key = jax.random.key(seed)
    ks = iter(jax.random.split(key, 40))
    f32 = jnp.float32

    def nrm(shape, scale):
        return jax.random.normal(next(ks), shape, f32) * scale

    def unif(shape, lo, hi):
        return jax.random.uniform(next(ks), shape, f32, minval=lo, maxval=hi)

    d = D_MODEL
    na, nb, nc = N_LAYERS_GDN, N_LAYERS_SSM, N_LAYERS_DSA
    gdn_dt = jnp.exp(unif((na, GDN_HEADS), math.log(1e-3), math.log(1e-1)))
    return {
        'x': nrm((BATCH, SEQ, d), 1.0),
        'ln_g': 1.0 + nrm((DEPTH, 2, d), 0.02),
        'ln_b': nrm((DEPTH, 2, d), 0.02),
        'moe_rg_w': nrm((DEPTH, d, MOE_GROUPS), d ** -0.5),
        'moe_rg_b': nrm((DEPTH, MOE_GROUPS), 0.01),
        'moe_re_w': nrm((DEPTH, d, MOE_EXPERTS), d ** -0.5),
        'moe_re_b': nrm((DEPTH, MOE_EXPERTS), 0.01),
        'moe_w_gate': nrm((DEPTH, MOE_EXPERTS, d, MOE_FF), d ** -0.5),
        'moe_w_up': nrm((DEPTH, MOE_EXPERTS, d, MOE_FF), d ** -0.5),
        'moe_w_down': nrm((DEPTH, MOE_EXPERTS, MOE_FF, d), DN_BETA * MOE_FF ** -0.5),
        'gdn_w_in': nrm((na, d, GDN_IN), d ** -0.5),
        'gdn_conv_w': nrm((na, GDN_CONV, GDN_CONV_CH), GDN_CONV ** -0.5),
        'gdn_a_log': jnp.log(unif((na, GDN_HEADS), 1.0, 16.0)),
        'gdn_dt_bias': gdn_dt + jnp.log(-jnp.expm1(-gdn_dt)),
        'gdn_norm_g': 1.0 + nrm((na, GDN_HEAD_DIM), 0.02),
        'gdn_w_out': nrm((na, GDN_V, d), DN_BETA * GDN_V ** -0.5),
        'ssm_w_in': nrm((nb, d, SSM_WIDTH), d ** -0.5),
        'ssm_b_re': nrm((nb, SSM_GROUPS, SSM_STATE, SSM_GROUP), (2.0 * SSM_GROUP) ** -0.5),
        'ssm_b_im': nrm((nb, SSM_GROUPS, SSM_STATE, SSM_GROUP), (2.0 * SSM_GROUP) ** -0.5),
        'ssm_c_re': nrm((nb, SSM_GROUPS, SSM_GROUP, SSM_STATE), (2.0 * SSM_STATE) ** -0.5),
        'ssm_c_im': nrm((nb, SSM_GROUPS, SSM_GROUP, SSM_STATE), (2.0 * SSM_STATE) ** -0.5),
        'ssm_a_re': -0.5 + nrm((nb, SSM_GROUPS, SSM_STATE), 0.01),
        'ssm_a_im': math.pi * jnp.arange(SSM_STATE, dtype=f32) + nrm((nb, SSM_GROUPS, SSM_STATE), 0.01),
        'ssm_log_dt': unif((nb, SSM_GROUPS), math.log(1e-3), math.log(1e-1)),
        'ssm_d': nrm((nb, SSM_WIDTH), 1.0),
        'ssm_w_glu': nrm((nb, SSM_WIDTH, SSM_WIDTH), SSM_WIDTH ** -0.5),
        'ssm_b_glu': nrm((nb, SSM_WIDTH), 0.01),
        'ssm_w_out': nrm((nb, SSM_WIDTH, d), DN_BETA * SSM_WIDTH ** -0.5),
        'dsa_w_in': nrm((nc, d, DSA_IN), d ** -0.5),
        'dsa_w_out': nrm((nc, ATT_HEADS * ATT_HEAD_DIM, d), DN_BETA * (ATT_HEADS * ATT_HEAD_DIM) ** -0.5),
    }


def reference(x, ln_g, ln_b, moe_rg_w, moe_rg_b, moe_re_w, moe_re_b, moe_w_gate, moe_w_up, moe_w_down,
              gdn_w_in, gdn_conv_w, gdn_a_log, gdn_dt_bias, gdn_norm_g, gdn_w_out,
              ssm_w_in, ssm_b_re, ssm_b_im, ssm_c_re, ssm_c_im, ssm_a_re, ssm_a_im, ssm_log_dt,
              ssm_d, ssm_w_glu, ssm_b_glu, ssm_w_out, dsa_w_in, dsa_w_out):
    i_gdn = 0
    i_ssm = 0
    i_dsa = 0
    for layer in range(DEPTH):
        kind = layer % N_MIXERS
        if kind == 0:
            h = gated_deltanet_mixer(x, gdn_w_in[i_gdn], gdn_conv_w[i_gdn], gdn_a_log[i_gdn],
                                     gdn_dt_bias[i_gdn], gdn_norm_g[i_gdn], gdn_w_out[i_gdn])
            i_gdn += 1
        elif kind == 1:
            h = s5_mixer(x, ssm_w_in[i_ssm], ssm_b_re[i_ssm], ssm_b_im[i_ssm], ssm_c_re[i_ssm],
                         ssm_c_im[i_ssm], ssm_a_re[i_ssm], ssm_a_im[i_ssm], ssm_log_dt[i_ssm],
                         ssm_d[i_ssm], ssm_w_glu[i_ssm], ssm_b_glu[i_ssm], ssm_w_out[i_ssm])
            i_ssm += 1
        else:
            h = dsa_mixer(x, dsa_w_in[i_dsa], dsa_w_out[i_dsa])
            i_dsa += 1
        x = layer_norm(DN_ALPHA * x + h, ln_g[layer, 0], ln_b[layer, 0])
        h = hierarchical_moe(x, moe_rg_w[layer], moe_rg_b[layer], moe_re_w[layer], moe_re_b[layer],
                             moe_w_gate[layer], moe_w_up[layer], moe_w_down[layer])
        x = layer_norm(DN_ALPHA * x + h, ln_g[layer, 1], ln_b[layer, 1])
    return x
```

```python
import contextlib
import math
import numpy as np
import concourse.bass as bass
import concourse.mybir as mybir
from concourse.bass_utils import run_bass_kernel_spmd

F32 = mybir.dt.float32
BF16 = mybir.dt.bfloat16
I32 = mybir.dt.int32
AF = mybir.ActivationFunctionType
ALU = mybir.AluOpType
AX = mybir.AxisListType

T = 2048
D = 2048
NT = 16
DEPTH = 4
DN_ALPHA = (2.0 * DEPTH) ** 0.25
LN_EPS = 1e-5
RMS_EPS = 1e-6
NEXP = 32
FF = 512
CAP = 256
NSLOT = NEXP * CAP
GDN_IN = 8224
DSA_IN = 4496
NEG = -30000.0


class Buf:
    __slots__ = ("name", "writer", "readers")

    def __init__(self, name=""):
        self.name = name
        self.writer = None
        self.readers = []


class Eng:
    def __init__(self, fw, name, hw, is_pe=False):
        self.fw = fw
        self.name = name
        self.hw = hw
        self.is_pe = is_pe
        self.sems = []
        self.cnt = 0
        self.waited = {}
        self.n_instr = 0

    def cur_sem(self):
        if not self.sems or self.cnt >= self.fw.EPOCH:
            self.sems.append(self.fw.new_sem(f"{self.name}_p{len(self.sems)}"))
            self.cnt = 0
        return self.sems[-1]


class FW:
    EPOCH = 60000
    NP = 24

    def __init__(self, nc, stack):
        self.nc = nc
        self.stack = stack
        self.sem_handles = {}
        self.nsem = 0
        self.pe = Eng(self, "pe", nc.tensor, is_pe=True)
        self.dve = Eng(self, "dve", nc.vector)
        self.act = Eng(self, "act", nc.scalar)
        self.pool = Eng(self, "pool", nc.gpsimd)
        self.sp = Eng(self, "sp", nc.sync)
        self.engs = [self.pe, self.dve, self.act, self.pool, self.sp]
        self.dma_pool = {}
        self.dma_rr = {}
        self.all_dma_tokens = []

    def new_sem(self, name):
        h = self.stack.enter_context(self.nc.semaphore(name))
        self.nsem += 1
        self.sem_handles[self.nsem] = h
        return self.nsem

    def _wait(self, eng, tok):
        if tok is None:
            return
        key, val, src = tok
        if eng.is_pe and src == "pe":
            return
        if eng.waited.get(key, 0) >= val:
            return
        eng.waited[key] = val
        eng.hw.wait_ge(self.sem_handles[key], val)

    def _note_read(self, b, tok):
        b.readers.append(tok)
        if len(b.readers) > 16:
            last = {}
            for r in b.readers:
                k2 = (r[2], r[0])
                if k2 not in last or last[k2][1] < r[1]:
                    last[k2] = r
            b.readers = list(last.values())

    def op(self, eng, fn, reads=(), writes=()):
        for b in reads:
            self._wait(eng, b.writer)
        for b in writes:
            self._wait(eng, b.writer)
            for r in b.readers:
                if r[2] == eng.name:
                    continue
                self._wait(eng, r)
        ins = fn(eng.hw)
        key = eng.cur_sem()
        eng.cnt += 1
        ins.then_inc(self.sem_handles[key], 1)
        tok = (key, eng.cnt, eng.name)
        for b in reads:
            self._note_read(b, tok)
        for b in writes:
            b.writer = tok
            b.readers = []
        eng.n_instr += 1
        return tok

    def dma(self, out, in_, reads=(), writes=(), q=None, indirect=None, **kw):
        eng = q or self.sp
        for b in reads:
            self._wait(eng, b.writer)
        for b in writes:
            self._wait(eng, b.writer)
            for r in b.readers:
                self._wait(eng, r)
        pool = self.dma_pool.setdefault(eng.name, [])
        if len(pool) < self.NP:
            pool.append([self.new_sem(f"dma_{eng.name}_{len(pool)}"), 0])
            slot = pool[-1]
        else:
            i = self.dma_rr.get(eng.name, 0)
            slot = pool[i % self.NP]
            self.dma_rr[eng.name] = i + 1
        key, uses = slot
        if uses > 0:
            self._wait(eng, (key, 16 * uses, "dma"))
        if indirect is None:
            ins = eng.hw.dma_start(out=out, in_=in_, **kw)
        else:
            ins = eng.hw.indirect_dma_start(out=out, in_=in_, **indirect)
        slot[1] = uses + 1
        ins.then_inc(self.sem_handles[key], 16)
        tok = (key, 16 * (uses + 1), "dma")
        for b in reads:
            self._note_read(b, tok)
        for b in writes:
            b.writer = tok
            b.readers = []
        self.all_dma_tokens.append(tok)
        if len(self.all_dma_tokens) > 400:
            self._compact_dma()
        eng.n_instr += 1
        return tok

    def _compact_dma(self):
        last = {}
        for t in self.all_dma_tokens:
            if t[0] not in last or last[t[0]][1] < t[1]:
                last[t[0]] = t
        self.all_dma_tokens = list(last.values())

    def barrier(self, engs=None):
        toks = []
        for e in self.engs:
            if e.sems and e.cnt > 0:
                toks.append((e.sems[-1], e.cnt, e.name))
        self._compact_dma()
        toks += self.all_dma_tokens
        for e in (engs or self.engs):
            for key, val, src in toks:
                if src == e.name:
                    continue
                if e.waited.get(key, 0) >= val:
                    continue
                e.waited[key] = val
                e.hw.wait_ge(self.sem_handles[key], val)

    def finish(self):
        self.barrier(engs=[self.sp])


class K:
    ARENA = 46000

    def __init__(self, nc, st, ext_in, ext_out):
        self.nc = nc
        self.st = st
        self.fw = FW(nc, st)
        self.ext_in = ext_in
        self.ext_out = ext_out
        self.arena = st.enter_context(nc.sbuf_tensor("arena", [128, self.ARENA], F32))
        self.psum = st.enter_context(nc.psum_tensor("psum", [128, 4096], F32))
        self.off = 0
        self.pbank = [Buf(f"bank{i}") for i in range(8)]
        self.dram = {}
        self.dbuf = {}
        self._consts()

    def sb(self, n, dt=F32):
        words = n if dt != BF16 else (n + 1) // 2
        words = (words + 7) // 8 * 8
        assert self.off + words <= self.ARENA, (self.off, words)
        ap = self.arena[:, self.off:self.off + words]
        self.off += words
        if dt == BF16:
            ap = ap.bitcast(BF16)[:, 0:n]
        elif dt == I32:
            ap = ap.bitcast(I32)[:, 0:n]
        else:
            ap = ap[:, 0:n]
        return ap

    def mark(self):
        return self.off

    def release(self, m):
        self.fw.barrier()
        self.off = m

    def bank(self, i, dt=F32):
        ap = self.psum[:, i * 512:(i + 1) * 512]
        if dt == BF16:
            ap = ap.bitcast(BF16)
        return ap

    def dt(self, name, shape, dtype):
        kind = "Internal"
        if name in self.ext_in:
            kind = "ExternalInput"
        elif name in self.ext_out:
            kind = "ExternalOutput"
        t = self.nc.dram_tensor(name, list(shape), dtype, kind=kind).ap()
        self.dram[name] = t
        self.dbuf[name] = Buf(name)
        return t

    def _consts(self):
        fw = self.fw
        P = fw.pool
        self.cb = Buf("consts")
        cb = self.cb
        self.ident = self.sb(128)
        self.ones = self.sb(128)
        self.identb = self.sb(128, BF16)
        self.onesb = self.sb(128, BF16)
        self.sltb = self.sb(128, BF16)
        slt = self.sb(128)
        fw.op(P, lambda e: e.memset(self.ident, 0.0), writes=[cb])
        fw.op(P, lambda e: e.affine_select(out=self.ident, in_=self.ident, pattern=[[-1, 128]],
                                           compare_op=ALU.not_equal, fill=1.0, base=0, channel_multiplier=1),
              reads=[cb], writes=[cb])
        fw.op(P, lambda e: e.memset(self.ones, 1.0), writes=[cb])
        fw.op(P, lambda e: e.memset(slt, 1.0), writes=[cb])
        fw.op(P, lambda e: e.affine_select(out=slt, in_=slt, pattern=[[1, 128]], compare_op=ALU.is_gt,
                                           fill=0.0, base=0, channel_multiplier=-1), reads=[cb], writes=[cb])
        fw.op(P, lambda e: e.tensor_copy(out=self.identb, in_=self.ident), reads=[cb], writes=[cb])
        fw.op(P, lambda e: e.tensor_copy(out=self.onesb, in_=self.ones), reads=[cb], writes=[cb])
        fw.op(P, lambda e: e.tensor_copy(out=self.sltb, in_=slt), reads=[cb], writes=[cb])
        self.ebase = self.sb(NEXP)
        fw.op(P, lambda e: e.iota(out=self.ebase, pattern=[[CAP, NEXP]], base=0, channel_multiplier=0,
                                  allow_small_or_imprecise_dtypes=True), writes=[cb])
        self.trash = self.sb(1)
        fw.op(P, lambda e: e.iota(out=self.trash, pattern=[[0, 1]], base=NSLOT, channel_multiplier=1,
                                  allow_small_or_imprecise_dtypes=True), writes=[cb])
        self.const_mark = self.off

    def load_row(self, dst, src_row, buf, n):
        src = src_row.rearrange("(o n) -> o n", o=1).to_broadcast([128, n]) if len(src_row.shape) == 1 \
            else src_row.to_broadcast([128, n])
        self.fw.dma(out=dst, in_=src, writes=[buf])

    def build_xT(self, src, src_buf, xT, xT_buf):
        fw = self.fw
        m = self.mark()
        xin = [self.sb(D) for _ in range(2)]
        xb = [Buf("xin0"), Buf("xin1")]
        for i in range(NT):
            s = i % 2
            fw.dma(out=xin[s], in_=src[i * 128:(i + 1) * 128, :], reads=[src_buf], writes=[xb[s]])
            for g in range(4):
                bk = (i * 4 + g) % 8
                pb = self.pbank[bk]
                for j in range(4):
                    fc = g * 4 + j
                    fw.op(fw.pe, lambda e, fc=fc, j=j, bk=bk, s=s: e.transpose(
                        out=self.bank(bk)[:, j * 128:(j + 1) * 128], in_=xin[s][:, fc * 128:(fc + 1) * 128],
                        identity=self.ident), reads=[xb[s], self.cb], writes=[pb])
                dst = xT[:, g * 4:(g + 1) * 4, i * 128:(i + 1) * 128]
                srcp = self.bank(bk).rearrange("p (a b) -> p a b", a=4)
                if g % 2 == 0:
                    fw.op(fw.dve, lambda e, dst=dst, srcp=srcp: e.tensor_copy(out=dst, in_=srcp),
                          reads=[pb], writes=[xT_buf])
                else:
                    fw.op(fw.act, lambda e, dst=dst, srcp=srcp: e.activation(out=dst, in_=srcp, func=AF.Copy),
                          reads=[pb], writes=[xT_buf])
        self.release(m)

    def ln_tail(self, z, zb, grow, brow, gbuf, tmp, tmpb, stat, statb, out, outb):
        fw = self.fw
        mean = stat[:, 0:1]
        ssq = stat[:, 1:2]
        rstd = stat[:, 2:3]
        nmean = stat[:, 3:4]
        fw.op(fw.dve, lambda e: e.tensor_reduce(out=mean, in_=z, axis=AX.X, op=ALU.add), reads=[zb], writes=[statb])
        fw.op(fw.dve, lambda e: e.tensor_scalar(out=nmean, in0=mean, scalar1=-1.0 / D, scalar2=None, op0=ALU.mult),
              reads=[statb], writes=[statb])
        fw.op(fw.act, lambda e: e.activation(out=tmp, in_=z, func=AF.Square, bias=nmean, scale=1.0, accum_out=ssq),
              reads=[zb, statb], writes=[tmpb, statb])
        fw.op(fw.dve, lambda e: e.tensor_scalar(out=rstd, in0=ssq, scalar1=1.0 / D, scalar2=LN_EPS, op0=ALU.mult,
                                                op1=ALU.add), reads=[statb], writes=[statb])
        fw.op(fw.act, lambda e: e.activation(out=rstd, in_=rstd, func=AF.Sqrt), reads=[statb], writes=[statb])
        fw.op(fw.dve, lambda e: e.reciprocal(out=rstd, in_=rstd), reads=[statb], writes=[statb])
        fw.op(fw.dve, lambda e: e.tensor_scalar(out=tmp, in0=z, scalar1=nmean, scalar2=rstd, op0=ALU.add,
                                                op1=ALU.mult), reads=[zb, statb, tmpb], writes=[tmpb])
        fw.op(fw.pool, lambda e: e.tensor_tensor(out=tmp, in0=tmp, in1=grow, op=ALU.mult), reads=[tmpb, gbuf],
              writes=[tmpb])
        fw.op(fw.dve, lambda e: e.tensor_tensor(out=out, in0=tmp, in1=brow, op=ALU.add), reads=[tmpb, gbuf],
              writes=[outb])

    def outproj_ln(self, OT, OTb, w_out, resid, resid_buf, ln_g, ln_b, dst, dst_buf):
        fw = self.fw
        m = self.mark()
        W = self.sb(16 * D, BF16)
        Wv = W.rearrange("p (a b) -> p a b", a=16)
        Wb = Buf("wout")
        wsrc = w_out.rearrange("(fc p) m -> p fc m", p=128)
        for q in range(4):
            fw.dma(out=Wv[:, q * 4:(q + 1) * 4, :], in_=wsrc[:, q * 4:(q + 1) * 4, :], writes=[Wb], q=fw.pool)
        grow = self.sb(D)
        brow = self.sb(D)
        gbuf = Buf("lnrows")
        self.load_row(grow, ln_g, gbuf, D)
        self.load_row(brow, ln_b, gbuf, D)
        oT = [self.sb(16 * 128, BF16) for _ in range(2)]
        oTb = [Buf(), Buf()]
        rz = [self.sb(D) for _ in range(2)]
        rzb = [Buf(), Buf()]
        tmp = self.sb(D)
        tmpb = Buf()
        stat = [self.sb(4) for _ in range(2)]
        statb = [Buf(), Buf()]
        OTv = OT.rearrange("fc p t -> p fc t")
        for i in range(NT):
            s = i % 2
            fw.dma(out=oT[s].rearrange("p (a b) -> p a b", a=16), in_=OTv[:, :, i * 128:(i + 1) * 128],
                   reads=[OTb], writes=[oTb[s]])
            fw.dma(out=rz[s], in_=resid[i * 128:(i + 1) * 128, :], reads=[resid_buf], writes=[rzb[s]])
            for mc in range(4):
                bk = (i * 4 + mc) % 8
                pb = self.pbank[bk]
                for fc in range(16):
                    fw.op(fw.pe, lambda e, fc=fc, mc=mc, bk=bk, s=s: e.matmul(
                        self.bank(bk), lhsT=oT[s][:, fc * 128:(fc + 1) * 128], rhs=Wv[:, fc, mc * 512:(mc + 1) * 512],
                        start=(fc == 0), stop=(fc == 15)), reads=[oTb[s], Wb], writes=[pb])
                zs = rz[s][:, mc * 512:(mc + 1) * 512]
                fw.op(fw.dve, lambda e, zs=zs, bk=bk: e.scalar_tensor_tensor(
                    out=zs, in0=zs, scalar=DN_ALPHA, in1=self.bank(bk), op0=ALU.mult, op1=ALU.add),
                    reads=[pb, rzb[s]], writes=[rzb[s]])
            self.ln_tail(rz[s], rzb[s], grow, brow, gbuf, tmp, tmpb, stat[s], statb[s], rz[s], rzb[s])
            fw.dma(out=dst[i * 128:(i + 1) * 128, :], in_=rz[s], reads=[rzb[s]], writes=[dst_buf])
        self.release(m)

    def moe(self, L, xm, xm_buf, W, dst, dst_buf):
        fw = self.fw
        XG, YG = self.dram["XG"], self.dram["YG"]
        XGb, YGb = self.dbuf["XG"], self.dbuf["YG"]
        m0 = self.mark()
        dest = self.sb(NT * 2, I32)
        gate = self.sb(NT * 2)
        routeb = Buf("route")
        acum = self.sb(NEXP)
        acumb = self.sb(NEXP, BF16)
        acb = Buf("acum")
        fw.op(fw.dve, lambda e: e.memset(acum, 0.0), writes=[acb])
        fw.op(fw.dve, lambda e: e.memset(acumb, 0.0), writes=[acb])
        m1 = self.mark()
        wr = self.sb(16 * 36)
        wrv = wr.rearrange("p (a b) -> p a b", a=16)
        wrb = Buf("wr")
        fw.dma(out=wrv[:, :, 0:4], in_=W["moe_rg_w"][L].rearrange("(fc p) g -> p fc g", p=128), writes=[wrb])
        fw.dma(out=wrv[:, :, 4:36], in_=W["moe_re_w"][L].rearrange("(fc p) g -> p fc g", p=128), writes=[wrb])
        brow = self.sb(36)
        self.load_row(brow[:, 0:4], W["moe_rg_b"][L], wrb, 4)
        self.load_row(brow[:, 4:36], W["moe_re_b"][L], wrb, 32)
        xin = [self.sb(D) for _ in range(2)]
        xinb = [Buf(), Buf()]
        xTf = [self.sb(16 * 128) for _ in range(2)]
        xTfb = [Buf(), Buf()]
        xbf = [self.sb(D, BF16) for _ in range(2)]
        xbfb = [Buf(), Buf()]
        sm = [self.sb(256) for _ in range(2)]
        smb = [Buf(), Buf()]
        for i in range(NT):
            s = i % 2
            fw.dma(out=xin[s], in_=xm[i * 128:(i + 1) * 128, :], reads=[xm_buf], writes=[xinb[s]])
            for g in range(4):
                bk = g
                pb = self.pbank[bk]
                for j in range(4):
                    fc = g * 4 + j
                    fw.op(fw.pe, lambda e, fc=fc, j=j, bk=bk, s=s: e.transpose(
                        out=self.bank(bk)[:, j * 128:(j + 1) * 128], in_=xin[s][:, fc * 128:(fc + 1) * 128],
                        identity=self.ident), reads=[xinb[s], self.cb], writes=[pb])
                dstp = xTf[s][:, g * 512:(g + 1) * 512]
                if g % 2 == 0:
                    fw.op(fw.dve, lambda e, dstp=dstp, bk=bk: e.tensor_copy(out=dstp, in_=self.bank(bk)),
                          reads=[pb], writes=[xTfb[s]])
                else:
                    fw.op(fw.act, lambda e, dstp=dstp, bk=bk: e.activation(out=dstp, in_=self.bank(bk), func=AF.Copy),
                          reads=[pb], writes=[xTfb[s]])
            fw.op(fw.pool, lambda e, s=s: e.tensor_copy(out=xbf[s], in_=xin[s]), reads=[xinb[s]], writes=[xbfb[s]])
            pl = self.pbank[4]
            lg_ps = self.bank(4)[:, 0:36]
            for fc in range(16):
                fw.op(fw.pe, lambda e, fc=fc, s=s: e.matmul(lg_ps, lhsT=xTf[s][:, fc * 128:(fc + 1) * 128],
                                                           rhs=wrv[:, fc, :], start=(fc == 0), stop=(fc == 15)),
                      reads=[xTfb[s], wrb], writes=[pl])
            S = sm[s]
            Sb = smb[s]
            lg = S[:, 0:36]
            fw.op(fw.dve, lambda e, lg=lg: e.tensor_tensor(out=lg, in0=lg_ps, in1=brow, op=ALU.add),
                  reads=[pl, wrb], writes=[Sb])
            gmax = S[:, 36:37]
            ngmax = S[:, 37:38]
            gsum = S[:, 38:39]
            pg = S[:, 39:40]
            ohg = S[:, 40:44]
            ex4 = S[:, 44:48]
            fw.op(fw.dve, lambda e: e.tensor_reduce(out=gmax, in_=lg[:, 0:4], axis=AX.X, op=ALU.max),
                  reads=[Sb], writes=[Sb])
            fw.op(fw.dve, lambda e: e.tensor_scalar(out=ngmax, in0=gmax, scalar1=-1.0, scalar2=None, op0=ALU.mult),
                  reads=[Sb], writes=[Sb])
            fw.op(fw.act, lambda e: e.activation(out=ex4, in_=lg[:, 0:4], func=AF.Exp, bias=ngmax, scale=1.0,
                                                 accum_out=gsum), reads=[Sb], writes=[Sb])
            fw.op(fw.dve, lambda e: e.reciprocal(out=pg, in_=gsum), reads=[Sb], writes=[Sb])
            fw.op(fw.dve, lambda e: e.tensor_scalar(out=ohg, in0=lg[:, 0:4], scalar1=gmax, scalar2=None,
                                                    op0=ALU.is_ge), reads=[Sb], writes=[Sb])
            esel = S[:, 48:56]
            le = lg[:, 4:36]
            fw.op(fw.dve, lambda e: e.tensor_scalar(out=esel, in0=le[:, 0:8], scalar1=ohg[:, 0:1], scalar2=None,
                                                    op0=ALU.mult), reads=[Sb], writes=[Sb])
            for g in range(1, 4):
                fw.op(fw.dve, lambda e, g=g: e.scalar_tensor_tensor(
                    out=esel, in0=le[:, g * 8:(g + 1) * 8], scalar=ohg[:, g:g + 1], in1=esel, op0=ALU.mult,
                    op1=ALU.add), reads=[Sb], writes=[Sb])
            top8 = S[:, 56:64]
            fw.op(fw.dve, lambda e: e.max(out=top8, in_=esel), reads=[Sb], writes=[Sb])
            oh1 = S[:, 64:72]
            oh2 = S[:, 72:80]
            fw.op(fw.dve, lambda e: e.tensor_scalar(out=oh1, in0=esel, scalar1=top8[:, 0:1], scalar2=None,
                                                    op0=ALU.is_equal), reads=[Sb], writes=[Sb])
            fw.op(fw.dve, lambda e: e.tensor_scalar(out=oh2, in0=esel, scalar1=top8[:, 1:2], scalar2=None,
                                                    op0=ALU.is_equal), reads=[Sb], writes=[Sb])
            dv = S[:, 80:82]
            fw.op(fw.dve, lambda e: e.tensor_tensor(out=dv[:, 0:1], in0=top8[:, 0:1], in1=top8[:, 1:2],
                                                    op=ALU.subtract), reads=[Sb], writes=[Sb])
            fw.op(fw.dve, lambda e: e.tensor_tensor(out=dv[:, 1:2], in0=top8[:, 1:2], in1=top8[:, 0:1],
                                                    op=ALU.subtract), reads=[Sb], writes=[Sb])
            p12 = S[:, 82:84]
            fw.op(fw.act, lambda e: e.activation(out=p12, in_=dv, func=AF.Sigmoid), reads=[Sb], writes=[Sb])
            fw.op(fw.dve, lambda e: e.tensor_scalar(out=p12, in0=p12, scalar1=pg, scalar2=None, op0=ALU.mult),
                  reads=[Sb], writes=[Sb])
            A1 = S[:, 96:128]
            A2 = S[:, 128:160]
            for g in range(4):
                fw.op(fw.dve, lambda e, g=g: e.tensor_scalar(out=A1[:, g * 8:(g + 1) * 8], in0=oh1,
                                                              scalar1=ohg[:, g:g + 1], scalar2=None, op0=ALU.mult),
                      reads=[Sb], writes=[Sb])
                fw.op(fw.dve, lambda e, g=g: e.tensor_scalar(out=A2[:, g * 8:(g + 1) * 8], in0=oh2,
                                                              scalar1=ohg[:, g:g + 1], scalar2=None, op0=ALU.mult),
                      reads=[Sb], writes=[Sb])
            A12 = S[:, 160:192]
            A12b = S[:, 192:208].bitcast(BF16)
            fw.op(fw.dve, lambda e: e.tensor_tensor(out=A12, in0=A1, in1=A2, op=ALU.add), reads=[Sb], writes=[Sb])
            fw.op(fw.dve, lambda e: e.tensor_copy(out=A12b, in_=A12), reads=[Sb], writes=[Sb])
            pp = self.pbank[5]
            pos_ps = self.bank(5)[:, 0:32]
            fw.op(fw.pe, lambda e: e.matmul(pos_ps, lhsT=self.onesb, rhs=acumb, start=True, stop=False),
                  reads=[acb, self.cb], writes=[pp])
            fw.op(fw.pe, lambda e: e.matmul(pos_ps, lhsT=self.sltb, rhs=A12b, start=False, stop=True),
                  reads=[Sb, self.cb], writes=[pp])
            slot = S[:, 208:240]
            fw.op(fw.dve, lambda e: e.tensor_tensor(out=slot, in0=pos_ps, in1=self.ebase, op=ALU.add),
                  reads=[pp, self.cb], writes=[Sb])
            fw.op(fw.pool, lambda e: e.tensor_tensor(out=acum, in0=acum, in1=A12, op=ALU.add), reads=[Sb, acb],
                  writes=[acb])
            fw.op(fw.pool, lambda e: e.tensor_copy(out=acumb, in_=acum), reads=[acb], writes=[acb])
            tmp32 = S[:, 240:256]
            dr = S[:, 84:86]
            pr = S[:, 86:88]
            for r, A in ((0, A1), (1, A2)):
                tt = S[:, 224:256]
            scr = xTf[s][:, 0:32]
            for r, A in ((0, A1), (1, A2)):
                fw.op(fw.dve, lambda e, r=r, A=A: e.scalar_tensor_tensor(
                    out=scr, in0=A, scalar=1.0, in1=slot, op0=ALU.mult, op1=ALU.mult, accum_out=dr[:, r:r + 1]),
                    reads=[Sb, xTfb[s], pl], writes=[Sb, xTfb[s]])
                fw.op(fw.dve, lambda e, r=r, A=A: e.scalar_tensor_tensor(
                    out=scr, in0=A, scalar=1.0, in1=pos_ps, op0=ALU.mult, op1=ALU.mult, accum_out=pr[:, r:r + 1]),
                    reads=[Sb, xTfb[s], pp], writes=[Sb, xTfb[s]])
            valid = S[:, 88:90]
            fw.op(fw.dve, lambda e: e.tensor_scalar(out=valid, in0=pr, scalar1=float(CAP) - 0.5, scalar2=None,
                                                    op0=ALU.is_lt), reads=[Sb], writes=[Sb])
            fw.op(fw.dve, lambda e: e.tensor_tensor(out=p12, in0=p12, in1=valid, op=ALU.mult), reads=[Sb],
                  writes=[Sb])
            fw.op(fw.dve, lambda e: e.tensor_scalar(out=dr, in0=dr, scalar1=self.trash, scalar2=None,
                                                    op0=ALU.subtract), reads=[Sb, self.cb], writes=[Sb])
            fw.op(fw.dve, lambda e: e.tensor_tensor(out=dr, in0=dr, in1=valid, op=ALU.mult), reads=[Sb], writes=[Sb])
            fw.op(fw.dve, lambda e: e.tensor_scalar(out=dr, in0=dr, scalar1=self.trash, scalar2=None, op0=ALU.add),
                  reads=[Sb, self.cb], writes=[Sb])
            fw.op(fw.dve, lambda e, i=i: e.tensor_copy(out=dest[:, 2 * i:2 * i + 2], in_=dr), reads=[Sb],
                  writes=[routeb])
            fw.op(fw.dve, lambda e, i=i: e.tensor_copy(out=gate[:, 2 * i:2 * i + 2], in_=p12), reads=[Sb],
                  writes=[routeb])
            for r in range(2):
                fw.dma(out=XG, in_=xbf[s], reads=[xbfb[s], routeb], writes=[XGb], q=fw.pool,
                       indirect=dict(out_offset=bass.IndirectOffsetOnAxis(ap=dest[:, 2 * i + r:2 * i + r + 1], axis=0),
                                     in_offset=None))
        self.release(m1)
        m2 = self.mark()
        NB = 2
        wg = [self.sb(16 * FF, BF16) for _ in range(NB)]
        wu = [self.sb(16 * FF, BF16) for _ in range(NB)]
        wd = [self.sb(4 * D, BF16) for _ in range(NB)]
        wbuf = [Buf() for _ in range(NB)]
        xg = [self.sb(D, BF16) for _ in range(2)]
        xgb = [Buf(), Buf()]
        xgT = self.sb(16 * CAP, BF16)
        xgTv = xgT.rearrange("p (a b) -> p a b", a=16)
        xgTb = Buf()
        hT = self.sb(4 * CAP, BF16)
        hTv = hT.rearrange("p (a b) -> p a b", a=4)
        hTb = Buf()
        sg = self.sb(CAP)
        sgb = Buf()
        yt = [self.sb(D) for _ in range(2)]
        ytb = [Buf(), Buf()]
        pbk = 0
        for ex in range(NEXP):
            s = ex % NB
            fw.dma(out=wg[s].rearrange("p (a b) -> p a b", a=16),
                   in_=W["moe_w_gate"][L, ex].rearrange("(kc p) f -> p kc f", p=128), writes=[wbuf[s]], q=fw.pool)
            fw.dma(out=wu[s].rearrange("p (a b) -> p a b", a=16),
                   in_=W["moe_w_up"][L, ex].rearrange("(kc p) f -> p kc f", p=128), writes=[wbuf[s]], q=fw.pool)
            fw.dma(out=wd[s].rearrange("p (a b) -> p a b", a=4),
                   in_=W["moe_w_down"][L, ex].rearrange("(fc p) m -> p fc m", p=128), writes=[wbuf[s]], q=fw.pool)
            wgv = wg[s].rearrange("p (a b) -> p a b", a=16)
            wuv = wu[s].rearrange("p (a b) -> p a b", a=16)
            wdv = wd[s].rearrange("p (a b) -> p a b", a=4)
            for stl in range(CAP // 128):
                xs = stl % 2
                fw.dma(out=xg[xs], in_=XG[ex * CAP + stl * 128: ex * CAP + (stl + 1) * 128, :], reads=[XGb],
                       writes=[xgb[xs]])
                for g in range(2):
                    bk = pbk % 8
                    pbk += 1
                    pb = self.pbank[bk]
                    bkb = self.bank(bk, BF16)
                    for j in range(8):
                        kc = g * 8 + j
                        fw.op(fw.pe, lambda e, kc=kc, j=j, bkb=bkb, xs=xs: e.transpose(
                            out=bkb[:, j * 128:(j + 1) * 128], in_=xg[xs][:, kc * 128:(kc + 1) * 128],
                            identity=self.identb), reads=[xgb[xs], self.cb], writes=[pb])
                    dstp = xgTv[:, g * 8:(g + 1) * 8, stl * 128:(stl + 1) * 128]
                    srcp = bkb.rearrange("p (a b) -> p a b", a=8)
                    if g == 0:
                        fw.op(fw.dve, lambda e, dstp=dstp, srcp=srcp: e.tensor_copy(out=dstp, in_=srcp),
                              reads=[pb], writes=[xgTb])
                    else:
                        fw.op(fw.act, lambda e, dstp=dstp, srcp=srcp: e.activation(out=dstp, in_=srcp, func=AF.Copy),
                              reads=[pb], writes=[xgTb])
            for fc in range(4):
                bkg = pbk % 8
                bku = (pbk + 1) % 8
                pbk += 2
                for (bk, wv) in ((bkg, wgv), (bku, wuv)):
                    for kc in range(16):
                        fw.op(fw.pe, lambda e, bk=bk, wv=wv, kc=kc, fc=fc: e.matmul(
                            self.bank(bk)[:, 0:CAP], lhsT=wv[:, kc, fc * 128:(fc + 1) * 128], rhs=xgTv[:, kc, :],
                            start=(kc == 0), stop=(kc == 15)), reads=[wbuf[s], xgTb], writes=[self.pbank[bk]])
                fw.op(fw.act, lambda e, bkg=bkg: e.activation(out=sg, in_=self.bank(bkg)[:, 0:CAP], func=AF.Silu),
                      reads=[self.pbank[bkg]], writes=[sgb])
                fw.op(fw.dve, lambda e, bku=bku, fc=fc: e.tensor_tensor(out=hTv[:, fc, :], in0=sg,
                                                                        in1=self.bank(bku)[:, 0:CAP], op=ALU.mult),
                      reads=[self.pbank[bku], sgb], writes=[hTb])
            for stl in range(CAP // 128):
                ys = (ex * 2 + stl) % 2
                for mc in range(4):
                    bk = pbk % 8
                    pbk += 1
                    for fc in range(4):
                        fw.op(fw.pe, lambda e, bk=bk, fc=fc, mc=mc, stl=stl: e.matmul(
                            self.bank(bk), lhsT=hTv[:, fc, stl * 128:(stl + 1) * 128],
                            rhs=wdv[:, fc, mc * 512:(mc + 1) * 512], start=(fc == 0), stop=(fc == 3)),
                            reads=[hTb, wbuf[s]], writes=[self.pbank[bk]])
                    dstp = yt[ys][:, mc * 512:(mc + 1) * 512]
                    if mc % 2 == 0:
                        fw.op(fw.dve, lambda e, dstp=dstp, bk=bk: e.tensor_copy(out=dstp, in_=self.bank(bk)),
                              reads=[self.pbank[bk]], writes=[ytb[ys]])
                    else:
                        fw.op(fw.act, lambda e, dstp=dstp, bk=bk: e.activation(out=dstp, in_=self.bank(bk),
                                                                               func=AF.Copy),
                              reads=[self.pbank[bk]], writes=[ytb[ys]])
                fw.dma(out=YG[ex * CAP + stl * 128: ex * CAP + (stl + 1) * 128, :], in_=yt[ys], reads=[ytb[ys]],
                       writes=[YGb])
        self.release(m2)
        m3 = self.mark()
        grow = self.sb(D)
        brow2 = self.sb(D)
        gbuf = Buf()
        self.load_row(grow, W["ln_g"][L, 1], gbuf, D)
        self.load_row(brow2, W["ln_b"][L, 1], gbuf, D)
        xr = [self.sb(D) for _ in range(2)]
        xrb = [Buf(), Buf()]
        y1 = [self.sb(D) for _ in range(2)]
        y1b = [Buf(), Buf()]
        y2 = [self.sb(D) for _ in range(2)]
        y2b = [Buf(), Buf()]
        tmp = self.sb(D)
        tmpb = Buf()
        stat = [self.sb(4) for _ in range(2)]
        statb = [Buf(), Buf()]
        for i in range(NT):
            s = i % 2
            fw.dma(out=xr[s], in_=xm[i * 128:(i + 1) * 128, :], reads=[xm_buf], writes=[xrb[s]])
            for (yy, yb, r) in ((y1[s], y1b[s], 0), (y2[s], y2b[s], 1)):
                fw.dma(out=yy, in_=YG, reads=[YGb, routeb], writes=[yb], q=fw.pool,
                       indirect=dict(out_offset=None,
                                     in_offset=bass.IndirectOffsetOnAxis(ap=dest[:, 2 * i + r:2 * i + r + 1], axis=0)))
            fw.op(fw.pool, lambda e, s=s, i=i: e.tensor_scalar(out=y1[s], in0=y1[s], scalar1=gate[:, 2 * i:2 * i + 1],
                                                               scalar2=None, op0=ALU.mult),
                  reads=[y1b[s], routeb], writes=[y1b[s]])
            fw.op(fw.dve, lambda e, s=s, i=i: e.scalar_tensor_tensor(
                out=y2[s], in0=y2[s], scalar=gate[:, 2 * i + 1:2 * i + 2], in1=y1[s], op0=ALU.mult, op1=ALU.add),
                reads=[y1b[s], y2b[s], routeb], writes=[y2b[s]])
            fw.op(fw.dve, lambda e, s=s: e.scalar_tensor_tensor(
                out=xr[s], in0=xr[s], scalar=DN_ALPHA, in1=y2[s], op0=ALU.mult, op1=ALU.add),
                reads=[xrb[s], y2b[s]], writes=[xrb[s]])
            self.ln_tail(xr[s], xrb[s], grow, brow2, gbuf, tmp, tmpb, stat[s], statb[s], xr[s], xrb[s])
            fw.dma(out=dst[i * 128:(i + 1) * 128, :], in_=xr[s], reads=[xrb[s]], writes=[dst_buf])
        self.release(m3)
        self.release(m0)

    def proj_fm(self, xTv, xTb, w_cols, wt, wtb, banks=(0, 1, 2, 3)):
        fw = self.fw
        fw.dma(out=wt.rearrange("p (a b) -> p a b", a=16), in_=w_cols.rearrange("(kc p) f -> p kc f", p=128),
               writes=[wtb], q=fw.pool)
        wv = wt.rearrange("p (a b) -> p a b", a=16)
        for tc in range(4):
            bk = banks[tc]
            for kc in range(16):
                fw.op(fw.pe, lambda e, bk=bk, kc=kc, tc=tc: e.matmul(
                    self.bank(bk), lhsT=wv[:, kc, :], rhs=xTv[:, kc, tc * 512:(tc + 1) * 512],
                    start=(kc == 0), stop=(kc == 15)), reads=[wtb, xTb], writes=[self.pbank[bk]])

    def sin_rr(self, eng, out, ang, buf, k, r, n_part=128, shift=0.0):
        fw = self.fw
        MAG = 12582912.0
        C1 = 6.28125
        C2 = 2.0 * math.pi - 6.28125
        fw.op(eng, lambda e: e.tensor_scalar(out=k, in0=ang, scalar1=1.0 / (2.0 * math.pi),
                                             scalar2=shift / (2.0 * math.pi), op0=ALU.mult, op1=ALU.add),
              reads=[buf], writes=[buf])
        fw.op(eng, lambda e: e.tensor_scalar(out=k, in0=k, scalar1=MAG, scalar2=None, op0=ALU.add),
              reads=[buf], writes=[buf])
        fw.op(eng, lambda e: e.tensor_scalar(out=k, in0=k, scalar1=-MAG, scalar2=None, op0=ALU.add),
              reads=[buf], writes=[buf])
        fw.op(eng, lambda e: e.scalar_tensor_tensor(out=r, in0=k, scalar=-C1, in1=ang, op0=ALU.mult, op1=ALU.add),
              reads=[buf], writes=[buf]) if eng is fw.dve else None
        if eng is not fw.dve:
            raise ValueError
        fw.op(eng, lambda e: e.scalar_tensor_tensor(out=r, in0=k, scalar=-C2, in1=r, op0=ALU.mult, op1=ALU.add),
              reads=[buf], writes=[buf])
        fw.op(eng, lambda e: e.tensor_scalar(out=r, in0=r, scalar1=shift, scalar2=math.pi, op0=ALU.add, op1=ALU.min),
              reads=[buf], writes=[buf])
        fw.op(eng, lambda e: e.tensor_scalar(out=r, in0=r, scalar1=-math.pi, scalar2=None, op0=ALU.max),
              reads=[buf], writes=[buf])
        fw.op(fw.act, lambda e: e.activation(out=out, in_=r, func=AF.Sin), reads=[buf], writes=[buf])

    def s5(self, L, src, src_buf, W, dst, dst_buf):
        fw = self.fw
        dve, act, pool, pe = fw.dve, fw.act, fw.pool, fw.pe
        U, Ub = self.dram["U"], self.dbuf["U"]
        YA, YAb = self.dram["YA"], self.dbuf["YA"]
        OT, OTb = self.dram["OT"], self.dbuf["OT"]
        m0 = self.mark()
        big = self.sb(16 * T, BF16)
        bigv = big.rearrange("p (a b) -> p a b", a=16)
        xTb = Buf("xT")
        self.build_xT(src, src_buf, bigv, xTb)
        mU = self.mark()
        wt = [self.sb(16 * 128, BF16) for _ in range(2)]
        wtb = [Buf(), Buf()]
        uf = [self.sb(T) for _ in range(2)]
        ufb = [Buf(), Buf()]
        for J in range(16):
            s = J % 2
            self.proj_fm(bigv, xTb, W["ssm_w_in"][0][:, J * 128:(J + 1) * 128], wt[s], wtb[s])
            for tc in range(4):
                eng = dve if tc % 2 == 0 else act
                dstp = uf[s][:, tc * 512:(tc + 1) * 512]
                if tc % 2 == 0:
                    fw.op(dve, lambda e, dstp=dstp, tc=tc: e.tensor_copy(out=dstp, in_=self.bank(tc)),
                          reads=[self.pbank[tc]], writes=[ufb[s]])
                else:
                    fw.op(act, lambda e, dstp=dstp, tc=tc: e.activation(out=dstp, in_=self.bank(tc), func=AF.Copy),
                          reads=[self.pbank[tc]], writes=[ufb[s]])
            fw.dma(out=U[J], in_=uf[s], reads=[ufb[s]], writes=[Ub])
        self.release(mU)
        uTb_buf = xTb
        for q in range(4):
            fw.dma(out=bigv[:, q * 4:(q + 1) * 4, :], in_=U.rearrange("j p t -> p j t")[:, q * 4:(q + 1) * 4, :],
                   reads=[Ub], writes=[uTb_buf], q=pool)
        mP = self.mark()
        PQ = self.sb(6 * 64)
        LB = [self.sb(16 * 128, BF16) for _ in range(2)]
        CL = [self.sb(16 * 128) for _ in range(2)]
        LBz = [self.sb(16 * 128, BF16) for _ in range(2)]
        LBzv = [a.rearrange("p (J q) -> p J q", J=16) for a in LBz]
        dsk = self.sb(16)
        tau = self.sb(512)
        m96 = self.sb(1)
        mT = self.mark()
        pb_ = Buf("s5par")
        A = lambda: self.sb(128)
        are, aim, dtb, mag, ang, kk, rr, sn, cs, den, fre, fim, t1, t2 = [A() for _ in range(14)]
        ldt = self.sb(2)
        fw.dma(out=are[0:64, :], in_=W["ssm_a_re"][0].rearrange("(j two) p -> j (two p)", two=2), writes=[pb_])
        fw.dma(out=aim[0:64, :], in_=W["ssm_a_im"][0].rearrange("(j two) p -> j (two p)", two=2), writes=[pb_])
        fw.dma(out=ldt[0:64, :], in_=W["ssm_log_dt"][0].rearrange("(j two) -> j two", two=2), writes=[pb_])
        h = slice(0, 64)
        fw.op(act, lambda e: e.activation(out=ldt[h, :], in_=ldt[h, :], func=AF.Exp), reads=[pb_], writes=[pb_])
        for two in range(2):
            fw.op(dve, lambda e, two=two: e.tensor_scalar(out=dtb[h, two * 64:(two + 1) * 64], in0=self.ones[h, 0:64],
                                                         scalar1=ldt[h, two:two + 1], scalar2=None, op0=ALU.mult),
                  reads=[pb_, self.cb], writes=[pb_])
        tt = lambda o, a, b, op: fw.op(dve, lambda e: e.tensor_tensor(out=o[h, :], in0=a[h, :], in1=b[h, :], op=op),
                                       reads=[pb_], writes=[pb_])
        tt(mag, are, dtb, ALU.mult)
        fw.op(act, lambda e: e.activation(out=mag[h, :], in_=mag[h, :], func=AF.Exp), reads=[pb_], writes=[pb_])
        tt(ang, aim, dtb, ALU.mult)
        self.sin_rr(dve, sn[h, :], ang[h, :], pb_, kk[h, :], rr[h, :])
        thr = A()
        fw.op(dve, lambda e: e.tensor_copy(out=thr[h, :], in_=rr[h, :]), reads=[pb_], writes=[pb_])
        self.sin_rr(dve, cs[h, :], ang[h, :], pb_, kk[h, :], rr[h, :], shift=math.pi / 2)
        lre, lim = A(), A()
        tt(lre, mag, cs, ALU.mult)
        tt(lim, mag, sn, ALU.mult)
        tt(t1, are, are, ALU.mult)
        tt(t2, aim, aim, ALU.mult)
        tt(den, t1, t2, ALU.add)
        fw.op(dve, lambda e: e.reciprocal(out=den[h, :], in_=den[h, :]), reads=[pb_], writes=[pb_])
        lm1 = A()
        fw.op(dve, lambda e: e.tensor_scalar(out=lm1[h, :], in0=lre[h, :], scalar1=-1.0, scalar2=None, op0=ALU.add),
              reads=[pb_], writes=[pb_])
        tt(t1, lm1, are, ALU.mult)
        tt(t2, lim, aim, ALU.mult)
        tt(fre, t1, t2, ALU.add)
        tt(fre, fre, den, ALU.mult)
        tt(t1, lim, are, ALU.mult)
        tt(t2, lm1, aim, ALU.mult)
        tt(fim, t1, t2, ALU.subtract)
        tt(fim, fim, den, ALU.mult)
        PQv = PQ.rearrange("p (a b) -> p a b", a=6)
        pqb = Buf("pq")
        for n_, srcp in enumerate((mag, thr, cs, sn, fre, fim)):
            fw.op(pe, lambda e, n_=n_, srcp=srcp: e.transpose(out=self.bank(0)[:, n_ * 64:(n_ + 1) * 64],
                                                              in_=srcp[0:64, :], identity=self.ident[0:64, 0:64]),
                  reads=[pb_, self.cb], writes=[self.pbank[0]])
        fw.op(dve, lambda e: e.tensor_copy(out=PQ, in_=self.bank(0)[:, 0:384]), reads=[self.pbank[0]], writes=[pqb])
        RHO, THR, COS1, SIN1, FRE, FIM = [PQv[:, n_, :] for n_ in range(6)]
        bre = self.sb(64 * 16)
        bim = self.sb(64 * 16)
        bbr = self.sb(64 * 16)
        bbi = self.sb(64 * 16)
        tb1 = self.sb(64 * 16)
        bb_ = Buf("bb")
        v3 = lambda a: a.rearrange("p (j c) -> p j c", c=16)
        fw.dma(out=v3(bre), in_=W["ssm_b_re"][0].rearrange("(j two) p c -> (two p) j c", two=2), writes=[bb_])
        fw.dma(out=v3(bim), in_=W["ssm_b_im"][0].rearrange("(j two) p c -> (two p) j c", two=2), writes=[bb_])
        bc = lambda a: a.unsqueeze(2).to_broadcast([128, 64, 16])
        t3 = lambda o, a, b, op: fw.op(dve, lambda e: e.tensor_tensor(out=v3(o), in0=v3(a), in1=b, op=op),
                                       reads=[bb_, pqb], writes=[bb_])
        t3(bbr, bre, bc(FRE), ALU.mult)
        t3(tb1, bim, bc(FIM), ALU.mult)
        t3(bbr, bbr, v3(tb1), ALU.subtract)
        t3(bbi, bim, bc(FRE), ALU.mult)
        t3(tb1, bre, bc(FIM), ALU.mult)
        t3(bbi, bbi, v3(tb1), ALU.add)
        LBv = [a.rearrange("p (J q) -> p J q", J=16) for a in LB]
        lbb = Buf("LB")
        arr = self.sb(16 * 128)
        arrb = Buf("arr")
        arr5 = arr.rearrange("p (J jj two c) -> p J jj two c", J=16, jj=4, two=2)
        for ri, bbx in enumerate((bbr, bbi)):
            fw.op(pool, lambda e: e.memset(arr, 0.0), writes=[arrb])
            b4 = bbx.rearrange("p (J jj c) -> p J jj c", J=16, jj=4)
            for two in range(2):
                ps = slice(two * 64, (two + 1) * 64)
                fw.op(pool, lambda e, two=two, ps=ps, b4=b4: e.tensor_copy(out=arr5[ps, :, :, two, :], in_=b4[ps]),
                      reads=[bb_, arrb], writes=[arrb])
            av = arr.rearrange("p (J x) -> p J x", J=16)
            for g in range(4):
                bk = 1 + g
                for j4 in range(4):
                    J = g * 4 + j4
                    fw.op(pe, lambda e, J=J, j4=j4, bk=bk: e.transpose(out=self.bank(bk)[:, j4 * 128:(j4 + 1) * 128],
                                                                       in_=av[:, J, :], identity=self.ident),
                          reads=[arrb, self.cb], writes=[self.pbank[bk]])
                fw.op(act, lambda e, g=g, bk=bk, ri=ri: e.activation(
                    out=LBv[ri][:, g * 4:(g + 1) * 4, :], in_=self.bank(bk).rearrange("p (a b) -> p a b", a=4),
                    func=AF.Copy), reads=[self.pbank[bk]], writes=[lbb])
        fw.op(pool, lambda e: e.memset(m96, 1.0), writes=[lbb])
        fw.op(pool, lambda e: e.affine_select(out=m96, in_=m96, pattern=[[0, 1]], compare_op=ALU.is_ge, fill=0.0,
                                              base=-96, channel_multiplier=1), reads=[lbb], writes=[lbb])
        for ri in range(2):
            fw.op(dve, lambda e, ri=ri: e.tensor_scalar(out=LBz[ri][64:128, :], in0=LB[ri][64:128, :],
                                                       scalar1=m96[64:128, :], scalar2=None, op0=ALU.mult),
                  reads=[lbb], writes=[lbb])
        CLv = [a.rearrange("p (J q) -> p J q", J=16) for a in CL]
        clb = Buf("CL")
        for ri, cname in enumerate(("ssm_c_re", "ssm_c_im")):
            fw.op(pool, lambda e: e.memset(arr, 0.0), writes=[arrb])
            a4 = arr.rearrange("p (J two q) -> p J two q", J=16, two=2)
            csrc = W[cname][0].rearrange("(J jj two) c p -> jj two c J p", jj=4, two=2)
            for jj in range(4):
                for two in range(2):
                    r0 = jj * 32 + two * 16
                    fw.dma(out=a4[r0:r0 + 16, :, two, :], in_=csrc[jj, two], writes=[arrb])
            av = arr.rearrange("p (J x) -> p J x", J=16)
            for g in range(4):
                bk = 1 + g
                for j4 in range(4):
                    J = g * 4 + j4
                    fw.op(pe, lambda e, J=J, j4=j4, bk=bk: e.transpose(out=self.bank(bk)[:, j4 * 128:(j4 + 1) * 128],
                                                                       in_=av[:, J, :], identity=self.ident),
                          reads=[arrb, self.cb], writes=[self.pbank[bk]])
                fw.op(act, lambda e, g=g, bk=bk, ri=ri: e.activation(
                    out=CLv[ri][:, g * 4:(g + 1) * 4, :], in_=self.bank(bk).rearrange("p (a b) -> p a b", a=4),
                    func=AF.Copy, scale=(1.0 if ri == 0 else -1.0)), reads=[self.pbank[bk]], writes=[clb])
        dskb = Buf("dsk")
        d16 = self.sb(128)
        fw.dma(out=d16[0:16, :], in_=W["ssm_d"][0].rearrange("(J p) -> J p", p=128), writes=[dskb])
        fw.op(pe, lambda e: e.transpose(out=self.bank(5)[:, 0:16], in_=d16[0:16, :], identity=self.ident[0:16, 0:16]),
              reads=[dskb, self.cb], writes=[self.pbank[5]])
        fw.op(dve, lambda e: e.tensor_copy(out=dsk, in_=self.bank(5)[:, 0:16]), reads=[self.pbank[5]], writes=[dskb])
        taub = Buf("tau")
        fw.op(pool, lambda e: e.iota(out=tau, pattern=[[1, 512]], base=0, channel_multiplier=0,
                                     allow_small_or_imprecise_dtypes=True), writes=[taub])
        self.release(mT)
        NW = 2
        tabc = [self.sb(512) for _ in range(NW)]
        tabs = [self.sb(512) for _ in range(NW)]
        tk = [self.sb(512) for _ in range(NW)]
        tr_ = [self.sb(512) for _ in range(NW)]
        tang = [self.sb(512) for _ in range(NW)]
        tabb = [Buf() for _ in range(NW)]
        wk = [[self.sb(512) for _ in range(6)] for _ in range(NW)]
        wkb = [Buf() for _ in range(NW)]
        st8 = [self.sb(8) for _ in range(4)]
        st8b = [Buf() for _ in range(4)]
        clm = [self.sb(4 * 128) for _ in range(2)]
        clmb = Buf("clm")
        ufl0 = self.sb(T)
        ufl = [ufl0, ufl0]
        uflb0 = Buf()
        uflb = [uflb0, uflb0]
        yo = [self.sb(512) for _ in range(2)]
        yob = [Buf(), Buf()]
        yab0 = self.sb(T, BF16)
        yab = [yab0, yab0]
        yabb0 = Buf()
        yabb = [yabb0, yabb0]
        cnt = 0
        for J in range(16):
            js = J % 2
            fw.dma(out=ufl[js], in_=U[J], reads=[Ub], writes=[uflb[js]])
            for ri in range(2):
                cm = clm[ri].rearrange("p (jj x) -> p jj x", jj=4)
                fw.op(pool, lambda e, ri=ri: e.memset(clm[ri], 0.0), writes=[clmb])
                for jj in range(4):
                    fw.op(pool, lambda e, ri=ri, jj=jj, cm=cm, J=J: e.tensor_copy(
                        out=cm[:, jj, jj * 32:(jj + 1) * 32], in_=CLv[ri][:, J, jj * 32:(jj + 1) * 32]),
                        reads=[clb, clmb], writes=[clmb])
            for jj in range(4):
                fw.op(dve, lambda e, jj=jj: e.memset(st8[jj], 0.0), writes=[st8b[jj]])
            tabsets = {}
            for tc in range(4):
                ybk = 7
                for jj in range(4):
                    j = J * 4 + jj
                    w = cnt % NW
                    cnt += 1
                    fw.op(dve, lambda e, w=w, j=j: e.tensor_scalar(out=tang[w], in0=tau, scalar1=THR[:, j:j + 1],
                                                                   scalar2=None, op0=ALU.mult),
                          reads=[taub, pqb], writes=[tabb[w]])
                    self.sin_rr(dve, tabs[w], tang[w], tabb[w], tk[w], tr_[w])
                    self.sin_rr(dve, tabc[w], tang[w], tabb[w], tk[w], tr_[w], shift=math.pi / 2)
                    rows = slice(jj * 32, (jj + 1) * 32) if jj < 3 else slice(64, 128)
                    LBu = LBv if jj < 3 else LBzv
                    for ri, bk in ((0, 5), (1, 6)):
                        fw.op(pe, lambda e, ri=ri, bk=bk, rows=rows, J=J, tc=tc, LBu=LBu: e.matmul(
                            self.bank(bk), lhsT=LBu[ri][rows, J, :], rhs=bigv[rows, J, tc * 512:(tc + 1) * 512],
                            start=True, stop=True), reads=[lbb, uTb_buf], writes=[self.pbank[bk]])
                    btr, bti, rre, rim, ta, tb = wk[w]
                    B = wkb[w]
                    fw.op(dve, lambda e, w=w, btr=btr: e.tensor_tensor(out=btr, in0=self.bank(5), in1=tabc[w], op=ALU.mult),
                          reads=[self.pbank[5], tabb[w]], writes=[B])
                    fw.op(dve, lambda e, w=w, ta=ta: e.tensor_tensor(out=ta, in0=self.bank(6), in1=tabs[w], op=ALU.mult),
                          reads=[self.pbank[6], tabb[w]], writes=[B])
                    fw.op(pool, lambda e, btr=btr, ta=ta: e.tensor_tensor(out=btr, in0=btr, in1=ta, op=ALU.add),
                          reads=[B], writes=[B])
                    fw.op(dve, lambda e, w=w, bti=bti: e.tensor_tensor(out=bti, in0=self.bank(6), in1=tabc[w], op=ALU.mult),
                          reads=[self.pbank[6], tabb[w]], writes=[B])
                    fw.op(dve, lambda e, w=w, tb=tb: e.tensor_tensor(out=tb, in0=self.bank(5), in1=tabs[w], op=ALU.mult),
                          reads=[self.pbank[5], tabb[w]], writes=[B])
                    fw.op(pool, lambda e, bti=bti, tb=tb: e.tensor_tensor(out=bti, in0=bti, in1=tb, op=ALU.subtract),
                          reads=[B], writes=[B])
                    c8 = st8[jj]
                    cb8 = st8b[jj]
                    fw.op(dve, lambda e, c8=c8, j=j: e.tensor_scalar(out=c8[:, 2:3], in0=c8[:, 0:1],
                                                                     scalar1=COS1[:, j:j + 1], scalar2=None, op0=ALU.mult),
                          reads=[cb8, pqb], writes=[cb8])
                    fw.op(dve, lambda e, c8=c8, j=j: e.tensor_scalar(out=c8[:, 4:5], in0=c8[:, 1:2],
                                                                     scalar1=SIN1[:, j:j + 1], scalar2=None, op0=ALU.mult),
                          reads=[cb8, pqb], writes=[cb8])
                    fw.op(dve, lambda e, c8=c8: e.tensor_tensor(out=c8[:, 2:3], in0=c8[:, 2:3], in1=c8[:, 4:5],
                                                                op=ALU.subtract), reads=[cb8], writes=[cb8])
                    fw.op(dve, lambda e, c8=c8, j=j: e.tensor_scalar(out=c8[:, 3:4], in0=c8[:, 0:1],
                                                                     scalar1=SIN1[:, j:j + 1], scalar2=None, op0=ALU.mult),
                          reads=[cb8, pqb], writes=[cb8])
                    fw.op(dve, lambda e, c8=c8, j=j: e.tensor_scalar(out=c8[:, 4:5], in0=c8[:, 1:2],
                                                                     scalar1=COS1[:, j:j + 1], scalar2=None, op0=ALU.mult),
                          reads=[cb8, pqb], writes=[cb8])
                    fw.op(dve, lambda e, c8=c8: e.tensor_tensor(out=c8[:, 3:4], in0=c8[:, 3:4], in1=c8[:, 4:5],
                                                                op=ALU.add), reads=[cb8], writes=[cb8])
                    rho_b = RHO[:, j:j + 1].to_broadcast([128, 512])
                    fw.op(dve, lambda e, rre=rre, btr=btr, c8=c8, rho_b=rho_b: e.tensor_tensor_scan(
                        out=rre, data0=rho_b, data1=btr, initial=c8[:, 2:3], op0=ALU.mult, op1=ALU.add),
                        reads=[B, cb8, pqb], writes=[B])
                    fw.op(dve, lambda e, rim=rim, bti=bti, c8=c8, rho_b=rho_b: e.tensor_tensor_scan(
                        out=rim, data0=rho_b, data1=bti, initial=c8[:, 3:4], op0=ALU.mult, op1=ALU.add),
                        reads=[B, cb8, pqb], writes=[B])
                    fw.op(pool, lambda e, w=w, ta=ta, rre=rre: e.tensor_tensor(out=ta, in0=rre, in1=tabc[w], op=ALU.mult),
                          reads=[B, tabb[w]], writes=[B])
                    fw.op(pool, lambda e, w=w, tb=tb, rim=rim: e.tensor_tensor(out=tb, in0=rim, in1=tabs[w], op=ALU.mult),
                          reads=[B, tabb[w]], writes=[B])
                    fw.op(pool, lambda e, w=w, ta=ta, tb=tb: e.tensor_tensor(out=ta, in0=ta, in1=tb, op=ALU.subtract),
                          reads=[B], writes=[B])
                    fw.op(pool, lambda e, w=w, tb=tb, rre=rre: e.tensor_tensor(out=tb, in0=rre, in1=tabs[w], op=ALU.mult),
                          reads=[B, tabb[w]], writes=[B])
                    fw.op(pool, lambda e, w=w, rre=rre, rim=rim: e.tensor_tensor(out=rre, in0=rim, in1=tabc[w], op=ALU.mult),
                          reads=[B, tabb[w]], writes=[B])
                    fw.op(pool, lambda e, tb=tb, rre=rre: e.tensor_tensor(out=tb, in0=tb, in1=rre, op=ALU.add),
                          reads=[B], writes=[B])
                    fw.op(dve, lambda e, c8=c8, ta=ta: e.tensor_copy(out=c8[:, 0:1], in_=ta[:, 511:512]),
                          reads=[B, cb8], writes=[cb8])
                    fw.op(dve, lambda e, c8=c8, tb=tb: e.tensor_copy(out=c8[:, 1:2], in_=tb[:, 511:512]),
                          reads=[B, cb8], writes=[cb8])
                    cm0 = clm[0].rearrange("p (jj x) -> p jj x", jj=4)
                    cm1 = clm[1].rearrange("p (jj x) -> p jj x", jj=4)
                    fw.op(pe, lambda e, ta=ta, jj=jj, cm0=cm0: e.matmul(self.bank(ybk), lhsT=cm0[:, jj, :], rhs=ta,
                                                                        start=(jj == 0), stop=False),
                          reads=[clmb, B], writes=[self.pbank[ybk]])
                    fw.op(pe, lambda e, tb=tb, jj=jj, cm1=cm1: e.matmul(self.bank(ybk), lhsT=cm1[:, jj, :], rhs=tb,
                                                                        start=False, stop=(jj == 3)),
                          reads=[clmb, B], writes=[self.pbank[ybk]])
                ys = (J * 4 + tc) % 2
                fw.op(dve, lambda e, ys=ys, js=js, J=J, tc=tc: e.scalar_tensor_tensor(
                    out=yo[ys], in0=ufl[js][:, tc * 512:(tc + 1) * 512], scalar=dsk[:, J:J + 1], in1=self.bank(ybk),
                    op0=ALU.mult, op1=ALU.add), reads=[uflb[js], dskb, self.pbank[ybk]], writes=[yob[ys]])
                fw.op(act, lambda e, ys=ys, js=js, tc=tc: e.activation(out=yab[js][:, tc * 512:(tc + 1) * 512],
                                                                     in_=yo[ys], func=AF.Gelu),
                      reads=[yob[ys]], writes=[yabb[js]])
            fw.dma(out=YA[J], in_=yab[js], reads=[yabb[js]], writes=[YAb])
        self.release(mP)
        for q in range(4):
            fw.dma(out=bigv[:, q * 4:(q + 1) * 4, :], in_=YA.rearrange("j p t -> p j t")[:, q * 4:(q + 1) * 4, :],
                   reads=[YAb], writes=[xTb])
        mG = self.mark()
        wt = [self.sb(16 * 128, BF16) for _ in range(2)]
        wtb = [Buf(), Buf()]
        bgl = self.sb(16)
        bglb = Buf()
        b16 = self.sb(128)
        fw.dma(out=b16[0:16, :], in_=W["ssm_b_glu"][0].rearrange("(J p) -> J p", p=128), writes=[bglb])
        fw.op(pe, lambda e: e.transpose(out=self.bank(5)[:, 0:16], in_=b16[0:16, :], identity=self.ident[0:16, 0:16]),
              reads=[bglb, self.cb], writes=[self.pbank[5]])
        fw.op(dve, lambda e: e.tensor_copy(out=bgl, in_=self.bank(5)[:, 0:16]), reads=[self.pbank[5]], writes=[bglb])
        sg = [self.sb(512) for _ in range(2)]
        sgb = [Buf(), Buf()]
        y2 = [self.sb(T, BF16) for _ in range(2)]
        y2b = [Buf(), Buf()]
        for mo in range(16):
            s = mo % 2
            self.proj_fm(bigv, xTb, W["ssm_w_glu"][0][:, mo * 128:(mo + 1) * 128], wt[s], wtb[s])
            for tc in range(4):
                s2 = tc % 2
                fw.op(act, lambda e, s2=s2, tc=tc, mo=mo: e.activation(out=sg[s2], in_=self.bank(tc), func=AF.Sigmoid,
                                                                     bias=bgl[:, mo:mo + 1], scale=1.0),
                      reads=[self.pbank[tc], bglb], writes=[sgb[s2]])
                fw.op(dve, lambda e, s=s, s2=s2, tc=tc, mo=mo: e.tensor_tensor(
                    out=y2[s][:, tc * 512:(tc + 1) * 512], in0=sg[s2], in1=bigv[:, mo, tc * 512:(tc + 1) * 512],
                    op=ALU.mult), reads=[sgb[s2], xTb], writes=[y2b[s]])
            fw.dma(out=OT[mo], in_=y2[s], reads=[y2b[s]], writes=[OTb])
        self.release(mG)
        self.release(m0)
        self.outproj_ln(OT, OTb, W["ssm_w_out"][0], src, src_buf, W["ln_g"][L, 0], W["ln_b"][L, 0], dst, dst_buf)

    def dsa(self, L, src, src_buf, W, dst, dst_buf):
        fw = self.fw
        dve, act, pool, pe = fw.dve, fw.act, fw.pool, fw.pe
        QT, QTb = self.dram["QT"], self.dbuf["QT"]
        QI, QIb = self.dram["QI"], self.dbuf["QI"]
        MT, MTb = self.dram["MASKT"], self.dbuf["MASKT"]
        OT, OTb = self.dram["OT"], self.dbuf["OT"]
        w_in = W["dsa_w_in"][0]
        m0 = self.mark()
        kT = self.sb(T, BF16)
        kiT = self.sb(T, BF16)
        vtok = self.sb(16 * 132, BF16)
        vtv = vtok.rearrange("p (a b) -> p a b", a=16)
        wi = self.sb(16 * 16)
        wiv = wi.rearrange("p (a b) -> p a b", a=16)
        resb = Buf("dsa_res")
        fw.op(pool, lambda e: e.memset(vtok, 1.0), writes=[resb])
        m1 = self.mark()
        big = self.sb(16 * T, BF16)
        bigv = big.rearrange("p (a b) -> p a b", a=16)
        xTb = Buf("xT")
        self.build_xT(src, src_buf, bigv, xTb)
        wt = [self.sb(16 * 128, BF16) for _ in range(2)]
        wtb = [Buf(), Buf()]
        ob = [self.sb(T, BF16) for _ in range(2)]
        obb = [Buf(), Buf()]
        QSC = 128.0 ** -0.5
        n = 0
        for (col0, cnt_, dstD, dstDb, scale) in ((0, 16, QT, QTb, QSC), (2304, 16, QI, QIb, 1.0)):
            for hh in range(cnt_):
                s = n % 2
                n += 1
                self.proj_fm(bigv, xTb, w_in[:, col0 + hh * 128: col0 + (hh + 1) * 128], wt[s], wtb[s])
                for tc in range(4):
                    dstp = ob[s][:, tc * 512:(tc + 1) * 512]
                    if tc % 2 == 0:
                        fw.op(dve, lambda e, dstp=dstp, tc=tc, scale=scale: e.tensor_scalar(
                            out=dstp, in0=self.bank(tc), scalar1=scale, scalar2=None, op0=ALU.mult),
                            reads=[self.pbank[tc]], writes=[obb[s]])
                    else:
                        fw.op(act, lambda e, dstp=dstp, tc=tc, scale=scale: e.activation(
                            out=dstp, in_=self.bank(tc), func=AF.Copy, scale=scale),
                            reads=[self.pbank[tc]], writes=[obb[s]])
                fw.dma(out=dstD[hh], in_=ob[s], reads=[obb[s]], writes=[dstDb])
        for (col0, dstT) in ((2048, kT), (4352, kiT)):
            s = n % 2
            n += 1
            self.proj_fm(bigv, xTb, w_in[:, col0: col0 + 128], wt[s], wtb[s])
            for tc in range(4):
                dstp = dstT[:, tc * 512:(tc + 1) * 512]
                fw.op(act, lambda e, dstp=dstp, tc=tc: e.activation(out=dstp, in_=self.bank(tc), func=AF.Copy),
                      reads=[self.pbank[tc]], writes=[resb])
        wv = wt[0]
        wvv = wv.rearrange("p (a b) -> p a b", a=16)
        fw.dma(out=wvv, in_=w_in[:, 2176:2304].rearrange("(kc p) f -> p kc f", p=128), writes=[wtb[0]], q=pool)
        ww = wt[1][:, 0:256]
        wwv = ww.rearrange("p (a b) -> p a b", a=16)
        fw.dma(out=wwv, in_=w_in[:, 4480:4496].rearrange("(kc p) f -> p kc f", p=128), writes=[wtb[1]], q=pool)
        WSC = (16.0 ** -0.5) * (128.0 ** -0.5)
        for i in range(NT):
            bk = 4 + (i % 2)
            for kc in range(16):
                fw.op(pe, lambda e, i=i, kc=kc, bk=bk: e.matmul(self.bank(bk)[:, 0:128], lhsT=bigv[:, kc, i * 128:(i + 1) * 128],
                                                               rhs=wvv[:, kc, :], start=(kc == 0), stop=(kc == 15)),
                      reads=[xTb, wtb[0]], writes=[self.pbank[bk]])
            fw.op(act, lambda e, i=i, bk=bk: e.activation(out=vtv[:, i, 0:128], in_=self.bank(bk)[:, 0:128], func=AF.Copy),
                  reads=[self.pbank[bk]], writes=[resb])
            bk2 = 6 + (i % 2)
            for kc in range(16):
                fw.op(pe, lambda e, i=i, kc=kc, bk2=bk2: e.matmul(self.bank(bk2)[:, 0:16], lhsT=bigv[:, kc, i * 128:(i + 1) * 128],
                                                                 rhs=wwv[:, kc, :], start=(kc == 0), stop=(kc == 15)),
                      reads=[xTb, wtb[1]], writes=[self.pbank[bk2]])
            fw.op(dve, lambda e, i=i, bk2=bk2: e.tensor_scalar(out=wiv[:, i, :], in0=self.bank(bk2)[:, 0:16], scalar1=WSC,
                                                               scalar2=None, op0=ALU.mult),
                  reads=[self.pbank[bk2]], writes=[resb])
        self.release(m1)
        m2 = self.mark()
        qit = [self.sb(16 * 128, BF16) for _ in range(2)]
        qitb = [Buf(), Buf()]
        acc = self.sb(T)
        accb = Buf("acc")
        work = self.sb(T)
        workb = Buf("work")
        rl = [self.sb(512, BF16) for _ in range(2)]
        rlb = [Buf(), Buf()]
        m8 = self.sb(8)
        m8b = Buf()
        mk = self.sb(T, BF16)
        mkb = Buf()
        mT = [self.sb(16 * 128, BF16) for _ in range(2)]
        mTb = [Buf(), Buf()]
        QIv = QI.rearrange("h p t -> p h t")
        MTv = MT.rearrange("b p t -> p b t")
        nr = 0
        for i in range(NT):
            s = i % 2
            S_ = 128 * (i + 1)
            fw.dma(out=qit[s].rearrange("p (a b) -> p a b", a=16), in_=QIv[:, :, i * 128:(i + 1) * 128], reads=[QIb],
                   writes=[qitb[s]])
            qv = qit[s].rearrange("p (a b) -> p a b", a=16)
            nsc = (S_ + 511) // 512
            for hh in range(16):
                for sc in range(nsc):
                    c0 = sc * 512
                    c1 = min(S_, c0 + 512)
                    bk = nr % 4
                    r2 = nr % 2
                    nr += 1
                    fw.op(pe, lambda e, hh=hh, c0=c0, c1=c1, bk=bk, qv=qv: e.matmul(
                        self.bank(bk)[:, 0:c1 - c0], lhsT=qv[:, hh, :], rhs=kiT[:, c0:c1], start=True, stop=True),
                        reads=[qitb[s], resb], writes=[self.pbank[bk]])
                    fw.op(act, lambda e, c0=c0, c1=c1, bk=bk, r2=r2: e.activation(
                        out=rl[r2][:, 0:c1 - c0], in_=self.bank(bk)[:, 0:c1 - c0], func=AF.Relu),
                        reads=[self.pbank[bk]], writes=[rlb[r2]])
                    if hh == 0:
                        fw.op(dve, lambda e, c0=c0, c1=c1, r2=r2, i=i: e.tensor_scalar(
                            out=acc[:, c0:c1], in0=rl[r2][:, 0:c1 - c0], scalar1=wiv[:, i, 0:1], scalar2=None,
                            op0=ALU.mult), reads=[rlb[r2], resb], writes=[accb])
                    else:
                        fw.op(dve, lambda e, c0=c0, c1=c1, r2=r2, i=i, hh=hh: e.scalar_tensor_tensor(
                            out=acc[:, c0:c1], in0=rl[r2][:, 0:c1 - c0], scalar=wiv[:, i, hh:hh + 1], in1=acc[:, c0:c1],
                            op0=ALU.mult, op1=ALU.add), reads=[rlb[r2], resb, accb], writes=[accb])
            fw.op(pool, lambda e, S_=S_: e.affine_select(out=acc[:, S_ - 128:S_], in_=acc[:, S_ - 128:S_],
                                                         pattern=[[-1, 128]], compare_op=ALU.is_ge, fill=-1e30, base=0,
                                                         channel_multiplier=1), reads=[accb], writes=[accb])
            if i >= 2:
                cur = acc
                curb = accb
                for rnd in range(32):
                    fw.op(dve, lambda e, cur=cur, S_=S_: e.max(out=m8, in_=cur[:, 0:S_]), reads=[curb], writes=[m8b])
                    if rnd < 31:
                        fw.op(dve, lambda e, cur=cur, S_=S_: e.match_replace(out=work[:, 0:S_], in_to_replace=m8,
                                                                             in_values=cur[:, 0:S_], imm_value=-3e38),
                              reads=[curb, m8b], writes=[workb])
                        cur = work
                        curb = workb
                fw.op(dve, lambda e, S_=S_: e.tensor_scalar(out=mk[:, 0:S_], in0=acc[:, 0:S_], scalar1=m8[:, 7:8],
                                                            scalar2=None, op0=ALU.is_ge), reads=[accb, m8b], writes=[mkb])
            else:
                fw.op(dve, lambda e, S_=S_: e.tensor_scalar(out=mk[:, 0:S_], in0=acc[:, 0:S_], scalar1=-1e29,
                                                            scalar2=None, op0=ALU.is_ge), reads=[accb], writes=[mkb])
            mTv = mT[s].rearrange("p (a b) -> p a b", a=16)
            for g in range((i + 8) // 8):
                bk = 4 + (g + i) % 2
                bkb = self.bank(bk, BF16)
                nb_ = min(8, i + 1 - g * 8)
                for j in range(nb_):
                    b = g * 8 + j
                    fw.op(pe, lambda e, b=b, j=j, bkb=bkb: e.transpose(out=bkb[:, j * 128:(j + 1) * 128],
                                                                      in_=mk[:, b * 128:(b + 1) * 128], identity=self.identb),
                          reads=[mkb, self.cb], writes=[self.pbank[bk]])
                fw.op(act, lambda e, g=g, nb_=nb_, bkb=bkb, mTv=mTv: e.activation(
                    out=mTv[:, g * 8:g * 8 + nb_, :], in_=bkb[:, 0:nb_ * 128].rearrange("p (a b) -> p a b", a=nb_),
                    func=AF.Copy), reads=[self.pbank[bk]], writes=[mTb[s]])
            fw.dma(out=MTv[:, 0:i + 1, i * 128:(i + 1) * 128], in_=mTv[:, 0:i + 1, :], reads=[mTb[s]], writes=[MTb])
        self.release(m2)
        m3 = self.mark()
        A1 = self.sb(2432)
        a1b = Buf("A1")
        fw.op(pool, lambda e: e.iota(out=A1, pattern=[[-1, 2432]], base=384, channel_multiplier=1,
                                     allow_small_or_imprecise_dtypes=True), writes=[a1b])
        fw.op(pool, lambda e: e.tensor_scalar(out=A1, in0=A1, scalar1=0.0, scalar2=None, op0=ALU.min), reads=[a1b],
              writes=[a1b])
        mres = self.sb(16 * T, BF16)
        mrv = mres.rearrange("p (a b) -> p a b", a=16)
        mrb = Buf("maskres")
        fw.op(pool, lambda e: e.memset(mres, 0.0), writes=[mrb])
        for b in range(16):
            fw.dma(out=mrv[:, b, b * 128:T], in_=MT[b][:, b * 128:T], reads=[MTb], writes=[mrb])
        qh = [self.sb(T, BF16) for _ in range(2)]
        qhb = [Buf(), Buf()]
        oth = [self.sb(T, BF16) for _ in range(2)]
        othb = [Buf(), Buf()]
        tmp = [self.sb(512) for _ in range(2)]
        tmpb = [Buf(), Buf()]
        pp = [self.sb(512, BF16) for _ in range(2)]
        ppb = [Buf(), Buf()]
        pm = [self.sb(512, BF16) for _ in range(2)]
        pmb = [Buf(), Buf()]
        rden = self.sb(4)
        rdb = Buf()
        on = self.sb(512, BF16)
        onb = Buf()
        nu = 0
        ng = 0
        for hh in range(16):
            s = hh % 2
            slope = 2.0 ** (-(hh + 1) / 2.0)
            fw.dma(out=qh[s], in_=QT[hh], reads=[QTb], writes=[qhb[s]])
            for c in range(4):
                tbk = 6 + (ng % 2)
                ng += 1
                nb = 4 * (c + 1)
                for b in range(nb):
                    u = nu % 2
                    nu += 1
                    lbk = u
                    fw.op(pe, lambda e, b=b, c=c, lbk=lbk, s=s: e.matmul(
                        self.bank(lbk), lhsT=kT[:, b * 128:(b + 1) * 128], rhs=qh[s][:, c * 512:(c + 1) * 512],
                        start=True, stop=True), reads=[resb, qhb[s]], writes=[self.pbank[lbk]])
                    off = 512 * c - 128 * b + 384
                    fw.op(dve, lambda e, u=u, off=off, lbk=lbk, slope=slope: e.scalar_tensor_tensor(
                        out=tmp[u], in0=A1[:, off:off + 512], scalar=slope, in1=self.bank(lbk), op0=ALU.mult, op1=ALU.add),
                        reads=[a1b, self.pbank[lbk]], writes=[tmpb[u]])
                    fw.op(act, lambda e, u=u: e.activation(out=pp[u], in_=tmp[u], func=AF.Exp), reads=[tmpb[u]],
                          writes=[ppb[u]])
                    fw.op(pool, lambda e, u=u, b=b, c=c: e.tensor_tensor(out=pm[u], in0=pp[u],
                                                                        in1=mrv[:, b, c * 512:(c + 1) * 512], op=ALU.mult),
                          reads=[ppb[u], mrb], writes=[pmb[u]])
                    for sub in range(4):
                        tt_ = 4 * c + sub
                        if b > tt_:
                            continue
                        obk = 2 + sub
                        fw.op(pe, lambda e, u=u, sub=sub, b=b, tt_=tt_, obk=obk: e.matmul(
                            self.bank(obk)[:, 0:129], lhsT=pm[u][:, sub * 128:(sub + 1) * 128],
                            rhs=vtv[:, b, 0:129], start=(b == 0), stop=(b == tt_)),
                            reads=[pmb[u], resb], writes=[self.pbank[obk]])
                for sub in range(4):
                    obk = 2 + sub
                    fw.op(dve, lambda e, obk=obk, sub=sub: e.reciprocal(out=rden[:, sub:sub + 1], in_=self.bank(obk)[:, 128:129]),
                          reads=[self.pbank[obk]], writes=[rdb])
                    fw.op(dve, lambda e, sub=sub, obk=obk: e.tensor_scalar(
                        out=on[:, sub * 128:(sub + 1) * 128], in0=self.bank(obk)[:, 0:128],
                        scalar1=rden[:, sub:sub + 1], scalar2=None, op0=ALU.mult),
                        reads=[self.pbank[obk], rdb], writes=[onb])
                tbb = self.bank(tbk, BF16)
                for sub in range(4):
                    fw.op(pe, lambda e, sub=sub, tbb=tbb: e.transpose(out=tbb[:, sub * 128:(sub + 1) * 128],
                                                                      in_=on[:, sub * 128:(sub + 1) * 128], identity=self.identb),
                          reads=[onb, self.cb], writes=[self.pbank[tbk]])
                fw.op(act, lambda e, s=s, c=c, tbb=tbb: e.activation(out=oth[s][:, c * 512:(c + 1) * 512], in_=tbb[:, 0:512],
                                                                      func=AF.Copy), reads=[self.pbank[tbk]], writes=[othb[s]])
            fw.dma(out=OT[hh], in_=oth[s], reads=[othb[s]], writes=[OTb])
        self.release(m3)
        self.release(m0)
        self.outproj_ln(OT, OTb, W["dsa_w_out"][0], src, src_buf, W["ln_g"][L, 0], W["ln_b"][L, 0], dst, dst_buf)

    def gdn(self, li, L, src, src_buf, W, dst, dst_buf):
        fw = self.fw
        dve, act, pool, pe = fw.dve, fw.act, fw.pool, fw.pe
        OT, OTb = self.dram["OT"], self.dbuf["OT"]
        w_in = W["gdn_w_in"][li]
        m0 = self.mark()
        A128 = lambda: self.sb(128)
        U2, B2, NEGM4 = A128(), A128(), self.sb(512)
        gcb = Buf("gdnconst")
        fw.op(pool, lambda e: e.memset(U2, 1.0), writes=[gcb])
        fw.op(pool, lambda e: e.affine_select(out=U2, in_=U2, pattern=[[1, 128]], compare_op=ALU.is_ge, fill=0.0,
                                              base=0, channel_multiplier=-1), reads=[gcb], writes=[gcb])
        fw.op(pool, lambda e: e.memset(U2[0:64, 64:128], 0.0), reads=[gcb], writes=[gcb])
        fw.op(pool, lambda e: e.memset(B2, 0.0), writes=[gcb])
        fw.op(pool, lambda e: e.memset(B2[0:64, 0:64], 1.0), reads=[gcb], writes=[gcb])
        fw.op(pool, lambda e: e.memset(B2[64:128, 64:128], 1.0), reads=[gcb], writes=[gcb])
        N4 = NEGM4.rearrange("p (u i) -> p u i", u=4)
        fw.op(pool, lambda e: e.memset(NEGM4, 0.0), writes=[gcb])
        fw.op(pool, lambda e: e.affine_select(out=N4, in_=N4, pattern=[[0, 4], [1, 128]], compare_op=ALU.is_ge, fill=NEG,
                                              base=0, channel_multiplier=-1), reads=[gcb], writes=[gcb])
        fw.op(pool, lambda e: e.memset(N4[0:64, :, 64:128], NEG), reads=[gcb], writes=[gcb])
        id4 = self.ident.unsqueeze(1).to_broadcast([128, 4, 128])
        ABt = self.sb(16 * 32)
        ABv = ABt.rearrange("p (a b) -> p a b", a=16)
        GT, GC, EGC, EKD, NGC, BT, NB = [self.sb(256) for _ in range(7)]
        v16 = lambda a: a.rearrange("p (a b) -> p a b", a=16)
        CW = self.sb(48 * 4)
        CWv = CW.rearrange("p (c j) -> p c j", j=4)
        NGrow = self.sb(128)
        gb = Buf("gdn_g")
        self.load_row(NGrow, W["gdn_norm_g"][li], gb, 128)
        arow = self.sb(32)
        self.load_row(arow[:, 0:16], W["gdn_a_log"][li], gb, 16)
        self.load_row(arow[:, 16:32], W["gdn_dt_bias"][li], gb, 16)
        cwrow = self.sb(6144)
        fw.dma(out=cwrow[0:4, :], in_=W["gdn_conv_w"][li], writes=[gb])
        for c in range(48):
            fw.op(pe, lambda e, c=c: e.transpose(out=self.bank(4)[:, c * 4:(c + 1) * 4], in_=cwrow[0:4, c * 128:(c + 1) * 128],
                                                 identity=self.ident[0:4, 0:4]), reads=[gb, self.cb], writes=[self.pbank[4]])
        fw.op(dve, lambda e: e.tensor_copy(out=CW, in_=self.bank(4)[:, 0:192]), reads=[self.pbank[4]], writes=[gb])
        self.off -= 6144 + 0
        fw.barrier()
        big = self.sb(16 * T, BF16)
        bigv = big.rearrange("p (a b) -> p a b", a=16)
        xTb = Buf("xT")
        self.build_xT(src, src_buf, bigv, xTb)
        mg = self.mark()
        wab = self.sb(16 * 32, BF16)
        wabv = wab.rearrange("p (a b) -> p a b", a=16)
        wabb = Buf()
        fw.dma(out=wabv, in_=w_in[:, 8192:8224].rearrange("(kc p) f -> p kc f", p=128), writes=[wabb], q=pool)
        for i in range(NT):
            bk = 4 + i % 4
            for kc in range(16):
                fw.op(pe, lambda e, i=i, kc=kc, bk=bk: e.matmul(self.bank(bk)[:, 0:32], lhsT=bigv[:, kc, i * 128:(i + 1) * 128],
                                                               rhs=wabv[:, kc, :], start=(kc == 0), stop=(kc == 15)),
                      reads=[xTb, wabb], writes=[self.pbank[bk]])
            fw.op(act, lambda e, i=i, bk=bk: e.activation(out=ABv[:, i, :], in_=self.bank(bk)[:, 0:32], func=AF.Copy),
                  reads=[self.pbank[bk]], writes=[gb])
        t1, t2, t3 = self.sb(256), self.sb(256), self.sb(256)
        nea = self.sb(16)
        dtb_b = arow[:, 16:32].unsqueeze(1).to_broadcast([128, 16, 16])
        fw.op(dve, lambda e: e.tensor_tensor(out=v16(t1), in0=ABv[:, :, 0:16], in1=dtb_b, op=ALU.add), reads=[gb], writes=[gb])
        fw.op(dve, lambda e: e.tensor_scalar(out=t2, in0=t1, scalar1=-1.0, scalar2=None, op0=ALU.mult), reads=[gb], writes=[gb])
        fw.op(dve, lambda e: e.tensor_tensor(out=t2, in0=t2, in1=t1, op=ALU.max), reads=[gb], writes=[gb])
        fw.op(act, lambda e: e.activation(out=t2, in_=t2, func=AF.Exp, scale=-1.0), reads=[gb], writes=[gb])
        fw.op(dve, lambda e: e.tensor_scalar(out=t2, in0=t2, scalar1=1.0, scalar2=None, op0=ALU.add), reads=[gb], writes=[gb])
        fw.op(act, lambda e: e.activation(out=t2, in_=t2, func=AF.Ln), reads=[gb], writes=[gb])
        fw.op(dve, lambda e: e.scalar_tensor_tensor(out=t3, in0=t1, scalar=0.0, in1=t2, op0=ALU.max, op1=ALU.add),
              reads=[gb], writes=[gb])
        fw.op(act, lambda e: e.activation(out=nea, in_=arow[:, 0:16], func=AF.Exp), reads=[gb], writes=[gb])
        fw.op(dve, lambda e: e.tensor_scalar(out=nea, in0=nea, scalar1=-1.0, scalar2=None, op0=ALU.mult), reads=[gb], writes=[gb])
        fw.op(dve, lambda e: e.tensor_tensor(out=v16(GT), in0=v16(t3), in1=nea.unsqueeze(1).to_broadcast([128, 16, 16]),
                                             op=ALU.mult), reads=[gb], writes=[gb])
        fw.op(act, lambda e: e.activation(out=v16(BT), in_=ABv[:, :, 16:32], func=AF.Sigmoid), reads=[gb], writes=[gb])
        fw.op(dve, lambda e: e.tensor_scalar(out=NB, in0=BT, scalar1=-1.0, scalar2=None, op0=ALU.mult), reads=[gb], writes=[gb])
        fw.op(pe, lambda e: e.matmul(self.bank(4)[:, 0:256], lhsT=U2, rhs=GT, start=True, stop=True), reads=[gb, gcb],
              writes=[self.pbank[4]])
        fw.op(pe, lambda e: e.matmul(self.bank(5)[:, 0:256], lhsT=B2, rhs=GT, start=True, stop=True), reads=[gb, gcb],
              writes=[self.pbank[5]])
        fw.op(dve, lambda e: e.tensor_copy(out=GC, in_=self.bank(4)[:, 0:256]), reads=[self.pbank[4]], writes=[gb])
        fw.op(act, lambda e: e.activation(out=EGC, in_=self.bank(4)[:, 0:256], func=AF.Exp), reads=[self.pbank[4]], writes=[gb])
        fw.op(dve, lambda e: e.tensor_tensor(out=t1, in0=self.bank(5)[:, 0:256], in1=GC, op=ALU.subtract),
              reads=[self.pbank[5], gb], writes=[gb])
        fw.op(act, lambda e: e.activation(out=EKD, in_=t1, func=AF.Exp), reads=[gb], writes=[gb])
        fw.op(dve, lambda e: e.tensor_scalar(out=NGC, in0=GC, scalar1=-1.0, scalar2=None, op0=ALU.mult), reads=[gb], writes=[gb])
        self.release(mg)
        GCv, EGCv, EKDv, NGCv, BTv, NBv = [v16(a) for a in (GC, EGC, EKD, NGC, BT, NB)]
        wt = [self.sb(16 * 128, BF16) for _ in range(2)]
        wtb = [Buf(), Buf()]
        qT, kT, vT = self.sb(T), self.sb(T), self.sb(T)
        qkvb = Buf("qkv")
        ktok, vtok, otok = self.sb(T), self.sb(T), self.sb(T)
        ktv, vtv, otv = [a.rearrange("p (a b) -> p a b", a=16) for a in (ktok, vtok, otok)]
        tokb = Buf("tok")
        otb = Buf("otok")
        regA = self.sb(8192)
        raw = regA[:, 0:2056]
        cacc = regA[:, 2056:2056 + 2048]
        tmpq = regA[:, 4104:4104 + 2048]
        rb = Buf("raw")
        names = ["DG4", "EGR4", "Dt4", "Mt4", "Nn4", "Qa", "Qta", "Qb", "Qtb", "X4", "AT4", "KG4", "KD4", "WT4", "BU4", "QD4"]
        QTL = {n_: regA[:, i_ * 512:(i_ + 1) * 512] for i_, n_ in enumerate(names)}
        QB = {n_: Buf(n_) for n_ in names}
        q3 = lambda a: a.rearrange("p (u i) -> p u i", u=4)
        VN = self.sb(128)
        vnb = Buf("VN")
        S = self.sb(128)
        Sb = Buf("S")
        oth = [self.sb(T, BF16) for _ in range(2)]
        othb = [Buf(), Buf()]
        sm = self.sb(64)
        smb = Buf()
        fw.op(dve, lambda e: e.memset(raw[:, 0:3], 0.0), writes=[rb])
        nbk = [0]

        def nb_():
            nbk[0] += 1
            return 4 + nbk[0] % 4

        for h in range(16):
            fw.barrier()
            for kind, col0, dstT in (("q", 0, qT), ("k", 2048, kT), ("v", 4096, vT)):
                s = nbk[0] % 2
                nbk[0] += 1
                self.proj_fm(bigv, xTb, w_in[:, col0 + h * 128: col0 + (h + 1) * 128], wt[s], wtb[s])
                for tc in range(4):
                    dstp = raw[:, 3 + tc * 512: 3 + (tc + 1) * 512]
                    if tc % 2 == 0:
                        fw.op(dve, lambda e, dstp=dstp, tc=tc: e.tensor_copy(out=dstp, in_=self.bank(tc)),
                              reads=[self.pbank[tc]], writes=[rb])
                    else:
                        fw.op(act, lambda e, dstp=dstp, tc=tc: e.activation(out=dstp, in_=self.bank(tc), func=AF.Copy),
                              reads=[self.pbank[tc]], writes=[rb])
                ch = (col0 // 128) + h
                fw.op(dve, lambda e, ch=ch: e.tensor_scalar(out=cacc, in0=raw[:, 0:T], scalar1=CWv[:, ch, 0:1], scalar2=None,
                                                            op0=ALU.mult), reads=[rb, gb], writes=[rb])
                for j in range(1, 4):
                    fw.op(dve, lambda e, ch=ch, j=j: e.scalar_tensor_tensor(
                        out=cacc, in0=raw[:, j:j + T], scalar=CWv[:, ch, j:j + 1], in1=cacc, op0=ALU.mult, op1=ALU.add),
                        reads=[rb, gb], writes=[rb])
                if kind == "v":
                    fw.op(act, lambda e: e.activation(out=vT, in_=cacc, func=AF.Silu), reads=[rb], writes=[qkvb])
                    continue
                fw.op(act, lambda e: e.activation(out=tmpq, in_=cacc, func=AF.Silu), reads=[rb], writes=[rb])
                fw.op(act, lambda e: e.activation(out=cacc, in_=tmpq, func=AF.Square), reads=[rb], writes=[rb])
                for tc in range(4):
                    fw.op(pe, lambda e, tc=tc: e.matmul(self.bank(tc), lhsT=self.ones, rhs=cacc[:, tc * 512:(tc + 1) * 512],
                                                        start=True, stop=True), reads=[rb, self.cb], writes=[self.pbank[tc]])
                    cs_ = cacc[:, tc * 512:(tc + 1) * 512]
                    fw.op(dve, lambda e, tc=tc, cs_=cs_: e.tensor_scalar(out=cs_, in0=self.bank(tc), scalar1=RMS_EPS,
                                                                         scalar2=None, op0=ALU.add),
                          reads=[self.pbank[tc], rb], writes=[rb])
                fw.op(act, lambda e: e.activation(out=cacc, in_=cacc, func=AF.Sqrt), reads=[rb], writes=[rb])
                fw.op(dve, lambda e: e.reciprocal(out=cacc, in_=cacc), reads=[rb], writes=[rb])
                sc_ = (128.0 ** -0.5) if kind == "q" else 1.0
                fw.op(dve, lambda e, dstT=dstT, sc_=sc_: e.scalar_tensor_tensor(out=dstT, in0=tmpq, scalar=sc_, in1=cacc,
                                                                                op0=ALU.mult, op1=ALU.mult),
                      reads=[rb], writes=[qkvb])
            for srcT, dv_ in ((kT, ktv), (vT, vtv)):
                for g in range(4):
                    bk = nb_()
                    for j in range(4):
                        P_ = g * 4 + j
                        fw.op(pe, lambda e, srcT=srcT, P_=P_, j=j, bk=bk: e.transpose(
                            out=self.bank(bk)[:, j * 128:(j + 1) * 128], in_=srcT[:, P_ * 128:(P_ + 1) * 128],
                            identity=self.ident), reads=[qkvb, self.cb], writes=[self.pbank[bk]])
                    fw.op(act, lambda e, dv_=dv_, g=g, bk=bk: e.activation(
                        out=dv_[:, g * 4:(g + 1) * 4, :], in_=self.bank(bk).rearrange("p (a b) -> p a b", a=4), func=AF.Copy),
                        reads=[self.pbank[bk]], writes=[tokb])
            fw.barrier()
            fw.op(dve, lambda e: e.memset(S, 0.0), writes=[Sb])
            for Q in range(4):
                cols4 = slice(Q * 512, (Q + 1) * 512)
                P0 = Q * 4
                bc4 = lambda a: a[:, P0:P0 + 4, h].unsqueeze(2).to_broadcast([128, 4, 128])
                L_ = QTL
                fw.op(pool, lambda e: e.tensor_tensor(out=q3(L_["DG4"]), in0=id4, in1=bc4(GCv), op=ALU.mult),
                      reads=[gb, self.cb], writes=[QB["DG4"]])
                bA, bB = nb_(), nb_()
                fw.op(pe, lambda e: e.matmul(self.bank(bA), lhsT=self.ones, rhs=L_["DG4"], start=True, stop=True),
                      reads=[QB["DG4"], self.cb], writes=[self.pbank[bA]])
                fw.op(pe, lambda e: e.matmul(self.bank(bB), lhsT=self.ones, rhs=L_["DG4"], start=True, stop=False),
                      reads=[QB["DG4"], self.cb], writes=[self.pbank[bB]])
                fw.op(pe, lambda e: e.matmul(self.bank(bB), lhsT=self.ident, rhs=NEGM4, start=False, stop=True),
                      reads=[gcb, self.cb], writes=[self.pbank[bB]])
                fw.op(act, lambda e: e.activation(out=L_["EGR4"], in_=self.bank(bA), func=AF.Exp), reads=[self.pbank[bA]],
                      writes=[QB["EGR4"]])
                for u in range(4):
                    fw.op(act, lambda e, u=u: e.activation(out=L_["Dt4"][:, u * 128:(u + 1) * 128],
                                                           in_=self.bank(bB)[:, u * 128:(u + 1) * 128], func=AF.Exp,
                                                           bias=NGCv[:, P0 + u, h:h + 1], scale=1.0),
                          reads=[self.pbank[bB], gb], writes=[QB["Dt4"]])
                bK, bQ = nb_(), nb_()
                for u in range(4):
                    cu = slice((P0 + u) * 128, (P0 + u + 1) * 128)
                    fw.op(pe, lambda e, u=u, cu=cu: e.matmul(self.bank(bK)[:, u * 128:(u + 1) * 128], lhsT=kT[:, cu], rhs=kT[:, cu],
                                                             start=True, stop=True), reads=[qkvb], writes=[self.pbank[bK]])
                for u in range(4):
                    cu = slice((P0 + u) * 128, (P0 + u + 1) * 128)
                    fw.op(pe, lambda e, u=u, cu=cu: e.matmul(self.bank(bQ)[:, u * 128:(u + 1) * 128], lhsT=kT[:, cu], rhs=qT[:, cu],
                                                             start=True, stop=True), reads=[qkvb], writes=[self.pbank[bQ]])
                fw.op(dve, lambda e: e.tensor_tensor(out=q3(L_["Mt4"]), in0=self.bank(bK).rearrange("p (u i) -> p u i", u=4),
                                                     in1=bc4(BTv), op=ALU.mult), reads=[self.pbank[bK], gb], writes=[QB["Mt4"]])
                fw.op(dve, lambda e: e.tensor_tensor(out=L_["Mt4"], in0=L_["Mt4"], in1=L_["Dt4"], op=ALU.mult),
                      reads=[QB["Dt4"], QB["Mt4"]], writes=[QB["Mt4"]])
                fw.op(pool, lambda e: e.affine_select(out=q3(L_["Mt4"]), in_=q3(L_["Mt4"]), pattern=[[0, 4], [1, 128]],
                                                      compare_op=ALU.not_equal, fill=0.0, base=0, channel_multiplier=-1),
                      reads=[QB["Mt4"]], writes=[QB["Mt4"]])
                fw.op(dve, lambda e: e.tensor_tensor(out=L_["AT4"], in0=self.bank(bQ), in1=L_["Dt4"], op=ALU.mult),
                      reads=[self.pbank[bQ], QB["Dt4"]], writes=[QB["AT4"]])
                bN = nb_()
                for u in range(4):
                    fw.op(pe, lambda e, u=u: e.transpose(out=self.bank(bN)[:, u * 128:(u + 1) * 128],
                                                         in_=L_["Mt4"][:, u * 128:(u + 1) * 128], identity=self.ident),
                          reads=[QB["Mt4"], self.cb], writes=[self.pbank[bN]])
                fw.op(act, lambda e: e.activation(out=L_["Nn4"], in_=self.bank(bN), func=AF.Copy), reads=[self.pbank[bN]],
                      writes=[QB["Nn4"]])
                fw.op(pool, lambda e: e.tensor_tensor(out=q3(L_["X4"]), in0=id4, in1=q3(L_["Mt4"]), op=ALU.subtract),
                      reads=[QB["Mt4"], self.cb], writes=[QB["X4"]])
                Qn, Qtn = "Mt4", "Nn4"
                pp_ = [("Qa", "Qta"), ("Qb", "Qtb")]
                for lvl in range(1, 6):
                    Qo, Qto = pp_[lvl % 2]
                    bt = nb_()
                    for u in range(4):
                        us = slice(u * 128, (u + 1) * 128)
                        fw.op(pe, lambda e, us=us, Qn=Qn, Qtn=Qtn, bt=bt: e.matmul(self.bank(bt)[:, us], lhsT=L_[Qn][:, us],
                                                                                   rhs=L_[Qtn][:, us], start=True, stop=True),
                              reads=[QB[Qn], QB[Qtn]], writes=[self.pbank[bt]])
                    fw.op(act, lambda e, Qto=Qto, bt=bt: e.activation(out=L_[Qto], in_=self.bank(bt), func=AF.Copy),
                          reads=[self.pbank[bt]], writes=[QB[Qto]])
                    if lvl < 5:
                        bq = nb_()
                        for u in range(4):
                            us = slice(u * 128, (u + 1) * 128)
                            fw.op(pe, lambda e, us=us, Qn=Qn, Qtn=Qtn, bq=bq: e.matmul(self.bank(bq)[:, us], lhsT=L_[Qtn][:, us],
                                                                                       rhs=L_[Qn][:, us], start=True, stop=True),
                                  reads=[QB[Qn], QB[Qtn]], writes=[self.pbank[bq]])
                        fw.op(dve, lambda e, Qo=Qo, bq=bq: e.tensor_copy(out=L_[Qo], in_=self.bank(bq)),
                              reads=[self.pbank[bq]], writes=[QB[Qo]])
                    bx = nb_()
                    for u in range(4):
                        us = slice(u * 128, (u + 1) * 128)
                        fw.op(pe, lambda e, us=us, Qto=Qto, bx=bx: e.matmul(self.bank(bx)[:, us], lhsT=L_[Qto][:, us],
                                                                            rhs=L_["X4"][:, us], start=True, stop=True),
                              reads=[QB[Qto], QB["X4"]], writes=[self.pbank[bx]])
                    fw.op(dve, lambda e, bx=bx: e.tensor_tensor(out=L_["X4"], in0=L_["X4"], in1=self.bank(bx), op=ALU.add),
                          reads=[self.pbank[bx], QB["X4"]], writes=[QB["X4"]])
                    Qn, Qtn = Qo, Qto
                fw.op(pool, lambda e: e.tensor_tensor(out=q3(L_["KG4"]), in0=ktv[:, P0:P0 + 4, :], in1=bc4(EGCv), op=ALU.mult),
                      reads=[tokb, gb], writes=[QB["KG4"]])
                fw.op(pool, lambda e: e.tensor_tensor(out=q3(L_["KD4"]), in0=ktv[:, P0:P0 + 4, :], in1=bc4(EKDv), op=ALU.mult),
                      reads=[tokb, gb], writes=[QB["KD4"]])
                bW, bU = nb_(), nb_()
                for u in range(4):
                    us = slice(u * 128, (u + 1) * 128)
                    fw.op(pe, lambda e, us=us: e.matmul(self.bank(bW)[:, us], lhsT=L_["KG4"][:, us], rhs=L_["X4"][:, us],
                                                        start=True, stop=True), reads=[QB["KG4"], QB["X4"]], writes=[self.pbank[bW]])
                for u in range(4):
                    us = slice(u * 128, (u + 1) * 128)
                    fw.op(pe, lambda e, us=us, u=u: e.matmul(self.bank(bU)[:, us], lhsT=L_["X4"][:, us], rhs=vtv[:, P0 + u, :],
                                                             start=True, stop=True), reads=[QB["X4"], tokb], writes=[self.pbank[bU]])
                fw.op(act, lambda e: e.activation(out=L_["WT4"], in_=self.bank(bW), func=AF.Copy), reads=[self.pbank[bW]],
                      writes=[QB["WT4"]])
                fw.op(dve, lambda e: e.tensor_tensor(out=q3(L_["BU4"]), in0=self.bank(bU).rearrange("p (u i) -> p u i", u=4),
                                                     in1=bc4(BTv), op=ALU.mult), reads=[self.pbank[bU], gb], writes=[QB["BU4"]])
                fw.op(dve, lambda e: e.tensor_tensor(out=L_["QD4"], in0=qT[:, cols4], in1=L_["EGR4"], op=ALU.mult),
                      reads=[qkvb, QB["EGR4"]], writes=[QB["QD4"]])
                for u in range(4):
                    us = slice(u * 128, (u + 1) * 128)
                    P_ = P0 + u
                    for c in range(2):
                        rows = slice(c * 64, (c + 1) * 64)
                        bw = nb_()
                        fw.op(pe, lambda e, us=us, bw=bw: e.matmul(self.bank(bw)[:, 0:128], lhsT=L_["WT4"][:, us], rhs=S,
                                                                   start=True, stop=True), reads=[QB["WT4"], Sb],
                              writes=[self.pbank[bw]])
                        fw.op(dve, lambda e, rows=rows, bw=bw, P_=P_, us=us: e.scalar_tensor_tensor(
                            out=VN[rows, :], in0=self.bank(bw)[rows, 0:128], scalar=NBv[rows, P_, h:h + 1],
                            in1=L_["BU4"][rows, us], op0=ALU.mult, op1=ALU.add),
                            reads=[self.pbank[bw], gb, QB["BU4"]], writes=[vnb])
                        bo = nb_()
                        fw.op(pe, lambda e, us=us, bo=bo: e.matmul(self.bank(bo)[:, 0:128], lhsT=L_["QD4"][:, us], rhs=S,
                                                                   start=True, stop=False), reads=[QB["QD4"], Sb],
                              writes=[self.pbank[bo]])
                        fw.op(pe, lambda e, us=us, bo=bo, rows=rows: e.matmul(self.bank(bo)[:, 0:128], lhsT=L_["AT4"][rows, us],
                                                                              rhs=VN[rows, :], start=False, stop=True),
                              reads=[QB["AT4"], vnb], writes=[self.pbank[bo]])
                        fw.op(act, lambda e, rows=rows, bo=bo, P_=P_: e.activation(out=otv[rows, P_, :], in_=self.bank(bo)[rows, 0:128],
                                                                                   func=AF.Copy), reads=[self.pbank[bo]], writes=[otb])
                        bs = nb_()
                        fw.op(pe, lambda e, us=us, bs=bs, rows=rows: e.matmul(self.bank(bs)[:, 0:128], lhsT=L_["KD4"][rows, us],
                                                                              rhs=VN[rows, :], start=True, stop=True),
                              reads=[QB["KD4"], vnb], writes=[self.pbank[bs]])
                        cdc = u * 128 + c * 64 + 63
                        fw.op(dve, lambda e, bs=bs, cdc=cdc: e.scalar_tensor_tensor(
                            out=S, in0=S, scalar=L_["EGR4"][:, cdc:cdc + 1], in1=self.bank(bs)[:, 0:128], op0=ALU.mult, op1=ALU.add),
                            reads=[self.pbank[bs], QB["EGR4"], Sb], writes=[Sb])
            fw.barrier()
            zs = ktok
            zsv = zs.rearrange("p (a b) -> p a b", a=16)
            zb = Buf("zs")
            s = nbk[0] % 2
            nbk[0] += 1
            wzv = wt[s].rearrange("p (a b) -> p a b", a=16)
            fw.dma(out=wzv, in_=w_in[:, 6144 + h * 128: 6144 + (h + 1) * 128].rearrange("(kc p) f -> p kc f", p=128),
                   writes=[wtb[s]], q=pool)
            for g in range(4):
                bk = g
                for j in range(4):
                    i = g * 4 + j
                    for kc in range(16):
                        fw.op(pe, lambda e, i=i, j=j, kc=kc, bk=bk: e.matmul(
                            self.bank(bk)[:, j * 128:(j + 1) * 128], lhsT=bigv[:, kc, i * 128:(i + 1) * 128], rhs=wzv[:, kc, :],
                            start=(kc == 0), stop=(kc == 15)), reads=[xTb, wtb[s]], writes=[self.pbank[bk]])
                fw.op(act, lambda e, g=g, bk=bk: e.activation(out=zsv[:, g * 4:(g + 1) * 4, :],
                                                              in_=self.bank(bk).rearrange("p (a b) -> p a b", a=4), func=AF.Silu),
                      reads=[self.pbank[bk]], writes=[zb])
            sq = regA[:, 0:T]
            sqb = Buf("sq")
            fw.op(pool, lambda e: e.tensor_tensor(out=sq, in0=otok, in1=otok, op=ALU.mult), reads=[otb], writes=[sqb])
            ms = sm[:, 0:16]
            fw.op(dve, lambda e: e.tensor_reduce(out=ms, in_=sq.rearrange("p (a b) -> p a b", a=16), axis=AX.X, op=ALU.add),
                  reads=[sqb], writes=[smb])
            fw.op(dve, lambda e: e.tensor_scalar(out=ms, in0=ms, scalar1=1.0 / 128.0, scalar2=RMS_EPS, op0=ALU.mult, op1=ALU.add),
                  reads=[smb], writes=[smb])
            fw.op(act, lambda e: e.activation(out=ms, in_=ms, func=AF.Sqrt), reads=[smb], writes=[smb])
            fw.op(dve, lambda e: e.reciprocal(out=ms, in_=ms), reads=[smb], writes=[smb])
            fw.op(dve, lambda e: e.tensor_tensor(out=otv, in0=otv, in1=ms.unsqueeze(2).to_broadcast([128, 16, 128]), op=ALU.mult),
                  reads=[smb, otb], writes=[otb])
            fw.op(pool, lambda e: e.tensor_tensor(out=otv, in0=otv, in1=NGrow.unsqueeze(1).to_broadcast([128, 16, 128]),
                                                  op=ALU.mult), reads=[gb, otb], writes=[otb])
            fw.op(dve, lambda e: e.tensor_tensor(out=otok, in0=otok, in1=zs, op=ALU.mult), reads=[zb, otb], writes=[otb])
            s2 = h % 2
            for g in range(4):
                bk = 4 + g
                for j in range(4):
                    P_ = g * 4 + j
                    fw.op(pe, lambda e, P_=P_, j=j, bk=bk: e.transpose(out=self.bank(bk)[:, j * 128:(j + 1) * 128], in_=otv[:, P_, :],
                                                                      identity=self.ident), reads=[otb, self.cb], writes=[self.pbank[bk]])
                fw.op(act, lambda e, g=g, bk=bk, s2=s2: e.activation(out=oth[s2][:, g * 512:(g + 1) * 512], in_=self.bank(bk),
                                                                    func=AF.Copy), reads=[self.pbank[bk]], writes=[othb[s2]])
            fw.dma(out=OT[h], in_=oth[s2], reads=[othb[s2]], writes=[OTb])
        self.release(m0)
        self.outproj_ln(OT, OTb, W["gdn_w_out"][li], src, src_buf, W["ln_g"][L, 0], W["ln_b"][L, 0], dst, dst_buf)

    def init_yg(self):
        fw = self.fw
        m = self.mark()
        z = self.sb(D)
        zb = Buf()
        fw.op(fw.dve, lambda e: e.memset(z, 0.0), writes=[zb])
        fw.dma(out=self.dram["YG"][NSLOT:NSLOT + 128, :], in_=z, reads=[zb], writes=[self.dbuf["YG"]])
        self.release(m)


WEIGHT_SPECS = [
    ("ln_g", (4, 2, 2048)), ("ln_b", (4, 2, 2048)), ("moe_rg_w", (4, 2048, 4)), ("moe_rg_b", (4, 4)),
    ("moe_re_w", (4, 2048, 32)), ("moe_re_b", (4, 32)), ("moe_w_gate", (4, 32, 2048, 512)),
    ("moe_w_up", (4, 32, 2048, 512)), ("moe_w_down", (4, 32, 512, 2048)), ("gdn_w_in", (2, 2048, 8224)),
    ("gdn_conv_w", (2, 4, 6144)), ("gdn_a_log", (2, 16)), ("gdn_dt_bias", (2, 16)), ("gdn_norm_g", (2, 128)),
    ("gdn_w_out", (2, 2048, 2048)), ("ssm_w_in", (1, 2048, 2048)), ("ssm_b_re", (1, 128, 64, 16)),
    ("ssm_b_im", (1, 128, 64, 16)), ("ssm_c_re", (1, 128, 16, 64)), ("ssm_c_im", (1, 128, 16, 64)),
    ("ssm_a_re", (1, 128, 64)), ("ssm_a_im", (1, 128, 64)), ("ssm_log_dt", (1, 128)), ("ssm_d", (1, 2048)),
    ("ssm_w_glu", (1, 2048, 2048)), ("ssm_b_glu", (1, 2048)), ("ssm_w_out", (1, 2048, 2048)),
    ("dsa_w_in", (1, 2048, 4496)), ("dsa_w_out", (1, 2048, 2048)),
]


def build(stages, ext_in=(), ext_out=(), weights=None):
    nc = bass.Bass("TRN2", target_bir_lowering=False)
    st = contextlib.ExitStack()
    with st:
        k = K(nc, st, set(ext_in), set(ext_out))
        fw = k.fw
        used = weights if weights is not None else [n for n, _ in WEIGHT_SPECS]
        W = {}
        for n, shp in WEIGHT_SPECS:
            if n in used:
                W[n] = nc.dram_tensor(n, list(shp), F32, kind="ExternalInput").ap()
        k.W = W
        names = set()
        for stg in stages:
            names.update(stg[2:])
        if "x" in names:
            k.dram["x"] = nc.dram_tensor("x", [T, D], F32, kind="ExternalInput").ap()
            k.dbuf["x"] = Buf("x")
        k.dram["out"] = nc.dram_tensor("out", [T, D], F32, kind="ExternalOutput").ap()
        k.dbuf["out"] = Buf("out")
        k.dt("XA", [T, D], F32)
        k.dt("XM", [T, D], F32)
        k.dt("OT", [16, 128, T], BF16)
        k.dt("XG", [NSLOT + 128, D], BF16)
        k.dt("YG", [NSLOT + 128, D], F32)
        k.dt("U", [16, 128, T], F32)
        k.dt("YA", [16, 128, T], BF16)
        k.dt("QT", [16, 128, T], BF16)
        k.dt("QI", [16, 128, T], BF16)
        k.dt("MASKT", [16, 128, T], BF16)
        k.init_yg()
        for stg in stages:
            kind, L, src, dst = stg
            S, Sb, Dd, Db = k.dram[src], k.dbuf[src], k.dram[dst], k.dbuf[dst]
            if kind == "moe":
                k.moe(L, S, Sb, W, Dd, Db)
            elif kind == "gdn":
                k.gdn(L // 3, L, S, Sb, W, Dd, Db)
            elif kind == "s5":
                k.s5(L, S, Sb, W, Dd, Db)
            elif kind == "dsa":
                k.dsa(L, S, Sb, W, Dd, Db)
            else:
                raise ValueError(kind)
        fw.finish()
        k.stats = {e.name: e.n_instr for e in fw.engs}
    return nc, k


_MIXERS = ("gdn", "s5", "dsa")


def _stages():
    st = []
    for L in range(DEPTH):
        src = "x" if L == 0 else "XA"
        st.append((_MIXERS[L % 3], L, src, "XM"))
        st.append(("moe", L, "XM", "out" if L == DEPTH - 1 else "XA"))
    return st


def kernel(**inputs):
    x = np.ascontiguousarray(np.asarray(inputs["x"], dtype=np.float32))
    nc, _k = build(_stages())
    wts = {n: np.ascontiguousarray(np.asarray(inputs[n], dtype=np.float32)) for n, _ in WEIGHT_SPECS}
    n_cores = 8
    in_maps = []
    for c in range(n_cores):
        m = dict(wts)
        m["x"] = x[c]
        in_maps.append(m)
    res = run_bass_kernel_spmd(nc, in_maps, core_ids=list(range(n_cores)))
    return np.stack([np.asarray(r["out"], dtype=np.float32) for r in res.results], axis=0)
```

```python
import contextlib
import math
import numpy as np
import concourse.bass as bass
import concourse.mybir as mybir
from concourse.bass_utils import run_bass_kernel_spmd

F32 = mybir.dt.float32
BF16 = mybir.dt.bfloat16
I32 = mybir.dt.int32
AF = mybir.ActivationFunctionType
ALU = mybir.AluOpType
AX = mybir.AxisListType

T = 2048
D = 2048
NT = 16
DEPTH = 4
DN_ALPHA = (2.0 * DEPTH) ** 0.25
LN_EPS = 1e-5
RMS_EPS = 1e-6
NEXP = 32
FF = 512
CAP = 256
NSLOT = NEXP * CAP
GDN_IN = 8224
DSA_IN = 4496
NEG = -30000.0


class Buf:
    __slots__ = ("name", "writer", "readers")

    def __init__(self, name=""):
        self.name = name
        self.writer = None
        self.readers = []


class Eng:
    def __init__(self, fw, name, hw, is_pe=False):
        self.fw = fw
        self.name = name
        self.hw = hw
        self.is_pe = is_pe
        self.sems = []
        self.cnt = 0
        self.waited = {}
        self.n_instr = 0

    def cur_sem(self):
        if not self.sems or self.cnt >= self.fw.EPOCH:
            self.sems.append(self.fw.new_sem(f"{self.name}_p{len(self.sems)}"))
            self.cnt = 0
        return self.sems[-1]


class FW:
    EPOCH = 60000
    NP = 24

    def __init__(self, nc, stack):
        self.nc = nc
        self.stack = stack
        self.sem_handles = {}
        self.nsem = 0
        self.pe = Eng(self, "pe", nc.tensor, is_pe=True)
        self.dve = Eng(self, "dve", nc.vector)
        self.act = Eng(self, "act", nc.scalar)
        self.pool = Eng(self, "pool", nc.gpsimd)
        self.sp = Eng(self, "sp", nc.sync)
        self.engs = [self.pe, self.dve, self.act, self.pool, self.sp]
        self.dma_pool = {}
        self.dma_rr = {}
        self.all_dma_tokens = []

    def new_sem(self, name):
        h = self.stack.enter_context(self.nc.semaphore(name))
        self.nsem += 1
        self.sem_handles[self.nsem] = h
        return self.nsem

    def _wait(self, eng, tok):
        if tok is None:
            return
        key, val, src = tok
        if eng.is_pe and src == "pe":
            return
        if eng.waited.get(key, 0) >= val:
            return
        eng.waited[key] = val
        eng.hw.wait_ge(self.sem_handles[key], val)

    def _note_read(self, b, tok):
        b.readers.append(tok)
        if len(b.readers) > 16:
            last = {}
            for r in b.readers:
                k2 = (r[2], r[0])
                if k2 not in last or last[k2][1] < r[1]:
                    last[k2] = r
            b.readers = list(last.values())

    def op(self, eng, fn, reads=(), writes=()):
        for b in reads:
            self._wait(eng, b.writer)
        for b in writes:
            self._wait(eng, b.writer)
            for r in b.readers:
                if r[2] == eng.name:
                    continue
                self._wait(eng, r)
        ins = fn(eng.hw)
        key = eng.cur_sem()
        eng.cnt += 1
        ins.then_inc(self.sem_handles[key], 1)
        tok = (key, eng.cnt, eng.name)
        for b in reads:
            self._note_read(b, tok)
        for b in writes:
            b.writer = tok
            b.readers = []
        eng.n_instr += 1
        return tok

    def dma(self, out, in_, reads=(), writes=(), q=None, indirect=None, **kw):
        eng = q or self.sp
        for b in reads:
            self._wait(eng, b.writer)
        for b in writes:
            self._wait(eng, b.writer)
            for r in b.readers:
                self._wait(eng, r)
        pool = self.dma_pool.setdefault(eng.name, [])
        if len(pool) < self.NP:
            pool.append([self.new_sem(f"dma_{eng.name}_{len(pool)}"), 0])
            slot = pool[-1]
        else:
            i = self.dma_rr.get(eng.name, 0)
            slot = pool[i % self.NP]
            self.dma_rr[eng.name] = i + 1
        key, uses = slot
        if uses > 0:
            self._wait(eng, (key, 16 * uses, "dma"))
        if indirect is None:
            ins = eng.hw.dma_start(out=out, in_=in_, **kw)
        else:
            ins = eng.hw.indirect_dma_start(out=out, in_=in_, **indirect)
        slot[1] = uses + 1
        ins.then_inc(self.sem_handles[key], 16)
        tok = (key, 16 * (uses + 1), "dma")
        for b in reads:
            self._note_read(b, tok)
        for b in writes:
            b.writer = tok
            b.readers = []
        self.all_dma_tokens.append(tok)
        if len(self.all_dma_tokens) > 400:
            self._compact_dma()
        eng.n_instr += 1
        return tok

    def _compact_dma(self):
        last = {}
        for t in self.all_dma_tokens:
            if t[0] not in last or last[t[0]][1] < t[1]:
                last[t[0]] = t
        self.all_dma_tokens = list(last.values())

    def barrier(self, engs=None):
        toks = []
        for e in self.engs:
            if e.sems and e.cnt > 0:
                toks.append((e.sems[-1], e.cnt, e.name))
        self._compact_dma()
        toks += self.all_dma_tokens
        for e in (engs or self.engs):
            for key, val, src in toks:
                if src == e.name:
                    continue
                if e.waited.get(key, 0) >= val:
                    continue
                e.waited[key] = val
                e.hw.wait_ge(self.sem_handles[key], val)

    def finish(self):
        self.barrier(engs=[self.sp])


class K:
    ARENA = 46000

    def __init__(self, nc, st, ext_in, ext_out):
        self.nc = nc
        self.st = st
        self.fw = FW(nc, st)
        self.ext_in = ext_in
        self.ext_out = ext_out
        self.arena = st.enter_context(nc.sbuf_tensor("arena", [128, self.ARENA], F32))
        self.psum = st.enter_context(nc.psum_tensor("psum", [128, 4096], F32))
        self.off = 0
        self.pbank = [Buf(f"bank{i}") for i in range(8)]
        self.dram = {}
        self.dbuf = {}
        self._consts()

    def sb(self, n, dt=F32):
        words = n if dt != BF16 else (n + 1) // 2
        words = (words + 7) // 8 * 8
        assert self.off + words <= self.ARENA, (self.off, words)
        ap = self.arena[:, self.off:self.off + words]
        self.off += words
        if dt == BF16:
            ap = ap.bitcast(BF16)[:, 0:n]
        elif dt == I32:
            ap = ap.bitcast(I32)[:, 0:n]
        else:
            ap = ap[:, 0:n]
        return ap

    def mark(self):
        return self.off

    def release(self, m):
        self.fw.barrier()
        self.off = m

    def bank(self, i, dt=F32):
        ap = self.psum[:, i * 512:(i + 1) * 512]
        if dt == BF16:
            ap = ap.bitcast(BF16)
        return ap

    def dt(self, name, shape, dtype):
        kind = "Internal"
        if name in self.ext_in:
            kind = "ExternalInput"
        elif name in self.ext_out:
            kind = "ExternalOutput"
        t = self.nc.dram_tensor(name, list(shape), dtype, kind=kind).ap()
        self.dram[name] = t
        self.dbuf[name] = Buf(name)
        return t

    def _consts(self):
        fw = self.fw
        P = fw.pool
        self.cb = Buf("consts")
        cb = self.cb
        self.ident = self.sb(128)
        self.ones = self.sb(128)
        self.identb = self.sb(128, BF16)
        self.onesb = self.sb(128, BF16)
        self.sltb = self.sb(128, BF16)
        slt = self.sb(128)
        fw.op(P, lambda e: e.memset(self.ident, 0.0), writes=[cb])
        fw.op(P, lambda e: e.affine_select(out=self.ident, in_=self.ident, pattern=[[-1, 128]],
                                           compare_op=ALU.not_equal, fill=1.0, base=0, channel_multiplier=1),
              reads=[cb], writes=[cb])
        fw.op(P, lambda e: e.memset(self.ones, 1.0), writes=[cb])
        fw.op(P, lambda e: e.memset(slt, 1.0), writes=[cb])
        fw.op(P, lambda e: e.affine_select(out=slt, in_=slt, pattern=[[1, 128]], compare_op=ALU.is_gt,
                                           fill=0.0, base=0, channel_multiplier=-1), reads=[cb], writes=[cb])
        fw.op(P, lambda e: e.tensor_copy(out=self.identb, in_=self.ident), reads=[cb], writes=[cb])
        fw.op(P, lambda e: e.tensor_copy(out=self.onesb, in_=self.ones), reads=[cb], writes=[cb])
        fw.op(P, lambda e: e.tensor_copy(out=self.sltb, in_=slt), reads=[cb], writes=[cb])
        self.ebase = self.sb(NEXP)
        fw.op(P, lambda e: e.iota(out=self.ebase, pattern=[[CAP, NEXP]], base=0, channel_multiplier=0,
                                  allow_small_or_imprecise_dtypes=True), writes=[cb])
        self.trash = self.sb(1)
        fw.op(P, lambda e: e.iota(out=self.trash, pattern=[[0, 1]], base=NSLOT, channel_multiplier=1,
                                  allow_small_or_imprecise_dtypes=True), writes=[cb])
        self.const_mark = self.off

    def load_row(self, dst, src_row, buf, n):
        src = src_row.rearrange("(o n) -> o n", o=1).to_broadcast([128, n]) if len(src_row.shape) == 1 \
            else src_row.to_broadcast([128, n])
        self.fw.dma(out=dst, in_=src, writes=[buf])

    def build_xT(self, src, src_buf, xT, xT_buf):
        fw = self.fw
        m = self.mark()
        xin = [self.sb(D) for _ in range(2)]
        xb = [Buf("xin0"), Buf("xin1")]
        for i in range(NT):
            s = i % 2
            fw.dma(out=xin[s], in_=src[i * 128:(i + 1) * 128, :], reads=[src_buf], writes=[xb[s]])
            for g in range(4):
                bk = (i * 4 + g) % 8
                pb = self.pbank[bk]
                for j in range(4):
                    fc = g * 4 + j
                    fw.op(fw.pe, lambda e, fc=fc, j=j, bk=bk, s=s: e.transpose(
                        out=self.bank(bk)[:, j * 128:(j + 1) * 128], in_=xin[s][:, fc * 128:(fc + 1) * 128],
                        identity=self.ident), reads=[xb[s], self.cb], writes=[pb])
                dst = xT[:, g * 4:(g + 1) * 4, i * 128:(i + 1) * 128]
                srcp = self.bank(bk).rearrange("p (a b) -> p a b", a=4)
                if g % 2 == 0:
                    fw.op(fw.dve, lambda e, dst=dst, srcp=srcp: e.tensor_copy(out=dst, in_=srcp),
                          reads=[pb], writes=[xT_buf])
                else:
                    fw.op(fw.act, lambda e, dst=dst, srcp=srcp: e.activation(out=dst, in_=srcp, func=AF.Copy),
                          reads=[pb], writes=[xT_buf])
        self.release(m)

    def ln_tail(self, z, zb, grow, brow, gbuf, tmp, tmpb, stat, statb, out, outb):
        fw = self.fw
        mean = stat[:, 0:1]
        ssq = stat[:, 1:2]
        rstd = stat[:, 2:3]
        nmean = stat[:, 3:4]
        fw.op(fw.dve, lambda e: e.tensor_reduce(out=mean, in_=z, axis=AX.X, op=ALU.add), reads=[zb], writes=[statb])
        fw.op(fw.dve, lambda e: e.tensor_scalar(out=nmean, in0=mean, scalar1=-1.0 / D, scalar2=None, op0=ALU.mult),
              reads=[statb], writes=[statb])
        fw.op(fw.act, lambda e: e.activation(out=tmp, in_=z, func=AF.Square, bias=nmean, scale=1.0, accum_out=ssq),
              reads=[zb, statb], writes=[tmpb, statb])
        fw.op(fw.dve, lambda e: e.tensor_scalar(out=rstd, in0=ssq, scalar1=1.0 / D, scalar2=LN_EPS, op0=ALU.mult,
                                                op1=ALU.add), reads=[statb], writes=[statb])
        fw.op(fw.act, lambda e: e.activation(out=rstd, in_=rstd, func=AF.Sqrt), reads=[statb], writes=[statb])
        fw.op(fw.dve, lambda e: e.reciprocal(out=rstd, in_=rstd), reads=[statb], writes=[statb])
        fw.op(fw.dve, lambda e: e.tensor_scalar(out=tmp, in0=z, scalar1=nmean, scalar2=rstd, op0=ALU.add,
                                                op1=ALU.mult), reads=[zb, statb, tmpb], writes=[tmpb])
        fw.op(fw.pool, lambda e: e.tensor_tensor(out=tmp, in0=tmp, in1=grow, op=ALU.mult), reads=[tmpb, gbuf],
              writes=[tmpb])
        fw.op(fw.dve, lambda e: e.tensor_tensor(out=out, in0=tmp, in1=brow, op=ALU.add), reads=[tmpb, gbuf],
              writes=[outb])

    def outproj_ln(self, OT, OTb, w_out, resid, resid_buf, ln_g, ln_b, dst, dst_buf):
        fw = self.fw
        m = self.mark()
        W = self.sb(16 * D, BF16)
        Wv = W.rearrange("p (a b) -> p a b", a=16)
        Wb = Buf("wout")
        wsrc = w_out.rearrange("(fc p) m -> p fc m", p=128)
        for q in range(4):
            fw.dma(out=Wv[:, q * 4:(q + 1) * 4, :], in_=wsrc[:, q * 4:(q + 1) * 4, :], writes=[Wb], q=fw.pool)
        grow = self.sb(D)
        brow = self.sb(D)
        gbuf = Buf("lnrows")
        self.load_row(grow, ln_g, gbuf, D)
        self.load_row(brow, ln_b, gbuf, D)
        oT = [self.sb(16 * 128, BF16) for _ in range(2)]
        oTb = [Buf(), Buf()]
        rz = [self.sb(D) for _ in range(2)]
        rzb = [Buf(), Buf()]
        tmp = self.sb(D)
        tmpb = Buf()
        stat = [self.sb(4) for _ in range(2)]
        statb = [Buf(), Buf()]
        OTv = OT.rearrange("fc p t -> p fc t")
        for i in range(NT):
            s = i % 2
            fw.dma(out=oT[s].rearrange("p (a b) -> p a b", a=16), in_=OTv[:, :, i * 128:(i + 1) * 128],
                   reads=[OTb], writes=[oTb[s]])
            fw.dma(out=rz[s], in_=resid[i * 128:(i + 1) * 128, :], reads=[resid_buf], writes=[rzb[s]])
            for mc in range(4):
                bk = (i * 4 + mc) % 8
                pb = self.pbank[bk]
                for fc in range(16):
                    fw.op(fw.pe, lambda e, fc=fc, mc=mc, bk=bk, s=s: e.matmul(
                        self.bank(bk), lhsT=oT[s][:, fc * 128:(fc + 1) * 128], rhs=Wv[:, fc, mc * 512:(mc + 1) * 512],
                        start=(fc == 0), stop=(fc == 15)), reads=[oTb[s], Wb], writes=[pb])
                zs = rz[s][:, mc * 512:(mc + 1) * 512]
                fw.op(fw.dve, lambda e, zs=zs, bk=bk: e.scalar_tensor_tensor(
                    out=zs, in0=zs, scalar=DN_ALPHA, in1=self.bank(bk), op0=ALU.mult, op1=ALU.add),
                    reads=[pb, rzb[s]], writes=[rzb[s]])
            self.ln_tail(rz[s], rzb[s], grow, brow, gbuf, tmp, tmpb, stat[s], statb[s], rz[s], rzb[s])
            fw.dma(out=dst[i * 128:(i + 1) * 128, :], in_=rz[s], reads=[rzb[s]], writes=[dst_buf])
        self.release(m)

    def moe(self, L, xm, xm_buf, W, dst, dst_buf):
        fw = self.fw
        XG, YG = self.dram["XG"], self.dram["YG"]
        XGb, YGb = self.dbuf["XG"], self.dbuf["YG"]
        m0 = self.mark()
        dest = self.sb(NT * 2, I32)
        gate = self.sb(NT * 2)
        routeb = Buf("route")
        acum = self.sb(NEXP)
        acumb = self.sb(NEXP, BF16)
        acb = Buf("acum")
        fw.op(fw.dve, lambda e: e.memset(acum, 0.0), writes=[acb])
        fw.op(fw.dve, lambda e: e.memset(acumb, 0.0), writes=[acb])
        m1 = self.mark()
        wr = self.sb(16 * 36)
        wrv = wr.rearrange("p (a b) -> p a b", a=16)
        wrb = Buf("wr")
        fw.dma(out=wrv[:, :, 0:4], in_=W["moe_rg_w"][L].rearrange("(fc p) g -> p fc g", p=128), writes=[wrb])
        fw.dma(out=wrv[:, :, 4:36], in_=W["moe_re_w"][L].rearrange("(fc p) g -> p fc g", p=128), writes=[wrb])
        brow = self.sb(36)
        self.load_row(brow[:, 0:4], W["moe_rg_b"][L], wrb, 4)
        self.load_row(brow[:, 4:36], W["moe_re_b"][L], wrb, 32)
        xin = [self.sb(D) for _ in range(2)]
        xinb = [Buf(), Buf()]
        xTf = [self.sb(16 * 128) for _ in range(2)]
        xTfb = [Buf(), Buf()]
        xbf = [self.sb(D, BF16) for _ in range(2)]
        xbfb = [Buf(), Buf()]
        sm = [self.sb(256) for _ in range(2)]
        smb = [Buf(), Buf()]
        for i in range(NT):
            s = i % 2
            fw.dma(out=xin[s], in_=xm[i * 128:(i + 1) * 128, :], reads=[xm_buf], writes=[xinb[s]])
            for g in range(4):
                bk = g
                pb = self.pbank[bk]
                for j in range(4):
                    fc = g * 4 + j
                    fw.op(fw.pe, lambda e, fc=fc, j=j, bk=bk, s=s: e.transpose(
                        out=self.bank(bk)[:, j * 128:(j + 1) * 128], in_=xin[s][:, fc * 128:(fc + 1) * 128],
                        identity=self.ident), reads=[xinb[s], self.cb], writes=[pb])
                dstp = xTf[s][:, g * 512:(g + 1) * 512]
                if g % 2 == 0:
                    fw.op(fw.dve, lambda e, dstp=dstp, bk=bk: e.tensor_copy(out=dstp, in_=self.bank(bk)),
                          reads=[pb], writes=[xTfb[s]])
                else:
                    fw.op(fw.act, lambda e, dstp=dstp, bk=bk: e.activation(out=dstp, in_=self.bank(bk), func=AF.Copy),
                          reads=[pb], writes=[xTfb[s]])
            fw.op(fw.pool, lambda e, s=s: e.tensor_copy(out=xbf[s], in_=xin[s]), reads=[xinb[s]], writes=[xbfb[s]])
            pl = self.pbank[4]
            lg_ps = self.bank(4)[:, 0:36]
            for fc in range(16):
                fw.op(fw.pe, lambda e, fc=fc, s=s: e.matmul(lg_ps, lhsT=xTf[s][:, fc * 128:(fc + 1) * 128],
                                                           rhs=wrv[:, fc, :], start=(fc == 0), stop=(fc == 15)),
                      reads=[xTfb[s], wrb], writes=[pl])
            S = sm[s]
            Sb = smb[s]
            lg = S[:, 0:36]
            fw.op(fw.dve, lambda e, lg=lg: e.tensor_tensor(out=lg, in0=lg_ps, in1=brow, op=ALU.add),
                  reads=[pl, wrb], writes=[Sb])
            gmax = S[:, 36:37]
            ngmax = S[:, 37:38]
            gsum = S[:, 38:39]
            pg = S[:, 39:40]
            ohg = S[:, 40:44]
            ex4 = S[:, 44:48]
            fw.op(fw.dve, lambda e: e.tensor_reduce(out=gmax, in_=lg[:, 0:4], axis=AX.X, op=ALU.max),
                  reads=[Sb], writes=[Sb])
            fw.op(fw.dve, lambda e: e.tensor_scalar(out=ngmax, in0=gmax, scalar1=-1.0, scalar2=None, op0=ALU.mult),
                  reads=[Sb], writes=[Sb])
            fw.op(fw.act, lambda e: e.activation(out=ex4, in_=lg[:, 0:4], func=AF.Exp, bias=ngmax, scale=1.0,
                                                 accum_out=gsum), reads=[Sb], writes=[Sb])
            fw.op(fw.dve, lambda e: e.reciprocal(out=pg, in_=gsum), reads=[Sb], writes=[Sb])
            fw.op(fw.dve, lambda e: e.tensor_scalar(out=ohg, in0=lg[:, 0:4], scalar1=gmax, scalar2=None,
                                                    op0=ALU.is_ge), reads=[Sb], writes=[Sb])
            esel = S[:, 48:56]
            le = lg[:, 4:36]
            fw.op(fw.dve, lambda e: e.tensor_scalar(out=esel, in0=le[:, 0:8], scalar1=ohg[:, 0:1], scalar2=None,
                                                    op0=ALU.mult), reads=[Sb], writes=[Sb])
            for g in range(1, 4):
                fw.op(fw.dve, lambda e, g=g: e.scalar_tensor_tensor(
                    out=esel, in0=le[:, g * 8:(g + 1) * 8], scalar=ohg[:, g:g + 1], in1=esel, op0=ALU.mult,
                    op1=ALU.add), reads=[Sb], writes=[Sb])
            top8 = S[:, 56:64]
            fw.op(fw.dve, lambda e: e.max(out=top8, in_=esel), reads=[Sb], writes=[Sb])
            oh1 = S[:, 64:72]
            oh2 = S[:, 72:80]
            fw.op(fw.dve, lambda e: e.tensor_scalar(out=oh1, in0=esel, scalar1=top8[:, 0:1], scalar2=None,
                                                    op0=ALU.is_equal), reads=[Sb], writes=[Sb])
            fw.op(fw.dve, lambda e: e.tensor_scalar(out=oh2, in0=esel, scalar1=top8[:, 1:2], scalar2=None,
                                                    op0=ALU.is_equal), reads=[Sb], writes=[Sb])
            dv = S[:, 80:82]
            fw.op(fw.dve, lambda e: e.tensor_tensor(out=dv[:, 0:1], in0=top8[:, 0:1], in1=top8[:, 1:2],
                                                    op=ALU.subtract), reads=[Sb], writes=[Sb])
            fw.op(fw.dve, lambda e: e.tensor_tensor(out=dv[:, 1:2], in0=top8[:, 1:2], in1=top8[:, 0:1],
                                                    op=ALU.subtract), reads=[Sb], writes=[Sb])
            p12 = S[:, 82:84]
            fw.op(fw.act, lambda e: e.activation(out=p12, in_=dv, func=AF.Sigmoid), reads=[Sb], writes=[Sb])
            fw.op(fw.dve, lambda e: e.tensor_scalar(out=p12, in0=p12, scalar1=pg, scalar2=None, op0=ALU.mult),
                  reads=[Sb], writes=[Sb])
            A1 = S[:, 96:128]
            A2 = S[:, 128:160]
            for g in range(4):
                fw.op(fw.dve, lambda e, g=g: e.tensor_scalar(out=A1[:, g * 8:(g + 1) * 8], in0=oh1,
                                                              scalar1=ohg[:, g:g + 1], scalar2=None, op0=ALU.mult),
                      reads=[Sb], writes=[Sb])
                fw.op(fw.dve, lambda e, g=g: e.tensor_scalar(out=A2[:, g * 8:(g + 1) * 8], in0=oh2,
                                                              scalar1=ohg[:, g:g + 1], scalar2=None, op0=ALU.mult),
                      reads=[Sb], writes=[Sb])
            A12 = S[:, 160:192]
            A12b = S[:, 192:208].bitcast(BF16)
            fw.op(fw.dve, lambda e: e.tensor_tensor(out=A12, in0=A1, in1=A2, op=ALU.add), reads=[Sb], writes=[Sb])
            fw.op(fw.dve, lambda e: e.tensor_copy(out=A12b, in_=A12), reads=[Sb], writes=[Sb])
            pp = self.pbank[5]
            pos_ps = self.bank(5)[:, 0:32]
            fw.op(fw.pe, lambda e: e.matmul(pos_ps, lhsT=self.onesb, rhs=acumb, start=True, stop=False),
                  reads=[acb, self.cb], writes=[pp])
            fw.op(fw.pe, lambda e: e.matmul(pos_ps, lhsT=self.sltb, rhs=A12b, start=False, stop=True),
                  reads=[Sb, self.cb], writes=[pp])
            slot = S[:, 208:240]
            fw.op(fw.dve, lambda e: e.tensor_tensor(out=slot, in0=pos_ps, in1=self.ebase, op=ALU.add),
                  reads=[pp, self.cb], writes=[Sb])
            fw.op(fw.pool, lambda e: e.tensor_tensor(out=acum, in0=acum, in1=A12, op=ALU.add), reads=[Sb, acb],
                  writes=[acb])
            fw.op(fw.pool, lambda e: e.tensor_copy(out=acumb, in_=acum), reads=[acb], writes=[acb])
            tmp32 = S[:, 240:256]
            dr = S[:, 84:86]
            pr = S[:, 86:88]
            for r, A in ((0, A1), (1, A2)):
                tt = S[:, 224:256]
            scr = xTf[s][:, 0:32]
            for r, A in ((0, A1), (1, A2)):
                fw.op(fw.dve, lambda e, r=r, A=A: e.scalar_tensor_tensor(
                    out=scr, in0=A, scalar=1.0, in1=slot, op0=ALU.mult, op1=ALU.mult, accum_out=dr[:, r:r + 1]),
                    reads=[Sb, xTfb[s], pl], writes=[Sb, xTfb[s]])
                fw.op(fw.dve, lambda e, r=r, A=A: e.scalar_tensor_tensor(
                    out=scr, in0=A, scalar=1.0, in1=pos_ps, op0=ALU.mult, op1=ALU.mult, accum_out=pr[:, r:r + 1]),
                    reads=[Sb, xTfb[s], pp], writes=[Sb, xTfb[s]])
            valid = S[:, 88:90]
            fw.op(fw.dve, lambda e: e.tensor_scalar(out=valid, in0=pr, scalar1=float(CAP) - 0.5, scalar2=None,
                                                    op0=ALU.is_lt), reads=[Sb], writes=[Sb])
            fw.op(fw.dve, lambda e: e.tensor_tensor(out=p12, in0=p12, in1=valid, op=ALU.mult), reads=[Sb],
                  writes=[Sb])
            fw.op(fw.dve, lambda e: e.tensor_scalar(out=dr, in0=dr, scalar1=self.trash, scalar2=None,
                                                    op0=ALU.subtract), reads=[Sb, self.cb], writes=[Sb])
            fw.op(fw.dve, lambda e: e.tensor_tensor(out=dr, in0=dr, in1=valid, op=ALU.mult), reads=[Sb], writes=[Sb])
            fw.op(fw.dve, lambda e: e.tensor_scalar(out=dr, in0=dr, scalar1=self.trash, scalar2=None, op0=ALU.add),
                  reads=[Sb, self.cb], writes=[Sb])
            fw.op(fw.dve, lambda e, i=i: e.tensor_copy(out=dest[:, 2 * i:2 * i + 2], in_=dr), reads=[Sb],
                  writes=[routeb])
            fw.op(fw.dve, lambda e, i=i: e.tensor_copy(out=gate[:, 2 * i:2 * i + 2], in_=p12), reads=[Sb],
                  writes=[routeb])
            for r in range(2):
                fw.dma(out=XG, in_=xbf[s], reads=[xbfb[s], routeb], writes=[XGb], q=fw.pool,
                       indirect=dict(out_offset=bass.IndirectOffsetOnAxis(ap=dest[:, 2 * i + r:2 * i + r + 1], axis=0),
                                     in_offset=None))
        self.release(m1)
        m2 = self.mark()
        NB = 2
        wg = [self.sb(16 * FF, BF16) for _ in range(NB)]
        wu = [self.sb(16 * FF, BF16) for _ in range(NB)]
        wd = [self.sb(4 * D, BF16) for _ in range(NB)]
        wbuf = [Buf() for _ in range(NB)]
        xg = [self.sb(D, BF16) for _ in range(2)]
        xgb = [Buf(), Buf()]
        xgT = self.sb(16 * CAP, BF16)
        xgTv = xgT.rearrange("p (a b) -> p a b", a=16)
        xgTb = Buf()
        hT = self.sb(4 * CAP, BF16)
        hTv = hT.rearrange("p (a b) -> p a b", a=4)
        hTb = Buf()
        sg = self.sb(CAP)
        sgb = Buf()
        yt = [self.sb(D) for _ in range(2)]
        ytb = [Buf(), Buf()]
        pbk = 0
        for ex in range(NEXP):
            s = ex % NB
            fw.dma(out=wg[s].rearrange("p (a b) -> p a b", a=16),
                   in_=W["moe_w_gate"][L, ex].rearrange("(kc p) f -> p kc f", p=128), writes=[wbuf[s]], q=fw.pool)
            fw.dma(out=wu[s].rearrange("p (a b) -> p a b", a=16),
                   in_=W["moe_w_up"][L, ex].rearrange("(kc p) f -> p kc f", p=128), writes=[wbuf[s]], q=fw.pool)
            fw.dma(out=wd[s].rearrange("p (a b) -> p a b", a=4),
                   in_=W["moe_w_down"][L, ex].rearrange("(fc p) m -> p fc m", p=128), writes=[wbuf[s]], q=fw.pool)
            wgv = wg[s].rearrange("p (a b) -> p a b", a=16)
            wuv = wu[s].rearrange("p (a b) -> p a b", a=16)
            wdv = wd[s].rearrange("p (a b) -> p a b", a=4)
            for stl in range(CAP // 128):
                xs = stl % 2
                fw.dma(out=xg[xs], in_=XG[ex * CAP + stl * 128: ex * CAP + (stl + 1) * 128, :], reads=[XGb],
                       writes=[xgb[xs]])
                for g in range(2):
                    bk = pbk % 8
                    pbk += 1
                    pb = self.pbank[bk]
                    bkb = self.bank(bk, BF16)
                    for j in range(8):
                        kc = g * 8 + j
                        fw.op(fw.pe, lambda e, kc=kc, j=j, bkb=bkb, xs=xs: e.transpose(
                            out=bkb[:, j * 128:(j + 1) * 128], in_=xg[xs][:, kc * 128:(kc + 1) * 128],
                            identity=self.identb), reads=[xgb[xs], self.cb], writes=[pb])
                    dstp = xgTv[:, g * 8:(g + 1) * 8, stl * 128:(stl + 1) * 128]
                    srcp = bkb.rearrange("p (a b) -> p a b", a=8)
                    if g == 0:
                        fw.op(fw.dve, lambda e, dstp=dstp, srcp=srcp: e.tensor_copy(out=dstp, in_=srcp),
                              reads=[pb], writes=[xgTb])
                    else:
                        fw.op(fw.act, lambda e, dstp=dstp, srcp=srcp: e.activation(out=dstp, in_=srcp, func=AF.Copy),
                              reads=[pb], writes=[xgTb])
            for fc in range(4):
                bkg = pbk % 8
                bku = (pbk + 1) % 8
                pbk += 2
                for (bk, wv) in ((bkg, wgv), (bku, wuv)):
                    for kc in range(16):
                        fw.op(fw.pe, lambda e, bk=bk, wv=wv, kc=kc, fc=fc: e.matmul(
                            self.bank(bk)[:, 0:CAP], lhsT=wv[:, kc, fc * 128:(fc + 1) * 128], rhs=xgTv[:, kc, :],
                            start=(kc == 0), stop=(kc == 15)), reads=[wbuf[s], xgTb], writes=[self.pbank[bk]])
                fw.op(fw.act, lambda e, bkg=bkg: e.activation(out=sg, in_=self.bank(bkg)[:, 0:CAP], func=AF.Silu),
                      reads=[self.pbank[bkg]], writes=[sgb])
                fw.op(fw.dve, lambda e, bku=bku, fc=fc: e.tensor_tensor(out=hTv[:, fc, :], in0=sg,
                                                                        in1=self.bank(bku)[:, 0:CAP], op=ALU.mult),
                      reads=[self.pbank[bku], sgb], writes=[hTb])
            for stl in range(CAP // 128):
                ys = (ex * 2 + stl) % 2
                for mc in range(4):
                    bk = pbk % 8
                    pbk += 1
                    for fc in range(4):
                        fw.op(fw.pe, lambda e, bk=bk, fc=fc, mc=mc, stl=stl: e.matmul(
                            self.bank(bk), lhsT=hTv[:, fc, stl * 128:(stl + 1) * 128],
                            rhs=wdv[:, fc, mc * 512:(mc + 1) * 512], start=(fc == 0), stop=(fc == 3)),
                            reads=[hTb, wbuf[s]], writes=[self.pbank[bk]])
                    dstp = yt[ys][:, mc * 512:(mc + 1) * 512]
                    if mc % 2 == 0:
                        fw.op(fw.dve, lambda e, dstp=dstp, bk=bk: e.tensor_copy(out=dstp, in_=self.bank(bk)),
                              reads=[self.pbank[bk]], writes=[ytb[ys]])
                    else:
                        fw.op(fw.act, lambda e, dstp=dstp, bk=bk: e.activation(out=dstp, in_=self.bank(bk),
                                                                               func=AF.Copy),
                              reads=[self.pbank[bk]], writes=[ytb[ys]])
                fw.dma(out=YG[ex * CAP + stl * 128: ex * CAP + (stl + 1) * 128, :], in_=yt[ys], reads=[ytb[ys]],
                       writes=[YGb])
        self.release(m2)
        m3 = self.mark()
        grow = self.sb(D)
        brow2 = self.sb(D)
        gbuf = Buf()
        self.load_row(grow, W["ln_g"][L, 1], gbuf, D)
        self.load_row(brow2, W["ln_b"][L, 1], gbuf, D)
        xr = [self.sb(D) for _ in range(2)]
        xrb = [Buf(), Buf()]
        y1 = [self.sb(D) for _ in range(2)]
        y1b = [Buf(), Buf()]
        y2 = [self.sb(D) for _ in range(2)]
        y2b = [Buf(), Buf()]
        tmp = self.sb(D)
        tmpb = Buf()
        stat = [self.sb(4) for _ in range(2)]
        statb = [Buf(), Buf()]
        for i in range(NT):
            s = i % 2
            fw.dma(out=xr[s], in_=xm[i * 128:(i + 1) * 128, :], reads=[xm_buf], writes=[xrb[s]])
            for (yy, yb, r) in ((y1[s], y1b[s], 0), (y2[s], y2b[s], 1)):
                fw.dma(out=yy, in_=YG, reads=[YGb, routeb], writes=[yb], q=fw.pool,
                       indirect=dict(out_offset=None,
                                     in_offset=bass.IndirectOffsetOnAxis(ap=dest[:, 2 * i + r:2 * i + r + 1], axis=0)))
            fw.op(fw.pool, lambda e, s=s, i=i: e.tensor_scalar(out=y1[s], in0=y1[s], scalar1=gate[:, 2 * i:2 * i + 1],
                                                               scalar2=None, op0=ALU.mult),
                  reads=[y1b[s], routeb], writes=[y1b[s]])
            fw.op(fw.dve, lambda e, s=s, i=i: e.scalar_tensor_tensor(
                out=y2[s], in0=y2[s], scalar=gate[:, 2 * i + 1:2 * i + 2], in1=y1[s], op0=ALU.mult, op1=ALU.add),
                reads=[y1b[s], y2b[s], routeb], writes=[y2b[s]])
            fw.op(fw.dve, lambda e, s=s: e.scalar_tensor_tensor(
                out=xr[s], in0=xr[s], scalar=DN_ALPHA, in1=y2[s], op0=ALU.mult, op1=ALU.add),
                reads=[xrb[s], y2b[s]], writes=[xrb[s]])
            self.ln_tail(xr[s], xrb[s], grow, brow2, gbuf, tmp, tmpb, stat[s], statb[s], xr[s], xrb[s])
            fw.dma(out=dst[i * 128:(i + 1) * 128, :], in_=xr[s], reads=[xrb[s]], writes=[dst_buf])
        self.release(m3)
        self.release(m0)

    def proj_fm(self, xTv, xTb, w_cols, wt, wtb, banks=(0, 1, 2, 3)):
        fw = self.fw
        fw.dma(out=wt.rearrange("p (a b) -> p a b", a=16), in_=w_cols.rearrange("(kc p) f -> p kc f", p=128),
               writes=[wtb], q=fw.pool)
        wv = wt.rearrange("p (a b) -> p a b", a=16)
        for tc in range(4):
            bk = banks[tc]
            for kc in range(16):
                fw.op(fw.pe, lambda e, bk=bk, kc=kc, tc=tc: e.matmul(
                    self.bank(bk), lhsT=wv[:, kc, :], rhs=xTv[:, kc, tc * 512:(tc + 1) * 512],
                    start=(kc == 0), stop=(kc == 15)), reads=[wtb, xTb], writes=[self.pbank[bk]])

    def sin_rr(self, eng, out, ang, buf, k, r, n_part=128, shift=0.0):
        fw = self.fw
        MAG = 12582912.0
        C1 = 6.28125
        C2 = 2.0 * math.pi - 6.28125
        fw.op(eng, lambda e: e.tensor_scalar(out=k, in0=ang, scalar1=1.0 / (2.0 * math.pi),
                                             scalar2=shift / (2.0 * math.pi), op0=ALU.mult, op1=ALU.add),
              reads=[buf], writes=[buf])
        fw.op(eng, lambda e: e.tensor_scalar(out=k, in0=k, scalar1=MAG, scalar2=None, op0=ALU.add),
              reads=[buf], writes=[buf])
        fw.op(eng, lambda e: e.tensor_scalar(out=k, in0=k, scalar1=-MAG, scalar2=None, op0=ALU.add),
              reads=[buf], writes=[buf])
        fw.op(eng, lambda e: e.scalar_tensor_tensor(out=r, in0=k, scalar=-C1, in1=ang, op0=ALU.mult, op1=ALU.add),
              reads=[buf], writes=[buf]) if eng is fw.dve else None
        if eng is not fw.dve:
            raise ValueError
        fw.op(eng, lambda e: e.scalar_tensor_tensor(out=r, in0=k, scalar=-C2, in1=r, op0=ALU.mult, op1=ALU.add),
              reads=[buf], writes=[buf])
        fw.op(eng, lambda e: e.tensor_scalar(out=r, in0=r, scalar1=shift, scalar2=math.pi, op0=ALU.add, op1=ALU.min),
              reads=[buf], writes=[buf])
        fw.op(eng, lambda e: e.tensor_scalar(out=r, in0=r, scalar1=-math.pi, scalar2=None, op0=ALU.max),
              reads=[buf], writes=[buf])
        fw.op(fw.act, lambda e: e.activation(out=out, in_=r, func=AF.Sin), reads=[buf], writes=[buf])

    def s5(self, L, src, src_buf, W, dst, dst_buf):
        fw = self.fw
        dve, act, pool, pe = fw.dve, fw.act, fw.pool, fw.pe
        U, Ub = self.dram["U"], self.dbuf["U"]
        YA, YAb = self.dram["YA"], self.dbuf["YA"]
        OT, OTb = self.dram["OT"], self.dbuf["OT"]
        m0 = self.mark()
        big = self.sb(16 * T, BF16)
        bigv = big.rearrange("p (a b) -> p a b", a=16)
        xTb = Buf("xT")
        self.build_xT(src, src_buf, bigv, xTb)
        mU = self.mark()
        wt = [self.sb(16 * 128, BF16) for _ in range(2)]
        wtb = [Buf(), Buf()]
        uf = [self.sb(T) for _ in range(2)]
        ufb = [Buf(), Buf()]
        for J in range(16):
            s = J % 2
            self.proj_fm(bigv, xTb, W["ssm_w_in"][0][:, J * 128:(J + 1) * 128], wt[s], wtb[s])
            for tc in range(4):
                eng = dve if tc % 2 == 0 else act
                dstp = uf[s][:, tc * 512:(tc + 1) * 512]
                if tc % 2 == 0:
                    fw.op(dve, lambda e, dstp=dstp, tc=tc: e.tensor_copy(out=dstp, in_=self.bank(tc)),
                          reads=[self.pbank[tc]], writes=[ufb[s]])
                else:
                    fw.op(act, lambda e, dstp=dstp, tc=tc: e.activation(out=dstp, in_=self.bank(tc), func=AF.Copy),
                          reads=[self.pbank[tc]], writes=[ufb[s]])
            fw.dma(out=U[J], in_=uf[s], reads=[ufb[s]], writes=[Ub])
        self.release(mU)
        uTb_buf = xTb
        for q in range(4):
            fw.dma(out=bigv[:, q * 4:(q + 1) * 4, :], in_=U.rearrange("j p t -> p j t")[:, q * 4:(q + 1) * 4, :],
                   reads=[Ub], writes=[uTb_buf], q=pool)
        mP = self.mark()
        PQ = self.sb(6 * 64)
        LB = [self.sb(16 * 128, BF16) for _ in range(2)]
        CL = [self.sb(16 * 128) for _ in range(2)]
        LBz = [self.sb(16 * 128, BF16) for _ in range(2)]
        LBzv = [a.rearrange("p (J q) -> p J q", J=16) for a in LBz]
        dsk = self.sb(16)
        tau = self.sb(520)
        m96 = self.sb(1)
        mT = self.mark()
        pb_ = Buf("s5par")
        A = lambda: self.sb(128)
        are, aim, dtb, mag, ang, kk, rr, sn, cs, den, fre, fim, t1, t2 = [A() for _ in range(14)]
        ldt = self.sb(2)
        fw.dma(out=are[0:64, :], in_=W["ssm_a_re"][0].rearrange("(j two) p -> j (two p)", two=2), writes=[pb_])
        fw.dma(out=aim[0:64, :], in_=W["ssm_a_im"][0].rearrange("(j two) p -> j (two p)", two=2), writes=[pb_])
        fw.dma(out=ldt[0:64, :], in_=W["ssm_log_dt"][0].rearrange("(j two) -> j two", two=2), writes=[pb_])
        h = slice(0, 64)
        fw.op(act, lambda e: e.activation(out=ldt[h, :], in_=ldt[h, :], func=AF.Exp), reads=[pb_], writes=[pb_])
        for two in range(2):
            fw.op(dve, lambda e, two=two: e.tensor_scalar(out=dtb[h, two * 64:(two + 1) * 64], in0=self.ones[h, 0:64],
                                                         scalar1=ldt[h, two:two + 1], scalar2=None, op0=ALU.mult),
                  reads=[pb_, self.cb], writes=[pb_])
        tt = lambda o, a, b, op: fw.op(dve, lambda e: e.tensor_tensor(out=o[h, :], in0=a[h, :], in1=b[h, :], op=op),
                                       reads=[pb_], writes=[pb_])
        tt(mag, are, dtb, ALU.mult)
        fw.op(act, lambda e: e.activation(out=mag[h, :], in_=mag[h, :], func=AF.Exp), reads=[pb_], writes=[pb_])
        tt(ang, aim, dtb, ALU.mult)
        self.sin_rr(dve, sn[h, :], ang[h, :], pb_, kk[h, :], rr[h, :])
        thr = A()
        fw.op(dve, lambda e: e.tensor_copy(out=thr[h, :], in_=rr[h, :]), reads=[pb_], writes=[pb_])
        self.sin_rr(dve, cs[h, :], ang[h, :], pb_, kk[h, :], rr[h, :], shift=math.pi / 2)
        lre, lim = A(), A()
        tt(lre, mag, cs, ALU.mult)
        tt(lim, mag, sn, ALU.mult)
        tt(t1, are, are, ALU.mult)
        tt(t2, aim, aim, ALU.mult)
        tt(den, t1, t2, ALU.add)
        fw.op(dve, lambda e: e.reciprocal(out=den[h, :], in_=den[h, :]), reads=[pb_], writes=[pb_])
        lm1 = A()
        fw.op(dve, lambda e: e.tensor_scalar(out=lm1[h, :], in0=lre[h, :], scalar1=-1.0, scalar2=None, op0=ALU.add),
              reads=[pb_], writes=[pb_])
        tt(t1, lm1, are, ALU.mult)
        tt(t2, lim, aim, ALU.mult)
        tt(fre, t1, t2, ALU.add)
        tt(fre, fre, den, ALU.mult)
        tt(t1, lim, are, ALU.mult)
        tt(t2, lm1, aim, ALU.mult)
        tt(fim, t1, t2, ALU.subtract)
        tt(fim, fim, den, ALU.mult)
        PQv = PQ.rearrange("p (a b) -> p a b", a=6)
        pqb = Buf("pq")
        for n_, srcp in enumerate((mag, thr, cs, sn, fre, fim)):
            fw.op(pe, lambda e, n_=n_, srcp=srcp: e.transpose(out=self.bank(0)[:, n_ * 64:(n_ + 1) * 64],
                                                              in_=srcp[0:64, :], identity=self.ident[0:64, 0:64]),
                  reads=[pb_, self.cb], writes=[self.pbank[0]])
        fw.op(dve, lambda e: e.tensor_copy(out=PQ, in_=self.bank(0)[:, 0:384]), reads=[self.pbank[0]], writes=[pqb])
        RHO, THR, COS1, SIN1, FRE, FIM = [PQv[:, n_, :] for n_ in range(6)]
        bre = self.sb(64 * 16)
        bim = self.sb(64 * 16)
        bbr = self.sb(64 * 16)
        bbi = self.sb(64 * 16)
        tb1 = self.sb(64 * 16)
        bb_ = Buf("bb")
        v3 = lambda a: a.rearrange("p (j c) -> p j c", c=16)
        fw.dma(out=v3(bre), in_=W["ssm_b_re"][0].rearrange("(j two) p c -> (two p) j c", two=2), writes=[bb_])
        fw.dma(out=v3(bim), in_=W["ssm_b_im"][0].rearrange("(j two) p c -> (two p) j c", two=2), writes=[bb_])
        bc = lambda a: a.unsqueeze(2).to_broadcast([128, 64, 16])
        t3 = lambda o, a, b, op: fw.op(dve, lambda e: e.tensor_tensor(out=v3(o), in0=v3(a), in1=b, op=op),
                                       reads=[bb_, pqb], writes=[bb_])
        t3(bbr, bre, bc(FRE), ALU.mult)
        t3(tb1, bim, bc(FIM), ALU.mult)
        t3(bbr, bbr, v3(tb1), ALU.subtract)
        t3(bbi, bim, bc(FRE), ALU.mult)
        t3(tb1, bre, bc(FIM), ALU.mult)
        t3(bbi, bbi, v3(tb1), ALU.add)
        LBv = [a.rearrange("p (J q) -> p J q", J=16) for a in LB]
        lbb = Buf("LB")
        arr = self.sb(16 * 128)
        arrb = Buf("arr")
        arr5 = arr.rearrange("p (J jj two c) -> p J jj two c", J=16, jj=4, two=2)
        for ri, bbx in enumerate((bbr, bbi)):
            fw.op(pool, lambda e: e.memset(arr, 0.0), writes=[arrb])
            b4 = bbx.rearrange("p (J jj c) -> p J jj c", J=16, jj=4)
            for two in range(2):
                ps = slice(two * 64, (two + 1) * 64)
                fw.op(pool, lambda e, two=two, ps=ps, b4=b4: e.tensor_copy(out=arr5[ps, :, :, two, :], in_=b4[ps]),
                      reads=[bb_, arrb], writes=[arrb])
            av = arr.rearrange("p (J x) -> p J x", J=16)
            for g in range(4):
                bk = 1 + g
                for j4 in range(4):
                    J = g * 4 + j4
                    fw.op(pe, lambda e, J=J, j4=j4, bk=bk: e.transpose(out=self.bank(bk)[:, j4 * 128:(j4 + 1) * 128],
                                                                       in_=av[:, J, :], identity=self.ident),
                          reads=[arrb, self.cb], writes=[self.pbank[bk]])
                fw.op(act, lambda e, g=g, bk=bk, ri=ri: e.activation(
                    out=LBv[ri][:, g * 4:(g + 1) * 4, :], in_=self.bank(bk).rearrange("p (a b) -> p a b", a=4),
                    func=AF.Copy), reads=[self.pbank[bk]], writes=[lbb])
        fw.op(pool, lambda e: e.memset(m96, 1.0), writes=[lbb])
        fw.op(pool, lambda e: e.affine_select(out=m96, in_=m96, pattern=[[0, 1]], compare_op=ALU.is_ge, fill=0.0,
                                              base=-96, channel_multiplier=1), reads=[lbb], writes=[lbb])
        for ri in range(2):
            fw.op(dve, lambda e, ri=ri: e.tensor_scalar(out=LBz[ri][64:128, :], in0=LB[ri][64:128, :],
                                                       scalar1=m96[64:128, :], scalar2=None, op0=ALU.mult),
                  reads=[lbb], writes=[lbb])
        CLv = [a.rearrange("p (J q) -> p J q", J=16) for a in CL]
        clb = Buf("CL")
        for ri, cname in enumerate(("ssm_c_re", "ssm_c_im")):
            fw.op(pool, lambda e: e.memset(arr, 0.0), writes=[arrb])
            a4 = arr.rearrange("p (J two q) -> p J two q", J=16, two=2)
            csrc = W[cname][0].rearrange("(J jj two) c p -> jj two c J p", jj=4, two=2)
            for jj in range(4):
                for two in range(2):
                    r0 = jj * 32 + two * 16
                    fw.dma(out=a4[r0:r0 + 16, :, two, :], in_=csrc[jj, two], writes=[arrb])
            av = arr.rearrange("p (J x) -> p J x", J=16)
            for g in range(4):
                bk = 1 + g
                for j4 in range(4):
                    J = g * 4 + j4
                    fw.op(pe, lambda e, J=J, j4=j4, bk=bk: e.transpose(out=self.bank(bk)[:, j4 * 128:(j4 + 1) * 128],
                                                                       in_=av[:, J, :], identity=self.ident),
                          reads=[arrb, self.cb], writes=[self.pbank[bk]])
                fw.op(act, lambda e, g=g, bk=bk, ri=ri: e.activation(
                    out=CLv[ri][:, g * 4:(g + 1) * 4, :], in_=self.bank(bk).rearrange("p (a b) -> p a b", a=4),
                    func=AF.Copy, scale=(1.0 if ri == 0 else -1.0)), reads=[self.pbank[bk]], writes=[clb])
        dskb = Buf("dsk")
        d16 = self.sb(128)
        fw.dma(out=d16[0:16, :], in_=W["ssm_d"][0].rearrange("(J p) -> J p", p=128), writes=[dskb])
        fw.op(pe, lambda e: e.transpose(out=self.bank(5)[:, 0:16], in_=d16[0:16, :], identity=self.ident[0:16, 0:16]),
              reads=[dskb, self.cb], writes=[self.pbank[5]])
        fw.op(dve, lambda e: e.tensor_copy(out=dsk, in_=self.bank(5)[:, 0:16]), reads=[self.pbank[5]], writes=[dskb])
        taub = Buf("tau")
        fw.op(pool, lambda e: e.iota(out=tau[:, 0:513], pattern=[[1, 513]], base=0, channel_multiplier=0,
                                     allow_small_or_imprecise_dtypes=True), writes=[taub])
        self.release(mT)
        NW = 2
        tabc_f = [self.sb(520) for _ in range(NW)]
        tabs_f = [self.sb(520) for _ in range(NW)]
        tk = [self.sb(520)[:, 0:513] for _ in range(NW)]
        tr_ = [self.sb(520)[:, 0:513] for _ in range(NW)]
        tang = [self.sb(520)[:, 0:513] for _ in range(NW)]
        tabc = [a[:, 0:512] for a in tabc_f]
        tabs = [a[:, 0:512] for a in tabs_f]
        tabb = [Buf() for _ in range(NW)]
        wk = [[self.sb(512) for _ in range(6)] for _ in range(NW)]
        wkb = [Buf() for _ in range(NW)]
        st8 = [self.sb(8) for _ in range(4)]
        st8b = [Buf() for _ in range(4)]
        clm = [self.sb(4 * 128) for _ in range(2)]
        clmb = Buf("clm")
        ufl0 = self.sb(T)
        ufl = [ufl0, ufl0]
        uflb0 = Buf()
        uflb = [uflb0, uflb0]
        yo = [self.sb(512) for _ in range(2)]
        yob = [Buf(), Buf()]
        yab0 = self.sb(T, BF16)
        yab = [yab0, yab0]
        yabb0 = Buf()
        yabb = [yabb0, yabb0]
        cnt = 0
        cnt2 = 0
        bu_done = {}
        bu_n = [0]

        def emit_bu(J_, jj_, tc_):
            bks = (5, 6) if bu_n[0] % 2 == 0 else (4, 7)
            bu_n[0] += 1
            rows_ = slice(jj_ * 32, (jj_ + 1) * 32) if jj_ < 3 else slice(64, 128)
            LBu_ = LBv if jj_ < 3 else LBzv
            for ri_, bk_ in ((0, bks[0]), (1, bks[1])):
                fw.op(pe, lambda e, ri_=ri_, bk_=bk_: e.matmul(
                    self.bank(bk_), lhsT=LBu_[ri_][rows_, J_, :], rhs=bigv[rows_, J_, tc_ * 512:(tc_ + 1) * 512],
                    start=True, stop=True), reads=[lbb, uTb_buf], writes=[self.pbank[bk_]])
            bu_done[(J_, jj_, tc_)] = bks

        for J in range(16):
            js = J % 2
            fw.dma(out=ufl[js], in_=U[J], reads=[Ub], writes=[uflb[js]])
            for ri in range(2):
                cm = clm[ri].rearrange("p (jj x) -> p jj x", jj=4)
                fw.op(pool, lambda e, ri=ri: e.memset(clm[ri], 0.0), writes=[clmb])
                for jj in range(4):
                    fw.op(pool, lambda e, ri=ri, jj=jj, cm=cm, J=J: e.tensor_copy(
                        out=cm[:, jj, jj * 32:(jj + 1) * 32], in_=CLv[ri][:, J, jj * 32:(jj + 1) * 32]),
                        reads=[clb, clmb], writes=[clmb])
            for jj in range(4):
                fw.op(dve, lambda e, jj=jj: e.memset(st8[jj], 0.0), writes=[st8b[jj]])
            for jj in range(4):
                j = J * 4 + jj
                w = cnt % NW
                cnt += 1
                fw.op(dve, lambda e, w=w, j=j: e.tensor_scalar(out=tang[w], in0=tau[:, 0:513], scalar1=THR[:, j:j + 1],
                                                               scalar2=None, op0=ALU.mult),
                      reads=[taub, pqb], writes=[tabb[w]])
                self.sin_rr(dve, tabs_f[w][:, 0:513], tang[w], tabb[w], tk[w], tr_[w])
                self.sin_rr(dve, tabc_f[w][:, 0:513], tang[w], tabb[w], tk[w], tr_[w], shift=math.pi / 2)
                C512 = tabc_f[w][:, 512:513]
                S512 = tabs_f[w][:, 512:513]
                for tc in range(4):
                    ybk = tc
                    w2 = cnt2 % NW
                    cnt2 += 1
                    if (J, jj, tc) not in bu_done:
                        emit_bu(J, jj, tc)
                    b5, b6 = bu_done[(J, jj, tc)]
                    nxt = (J, jj, tc + 1) if tc < 3 else ((J, jj + 1, 0) if jj < 3 else ((J + 1, 0, 0) if J < 15 else None))
                    if nxt is not None:
                        emit_bu(*nxt)
                    btr, bti, rre, rim, ta, tb = wk[w2]
                    B = wkb[w2]
                    fw.op(dve, lambda e, w=w, btr=btr: e.tensor_tensor(out=btr, in0=self.bank(b5), in1=tabc[w], op=ALU.mult),
                          reads=[self.pbank[b5], tabb[w]], writes=[B])
                    fw.op(dve, lambda e, w=w, ta=ta: e.tensor_tensor(out=ta, in0=self.bank(b6), in1=tabs[w], op=ALU.mult),
                          reads=[self.pbank[b6], tabb[w]], writes=[B])
                    fw.op(dve, lambda e, btr=btr, ta=ta: e.tensor_tensor(out=btr, in0=btr, in1=ta, op=ALU.add),
                          reads=[B], writes=[B])
                    fw.op(dve, lambda e, w=w, bti=bti: e.tensor_tensor(out=bti, in0=self.bank(b6), in1=tabc[w], op=ALU.mult),
                          reads=[self.pbank[b6], tabb[w]], writes=[B])
                    fw.op(dve, lambda e, w=w, tb=tb: e.tensor_tensor(out=tb, in0=self.bank(b5), in1=tabs[w], op=ALU.mult),
                          reads=[self.pbank[b5], tabb[w]], writes=[B])
                    fw.op(dve, lambda e, bti=bti, tb=tb: e.tensor_tensor(out=bti, in0=bti, in1=tb, op=ALU.subtract),
                          reads=[B], writes=[B])
                    c8 = st8[jj]
                    cb8 = st8b[jj]
                    fw.op(dve, lambda e, c8=c8, j=j: e.tensor_scalar(out=c8[:, 2:3], in0=c8[:, 0:1],
                                                                     scalar1=C512, scalar2=None, op0=ALU.mult),
                          reads=[cb8, tabb[w]], writes=[cb8])
                    fw.op(dve, lambda e, c8=c8, j=j: e.tensor_scalar(out=c8[:, 4:5], in0=c8[:, 1:2],
                                                                     scalar1=S512, scalar2=None, op0=ALU.mult),
                          reads=[cb8, tabb[w]], writes=[cb8])
                    fw.op(dve, lambda e, c8=c8: e.tensor_tensor(out=c8[:, 2:3], in0=c8[:, 2:3], in1=c8[:, 4:5],
                                                                op=ALU.subtract), reads=[cb8], writes=[cb8])
                    fw.op(dve, lambda e, c8=c8, j=j: e.tensor_scalar(out=c8[:, 3:4], in0=c8[:, 0:1],
                                                                     scalar1=S512, scalar2=None, op0=ALU.mult),
                          reads=[cb8, tabb[w]], writes=[cb8])
                    fw.op(dve, lambda e, c8=c8, j=j: e.tensor_scalar(out=c8[:, 4:5], in0=c8[:, 1:2],
                                                                     scalar1=C512, scalar2=None, op0=ALU.mult),
                          reads=[cb8, tabb[w]], writes=[cb8])
                    fw.op(dve, lambda e, c8=c8: e.tensor_tensor(out=c8[:, 3:4], in0=c8[:, 3:4], in1=c8[:, 4:5],
                                                                op=ALU.add), reads=[cb8], writes=[cb8])
                    rho_b = RHO[:, j:j + 1].to_broadcast([128, 512])
                    fw.op(dve, lambda e, rre=rre, btr=btr, c8=c8, rho_b=rho_b: e.tensor_tensor_scan(
                        out=rre, data0=rho_b, data1=btr, initial=c8[:, 2:3], op0=ALU.mult, op1=ALU.add),
                        reads=[B, cb8, pqb], writes=[B])
                    fw.op(dve, lambda e, rim=rim, bti=bti, c8=c8, rho_b=rho_b: e.tensor_tensor_scan(
                        out=rim, data0=rho_b, data1=bti, initial=c8[:, 3:4], op0=ALU.mult, op1=ALU.add),
                        reads=[B, cb8, pqb], writes=[B])
                    fw.op(dve, lambda e, c8=c8, rre=rre: e.tensor_copy(out=c8[:, 0:1], in_=rre[:, 511:512]),
                          reads=[B, cb8], writes=[cb8])
                    fw.op(dve, lambda e, c8=c8, rim=rim: e.tensor_copy(out=c8[:, 1:2], in_=rim[:, 511:512]),
                          reads=[B, cb8], writes=[cb8])
                    fw.op(pool, lambda e, w=w, ta=ta, rre=rre: e.tensor_tensor(out=ta, in0=rre, in1=tabc[w], op=ALU.mult),
                          reads=[B, tabb[w]], writes=[B])
                    fw.op(pool, lambda e, w=w, tb=tb, rim=rim: e.tensor_tensor(out=tb, in0=rim, in1=tabs[w], op=ALU.mult),
                          reads=[B, tabb[w]], writes=[B])
                    fw.op(pool, lambda e, w=w, ta=ta, tb=tb: e.tensor_tensor(out=ta, in0=ta, in1=tb, op=ALU.subtract),
                          reads=[B], writes=[B])
                    fw.op(pool, lambda e, w=w, tb=tb, rre=rre: e.tensor_tensor(out=tb, in0=rre, in1=tabs[w], op=ALU.mult),
                          reads=[B, tabb[w]], writes=[B])
                    fw.op(pool, lambda e, w=w, rre=rre, rim=rim: e.tensor_tensor(out=rre, in0=rim, in1=tabc[w], op=ALU.mult),
                          reads=[B, tabb[w]], writes=[B])
                    fw.op(pool, lambda e, tb=tb, rre=rre: e.tensor_tensor(out=tb, in0=tb, in1=rre, op=ALU.add),
                          reads=[B], writes=[B])
                    cm0 = clm[0].rearrange("p (jj x) -> p jj x", jj=4)
                    cm1 = clm[1].rearrange("p (jj x) -> p jj x", jj=4)
                    fw.op(pe, lambda e, ta=ta, jj=jj, cm0=cm0: e.matmul(self.bank(ybk), lhsT=cm0[:, jj, :], rhs=ta,
                                                                        start=(jj == 0), stop=False),
                          reads=[clmb, B], writes=[self.pbank[ybk]])
                    fw.op(pe, lambda e, tb=tb, jj=jj, cm1=cm1: e.matmul(self.bank(ybk), lhsT=cm1[:, jj, :], rhs=tb,
                                                                        start=False, stop=(jj == 3)),
                          reads=[clmb, B], writes=[self.pbank[ybk]])
            for tc in range(4):
                ybk = tc
                ys = (J * 4 + tc) % 2
                fw.op(dve, lambda e, ys=ys, js=js, J=J, tc=tc: e.scalar_tensor_tensor(
                    out=yo[ys], in0=ufl[js][:, tc * 512:(tc + 1) * 512], scalar=dsk[:, J:J + 1], in1=self.bank(ybk),
                    op0=ALU.mult, op1=ALU.add), reads=[uflb[js], dskb, self.pbank[ybk]], writes=[yob[ys]])
                fw.op(act, lambda e, ys=ys, js=js, tc=tc: e.activation(out=yab[js][:, tc * 512:(tc + 1) * 512],
                                                                     in_=yo[ys], func=AF.Gelu),
                      reads=[yob[ys]], writes=[yabb[js]])
            fw.dma(out=YA[J], in_=yab[js], reads=[yabb[js]], writes=[YAb])
        self.release(mP)
        for q in range(4):
            fw.dma(out=bigv[:, q * 4:(q + 1) * 4, :], in_=YA.rearrange("j p t -> p j t")[:, q * 4:(q + 1) * 4, :],
                   reads=[YAb], writes=[xTb])
        mG = self.mark()
        wt = [self.sb(16 * 128, BF16) for _ in range(2)]
        wtb = [Buf(), Buf()]
        bgl = self.sb(16)
        bglb = Buf()
        b16 = self.sb(128)
        fw.dma(out=b16[0:16, :], in_=W["ssm_b_glu"][0].rearrange("(J p) -> J p", p=128), writes=[bglb])
        fw.op(pe, lambda e: e.transpose(out=self.bank(5)[:, 0:16], in_=b16[0:16, :], identity=self.ident[0:16, 0:16]),
              reads=[bglb, self.cb], writes=[self.pbank[5]])
        fw.op(dve, lambda e: e.tensor_copy(out=bgl, in_=self.bank(5)[:, 0:16]), reads=[self.pbank[5]], writes=[bglb])
        sg = [self.sb(512) for _ in range(2)]
        sgb = [Buf(), Buf()]
        y2 = [self.sb(T, BF16) for _ in range(2)]
        y2b = [Buf(), Buf()]
        for mo in range(16):
            s = mo % 2
            self.proj_fm(bigv, xTb, W["ssm_w_glu"][0][:, mo * 128:(mo + 1) * 128], wt[s], wtb[s])
            for tc in range(4):
                s2 = tc % 2
                fw.op(act, lambda e, s2=s2, tc=tc, mo=mo: e.activation(out=sg[s2], in_=self.bank(tc), func=AF.Sigmoid,
                                                                     bias=bgl[:, mo:mo + 1], scale=1.0),
                      reads=[self.pbank[tc], bglb], writes=[sgb[s2]])
                fw.op(dve, lambda e, s=s, s2=s2, tc=tc, mo=mo: e.tensor_tensor(
                    out=y2[s][:, tc * 512:(tc + 1) * 512], in0=sg[s2], in1=bigv[:, mo, tc * 512:(tc + 1) * 512],
                    op=ALU.mult), reads=[sgb[s2], xTb], writes=[y2b[s]])
            fw.dma(out=OT[mo], in_=y2[s], reads=[y2b[s]], writes=[OTb])
        self.release(mG)
        self.release(m0)
        self.outproj_ln(OT, OTb, W["ssm_w_out"][0], src, src_buf, W["ln_g"][L, 0], W["ln_b"][L, 0], dst, dst_buf)

    def dsa(self, L, src, src_buf, W, dst, dst_buf):
        fw = self.fw
        dve, act, pool, pe = fw.dve, fw.act, fw.pool, fw.pe
        QT, QTb = self.dram["QT"], self.dbuf["QT"]
        QI, QIb = self.dram["QI"], self.dbuf["QI"]
        MT, MTb = self.dram["MASKT"], self.dbuf["MASKT"]
        OT, OTb = self.dram["OT"], self.dbuf["OT"]
        w_in = W["dsa_w_in"][0]
        m0 = self.mark()
        kT = self.sb(T, BF16)
        kiT = self.sb(T, BF16)
        vtok = self.sb(16 * 132, BF16)
        vtv = vtok.rearrange("p (a b) -> p a b", a=16)
        wi = self.sb(16 * 16)
        wiv = wi.rearrange("p (a b) -> p a b", a=16)
        resb = Buf("dsa_res")
        fw.op(pool, lambda e: e.memset(vtok, 1.0), writes=[resb])
        m1 = self.mark()
        big = self.sb(16 * T, BF16)
        bigv = big.rearrange("p (a b) -> p a b", a=16)
        xTb = Buf("xT")
        self.build_xT(src, src_buf, bigv, xTb)
        wt = [self.sb(16 * 128, BF16) for _ in range(2)]
        wtb = [Buf(), Buf()]
        ob = [self.sb(T, BF16) for _ in range(2)]
        obb = [Buf(), Buf()]
        QSC = 128.0 ** -0.5
        n = 0
        for (col0, cnt_, dstD, dstDb, scale) in ((0, 16, QT, QTb, QSC), (2304, 16, QI, QIb, 1.0)):
            for hh in range(cnt_):
                s = n % 2
                n += 1
                self.proj_fm(bigv, xTb, w_in[:, col0 + hh * 128: col0 + (hh + 1) * 128], wt[s], wtb[s])
                for tc in range(4):
                    dstp = ob[s][:, tc * 512:(tc + 1) * 512]
                    if tc % 2 == 0:
                        fw.op(dve, lambda e, dstp=dstp, tc=tc, scale=scale: e.tensor_scalar(
                            out=dstp, in0=self.bank(tc), scalar1=scale, scalar2=None, op0=ALU.mult),
                            reads=[self.pbank[tc]], writes=[obb[s]])
                    else:
                        fw.op(act, lambda e, dstp=dstp, tc=tc, scale=scale: e.activation(
                            out=dstp, in_=self.bank(tc), func=AF.Copy, scale=scale),
                            reads=[self.pbank[tc]], writes=[obb[s]])
                fw.dma(out=dstD[hh], in_=ob[s], reads=[obb[s]], writes=[dstDb])
        for (col0, dstT) in ((2048, kT), (4352, kiT)):
            s = n % 2
            n += 1
            self.proj_fm(bigv, xTb, w_in[:, col0: col0 + 128], wt[s], wtb[s])
            for tc in range(4):
                dstp = dstT[:, tc * 512:(tc + 1) * 512]
                fw.op(act, lambda e, dstp=dstp, tc=tc: e.activation(out=dstp, in_=self.bank(tc), func=AF.Copy),
                      reads=[self.pbank[tc]], writes=[resb])
        wv = wt[0]
        wvv = wv.rearrange("p (a b) -> p a b", a=16)
        fw.dma(out=wvv, in_=w_in[:, 2176:2304].rearrange("(kc p) f -> p kc f", p=128), writes=[wtb[0]], q=pool)
        ww = wt[1][:, 0:256]
        wwv = ww.rearrange("p (a b) -> p a b", a=16)
        fw.dma(out=wwv, in_=w_in[:, 4480:4496].rearrange("(kc p) f -> p kc f", p=128), writes=[wtb[1]], q=pool)
        WSC = (16.0 ** -0.5) * (128.0 ** -0.5)
        for i in range(NT):
            bk = 4 + (i % 2)
            for kc in range(16):
                fw.op(pe, lambda e, i=i, kc=kc, bk=bk: e.matmul(self.bank(bk)[:, 0:128], lhsT=bigv[:, kc, i * 128:(i + 1) * 128],
                                                               rhs=wvv[:, kc, :], start=(kc == 0), stop=(kc == 15)),
                      reads=[xTb, wtb[0]], writes=[self.pbank[bk]])
            fw.op(act, lambda e, i=i, bk=bk: e.activation(out=vtv[:, i, 0:128], in_=self.bank(bk)[:, 0:128], func=AF.Copy),
                  reads=[self.pbank[bk]], writes=[resb])
            bk2 = 6 + (i % 2)
            for kc in range(16):
                fw.op(pe, lambda e, i=i, kc=kc, bk2=bk2: e.matmul(self.bank(bk2)[:, 0:16], lhsT=bigv[:, kc, i * 128:(i + 1) * 128],
                                                                 rhs=wwv[:, kc, :], start=(kc == 0), stop=(kc == 15)),
                      reads=[xTb, wtb[1]], writes=[self.pbank[bk2]])
            fw.op(dve, lambda e, i=i, bk2=bk2: e.tensor_scalar(out=wiv[:, i, :], in0=self.bank(bk2)[:, 0:16], scalar1=WSC,
                                                               scalar2=None, op0=ALU.mult),
                  reads=[self.pbank[bk2]], writes=[resb])
        self.release(m1)
        m2 = self.mark()
        qit = [self.sb(16 * 128, BF16) for _ in range(2)]
        qitb = [Buf(), Buf()]
        acc = self.sb(T)
        accb = Buf("acc")
        work = self.sb(T)
        workb = Buf("work")
        rl = [self.sb(512, BF16) for _ in range(2)]
        rlb = [Buf(), Buf()]
        m8 = self.sb(8)
        m8b = Buf()
        mk = self.sb(T, BF16)
        mkb = Buf()
        mT = [self.sb(16 * 128, BF16) for _ in range(2)]
        mTb = [Buf(), Buf()]
        QIv = QI.rearrange("h p t -> p h t")
        MTv = MT.rearrange("b p t -> p b t")
        nr = 0
        for i in range(NT):
            s = i % 2
            S_ = 128 * (i + 1)
            fw.dma(out=qit[s].rearrange("p (a b) -> p a b", a=16), in_=QIv[:, :, i * 128:(i + 1) * 128], reads=[QIb],
                   writes=[qitb[s]])
            qv = qit[s].rearrange("p (a b) -> p a b", a=16)
            nsc = (S_ + 511) // 512
            for hh in range(16):
                for sc in range(nsc):
                    c0 = sc * 512
                    c1 = min(S_, c0 + 512)
                    bk = nr % 4
                    r2 = nr % 2
                    nr += 1
                    fw.op(pe, lambda e, hh=hh, c0=c0, c1=c1, bk=bk, qv=qv: e.matmul(
                        self.bank(bk)[:, 0:c1 - c0], lhsT=qv[:, hh, :], rhs=kiT[:, c0:c1], start=True, stop=True),
                        reads=[qitb[s], resb], writes=[self.pbank[bk]])
                    fw.op(act, lambda e, c0=c0, c1=c1, bk=bk, r2=r2: e.activation(
                        out=rl[r2][:, 0:c1 - c0], in_=self.bank(bk)[:, 0:c1 - c0], func=AF.Relu),
                        reads=[self.pbank[bk]], writes=[rlb[r2]])
                    if hh == 0:
                        fw.op(dve, lambda e, c0=c0, c1=c1, r2=r2, i=i: e.tensor_scalar(
                            out=acc[:, c0:c1], in0=rl[r2][:, 0:c1 - c0], scalar1=wiv[:, i, 0:1], scalar2=None,
                            op0=ALU.mult), reads=[rlb[r2], resb], writes=[accb])
                    else:
                        fw.op(dve, lambda e, c0=c0, c1=c1, r2=r2, i=i, hh=hh: e.scalar_tensor_tensor(
                            out=acc[:, c0:c1], in0=rl[r2][:, 0:c1 - c0], scalar=wiv[:, i, hh:hh + 1], in1=acc[:, c0:c1],
                            op0=ALU.mult, op1=ALU.add), reads=[rlb[r2], resb, accb], writes=[accb])
            fw.op(pool, lambda e, S_=S_: e.affine_select(out=acc[:, S_ - 128:S_], in_=acc[:, S_ - 128:S_],
                                                         pattern=[[-1, 128]], compare_op=ALU.is_ge, fill=-1e30, base=0,
                                                         channel_multiplier=1), reads=[accb], writes=[accb])
            if i >= 2:
                cur = acc
                curb = accb
                for rnd in range(32):
                    fw.op(dve, lambda e, cur=cur, S_=S_: e.max(out=m8, in_=cur[:, 0:S_]), reads=[curb], writes=[m8b])
                    if rnd < 31:
                        fw.op(dve, lambda e, cur=cur, S_=S_: e.match_replace(out=work[:, 0:S_], in_to_replace=m8,
                                                                             in_values=cur[:, 0:S_], imm_value=-3e38),
                              reads=[curb, m8b], writes=[workb])
                        cur = work
                        curb = workb
                fw.op(dve, lambda e, S_=S_: e.tensor_scalar(out=mk[:, 0:S_], in0=acc[:, 0:S_], scalar1=m8[:, 7:8],
                                                            scalar2=None, op0=ALU.is_ge), reads=[accb, m8b], writes=[mkb])
            else:
                fw.op(dve, lambda e, S_=S_: e.tensor_scalar(out=mk[:, 0:S_], in0=acc[:, 0:S_], scalar1=-1e29,
                                                            scalar2=None, op0=ALU.is_ge), reads=[accb], writes=[mkb])
            mTv = mT[s].rearrange("p (a b) -> p a b", a=16)
            for g in range((i + 8) // 8):
                bk = 4 + (g + i) % 2
                bkb = self.bank(bk, BF16)
                nb_ = min(8, i + 1 - g * 8)
                for j in range(nb_):
                    b = g * 8 + j
                    fw.op(pe, lambda e, b=b, j=j, bkb=bkb: e.transpose(out=bkb[:, j * 128:(j + 1) * 128],
                                                                      in_=mk[:, b * 128:(b + 1) * 128], identity=self.identb),
                          reads=[mkb, self.cb], writes=[self.pbank[bk]])
                fw.op(act, lambda e, g=g, nb_=nb_, bkb=bkb, mTv=mTv: e.activation(
                    out=mTv[:, g * 8:g * 8 + nb_, :], in_=bkb[:, 0:nb_ * 128].rearrange("p (a b) -> p a b", a=nb_),
                    func=AF.Copy), reads=[self.pbank[bk]], writes=[mTb[s]])
            fw.dma(out=MTv[:, 0:i + 1, i * 128:(i + 1) * 128], in_=mTv[:, 0:i + 1, :], reads=[mTb[s]], writes=[MTb])
        self.release(m2)
        m3 = self.mark()
        A1 = self.sb(2432)
        a1b = Buf("A1")
        fw.op(pool, lambda e: e.iota(out=A1, pattern=[[-1, 2432]], base=384, channel_multiplier=1,
                                     allow_small_or_imprecise_dtypes=True), writes=[a1b])
        fw.op(pool, lambda e: e.tensor_scalar(out=A1, in0=A1, scalar1=0.0, scalar2=None, op0=ALU.min), reads=[a1b],
              writes=[a1b])
        mres = self.sb(16 * T, BF16)
        mrv = mres.rearrange("p (a b) -> p a b", a=16)
        mrb = Buf("maskres")
        fw.op(pool, lambda e: e.memset(mres, 0.0), writes=[mrb])
        for b in range(16):
            fw.dma(out=mrv[:, b, b * 128:T], in_=MT[b][:, b * 128:T], reads=[MTb], writes=[mrb])
        qh = [self.sb(T, BF16) for _ in range(2)]
        qhb = [Buf(), Buf()]
        oth = [self.sb(T, BF16) for _ in range(2)]
        othb = [Buf(), Buf()]
        tmp = [self.sb(512) for _ in range(2)]
        tmpb = [Buf(), Buf()]
        pp = [self.sb(512, BF16) for _ in range(2)]
        ppb = [Buf(), Buf()]
        pm = [self.sb(512, BF16) for _ in range(2)]
        pmb = [Buf(), Buf()]
        rden = self.sb(4)
        rdb = Buf()
        on = self.sb(512, BF16)
        onb = Buf()
        nu = 0
        ng = 0
        for hh in range(16):
            s = hh % 2
            slope = 2.0 ** (-(hh + 1) / 2.0)
            fw.dma(out=qh[s], in_=QT[hh], reads=[QTb], writes=[qhb[s]])
            for c in range(4):
                tbk = 6 + (ng % 2)
                ng += 1
                nb = 4 * (c + 1)
                for b in range(nb):
                    u = nu % 2
                    nu += 1
                    lbk = u
                    fw.op(pe, lambda e, b=b, c=c, lbk=lbk, s=s: e.matmul(
                        self.bank(lbk), lhsT=kT[:, b * 128:(b + 1) * 128], rhs=qh[s][:, c * 512:(c + 1) * 512],
                        start=True, stop=True), reads=[resb, qhb[s]], writes=[self.pbank[lbk]])
                    off = 512 * c - 128 * b + 384
                    fw.op(dve, lambda e, u=u, off=off, lbk=lbk, slope=slope: e.scalar_tensor_tensor(
                        out=tmp[u], in0=A1[:, off:off + 512], scalar=slope, in1=self.bank(lbk), op0=ALU.mult, op1=ALU.add),
                        reads=[a1b, self.pbank[lbk]], writes=[tmpb[u]])
                    fw.op(act, lambda e, u=u: e.activation(out=pp[u], in_=tmp[u], func=AF.Exp), reads=[tmpb[u]],
                          writes=[ppb[u]])
                    fw.op(pool, lambda e, u=u, b=b, c=c: e.tensor_tensor(out=pm[u], in0=pp[u],
                                                                        in1=mrv[:, b, c * 512:(c + 1) * 512], op=ALU.mult),
                          reads=[ppb[u], mrb], writes=[pmb[u]])
                    for sub in range(4):
                        tt_ = 4 * c + sub
                        if b > tt_:
                            continue
                        obk = 2 + sub
                        fw.op(pe, lambda e, u=u, sub=sub, b=b, tt_=tt_, obk=obk: e.matmul(
                            self.bank(obk)[:, 0:129], lhsT=pm[u][:, sub * 128:(sub + 1) * 128],
                            rhs=vtv[:, b, 0:129], start=(b == 0), stop=(b == tt_)),
                            reads=[pmb[u], resb], writes=[self.pbank[obk]])
                for sub in range(4):
                    obk = 2 + sub
                    fw.op(dve, lambda e, obk=obk, sub=sub: e.reciprocal(out=rden[:, sub:sub + 1], in_=self.bank(obk)[:, 128:129]),
                          reads=[self.pbank[obk]], writes=[rdb])
                    fw.op(dve, lambda e, sub=sub, obk=obk: e.tensor_scalar(
                        out=on[:, sub * 128:(sub + 1) * 128], in0=self.bank(obk)[:, 0:128],
                        scalar1=rden[:, sub:sub + 1], scalar2=None, op0=ALU.mult),
                        reads=[self.pbank[obk], rdb], writes=[onb])
                tbb = self.bank(tbk, BF16)
                for sub in range(4):
                    fw.op(pe, lambda e, sub=sub, tbb=tbb: e.transpose(out=tbb[:, sub * 128:(sub + 1) * 128],
                                                                      in_=on[:, sub * 128:(sub + 1) * 128], identity=self.identb),
                          reads=[onb, self.cb], writes=[self.pbank[tbk]])
                fw.op(act, lambda e, s=s, c=c, tbb=tbb: e.activation(out=oth[s][:, c * 512:(c + 1) * 512], in_=tbb[:, 0:512],
                                                                      func=AF.Copy), reads=[self.pbank[tbk]], writes=[othb[s]])
            fw.dma(out=OT[hh], in_=oth[s], reads=[othb[s]], writes=[OTb])
        self.release(m3)
        self.release(m0)
        self.outproj_ln(OT, OTb, W["dsa_w_out"][0], src, src_buf, W["ln_g"][L, 0], W["ln_b"][L, 0], dst, dst_buf)

    def gdn(self, li, L, src, src_buf, W, dst, dst_buf):
        fw = self.fw
        dve, act, pool, pe = fw.dve, fw.act, fw.pool, fw.pe
        OT, OTb = self.dram["OT"], self.dbuf["OT"]
        w_in = W["gdn_w_in"][li]
        m0 = self.mark()
        A128 = lambda: self.sb(128)
        U2, B2, NEGM4 = A128(), A128(), self.sb(512)
        gcb = Buf("gdnconst")
        fw.op(pool, lambda e: e.memset(U2, 1.0), writes=[gcb])
        fw.op(pool, lambda e: e.affine_select(out=U2, in_=U2, pattern=[[1, 128]], compare_op=ALU.is_ge, fill=0.0,
                                              base=0, channel_multiplier=-1), reads=[gcb], writes=[gcb])
        fw.op(pool, lambda e: e.memset(U2[0:64, 64:128], 0.0), reads=[gcb], writes=[gcb])
        fw.op(pool, lambda e: e.memset(B2, 0.0), writes=[gcb])
        fw.op(pool, lambda e: e.memset(B2[0:64, 0:64], 1.0), reads=[gcb], writes=[gcb])
        fw.op(pool, lambda e: e.memset(B2[64:128, 64:128], 1.0), reads=[gcb], writes=[gcb])
        N4 = NEGM4.rearrange("p (u i) -> p u i", u=4)
        fw.op(pool, lambda e: e.memset(NEGM4, 0.0), writes=[gcb])
        fw.op(pool, lambda e: e.affine_select(out=N4, in_=N4, pattern=[[0, 4], [1, 128]], compare_op=ALU.is_ge, fill=NEG,
                                              base=0, channel_multiplier=-1), reads=[gcb], writes=[gcb])
        fw.op(pool, lambda e: e.memset(N4[0:64, :, 64:128], NEG), reads=[gcb], writes=[gcb])
        id4 = self.ident.unsqueeze(1).to_broadcast([128, 4, 128])
        ABt = self.sb(16 * 32)
        ABv = ABt.rearrange("p (a b) -> p a b", a=16)
        GT, GC, EGC, EKD, NGC, BT, NB = [self.sb(256) for _ in range(7)]
        v16 = lambda a: a.rearrange("p (a b) -> p a b", a=16)
        CW = self.sb(48 * 4)
        CWv = CW.rearrange("p (c j) -> p c j", j=4)
        NGrow = self.sb(128)
        gb = Buf("gdn_g")
        self.load_row(NGrow, W["gdn_norm_g"][li], gb, 128)
        arow = self.sb(32)
        self.load_row(arow[:, 0:16], W["gdn_a_log"][li], gb, 16)
        self.load_row(arow[:, 16:32], W["gdn_dt_bias"][li], gb, 16)
        cwrow = self.sb(6144)
        fw.dma(out=cwrow[0:4, :], in_=W["gdn_conv_w"][li], writes=[gb])
        for c in range(48):
            fw.op(pe, lambda e, c=c: e.transpose(out=self.bank(4)[:, c * 4:(c + 1) * 4], in_=cwrow[0:4, c * 128:(c + 1) * 128],
                                                 identity=self.ident[0:4, 0:4]), reads=[gb, self.cb], writes=[self.pbank[4]])
        fw.op(dve, lambda e: e.tensor_copy(out=CW, in_=self.bank(4)[:, 0:192]), reads=[self.pbank[4]], writes=[gb])
        self.off -= 6144 + 0
        fw.barrier()
        big = self.sb(16 * T, BF16)
        bigv = big.rearrange("p (a b) -> p a b", a=16)
        xTb = Buf("xT")
        self.build_xT(src, src_buf, bigv, xTb)
        mg = self.mark()
        wab = self.sb(16 * 32, BF16)
        wabv = wab.rearrange("p (a b) -> p a b", a=16)
        wabb = Buf()
        fw.dma(out=wabv, in_=w_in[:, 8192:8224].rearrange("(kc p) f -> p kc f", p=128), writes=[wabb], q=pool)
        for i in range(NT):
            bk = 4 + i % 4
            for kc in range(16):
                fw.op(pe, lambda e, i=i, kc=kc, bk=bk: e.matmul(self.bank(bk)[:, 0:32], lhsT=bigv[:, kc, i * 128:(i + 1) * 128],
                                                               rhs=wabv[:, kc, :], start=(kc == 0), stop=(kc == 15)),
                      reads=[xTb, wabb], writes=[self.pbank[bk]])
            fw.op(act, lambda e, i=i, bk=bk: e.activation(out=ABv[:, i, :], in_=self.bank(bk)[:, 0:32], func=AF.Copy),
                  reads=[self.pbank[bk]], writes=[gb])
        t1, t2, t3 = self.sb(256), self.sb(256), self.sb(256)
        nea = self.sb(16)
        dtb_b = arow[:, 16:32].unsqueeze(1).to_broadcast([128, 16, 16])
        fw.op(dve, lambda e: e.tensor_tensor(out=v16(t1), in0=ABv[:, :, 0:16], in1=dtb_b, op=ALU.add), reads=[gb], writes=[gb])
        fw.op(dve, lambda e: e.tensor_scalar(out=t2, in0=t1, scalar1=-1.0, scalar2=None, op0=ALU.mult), reads=[gb], writes=[gb])
        fw.op(dve, lambda e: e.tensor_tensor(out=t2, in0=t2, in1=t1, op=ALU.max), reads=[gb], writes=[gb])
        fw.op(act, lambda e: e.activation(out=t2, in_=t2, func=AF.Exp, scale=-1.0), reads=[gb], writes=[gb])
        fw.op(dve, lambda e: e.tensor_scalar(out=t2, in0=t2, scalar1=1.0, scalar2=None, op0=ALU.add), reads=[gb], writes=[gb])
        fw.op(act, lambda e: e.activation(out=t2, in_=t2, func=AF.Ln), reads=[gb], writes=[gb])
        fw.op(dve, lambda e: e.scalar_tensor_tensor(out=t3, in0=t1, scalar=0.0, in1=t2, op0=ALU.max, op1=ALU.add),
              reads=[gb], writes=[gb])
        fw.op(act, lambda e: e.activation(out=nea, in_=arow[:, 0:16], func=AF.Exp), reads=[gb], writes=[gb])
        fw.op(dve, lambda e: e.tensor_scalar(out=nea, in0=nea, scalar1=-1.0, scalar2=None, op0=ALU.mult), reads=[gb], writes=[gb])
        fw.op(dve, lambda e: e.tensor_tensor(out=v16(GT), in0=v16(t3), in1=nea.unsqueeze(1).to_broadcast([128, 16, 16]),
                                             op=ALU.mult), reads=[gb], writes=[gb])
        fw.op(act, lambda e: e.activation(out=v16(BT), in_=ABv[:, :, 16:32], func=AF.Sigmoid), reads=[gb], writes=[gb])
        fw.op(dve, lambda e: e.tensor_scalar(out=NB, in0=BT, scalar1=-1.0, scalar2=None, op0=ALU.mult), reads=[gb], writes=[gb])
        fw.op(pe, lambda e: e.matmul(self.bank(4)[:, 0:256], lhsT=U2, rhs=GT, start=True, stop=True), reads=[gb, gcb],
              writes=[self.pbank[4]])
        fw.op(pe, lambda e: e.matmul(self.bank(5)[:, 0:256], lhsT=B2, rhs=GT, start=True, stop=True), reads=[gb, gcb],
              writes=[self.pbank[5]])
        fw.op(dve, lambda e: e.tensor_copy(out=GC, in_=self.bank(4)[:, 0:256]), reads=[self.pbank[4]], writes=[gb])
        fw.op(act, lambda e: e.activation(out=EGC, in_=self.bank(4)[:, 0:256], func=AF.Exp), reads=[self.pbank[4]], writes=[gb])
        fw.op(dve, lambda e: e.tensor_tensor(out=t1, in0=self.bank(5)[:, 0:256], in1=GC, op=ALU.subtract),
              reads=[self.pbank[5], gb], writes=[gb])
        fw.op(act, lambda e: e.activation(out=EKD, in_=t1, func=AF.Exp), reads=[gb], writes=[gb])
        fw.op(dve, lambda e: e.tensor_scalar(out=NGC, in0=GC, scalar1=-1.0, scalar2=None, op0=ALU.mult), reads=[gb], writes=[gb])
        self.release(mg)
        GCv, EGCv, EKDv, NGCv, BTv, NBv = [v16(a) for a in (GC, EGC, EKD, NGC, BT, NB)]
        wt = [self.sb(16 * 128, BF16) for _ in range(2)]
        wtb = [Buf(), Buf()]
        qT, kT, vT = [self.sb(T).bitcast(BF16)[:, 0:T] for _ in range(3)]
        qkvb = Buf("qkv")
        ktok_f, vtok_f, otok = self.sb(T), self.sb(T), self.sb(T)
        ktok, vtok = ktok_f.bitcast(BF16)[:, 0:T], vtok_f.bitcast(BF16)[:, 0:T]
        ktv, vtv, otv = [a.rearrange("p (a b) -> p a b", a=16) for a in (ktok, vtok, otok)]
        tokb = Buf("tok")
        otb = Buf("otok")
        regA = self.sb(8192)
        raw = regA[:, 0:2056]
        cacc = regA[:, 2056:2056 + 2048]
        tmpq = regA[:, 4104:4104 + 2048]
        rb = Buf("raw")
        names = ["DG4", "EGR4", "Dt4", "Mt4", "Nn4", "Qa", "Qta", "Qb", "Qtb", "X4", "AT4", "KG4", "KD4", "WT4", "BU4", "QD4"]
        F32T = ("DG4", "EGR4", "Dt4", "BU4")
        QTL = {n_: (regA[:, i_ * 512:(i_ + 1) * 512] if n_ in F32T else regA[:, i_ * 512:(i_ + 1) * 512].bitcast(BF16)[:, 0:512])
               for i_, n_ in enumerate(names)}
        QB = {n_: Buf(n_) for n_ in names}
        q3 = lambda a: a.rearrange("p (u i) -> p u i", u=4)
        VN = self.sb(128, BF16)
        vnb = Buf("VN")
        S = self.sb(128)
        Sb = Buf("S")
        Sbf = self.sb(128, BF16)
        Sbfb = Buf("Sbf")
        oth = [self.sb(T, BF16) for _ in range(2)]
        othb = [Buf(), Buf()]
        sm = self.sb(64)
        smb = Buf()
        fw.op(dve, lambda e: e.memset(raw[:, 0:3], 0.0), writes=[rb])
        nbk = [0]

        def nb_():
            nbk[0] += 1
            return 4 + nbk[0] % 4

        for h in range(16):
            fw.barrier()
            for kind, col0, dstT in (("q", 0, qT), ("k", 2048, kT), ("v", 4096, vT)):
                s = nbk[0] % 2
                nbk[0] += 1
                self.proj_fm(bigv, xTb, w_in[:, col0 + h * 128: col0 + (h + 1) * 128], wt[s], wtb[s])
                for tc in range(4):
                    dstp = raw[:, 3 + tc * 512: 3 + (tc + 1) * 512]
                    if tc % 2 == 0:
                        fw.op(dve, lambda e, dstp=dstp, tc=tc: e.tensor_copy(out=dstp, in_=self.bank(tc)),
                              reads=[self.pbank[tc]], writes=[rb])
                    else:
                        fw.op(act, lambda e, dstp=dstp, tc=tc: e.activation(out=dstp, in_=self.bank(tc), func=AF.Copy),
                              reads=[self.pbank[tc]], writes=[rb])
                ch = (col0 // 128) + h
                fw.op(dve, lambda e, ch=ch: e.tensor_scalar(out=cacc, in0=raw[:, 0:T], scalar1=CWv[:, ch, 0:1], scalar2=None,
                                                            op0=ALU.mult), reads=[rb, gb], writes=[rb])
                for j in range(1, 4):
                    fw.op(dve, lambda e, ch=ch, j=j: e.scalar_tensor_tensor(
                        out=cacc, in0=raw[:, j:j + T], scalar=CWv[:, ch, j:j + 1], in1=cacc, op0=ALU.mult, op1=ALU.add),
                        reads=[rb, gb], writes=[rb])
                if kind == "v":
                    fw.op(act, lambda e: e.activation(out=vT, in_=cacc, func=AF.Silu), reads=[rb], writes=[qkvb])
                    continue
                fw.op(act, lambda e: e.activation(out=tmpq, in_=cacc, func=AF.Silu), reads=[rb], writes=[rb])
                sq16 = raw[:, 8:8 + T // 2].bitcast(BF16)
                fw.op(act, lambda e: e.activation(out=sq16, in_=tmpq, func=AF.Square), reads=[rb], writes=[rb])
                for tc in range(4):
                    fw.op(pe, lambda e, tc=tc: e.matmul(self.bank(tc), lhsT=self.onesb, rhs=sq16[:, tc * 512:(tc + 1) * 512],
                                                        start=True, stop=True), reads=[rb, self.cb], writes=[self.pbank[tc]])
                    cs_ = cacc[:, tc * 512:(tc + 1) * 512]
                    fw.op(dve, lambda e, tc=tc, cs_=cs_: e.tensor_scalar(out=cs_, in0=self.bank(tc), scalar1=RMS_EPS,
                                                                         scalar2=None, op0=ALU.add),
                          reads=[self.pbank[tc], rb], writes=[rb])
                fw.op(act, lambda e: e.activation(out=cacc, in_=cacc, func=AF.Sqrt), reads=[rb], writes=[rb])
                fw.op(dve, lambda e: e.reciprocal(out=cacc, in_=cacc), reads=[rb], writes=[rb])
                sc_ = (128.0 ** -0.5) if kind == "q" else 1.0
                fw.op(dve, lambda e, dstT=dstT, sc_=sc_: e.scalar_tensor_tensor(out=dstT, in0=tmpq, scalar=sc_, in1=cacc,
                                                                                op0=ALU.mult, op1=ALU.mult),
                      reads=[rb], writes=[qkvb])
            for srcT, dv_ in ((kT, ktv), (vT, vtv)):
                for g in range(4):
                    bk = nb_()
                    bkb = self.bank(bk, BF16)
                    for j in range(4):
                        P_ = g * 4 + j
                        fw.op(pe, lambda e, srcT=srcT, P_=P_, j=j, bkb=bkb: e.transpose(
                            out=bkb[:, j * 128:(j + 1) * 128], in_=srcT[:, P_ * 128:(P_ + 1) * 128],
                            identity=self.identb), reads=[qkvb, self.cb], writes=[self.pbank[bk]])
                    fw.op(act, lambda e, dv_=dv_, g=g, bkb=bkb: e.activation(
                        out=dv_[:, g * 4:(g + 1) * 4, :], in_=bkb[:, 0:512].rearrange("p (a b) -> p a b", a=4), func=AF.Copy),
                        reads=[self.pbank[bk]], writes=[tokb])
            fw.barrier()
            fw.op(dve, lambda e: e.memset(S, 0.0), writes=[Sb])
            fw.op(dve, lambda e: e.memset(Sbf, 0.0), writes=[Sbfb])
            for Q in range(4):
                cols4 = slice(Q * 512, (Q + 1) * 512)
                P0 = Q * 4
                bc4 = lambda a: a[:, P0:P0 + 4, h].unsqueeze(2).to_broadcast([128, 4, 128])
                L_ = QTL
                fw.op(pool, lambda e: e.tensor_tensor(out=q3(L_["DG4"]), in0=id4, in1=bc4(GCv), op=ALU.mult),
                      reads=[gb, self.cb], writes=[QB["DG4"]])
                bA, bB = nb_(), nb_()
                fw.op(pe, lambda e: e.matmul(self.bank(bA), lhsT=self.ones, rhs=L_["DG4"], start=True, stop=True),
                      reads=[QB["DG4"], self.cb], writes=[self.pbank[bA]])
                fw.op(pe, lambda e: e.matmul(self.bank(bB), lhsT=self.ones, rhs=L_["DG4"], start=True, stop=False),
                      reads=[QB["DG4"], self.cb], writes=[self.pbank[bB]])
                fw.op(pe, lambda e: e.matmul(self.bank(bB), lhsT=self.ident, rhs=NEGM4, start=False, stop=True),
                      reads=[gcb, self.cb], writes=[self.pbank[bB]])
                fw.op(act, lambda e: e.activation(out=L_["EGR4"], in_=self.bank(bA), func=AF.Exp), reads=[self.pbank[bA]],
                      writes=[QB["EGR4"]])
                for u in range(4):
                    fw.op(act, lambda e, u=u: e.activation(out=L_["Dt4"][:, u * 128:(u + 1) * 128],
                                                           in_=self.bank(bB)[:, u * 128:(u + 1) * 128], func=AF.Exp,
                                                           bias=NGCv[:, P0 + u, h:h + 1], scale=1.0),
                          reads=[self.pbank[bB], gb], writes=[QB["Dt4"]])
                bK, bQ = nb_(), nb_()
                for u in range(4):
                    cu = slice((P0 + u) * 128, (P0 + u + 1) * 128)
                    fw.op(pe, lambda e, u=u, cu=cu: e.matmul(self.bank(bK)[:, u * 128:(u + 1) * 128], lhsT=kT[:, cu], rhs=kT[:, cu],
                                                             start=True, stop=True), reads=[qkvb], writes=[self.pbank[bK]])
                for u in range(4):
                    cu = slice((P0 + u) * 128, (P0 + u + 1) * 128)
                    fw.op(pe, lambda e, u=u, cu=cu: e.matmul(self.bank(bQ)[:, u * 128:(u + 1) * 128], lhsT=kT[:, cu], rhs=qT[:, cu],
                                                             start=True, stop=True), reads=[qkvb], writes=[self.pbank[bQ]])
                fw.op(dve, lambda e: e.tensor_tensor(out=q3(L_["Mt4"]), in0=self.bank(bK).rearrange("p (u i) -> p u i", u=4),
                                                     in1=bc4(BTv), op=ALU.mult), reads=[self.pbank[bK], gb], writes=[QB["Mt4"]])
                fw.op(dve, lambda e: e.tensor_tensor(out=L_["Mt4"], in0=L_["Mt4"], in1=L_["Dt4"], op=ALU.mult),
                      reads=[QB["Dt4"], QB["Mt4"]], writes=[QB["Mt4"]])
                fw.op(pool, lambda e: e.affine_select(out=q3(L_["Mt4"]), in_=q3(L_["Mt4"]), pattern=[[0, 4], [1, 128]],
                                                      compare_op=ALU.not_equal, fill=0.0, base=0, channel_multiplier=-1),
                      reads=[QB["Mt4"]], writes=[QB["Mt4"]])
                fw.op(dve, lambda e: e.tensor_tensor(out=L_["AT4"], in0=self.bank(bQ), in1=L_["Dt4"], op=ALU.mult),
                      reads=[self.pbank[bQ], QB["Dt4"]], writes=[QB["AT4"]])
                bN = nb_()
                bNb = self.bank(bN, BF16)
                for u in range(4):
                    fw.op(pe, lambda e, u=u: e.transpose(out=bNb[:, u * 128:(u + 1) * 128],
                                                         in_=L_["Mt4"][:, u * 128:(u + 1) * 128], identity=self.identb),
                          reads=[QB["Mt4"], self.cb], writes=[self.pbank[bN]])
                fw.op(act, lambda e: e.activation(out=L_["Nn4"], in_=bNb[:, 0:512], func=AF.Copy), reads=[self.pbank[bN]],
                      writes=[QB["Nn4"]])
                fw.op(pool, lambda e: e.tensor_tensor(out=q3(L_["X4"]), in0=id4, in1=q3(L_["Mt4"]), op=ALU.subtract),
                      reads=[QB["Mt4"], self.cb], writes=[QB["X4"]])
                Qn, Qtn = "Mt4", "Nn4"
                pp_ = [("Qa", "Qta"), ("Qb", "Qtb")]
                for lvl in range(1, 6):
                    Qo, Qto = pp_[lvl % 2]
                    bt = nb_()
                    for u in range(4):
                        us = slice(u * 128, (u + 1) * 128)
                        fw.op(pe, lambda e, us=us, Qn=Qn, Qtn=Qtn, bt=bt: e.matmul(self.bank(bt)[:, us], lhsT=L_[Qn][:, us],
                                                                                   rhs=L_[Qtn][:, us], start=True, stop=True),
                              reads=[QB[Qn], QB[Qtn]], writes=[self.pbank[bt]])
                    fw.op(act, lambda e, Qto=Qto, bt=bt: e.activation(out=L_[Qto], in_=self.bank(bt), func=AF.Copy),
                          reads=[self.pbank[bt]], writes=[QB[Qto]])
                    if lvl < 5:
                        bq = nb_()
                        for u in range(4):
                            us = slice(u * 128, (u + 1) * 128)
                            fw.op(pe, lambda e, us=us, Qn=Qn, Qtn=Qtn, bq=bq: e.matmul(self.bank(bq)[:, us], lhsT=L_[Qtn][:, us],
                                                                                       rhs=L_[Qn][:, us], start=True, stop=True),
                                  reads=[QB[Qn], QB[Qtn]], writes=[self.pbank[bq]])
                        fw.op(dve, lambda e, Qo=Qo, bq=bq: e.tensor_copy(out=L_[Qo], in_=self.bank(bq)),
                              reads=[self.pbank[bq]], writes=[QB[Qo]])
                    bx = nb_()
                    for u in range(4):
                        us = slice(u * 128, (u + 1) * 128)
                        fw.op(pe, lambda e, us=us, Qto=Qto, bx=bx: e.matmul(self.bank(bx)[:, us], lhsT=L_[Qto][:, us],
                                                                            rhs=L_["X4"][:, us], start=True, stop=True),
                              reads=[QB[Qto], QB["X4"]], writes=[self.pbank[bx]])
                    fw.op(dve, lambda e, bx=bx: e.tensor_tensor(out=L_["X4"], in0=L_["X4"], in1=self.bank(bx), op=ALU.add),
                          reads=[self.pbank[bx], QB["X4"]], writes=[QB["X4"]])
                    Qn, Qtn = Qo, Qto
                fw.op(pool, lambda e: e.tensor_tensor(out=q3(L_["KG4"]), in0=ktv[:, P0:P0 + 4, :], in1=bc4(EGCv), op=ALU.mult),
                      reads=[tokb, gb], writes=[QB["KG4"]])
                fw.op(pool, lambda e: e.tensor_tensor(out=q3(L_["KD4"]), in0=ktv[:, P0:P0 + 4, :], in1=bc4(EKDv), op=ALU.mult),
                      reads=[tokb, gb], writes=[QB["KD4"]])
                bW, bU = nb_(), nb_()
                for u in range(4):
                    us = slice(u * 128, (u + 1) * 128)
                    fw.op(pe, lambda e, us=us: e.matmul(self.bank(bW)[:, us], lhsT=L_["KG4"][:, us], rhs=L_["X4"][:, us],
                                                        start=True, stop=True), reads=[QB["KG4"], QB["X4"]], writes=[self.pbank[bW]])
                for u in range(4):
                    us = slice(u * 128, (u + 1) * 128)
                    fw.op(pe, lambda e, us=us, u=u: e.matmul(self.bank(bU)[:, us], lhsT=L_["X4"][:, us], rhs=vtv[:, P0 + u, :],
                                                             start=True, stop=True), reads=[QB["X4"], tokb], writes=[self.pbank[bU]])
                fw.op(act, lambda e: e.activation(out=L_["WT4"], in_=self.bank(bW), func=AF.Copy), reads=[self.pbank[bW]],
                      writes=[QB["WT4"]])
                fw.op(dve, lambda e: e.tensor_tensor(out=q3(L_["BU4"]), in0=self.bank(bU).rearrange("p (u i) -> p u i", u=4),
                                                     in1=bc4(BTv), op=ALU.mult), reads=[self.pbank[bU], gb], writes=[QB["BU4"]])
                fw.op(dve, lambda e: e.tensor_tensor(out=L_["QD4"], in0=qT[:, cols4], in1=L_["EGR4"], op=ALU.mult),
                      reads=[qkvb, QB["EGR4"]], writes=[QB["QD4"]])
                for u in range(4):
                    us = slice(u * 128, (u + 1) * 128)
                    P_ = P0 + u
                    for c in range(2):
                        rows = slice(c * 64, (c + 1) * 64)
                        bw = nb_()
                        fw.op(pe, lambda e, us=us, bw=bw: e.matmul(self.bank(bw)[:, 0:128], lhsT=L_["WT4"][:, us], rhs=Sbf,
                                                                   start=True, stop=True), reads=[QB["WT4"], Sbfb],
                              writes=[self.pbank[bw]])
                        fw.op(dve, lambda e, rows=rows, bw=bw, P_=P_, us=us: e.scalar_tensor_tensor(
                            out=VN[rows, :], in0=self.bank(bw)[rows, 0:128], scalar=NBv[rows, P_, h:h + 1],
                            in1=L_["BU4"][rows, us], op0=ALU.mult, op1=ALU.add),
                            reads=[self.pbank[bw], gb, QB["BU4"]], writes=[vnb])
                        bo = nb_()
                        fw.op(pe, lambda e, us=us, bo=bo: e.matmul(self.bank(bo)[:, 0:128], lhsT=L_["QD4"][:, us], rhs=Sbf,
                                                                   start=True, stop=False), reads=[QB["QD4"], Sbfb],
                              writes=[self.pbank[bo]])
                        fw.op(pe, lambda e, us=us, bo=bo, rows=rows: e.matmul(self.bank(bo)[:, 0:128], lhsT=L_["AT4"][rows, us],
                                                                              rhs=VN[rows, :], start=False, stop=True),
                              reads=[QB["AT4"], vnb], writes=[self.pbank[bo]])
                        fw.op(act, lambda e, rows=rows, bo=bo, P_=P_: e.activation(out=otv[rows, P_, :], in_=self.bank(bo)[rows, 0:128],
                                                                                   func=AF.Copy), reads=[self.pbank[bo]], writes=[otb])
                        bs = nb_()
                        fw.op(pe, lambda e, us=us, bs=bs, rows=rows: e.matmul(self.bank(bs)[:, 0:128], lhsT=L_["KD4"][rows, us],
                                                                              rhs=VN[rows, :], start=True, stop=True),
                              reads=[QB["KD4"], vnb], writes=[self.pbank[bs]])
                        cdc = u * 128 + c * 64 + 63
                        fw.op(dve, lambda e, bs=bs, cdc=cdc: e.scalar_tensor_tensor(
                            out=S, in0=S, scalar=L_["EGR4"][:, cdc:cdc + 1], in1=self.bank(bs)[:, 0:128], op0=ALU.mult, op1=ALU.add),
                            reads=[self.pbank[bs], QB["EGR4"], Sb], writes=[Sb])
                        fw.op(act, lambda e: e.activation(out=Sbf, in_=S, func=AF.Copy), reads=[Sb], writes=[Sbfb])
            fw.barrier()
            zs = ktok_f
            zsv = zs.rearrange("p (a b) -> p a b", a=16)
            zb = Buf("zs")
            s = nbk[0] % 2
            nbk[0] += 1
            wzv = wt[s].rearrange("p (a b) -> p a b", a=16)
            fw.dma(out=wzv, in_=w_in[:, 6144 + h * 128: 6144 + (h + 1) * 128].rearrange("(kc p) f -> p kc f", p=128),
                   writes=[wtb[s]], q=pool)
            for g in range(4):
                bk = g
                for j in range(4):
                    i = g * 4 + j
                    for kc in range(16):
                        fw.op(pe, lambda e, i=i, j=j, kc=kc, bk=bk: e.matmul(
                            self.bank(bk)[:, j * 128:(j + 1) * 128], lhsT=bigv[:, kc, i * 128:(i + 1) * 128], rhs=wzv[:, kc, :],
                            start=(kc == 0), stop=(kc == 15)), reads=[xTb, wtb[s]], writes=[self.pbank[bk]])
                fw.op(act, lambda e, g=g, bk=bk: e.activation(out=zsv[:, g * 4:(g + 1) * 4, :],
                                                              in_=self.bank(bk).rearrange("p (a b) -> p a b", a=4), func=AF.Silu),
                      reads=[self.pbank[bk]], writes=[zb])
            sq = regA[:, 0:T]
            sqb = Buf("sq")
            fw.op(pool, lambda e: e.tensor_tensor(out=sq, in0=otok, in1=otok, op=ALU.mult), reads=[otb], writes=[sqb])
            ms = sm[:, 0:16]
            fw.op(dve, lambda e: e.tensor_reduce(out=ms, in_=sq.rearrange("p (a b) -> p a b", a=16), axis=AX.X, op=ALU.add),
                  reads=[sqb], writes=[smb])
            fw.op(dve, lambda e: e.tensor_scalar(out=ms, in0=ms, scalar1=1.0 / 128.0, scalar2=RMS_EPS, op0=ALU.mult, op1=ALU.add),
                  reads=[smb], writes=[smb])
            fw.op(act, lambda e: e.activation(out=ms, in_=ms, func=AF.Sqrt), reads=[smb], writes=[smb])
            fw.op(dve, lambda e: e.reciprocal(out=ms, in_=ms), reads=[smb], writes=[smb])
            fw.op(dve, lambda e: e.tensor_tensor(out=otv, in0=otv, in1=ms.unsqueeze(2).to_broadcast([128, 16, 128]), op=ALU.mult),
                  reads=[smb, otb], writes=[otb])
            fw.op(pool, lambda e: e.tensor_tensor(out=otv, in0=otv, in1=NGrow.unsqueeze(1).to_broadcast([128, 16, 128]),
                                                  op=ALU.mult), reads=[gb, otb], writes=[otb])
            fw.op(dve, lambda e: e.tensor_tensor(out=otok, in0=otok, in1=zs, op=ALU.mult), reads=[zb, otb], writes=[otb])
            s2 = h % 2
            for g in range(4):
                bk = 4 + g
                for j in range(4):
                    P_ = g * 4 + j
                    fw.op(pe, lambda e, P_=P_, j=j, bk=bk: e.transpose(out=self.bank(bk)[:, j * 128:(j + 1) * 128], in_=otv[:, P_, :],
                                                                      identity=self.ident), reads=[otb, self.cb], writes=[self.pbank[bk]])
                fw.op(act, lambda e, g=g, bk=bk, s2=s2: e.activation(out=oth[s2][:, g * 512:(g + 1) * 512], in_=self.bank(bk),
                                                                    func=AF.Copy), reads=[self.pbank[bk]], writes=[othb[s2]])
            fw.dma(out=OT[h], in_=oth[s2], reads=[othb[s2]], writes=[OTb])
        self.release(m0)
        self.outproj_ln(OT, OTb, W["gdn_w_out"][li], src, src_buf, W["ln_g"][L, 0], W["ln_b"][L, 0], dst, dst_buf)

    def init_yg(self):
        fw = self.fw
        m = self.mark()
        z = self.sb(D)
        zb = Buf()
        fw.op(fw.dve, lambda e: e.memset(z, 0.0), writes=[zb])
        fw.dma(out=self.dram["YG"][NSLOT:NSLOT + 128, :], in_=z, reads=[zb], writes=[self.dbuf["YG"]])
        self.release(m)


WEIGHT_SPECS = [
    ("ln_g", (4, 2, 2048)), ("ln_b", (4, 2, 2048)), ("moe_rg_w", (4, 2048, 4)), ("moe_rg_b", (4, 4)),
    ("moe_re_w", (4, 2048, 32)), ("moe_re_b", (4, 32)), ("moe_w_gate", (4, 32, 2048, 512)),
    ("moe_w_up", (4, 32, 2048, 512)), ("moe_w_down", (4, 32, 512, 2048)), ("gdn_w_in", (2, 2048, 8224)),
    ("gdn_conv_w", (2, 4, 6144)), ("gdn_a_log", (2, 16)), ("gdn_dt_bias", (2, 16)), ("gdn_norm_g", (2, 128)),
    ("gdn_w_out", (2, 2048, 2048)), ("ssm_w_in", (1, 2048, 2048)), ("ssm_b_re", (1, 128, 64, 16)),
    ("ssm_b_im", (1, 128, 64, 16)), ("ssm_c_re", (1, 128, 16, 64)), ("ssm_c_im", (1, 128, 16, 64)),
    ("ssm_a_re", (1, 128, 64)), ("ssm_a_im", (1, 128, 64)), ("ssm_log_dt", (1, 128)), ("ssm_d", (1, 2048)),
    ("ssm_w_glu", (1, 2048, 2048)), ("ssm_b_glu", (1, 2048)), ("ssm_w_out", (1, 2048, 2048)),
    ("dsa_w_in", (1, 2048, 4496)), ("dsa_w_out", (1, 2048, 2048)),
]


def build(stages, ext_in=(), ext_out=(), weights=None):
    nc = bass.Bass("TRN2", target_bir_lowering=False)
    st = contextlib.ExitStack()
    with st:
        k = K(nc, st, set(ext_in), set(ext_out))
        fw = k.fw
        used = weights if weights is not None else [n for n, _ in WEIGHT_SPECS]
        W = {}
        for n, shp in WEIGHT_SPECS:
            if n in used:
                W[n] = nc.dram_tensor(n, list(shp), F32, kind="ExternalInput").ap()
        k.W = W
        names = set()
        for stg in stages:
            names.update(stg[2:])
        if "x" in names:
            k.dram["x"] = nc.dram_tensor("x", [T, D], F32, kind="ExternalInput").ap()
            k.dbuf["x"] = Buf("x")
        k.dram["out"] = nc.dram_tensor("out", [T, D], F32, kind="ExternalOutput").ap()
        k.dbuf["out"] = Buf("out")
        k.dt("XA", [T, D], F32)
        k.dt("XM", [T, D], F32)
        k.dt("OT", [16, 128, T], BF16)
        k.dt("XG", [NSLOT + 128, D], BF16)
        k.dt("YG", [NSLOT + 128, D], F32)
        k.dt("U", [16, 128, T], F32)
        k.dt("YA", [16, 128, T], BF16)
        k.dt("QT", [16, 128, T], BF16)
        k.dt("QI", [16, 128, T], BF16)
        k.dt("MASKT", [16, 128, T], BF16)
        k.init_yg()
        for stg in stages:
            kind, L, src, dst = stg
            S, Sb, Dd, Db = k.dram[src], k.dbuf[src], k.dram[dst], k.dbuf[dst]
            if kind == "moe":
                k.moe(L, S, Sb, W, Dd, Db)
            elif kind == "gdn":
                k.gdn(L // 3, L, S, Sb, W, Dd, Db)
            elif kind == "s5":
                k.s5(L, S, Sb, W, Dd, Db)
            elif kind == "dsa":
                k.dsa(L, S, Sb, W, Dd, Db)
            else:
                raise ValueError(kind)
        fw.finish()
        k.stats = {e.name: e.n_instr for e in fw.engs}
    return nc, k


_MIXERS = ("gdn", "s5", "dsa")


def _stages():
    st = []
    for L in range(DEPTH):
        src = "x" if L == 0 else "XA"
        st.append((_MIXERS[L % 3], L, src, "XM"))
        st.append(("moe", L, "XM", "out" if L == DEPTH - 1 else "XA"))
    return st


def kernel(**inputs):
    x = np.ascontiguousarray(np.asarray(inputs["x"], dtype=np.float32))
    nc, _k = build(_stages())
    wts = {n: np.ascontiguousarray(np.asarray(inputs[n], dtype=np.float32)) for n, _ in WEIGHT_SPECS}
    n_cores = 8
    in_maps = []
    for c in range(n_cores):
        m = dict(wts)
        m["x"] = x[c]
        in_maps.append(m)
    res = run_bass_kernel_spmd(nc, in_maps, core_ids=list(range(n_cores)))
    return np.stack([np.asarray(r["out"], dtype=np.float32) for r in res.results], axis=0)
```

```python
import contextlib
import math
import numpy as np
import concourse.bass as bass
import concourse.mybir as mybir
from concourse.bass_utils import run_bass_kernel_spmd

F32 = mybir.dt.float32
BF16 = mybir.dt.bfloat16
I32 = mybir.dt.int32
AF = mybir.ActivationFunctionType
ALU = mybir.AluOpType
AX = mybir.AxisListType

T = 2048
D = 2048
NT = 16
DEPTH = 4
DN_ALPHA = (2.0 * DEPTH) ** 0.25
LN_EPS = 1e-5
RMS_EPS = 1e-6
NEXP = 32
FF = 512
CAP = 256
NSLOT = NEXP * CAP
GDN_IN = 8224
DSA_IN = 4496
NEG = -30000.0


class Buf:
    __slots__ = ("name", "writer", "readers")

    def __init__(self, name=""):
        self.name = name
        self.writer = None
        self.readers = []


class Eng:
    def __init__(self, fw, name, hw, is_pe=False):
        self.fw = fw
        self.name = name
        self.hw = hw
        self.is_pe = is_pe
        self.sems = []
        self.cnt = 0
        self.waited = {}
        self.n_instr = 0

    def cur_sem(self):
        if not self.sems or self.cnt >= self.fw.EPOCH:
            self.sems.append(self.fw.new_sem(f"{self.name}_p{len(self.sems)}"))
            self.cnt = 0
        return self.sems[-1]


class FW:
    EPOCH = 60000
    NP = 24

    def __init__(self, nc, stack):
        self.nc = nc
        self.stack = stack
        self.sem_handles = {}
        self.nsem = 0
        self.pe = Eng(self, "pe", nc.tensor, is_pe=True)
        self.dve = Eng(self, "dve", nc.vector)
        self.act = Eng(self, "act", nc.scalar)
        self.pool = Eng(self, "pool", nc.gpsimd)
        self.sp = Eng(self, "sp", nc.sync)
        self.engs = [self.pe, self.dve, self.act, self.pool, self.sp]
        self.dma_pool = {}
        self.dma_rr = {}
        self.all_dma_tokens = []

    def new_sem(self, name):
        h = self.stack.enter_context(self.nc.semaphore(name))
        self.nsem += 1
        self.sem_handles[self.nsem] = h
        return self.nsem

    def _wait(self, eng, tok):
        if tok is None:
            return
        key, val, src = tok
        if eng.is_pe and src == "pe":
            return
        if eng.waited.get(key, 0) >= val:
            return
        eng.waited[key] = val
        eng.hw.wait_ge(self.sem_handles[key], val)

    def _note_read(self, b, tok):
        b.readers.append(tok)
        if len(b.readers) > 16:
            last = {}
            for r in b.readers:
                k2 = (r[2], r[0])
                if k2 not in last or last[k2][1] < r[1]:
                    last[k2] = r
            b.readers = list(last.values())

    def op(self, eng, fn, reads=(), writes=()):
        for b in reads:
            self._wait(eng, b.writer)
        for b in writes:
            self._wait(eng, b.writer)
            for r in b.readers:
                if r[2] == eng.name:
                    continue
                self._wait(eng, r)
        ins = fn(eng.hw)
        key = eng.cur_sem()
        eng.cnt += 1
        ins.then_inc(self.sem_handles[key], 1)
        tok = (key, eng.cnt, eng.name)
        for b in reads:
            self._note_read(b, tok)
        for b in writes:
            b.writer = tok
            b.readers = []
        eng.n_instr += 1
        return tok

    def dma(self, out, in_, reads=(), writes=(), q=None, indirect=None, **kw):
        eng = q or self.sp
        for b in reads:
            self._wait(eng, b.writer)
        for b in writes:
            self._wait(eng, b.writer)
            for r in b.readers:
                self._wait(eng, r)
        pool = self.dma_pool.setdefault(eng.name, [])
        if len(pool) < self.NP:
            pool.append([self.new_sem(f"dma_{eng.name}_{len(pool)}"), 0])
            slot = pool[-1]
        else:
            i = self.dma_rr.get(eng.name, 0)
            slot = pool[i % self.NP]
            self.dma_rr[eng.name] = i + 1
        key, uses = slot
        if uses > 0:
            self._wait(eng, (key, 16 * uses, "dma"))
        if indirect is None:
            ins = eng.hw.dma_start(out=out, in_=in_, **kw)
        else:
            ins = eng.hw.indirect_dma_start(out=out, in_=in_, **indirect)
        slot[1] = uses + 1
        ins.then_inc(self.sem_handles[key], 16)
        tok = (key, 16 * (uses + 1), "dma")
        for b in reads:
            self._note_read(b, tok)
        for b in writes:
            b.writer = tok
            b.readers = []
        self.all_dma_tokens.append(tok)
        if len(self.all_dma_tokens) > 400:
            self._compact_dma()
        eng.n_instr += 1
        return tok

    def _compact_dma(self):
        last = {}
        for t in self.all_dma_tokens:
            if t[0] not in last or last[t[0]][1] < t[1]:
                last[t[0]] = t
        self.all_dma_tokens = list(last.values())

    def barrier(self, engs=None):
        toks = []
        for e in self.engs:
            if e.sems and e.cnt > 0:
                toks.append((e.sems[-1], e.cnt, e.name))
        self._compact_dma()
        toks += self.all_dma_tokens
        for e in (engs or self.engs):
            for key, val, src in toks:
                if src == e.name:
                    continue
                if e.waited.get(key, 0) >= val:
                    continue
                e.waited[key] = val
                e.hw.wait_ge(self.sem_handles[key], val)

    def finish(self):
        self.barrier(engs=[self.sp])


class K:
    ARENA = 46000

    def __init__(self, nc, st, ext_in, ext_out):
        self.nc = nc
        self.st = st
        self.fw = FW(nc, st)
        self.ext_in = ext_in
        self.ext_out = ext_out
        self.arena = st.enter_context(nc.sbuf_tensor("arena", [128, self.ARENA], F32))
        self.psum = st.enter_context(nc.psum_tensor("psum", [128, 4096], F32))
        self.off = 0
        self.pbank = [Buf(f"bank{i}") for i in range(8)]
        self.dram = {}
        self.dbuf = {}
        self._consts()

    def sb(self, n, dt=F32):
        words = n if dt != BF16 else (n + 1) // 2
        words = (words + 7) // 8 * 8
        assert self.off + words <= self.ARENA, (self.off, words)
        ap = self.arena[:, self.off:self.off + words]
        self.off += words
        if dt == BF16:
            ap = ap.bitcast(BF16)[:, 0:n]
        elif dt == I32:
            ap = ap.bitcast(I32)[:, 0:n]
        else:
            ap = ap[:, 0:n]
        return ap

    def mark(self):
        return self.off

    def release(self, m):
        self.fw.barrier()
        self.off = m

    def bank(self, i, dt=F32):
        ap = self.psum[:, i * 512:(i + 1) * 512]
        if dt == BF16:
            ap = ap.bitcast(BF16)
        return ap

    def dt(self, name, shape, dtype):
        kind = "Internal"
        if name in self.ext_in:
            kind = "ExternalInput"
        elif name in self.ext_out:
            kind = "ExternalOutput"
        t = self.nc.dram_tensor(name, list(shape), dtype, kind=kind).ap()
        self.dram[name] = t
        self.dbuf[name] = Buf(name)
        return t

    def _consts(self):
        fw = self.fw
        P = fw.pool
        self.cb = Buf("consts")
        cb = self.cb
        self.ident = self.sb(128)
        self.ones = self.sb(128)
        self.identb = self.sb(128, BF16)
        self.onesb = self.sb(128, BF16)
        self.sltb = self.sb(128, BF16)
        slt = self.sb(128)
        fw.op(P, lambda e: e.memset(self.ident, 0.0), writes=[cb])
        fw.op(P, lambda e: e.affine_select(out=self.ident, in_=self.ident, pattern=[[-1, 128]],
                                           compare_op=ALU.not_equal, fill=1.0, base=0, channel_multiplier=1),
              reads=[cb], writes=[cb])
        fw.op(P, lambda e: e.memset(self.ones, 1.0), writes=[cb])
        fw.op(P, lambda e: e.memset(slt, 1.0), writes=[cb])
        fw.op(P, lambda e: e.affine_select(out=slt, in_=slt, pattern=[[1, 128]], compare_op=ALU.is_gt,
                                           fill=0.0, base=0, channel_multiplier=-1), reads=[cb], writes=[cb])
        fw.op(P, lambda e: e.tensor_copy(out=self.identb, in_=self.ident), reads=[cb], writes=[cb])
        fw.op(P, lambda e: e.tensor_copy(out=self.onesb, in_=self.ones), reads=[cb], writes=[cb])
        fw.op(P, lambda e: e.tensor_copy(out=self.sltb, in_=slt), reads=[cb], writes=[cb])
        self.ebase = self.sb(NEXP)
        fw.op(P, lambda e: e.iota(out=self.ebase, pattern=[[CAP, NEXP]], base=0, channel_multiplier=0,
                                  allow_small_or_imprecise_dtypes=True), writes=[cb])
        self.trash = self.sb(1)
        fw.op(P, lambda e: e.iota(out=self.trash, pattern=[[0, 1]], base=NSLOT, channel_multiplier=1,
                                  allow_small_or_imprecise_dtypes=True), writes=[cb])
        self.const_mark = self.off

    def load_row(self, dst, src_row, buf, n):
        src = src_row.rearrange("(o n) -> o n", o=1).to_broadcast([128, n]) if len(src_row.shape) == 1 \
            else src_row.to_broadcast([128, n])
        self.fw.dma(out=dst, in_=src, writes=[buf])

    def build_xT(self, src, src_buf, xT, xT_buf):
        fw = self.fw
        m = self.mark()
        xin = [self.sb(D) for _ in range(2)]
        xb = [Buf("xin0"), Buf("xin1")]
        for i in range(NT):
            s = i % 2
            fw.dma(out=xin[s], in_=src[i * 128:(i + 1) * 128, :], reads=[src_buf], writes=[xb[s]])
            for g in range(4):
                bk = (i * 4 + g) % 8
                pb = self.pbank[bk]
                for j in range(4):
                    fc = g * 4 + j
                    fw.op(fw.pe, lambda e, fc=fc, j=j, bk=bk, s=s: e.transpose(
                        out=self.bank(bk)[:, j * 128:(j + 1) * 128], in_=xin[s][:, fc * 128:(fc + 1) * 128],
                        identity=self.ident), reads=[xb[s], self.cb], writes=[pb])
                dst = xT[:, g * 4:(g + 1) * 4, i * 128:(i + 1) * 128]
                srcp = self.bank(bk).rearrange("p (a b) -> p a b", a=4)
                if g % 2 == 0:
                    fw.op(fw.dve, lambda e, dst=dst, srcp=srcp: e.tensor_copy(out=dst, in_=srcp),
                          reads=[pb], writes=[xT_buf])
                else:
                    fw.op(fw.act, lambda e, dst=dst, srcp=srcp: e.activation(out=dst, in_=srcp, func=AF.Copy),
                          reads=[pb], writes=[xT_buf])
        self.release(m)

    def ln_tail(self, z, zb, grow, brow, gbuf, tmp, tmpb, stat, statb, out, outb):
        fw = self.fw
        mean = stat[:, 0:1]
        ssq = stat[:, 1:2]
        rstd = stat[:, 2:3]
        nmean = stat[:, 3:4]
        fw.op(fw.dve, lambda e: e.tensor_reduce(out=mean, in_=z, axis=AX.X, op=ALU.add), reads=[zb], writes=[statb])
        fw.op(fw.dve, lambda e: e.tensor_scalar(out=nmean, in0=mean, scalar1=-1.0 / D, scalar2=None, op0=ALU.mult),
              reads=[statb], writes=[statb])
        fw.op(fw.act, lambda e: e.activation(out=tmp, in_=z, func=AF.Square, bias=nmean, scale=1.0, accum_out=ssq),
              reads=[zb, statb], writes=[tmpb, statb])
        fw.op(fw.dve, lambda e: e.tensor_scalar(out=rstd, in0=ssq, scalar1=1.0 / D, scalar2=LN_EPS, op0=ALU.mult,
                                                op1=ALU.add), reads=[statb], writes=[statb])
        fw.op(fw.act, lambda e: e.activation(out=rstd, in_=rstd, func=AF.Sqrt), reads=[statb], writes=[statb])
        fw.op(fw.dve, lambda e: e.reciprocal(out=rstd, in_=rstd), reads=[statb], writes=[statb])
        fw.op(fw.dve, lambda e: e.tensor_scalar(out=tmp, in0=z, scalar1=nmean, scalar2=rstd, op0=ALU.add,
                                                op1=ALU.mult), reads=[zb, statb, tmpb], writes=[tmpb])
        fw.op(fw.pool, lambda e: e.tensor_tensor(out=tmp, in0=tmp, in1=grow, op=ALU.mult), reads=[tmpb, gbuf],
              writes=[tmpb])
        fw.op(fw.dve, lambda e: e.tensor_tensor(out=out, in0=tmp, in1=brow, op=ALU.add), reads=[tmpb, gbuf],
              writes=[outb])

    def outproj_ln(self, OT, OTb, w_out, resid, resid_buf, ln_g, ln_b, dst, dst_buf):
        fw = self.fw
        m = self.mark()
        W = self.sb(16 * D, BF16)
        Wv = W.rearrange("p (a b) -> p a b", a=16)
        Wb = Buf("wout")
        wsrc = w_out.rearrange("(fc p) m -> p fc m", p=128)
        for q in range(4):
            fw.dma(out=Wv[:, q * 4:(q + 1) * 4, :], in_=wsrc[:, q * 4:(q + 1) * 4, :], writes=[Wb], q=fw.pool)
        grow = self.sb(D)
        brow = self.sb(D)
        gbuf = Buf("lnrows")
        self.load_row(grow, ln_g, gbuf, D)
        self.load_row(brow, ln_b, gbuf, D)
        oT = [self.sb(16 * 128, BF16) for _ in range(2)]
        oTb = [Buf(), Buf()]
        rz = [self.sb(D) for _ in range(2)]
        rzb = [Buf(), Buf()]
        tmp = self.sb(D)
        tmpb = Buf()
        stat = [self.sb(4) for _ in range(2)]
        statb = [Buf(), Buf()]
        OTv = OT.rearrange("fc p t -> p fc t")
        for i in range(NT):
            s = i % 2
            fw.dma(out=oT[s].rearrange("p (a b) -> p a b", a=16), in_=OTv[:, :, i * 128:(i + 1) * 128],
                   reads=[OTb], writes=[oTb[s]])
            fw.dma(out=rz[s], in_=resid[i * 128:(i + 1) * 128, :], reads=[resid_buf], writes=[rzb[s]])
            for mc in range(4):
                bk = (i * 4 + mc) % 8
                pb = self.pbank[bk]
                for fc in range(16):
                    fw.op(fw.pe, lambda e, fc=fc, mc=mc, bk=bk, s=s: e.matmul(
                        self.bank(bk), lhsT=oT[s][:, fc * 128:(fc + 1) * 128], rhs=Wv[:, fc, mc * 512:(mc + 1) * 512],
                        start=(fc == 0), stop=(fc == 15)), reads=[oTb[s], Wb], writes=[pb])
                zs = rz[s][:, mc * 512:(mc + 1) * 512]
                fw.op(fw.dve, lambda e, zs=zs, bk=bk: e.scalar_tensor_tensor(
                    out=zs, in0=zs, scalar=DN_ALPHA, in1=self.bank(bk), op0=ALU.mult, op1=ALU.add),
                    reads=[pb, rzb[s]], writes=[rzb[s]])
            self.ln_tail(rz[s], rzb[s], grow, brow, gbuf, tmp, tmpb, stat[s], statb[s], rz[s], rzb[s])
            fw.dma(out=dst[i * 128:(i + 1) * 128, :], in_=rz[s], reads=[rzb[s]], writes=[dst_buf])
        self.release(m)

    def moe(self, L, xm, xm_buf, W, dst, dst_buf):
        fw = self.fw
        XG, YG = self.dram["XG"], self.dram["YG"]
        XGb, YGb = self.dbuf["XG"], self.dbuf["YG"]
        m0 = self.mark()
        dest = self.sb(NT * 2, I32)
        gate = self.sb(NT * 2)
        routeb = Buf("route")
        acum = self.sb(NEXP)
        acumb = self.sb(NEXP, BF16)
        acb = Buf("acum")
        fw.op(fw.dve, lambda e: e.memset(acum, 0.0), writes=[acb])
        fw.op(fw.dve, lambda e: e.memset(acumb, 0.0), writes=[acb])
        m1 = self.mark()
        wr = self.sb(16 * 36)
        wrv = wr.rearrange("p (a b) -> p a b", a=16)
        wrb = Buf("wr")
        fw.dma(out=wrv[:, :, 0:4], in_=W["moe_rg_w"][L].rearrange("(fc p) g -> p fc g", p=128), writes=[wrb])
        fw.dma(out=wrv[:, :, 4:36], in_=W["moe_re_w"][L].rearrange("(fc p) g -> p fc g", p=128), writes=[wrb])
        brow = self.sb(36)
        self.load_row(brow[:, 0:4], W["moe_rg_b"][L], wrb, 4)
        self.load_row(brow[:, 4:36], W["moe_re_b"][L], wrb, 32)
        xin = [self.sb(D) for _ in range(2)]
        xinb = [Buf(), Buf()]
        xTf = [self.sb(16 * 128) for _ in range(2)]
        xTfb = [Buf(), Buf()]
        xbf = [self.sb(D, BF16) for _ in range(2)]
        xbfb = [Buf(), Buf()]
        sm = [self.sb(256) for _ in range(2)]
        smb = [Buf(), Buf()]
        for i in range(NT):
            s = i % 2
            fw.dma(out=xin[s], in_=xm[i * 128:(i + 1) * 128, :], reads=[xm_buf], writes=[xinb[s]])
            for g in range(4):
                bk = g
                pb = self.pbank[bk]
                for j in range(4):
                    fc = g * 4 + j
                    fw.op(fw.pe, lambda e, fc=fc, j=j, bk=bk, s=s: e.transpose(
                        out=self.bank(bk)[:, j * 128:(j + 1) * 128], in_=xin[s][:, fc * 128:(fc + 1) * 128],
                        identity=self.ident), reads=[xinb[s], self.cb], writes=[pb])
                dstp = xTf[s][:, g * 512:(g + 1) * 512]
                if g % 2 == 0:
                    fw.op(fw.dve, lambda e, dstp=dstp, bk=bk: e.tensor_copy(out=dstp, in_=self.bank(bk)),
                          reads=[pb], writes=[xTfb[s]])
                else:
                    fw.op(fw.act, lambda e, dstp=dstp, bk=bk: e.activation(out=dstp, in_=self.bank(bk), func=AF.Copy),
                          reads=[pb], writes=[xTfb[s]])
            fw.op(fw.pool, lambda e, s=s: e.tensor_copy(out=xbf[s], in_=xin[s]), reads=[xinb[s]], writes=[xbfb[s]])
            pl = self.pbank[4]
            lg_ps = self.bank(4)[:, 0:36]
            for fc in range(16):
                fw.op(fw.pe, lambda e, fc=fc, s=s: e.matmul(lg_ps, lhsT=xTf[s][:, fc * 128:(fc + 1) * 128],
                                                           rhs=wrv[:, fc, :], start=(fc == 0), stop=(fc == 15)),
                      reads=[xTfb[s], wrb], writes=[pl])
            S = sm[s]
            Sb = smb[s]
            lg = S[:, 0:36]
            fw.op(fw.dve, lambda e, lg=lg: e.tensor_tensor(out=lg, in0=lg_ps, in1=brow, op=ALU.add),
                  reads=[pl, wrb], writes=[Sb])
            gmax = S[:, 36:37]
            ngmax = S[:, 37:38]
            gsum = S[:, 38:39]
            pg = S[:, 39:40]
            ohg = S[:, 40:44]
            ex4 = S[:, 44:48]
            fw.op(fw.dve, lambda e: e.tensor_reduce(out=gmax, in_=lg[:, 0:4], axis=AX.X, op=ALU.max),
                  reads=[Sb], writes=[Sb])
            fw.op(fw.dve, lambda e: e.tensor_scalar(out=ngmax, in0=gmax, scalar1=-1.0, scalar2=None, op0=ALU.mult),
                  reads=[Sb], writes=[Sb])
            fw.op(fw.act, lambda e: e.activation(out=ex4, in_=lg[:, 0:4], func=AF.Exp, bias=ngmax, scale=1.0,
                                                 accum_out=gsum), reads=[Sb], writes=[Sb])
            fw.op(fw.dve, lambda e: e.reciprocal(out=pg, in_=gsum), reads=[Sb], writes=[Sb])
            fw.op(fw.dve, lambda e: e.tensor_scalar(out=ohg, in0=lg[:, 0:4], scalar1=gmax, scalar2=None,
                                                    op0=ALU.is_ge), reads=[Sb], writes=[Sb])
            esel = S[:, 48:56]
            le = lg[:, 4:36]
            fw.op(fw.dve, lambda e: e.tensor_scalar(out=esel, in0=le[:, 0:8], scalar1=ohg[:, 0:1], scalar2=None,
                                                    op0=ALU.mult), reads=[Sb], writes=[Sb])
            for g in range(1, 4):
                fw.op(fw.dve, lambda e, g=g: e.scalar_tensor_tensor(
                    out=esel, in0=le[:, g * 8:(g + 1) * 8], scalar=ohg[:, g:g + 1], in1=esel, op0=ALU.mult,
                    op1=ALU.add), reads=[Sb], writes=[Sb])
            top8 = S[:, 56:64]
            fw.op(fw.dve, lambda e: e.max(out=top8, in_=esel), reads=[Sb], writes=[Sb])
            oh1 = S[:, 64:72]
            oh2 = S[:, 72:80]
            fw.op(fw.dve, lambda e: e.tensor_scalar(out=oh1, in0=esel, scalar1=top8[:, 0:1], scalar2=None,
                                                    op0=ALU.is_equal), reads=[Sb], writes=[Sb])
            fw.op(fw.dve, lambda e: e.tensor_scalar(out=oh2, in0=esel, scalar1=top8[:, 1:2], scalar2=None,
                                                    op0=ALU.is_equal), reads=[Sb], writes=[Sb])
            dv = S[:, 80:82]
            fw.op(fw.dve, lambda e: e.tensor_tensor(out=dv[:, 0:1], in0=top8[:, 0:1], in1=top8[:, 1:2],
                                                    op=ALU.subtract), reads=[Sb], writes=[Sb])
            fw.op(fw.dve, lambda e: e.tensor_tensor(out=dv[:, 1:2], in0=top8[:, 1:2], in1=top8[:, 0:1],
                                                    op=ALU.subtract), reads=[Sb], writes=[Sb])
            p12 = S[:, 82:84]
            fw.op(fw.act, lambda e: e.activation(out=p12, in_=dv, func=AF.Sigmoid), reads=[Sb], writes=[Sb])
            fw.op(fw.dve, lambda e: e.tensor_scalar(out=p12, in0=p12, scalar1=pg, scalar2=None, op0=ALU.mult),
                  reads=[Sb], writes=[Sb])
            A1 = S[:, 96:128]
            A2 = S[:, 128:160]
            for g in range(4):
                fw.op(fw.dve, lambda e, g=g: e.tensor_scalar(out=A1[:, g * 8:(g + 1) * 8], in0=oh1,
                                                              scalar1=ohg[:, g:g + 1], scalar2=None, op0=ALU.mult),
                      reads=[Sb], writes=[Sb])
                fw.op(fw.dve, lambda e, g=g: e.tensor_scalar(out=A2[:, g * 8:(g + 1) * 8], in0=oh2,
                                                              scalar1=ohg[:, g:g + 1], scalar2=None, op0=ALU.mult),
                      reads=[Sb], writes=[Sb])
            A12 = S[:, 160:192]
            A12b = S[:, 192:208].bitcast(BF16)
            fw.op(fw.dve, lambda e: e.tensor_tensor(out=A12, in0=A1, in1=A2, op=ALU.add), reads=[Sb], writes=[Sb])
            fw.op(fw.dve, lambda e: e.tensor_copy(out=A12b, in_=A12), reads=[Sb], writes=[Sb])
            pp = self.pbank[5]
            pos_ps = self.bank(5)[:, 0:32]
            fw.op(fw.pe, lambda e: e.matmul(pos_ps, lhsT=self.onesb, rhs=acumb, start=True, stop=False),
                  reads=[acb, self.cb], writes=[pp])
            fw.op(fw.pe, lambda e: e.matmul(pos_ps, lhsT=self.sltb, rhs=A12b, start=False, stop=True),
                  reads=[Sb, self.cb], writes=[pp])
            slot = S[:, 208:240]
            fw.op(fw.dve, lambda e: e.tensor_tensor(out=slot, in0=pos_ps, in1=self.ebase, op=ALU.add),
                  reads=[pp, self.cb], writes=[Sb])
            fw.op(fw.pool, lambda e: e.tensor_tensor(out=acum, in0=acum, in1=A12, op=ALU.add), reads=[Sb, acb],
                  writes=[acb])
            fw.op(fw.pool, lambda e: e.tensor_copy(out=acumb, in_=acum), reads=[acb], writes=[acb])
            tmp32 = S[:, 240:256]
            dr = S[:, 84:86]
            pr = S[:, 86:88]
            for r, A in ((0, A1), (1, A2)):
                tt = S[:, 224:256]
            scr = xTf[s][:, 0:32]
            for r, A in ((0, A1), (1, A2)):
                fw.op(fw.dve, lambda e, r=r, A=A: e.scalar_tensor_tensor(
                    out=scr, in0=A, scalar=1.0, in1=slot, op0=ALU.mult, op1=ALU.mult, accum_out=dr[:, r:r + 1]),
                    reads=[Sb, xTfb[s], pl], writes=[Sb, xTfb[s]])
                fw.op(fw.dve, lambda e, r=r, A=A: e.scalar_tensor_tensor(
                    out=scr, in0=A, scalar=1.0, in1=pos_ps, op0=ALU.mult, op1=ALU.mult, accum_out=pr[:, r:r + 1]),
                    reads=[Sb, xTfb[s], pp], writes=[Sb, xTfb[s]])
            valid = S[:, 88:90]
            fw.op(fw.dve, lambda e: e.tensor_scalar(out=valid, in0=pr, scalar1=float(CAP) - 0.5, scalar2=None,
                                                    op0=ALU.is_lt), reads=[Sb], writes=[Sb])
            fw.op(fw.dve, lambda e: e.tensor_tensor(out=p12, in0=p12, in1=valid, op=ALU.mult), reads=[Sb],
                  writes=[Sb])
            fw.op(fw.dve, lambda e: e.tensor_scalar(out=dr, in0=dr, scalar1=self.trash, scalar2=None,
                                                    op0=ALU.subtract), reads=[Sb, self.cb], writes=[Sb])
            fw.op(fw.dve, lambda e: e.tensor_tensor(out=dr, in0=dr, in1=valid, op=ALU.mult), reads=[Sb], writes=[Sb])
            fw.op(fw.dve, lambda e: e.tensor_scalar(out=dr, in0=dr, scalar1=self.trash, scalar2=None, op0=ALU.add),
                  reads=[Sb, self.cb], writes=[Sb])
            fw.op(fw.dve, lambda e, i=i: e.tensor_copy(out=dest[:, 2 * i:2 * i + 2], in_=dr), reads=[Sb],
                  writes=[routeb])
            fw.op(fw.dve, lambda e, i=i: e.tensor_copy(out=gate[:, 2 * i:2 * i + 2], in_=p12), reads=[Sb],
                  writes=[routeb])
            for r in range(2):
                fw.dma(out=XG, in_=xbf[s], reads=[xbfb[s], routeb], writes=[XGb], q=fw.pool,
                       indirect=dict(out_offset=bass.IndirectOffsetOnAxis(ap=dest[:, 2 * i + r:2 * i + r + 1], axis=0),
                                     in_offset=None))
        self.release(m1)
        m2 = self.mark()
        NB = 2
        wg = [self.sb(16 * FF, BF16) for _ in range(NB)]
        wu = [self.sb(16 * FF, BF16) for _ in range(NB)]
        wd = [self.sb(4 * D, BF16) for _ in range(NB)]
        wbuf = [Buf() for _ in range(NB)]
        xg = [self.sb(D, BF16) for _ in range(2)]
        xgb = [Buf(), Buf()]
        xgT = self.sb(16 * CAP, BF16)
        xgTv = xgT.rearrange("p (a b) -> p a b", a=16)
        xgTb = Buf()
        hT = self.sb(4 * CAP, BF16)
        hTv = hT.rearrange("p (a b) -> p a b", a=4)
        hTb = Buf()
        sg = self.sb(CAP)
        sgb = Buf()
        yt = [self.sb(D) for _ in range(2)]
        ytb = [Buf(), Buf()]
        pbk = 0
        for ex in range(NEXP):
            s = ex % NB
            fw.dma(out=wg[s].rearrange("p (a b) -> p a b", a=16),
                   in_=W["moe_w_gate"][L, ex].rearrange("(kc p) f -> p kc f", p=128), writes=[wbuf[s]], q=fw.pool)
            fw.dma(out=wu[s].rearrange("p (a b) -> p a b", a=16),
                   in_=W["moe_w_up"][L, ex].rearrange("(kc p) f -> p kc f", p=128), writes=[wbuf[s]], q=fw.pool)
            fw.dma(out=wd[s].rearrange("p (a b) -> p a b", a=4),
                   in_=W["moe_w_down"][L, ex].rearrange("(fc p) m -> p fc m", p=128), writes=[wbuf[s]], q=fw.pool)
            wgv = wg[s].rearrange("p (a b) -> p a b", a=16)
            wuv = wu[s].rearrange("p (a b) -> p a b", a=16)
            wdv = wd[s].rearrange("p (a b) -> p a b", a=4)
            for stl in range(CAP // 128):
                xs = stl % 2
                fw.dma(out=xg[xs], in_=XG[ex * CAP + stl * 128: ex * CAP + (stl + 1) * 128, :], reads=[XGb],
                       writes=[xgb[xs]])
                for g in range(2):
                    bk = pbk % 8
                    pbk += 1
                    pb = self.pbank[bk]
                    bkb = self.bank(bk, BF16)
                    for j in range(8):
                        kc = g * 8 + j
                        fw.op(fw.pe, lambda e, kc=kc, j=j, bkb=bkb, xs=xs: e.transpose(
                            out=bkb[:, j * 128:(j + 1) * 128], in_=xg[xs][:, kc * 128:(kc + 1) * 128],
                            identity=self.identb), reads=[xgb[xs], self.cb], writes=[pb])
                    dstp = xgTv[:, g * 8:(g + 1) * 8, stl * 128:(stl + 1) * 128]
                    srcp = bkb.rearrange("p (a b) -> p a b", a=8)
                    if g == 0:
                        fw.op(fw.dve, lambda e, dstp=dstp, srcp=srcp: e.tensor_copy(out=dstp, in_=srcp),
                              reads=[pb], writes=[xgTb])
                    else:
                        fw.op(fw.act, lambda e, dstp=dstp, srcp=srcp: e.activation(out=dstp, in_=srcp, func=AF.Copy),
                              reads=[pb], writes=[xgTb])
            for fc in range(4):
                bkg = pbk % 8
                bku = (pbk + 1) % 8
                pbk += 2
                for (bk, wv) in ((bkg, wgv), (bku, wuv)):
                    for kc in range(16):
                        fw.op(fw.pe, lambda e, bk=bk, wv=wv, kc=kc, fc=fc: e.matmul(
                            self.bank(bk)[:, 0:CAP], lhsT=wv[:, kc, fc * 128:(fc + 1) * 128], rhs=xgTv[:, kc, :],
                            start=(kc == 0), stop=(kc == 15)), reads=[wbuf[s], xgTb], writes=[self.pbank[bk]])
                fw.op(fw.act, lambda e, bkg=bkg: e.activation(out=sg, in_=self.bank(bkg)[:, 0:CAP], func=AF.Silu),
                      reads=[self.pbank[bkg]], writes=[sgb])
                fw.op(fw.dve, lambda e, bku=bku, fc=fc: e.tensor_tensor(out=hTv[:, fc, :], in0=sg,
                                                                        in1=self.bank(bku)[:, 0:CAP], op=ALU.mult),
                      reads=[self.pbank[bku], sgb], writes=[hTb])
            for stl in range(CAP // 128):
                ys = (ex * 2 + stl) % 2
                for mc in range(4):
                    bk = pbk % 8
                    pbk += 1
                    for fc in range(4):
                        fw.op(fw.pe, lambda e, bk=bk, fc=fc, mc=mc, stl=stl: e.matmul(
                            self.bank(bk), lhsT=hTv[:, fc, stl * 128:(stl + 1) * 128],
                            rhs=wdv[:, fc, mc * 512:(mc + 1) * 512], start=(fc == 0), stop=(fc == 3)),
                            reads=[hTb, wbuf[s]], writes=[self.pbank[bk]])
                    dstp = yt[ys][:, mc * 512:(mc + 1) * 512]
                    if mc % 2 == 0:
                        fw.op(fw.dve, lambda e, dstp=dstp, bk=bk: e.tensor_copy(out=dstp, in_=self.bank(bk)),
                              reads=[self.pbank[bk]], writes=[ytb[ys]])
                    else:
                        fw.op(fw.act, lambda e, dstp=dstp, bk=bk: e.activation(out=dstp, in_=self.bank(bk),
                                                                               func=AF.Copy),
                              reads=[self.pbank[bk]], writes=[ytb[ys]])
                fw.dma(out=YG[ex * CAP + stl * 128: ex * CAP + (stl + 1) * 128, :], in_=yt[ys], reads=[ytb[ys]],
                       writes=[YGb])
        self.release(m2)
        m3 = self.mark()
        grow = self.sb(D)
        brow2 = self.sb(D)
        gbuf = Buf()
        self.load_row(grow, W["ln_g"][L, 1], gbuf, D)
        self.load_row(brow2, W["ln_b"][L, 1], gbuf, D)
        xr = [self.sb(D) for _ in range(2)]
        xrb = [Buf(), Buf()]
        y1 = [self.sb(D) for _ in range(2)]
        y1b = [Buf(), Buf()]
        y2 = [self.sb(D) for _ in range(2)]
        y2b = [Buf(), Buf()]
        tmp = self.sb(D)
        tmpb = Buf()
        stat = [self.sb(4) for _ in range(2)]
        statb = [Buf(), Buf()]
        for i in range(NT):
            s = i % 2
            fw.dma(out=xr[s], in_=xm[i * 128:(i + 1) * 128, :], reads=[xm_buf], writes=[xrb[s]])
            for (yy, yb, r) in ((y1[s], y1b[s], 0), (y2[s], y2b[s], 1)):
                fw.dma(out=yy, in_=YG, reads=[YGb, routeb], writes=[yb], q=fw.pool,
                       indirect=dict(out_offset=None,
                                     in_offset=bass.IndirectOffsetOnAxis(ap=dest[:, 2 * i + r:2 * i + r + 1], axis=0)))
            fw.op(fw.act, lambda e, s=s, i=i: e.activation(out=y1[s], in_=y1[s], func=AF.Identity,
                                                           scale=gate[:, 2 * i:2 * i + 1]),
                  reads=[y1b[s], routeb], writes=[y1b[s]])
            fw.op(fw.dve, lambda e, s=s, i=i: e.scalar_tensor_tensor(
                out=y2[s], in0=y2[s], scalar=gate[:, 2 * i + 1:2 * i + 2], in1=y1[s], op0=ALU.mult, op1=ALU.add),
                reads=[y1b[s], y2b[s], routeb], writes=[y2b[s]])
            fw.op(fw.dve, lambda e, s=s: e.scalar_tensor_tensor(
                out=xr[s], in0=xr[s], scalar=DN_ALPHA, in1=y2[s], op0=ALU.mult, op1=ALU.add),
                reads=[xrb[s], y2b[s]], writes=[xrb[s]])
            self.ln_tail(xr[s], xrb[s], grow, brow2, gbuf, tmp, tmpb, stat[s], statb[s], xr[s], xrb[s])
            fw.dma(out=dst[i * 128:(i + 1) * 128, :], in_=xr[s], reads=[xrb[s]], writes=[dst_buf])
        self.release(m3)
        self.release(m0)

    def proj_fm(self, xTv, xTb, w_cols, wt, wtb, banks=(0, 1, 2, 3)):
        fw = self.fw
        fw.dma(out=wt.rearrange("p (a b) -> p a b", a=16), in_=w_cols.rearrange("(kc p) f -> p kc f", p=128),
               writes=[wtb], q=fw.pool)
        wv = wt.rearrange("p (a b) -> p a b", a=16)
        for tc in range(4):
            bk = banks[tc]
            for kc in range(16):
                fw.op(fw.pe, lambda e, bk=bk, kc=kc, tc=tc: e.matmul(
                    self.bank(bk), lhsT=wv[:, kc, :], rhs=xTv[:, kc, tc * 512:(tc + 1) * 512],
                    start=(kc == 0), stop=(kc == 15)), reads=[wtb, xTb], writes=[self.pbank[bk]])

    def sin_rr(self, eng, out, ang, buf, k, r, n_part=128, shift=0.0):
        fw = self.fw
        MAG = 12582912.0
        C1 = 6.28125
        C2 = 2.0 * math.pi - 6.28125
        fw.op(eng, lambda e: e.tensor_scalar(out=k, in0=ang, scalar1=1.0 / (2.0 * math.pi),
                                             scalar2=shift / (2.0 * math.pi), op0=ALU.mult, op1=ALU.add),
              reads=[buf], writes=[buf])
        fw.op(eng, lambda e: e.tensor_scalar(out=k, in0=k, scalar1=MAG, scalar2=None, op0=ALU.add),
              reads=[buf], writes=[buf])
        fw.op(eng, lambda e: e.tensor_scalar(out=k, in0=k, scalar1=-MAG, scalar2=None, op0=ALU.add),
              reads=[buf], writes=[buf])
        fw.op(eng, lambda e: e.scalar_tensor_tensor(out=r, in0=k, scalar=-C1, in1=ang, op0=ALU.mult, op1=ALU.add),
              reads=[buf], writes=[buf]) if eng is fw.dve else None
        if eng is not fw.dve:
            raise ValueError
        fw.op(eng, lambda e: e.scalar_tensor_tensor(out=r, in0=k, scalar=-C2, in1=r, op0=ALU.mult, op1=ALU.add),
              reads=[buf], writes=[buf])
        fw.op(eng, lambda e: e.tensor_scalar(out=r, in0=r, scalar1=shift, scalar2=math.pi, op0=ALU.add, op1=ALU.min),
              reads=[buf], writes=[buf])
        fw.op(eng, lambda e: e.tensor_scalar(out=r, in0=r, scalar1=-math.pi, scalar2=None, op0=ALU.max),
              reads=[buf], writes=[buf])
        fw.op(fw.act, lambda e: e.activation(out=out, in_=r, func=AF.Sin), reads=[buf], writes=[buf])

    def s5(self, L, src, src_buf, W, dst, dst_buf):
        fw = self.fw
        dve, act, pool, pe = fw.dve, fw.act, fw.pool, fw.pe
        U, Ub = self.dram["U"], self.dbuf["U"]
        YA, YAb = self.dram["YA"], self.dbuf["YA"]
        OT, OTb = self.dram["OT"], self.dbuf["OT"]
        m0 = self.mark()
        big = self.sb(16 * T, BF16)
        bigv = big.rearrange("p (a b) -> p a b", a=16)
        xTb = Buf("xT")
        self.build_xT(src, src_buf, bigv, xTb)
        mU = self.mark()
        wt = [self.sb(16 * 128, BF16) for _ in range(2)]
        wtb = [Buf(), Buf()]
        uf = [self.sb(T) for _ in range(2)]
        ufb = [Buf(), Buf()]
        for J in range(16):
            s = J % 2
            self.proj_fm(bigv, xTb, W["ssm_w_in"][0][:, J * 128:(J + 1) * 128], wt[s], wtb[s])
            for tc in range(4):
                eng = dve if tc % 2 == 0 else act
                dstp = uf[s][:, tc * 512:(tc + 1) * 512]
                if tc % 2 == 0:
                    fw.op(dve, lambda e, dstp=dstp, tc=tc: e.tensor_copy(out=dstp, in_=self.bank(tc)),
                          reads=[self.pbank[tc]], writes=[ufb[s]])
                else:
                    fw.op(act, lambda e, dstp=dstp, tc=tc: e.activation(out=dstp, in_=self.bank(tc), func=AF.Copy),
                          reads=[self.pbank[tc]], writes=[ufb[s]])
            fw.dma(out=U[J], in_=uf[s], reads=[ufb[s]], writes=[Ub])
        self.release(mU)
        uTb_buf = xTb
        for q in range(4):
            fw.dma(out=bigv[:, q * 4:(q + 1) * 4, :], in_=U.rearrange("j p t -> p j t")[:, q * 4:(q + 1) * 4, :],
                   reads=[Ub], writes=[uTb_buf], q=pool)
        mP = self.mark()
        PQ = self.sb(6 * 64)
        LB = [self.sb(16 * 128, BF16) for _ in range(2)]
        CL = [self.sb(16 * 128) for _ in range(2)]
        LBz = [self.sb(16 * 128, BF16) for _ in range(2)]
        LBzv = [a.rearrange("p (J q) -> p J q", J=16) for a in LBz]
        dsk = self.sb(16)
        tau = self.sb(520)
        m96 = self.sb(1)
        mT = self.mark()
        pb_ = Buf("s5par")
        A = lambda: self.sb(128)
        are, aim, dtb, mag, ang, kk, rr, sn, cs, den, fre, fim, t1, t2 = [A() for _ in range(14)]
        ldt = self.sb(2)
        fw.dma(out=are[0:64, :], in_=W["ssm_a_re"][0].rearrange("(j two) p -> j (two p)", two=2), writes=[pb_])
        fw.dma(out=aim[0:64, :], in_=W["ssm_a_im"][0].rearrange("(j two) p -> j (two p)", two=2), writes=[pb_])
        fw.dma(out=ldt[0:64, :], in_=W["ssm_log_dt"][0].rearrange("(j two) -> j two", two=2), writes=[pb_])
        h = slice(0, 64)
        fw.op(act, lambda e: e.activation(out=ldt[h, :], in_=ldt[h, :], func=AF.Exp), reads=[pb_], writes=[pb_])
        for two in range(2):
            fw.op(dve, lambda e, two=two: e.tensor_scalar(out=dtb[h, two * 64:(two + 1) * 64], in0=self.ones[h, 0:64],
                                                         scalar1=ldt[h, two:two + 1], scalar2=None, op0=ALU.mult),
                  reads=[pb_, self.cb], writes=[pb_])
        tt = lambda o, a, b, op: fw.op(dve, lambda e: e.tensor_tensor(out=o[h, :], in0=a[h, :], in1=b[h, :], op=op),
                                       reads=[pb_], writes=[pb_])
        tt(mag, are, dtb, ALU.mult)
        fw.op(act, lambda e: e.activation(out=mag[h, :], in_=mag[h, :], func=AF.Exp), reads=[pb_], writes=[pb_])
        tt(ang, aim, dtb, ALU.mult)
        self.sin_rr(dve, sn[h, :], ang[h, :], pb_, kk[h, :], rr[h, :])
        thr = A()
        fw.op(dve, lambda e: e.tensor_copy(out=thr[h, :], in_=rr[h, :]), reads=[pb_], writes=[pb_])
        self.sin_rr(dve, cs[h, :], ang[h, :], pb_, kk[h, :], rr[h, :], shift=math.pi / 2)
        lre, lim = A(), A()
        tt(lre, mag, cs, ALU.mult)
        tt(lim, mag, sn, ALU.mult)
        tt(t1, are, are, ALU.mult)
        tt(t2, aim, aim, ALU.mult)
        tt(den, t1, t2, ALU.add)
        fw.op(dve, lambda e: e.reciprocal(out=den[h, :], in_=den[h, :]), reads=[pb_], writes=[pb_])
        lm1 = A()
        fw.op(dve, lambda e: e.tensor_scalar(out=lm1[h, :], in0=lre[h, :], scalar1=-1.0, scalar2=None, op0=ALU.add),
              reads=[pb_], writes=[pb_])
        tt(t1, lm1, are, ALU.mult)
        tt(t2, lim, aim, ALU.mult)
        tt(fre, t1, t2, ALU.add)
        tt(fre, fre, den, ALU.mult)
        tt(t1, lim, are, ALU.mult)
        tt(t2, lm1, aim, ALU.mult)
        tt(fim, t1, t2, ALU.subtract)
        tt(fim, fim, den, ALU.mult)
        PQv = PQ.rearrange("p (a b) -> p a b", a=6)
        pqb = Buf("pq")
        for n_, srcp in enumerate((mag, thr, cs, sn, fre, fim)):
            fw.op(pe, lambda e, n_=n_, srcp=srcp: e.transpose(out=self.bank(0)[:, n_ * 64:(n_ + 1) * 64],
                                                              in_=srcp[0:64, :], identity=self.ident[0:64, 0:64]),
                  reads=[pb_, self.cb], writes=[self.pbank[0]])
        fw.op(dve, lambda e: e.tensor_copy(out=PQ, in_=self.bank(0)[:, 0:384]), reads=[self.pbank[0]], writes=[pqb])
        RHO, THR, COS1, SIN1, FRE, FIM = [PQv[:, n_, :] for n_ in range(6)]
        bre = self.sb(64 * 16)
        bim = self.sb(64 * 16)
        bbr = self.sb(64 * 16)
        bbi = self.sb(64 * 16)
        tb1 = self.sb(64 * 16)
        bb_ = Buf("bb")
        v3 = lambda a: a.rearrange("p (j c) -> p j c", c=16)
        fw.dma(out=v3(bre), in_=W["ssm_b_re"][0].rearrange("(j two) p c -> (two p) j c", two=2), writes=[bb_])
        fw.dma(out=v3(bim), in_=W["ssm_b_im"][0].rearrange("(j two) p c -> (two p) j c", two=2), writes=[bb_])
        bc = lambda a: a.unsqueeze(2).to_broadcast([128, 64, 16])
        t3 = lambda o, a, b, op: fw.op(dve, lambda e: e.tensor_tensor(out=v3(o), in0=v3(a), in1=b, op=op),
                                       reads=[bb_, pqb], writes=[bb_])
        t3(bbr, bre, bc(FRE), ALU.mult)
        t3(tb1, bim, bc(FIM), ALU.mult)
        t3(bbr, bbr, v3(tb1), ALU.subtract)
        t3(bbi, bim, bc(FRE), ALU.mult)
        t3(tb1, bre, bc(FIM), ALU.mult)
        t3(bbi, bbi, v3(tb1), ALU.add)
        LBv = [a.rearrange("p (J q) -> p J q", J=16) for a in LB]
        lbb = Buf("LB")
        arr = self.sb(16 * 128)
        arrb = Buf("arr")
        arr5 = arr.rearrange("p (J jj two c) -> p J jj two c", J=16, jj=4, two=2)
        for ri, bbx in enumerate((bbr, bbi)):
            fw.op(pool, lambda e: e.memset(arr, 0.0), writes=[arrb])
            b4 = bbx.rearrange("p (J jj c) -> p J jj c", J=16, jj=4)
            for two in range(2):
                ps = slice(two * 64, (two + 1) * 64)
                fw.op(pool, lambda e, two=two, ps=ps, b4=b4: e.tensor_copy(out=arr5[ps, :, :, two, :], in_=b4[ps]),
                      reads=[bb_, arrb], writes=[arrb])
            av = arr.rearrange("p (J x) -> p J x", J=16)
            for g in range(4):
                bk = 1 + g
                for j4 in range(4):
                    J = g * 4 + j4
                    fw.op(pe, lambda e, J=J, j4=j4, bk=bk: e.transpose(out=self.bank(bk)[:, j4 * 128:(j4 + 1) * 128],
                                                                       in_=av[:, J, :], identity=self.ident),
                          reads=[arrb, self.cb], writes=[self.pbank[bk]])
                fw.op(act, lambda e, g=g, bk=bk, ri=ri: e.activation(
                    out=LBv[ri][:, g * 4:(g + 1) * 4, :], in_=self.bank(bk).rearrange("p (a b) -> p a b", a=4),
                    func=AF.Copy), reads=[self.pbank[bk]], writes=[lbb])
        fw.op(pool, lambda e: e.memset(m96, 1.0), writes=[lbb])
        fw.op(pool, lambda e: e.affine_select(out=m96, in_=m96, pattern=[[0, 1]], compare_op=ALU.is_ge, fill=0.0,
                                              base=-96, channel_multiplier=1), reads=[lbb], writes=[lbb])
        for ri in range(2):
            fw.op(dve, lambda e, ri=ri: e.tensor_scalar(out=LBz[ri][64:128, :], in0=LB[ri][64:128, :],
                                                       scalar1=m96[64:128, :], scalar2=None, op0=ALU.mult),
                  reads=[lbb], writes=[lbb])
        CLv = [a.rearrange("p (J q) -> p J q", J=16) for a in CL]
        clb = Buf("CL")
        for ri, cname in enumerate(("ssm_c_re", "ssm_c_im")):
            fw.op(pool, lambda e: e.memset(arr, 0.0), writes=[arrb])
            a4 = arr.rearrange("p (J two q) -> p J two q", J=16, two=2)
            csrc = W[cname][0].rearrange("(J jj two) c p -> jj two c J p", jj=4, two=2)
            for jj in range(4):
                for two in range(2):
                    r0 = jj * 32 + two * 16
                    fw.dma(out=a4[r0:r0 + 16, :, two, :], in_=csrc[jj, two], writes=[arrb])
            av = arr.rearrange("p (J x) -> p J x", J=16)
            for g in range(4):
                bk = 1 + g
                for j4 in range(4):
                    J = g * 4 + j4
                    fw.op(pe, lambda e, J=J, j4=j4, bk=bk: e.transpose(out=self.bank(bk)[:, j4 * 128:(j4 + 1) * 128],
                                                                       in_=av[:, J, :], identity=self.ident),
                          reads=[arrb, self.cb], writes=[self.pbank[bk]])
                fw.op(act, lambda e, g=g, bk=bk, ri=ri: e.activation(
                    out=CLv[ri][:, g * 4:(g + 1) * 4, :], in_=self.bank(bk).rearrange("p (a b) -> p a b", a=4),
                    func=AF.Copy, scale=(1.0 if ri == 0 else -1.0)), reads=[self.pbank[bk]], writes=[clb])
        dskb = Buf("dsk")
        d16 = self.sb(128)
        fw.dma(out=d16[0:16, :], in_=W["ssm_d"][0].rearrange("(J p) -> J p", p=128), writes=[dskb])
        fw.op(pe, lambda e: e.transpose(out=self.bank(5)[:, 0:16], in_=d16[0:16, :], identity=self.ident[0:16, 0:16]),
              reads=[dskb, self.cb], writes=[self.pbank[5]])
        fw.op(dve, lambda e: e.tensor_copy(out=dsk, in_=self.bank(5)[:, 0:16]), reads=[self.pbank[5]], writes=[dskb])
        taub = Buf("tau")
        fw.op(pool, lambda e: e.iota(out=tau[:, 0:513], pattern=[[1, 513]], base=0, channel_multiplier=0,
                                     allow_small_or_imprecise_dtypes=True), writes=[taub])
        self.release(mT)
        NW = 2
        tabc_f = [self.sb(520) for _ in range(NW)]
        tabs_f = [self.sb(520) for _ in range(NW)]
        tk = [self.sb(520)[:, 0:513] for _ in range(NW)]
        tr_ = [self.sb(520)[:, 0:513] for _ in range(NW)]
        tang = [self.sb(520)[:, 0:513] for _ in range(NW)]
        tabc = [a[:, 0:512] for a in tabc_f]
        tabs = [a[:, 0:512] for a in tabs_f]
        tabb = [Buf() for _ in range(NW)]
        wk = [[self.sb(512) for _ in range(6)] for _ in range(NW)]
        wkb = [Buf() for _ in range(NW)]
        st8 = [self.sb(8) for _ in range(4)]
        st8b = [Buf() for _ in range(4)]
        clm = [self.sb(4 * 128) for _ in range(2)]
        clmb = Buf("clm")
        ufl0 = self.sb(T)
        ufl = [ufl0, ufl0]
        uflb0 = Buf()
        uflb = [uflb0, uflb0]
        yo = [self.sb(512) for _ in range(2)]
        yob = [Buf(), Buf()]
        yab0 = self.sb(T, BF16)
        yab = [yab0, yab0]
        yabb0 = Buf()
        yabb = [yabb0, yabb0]
        cnt = 0
        cnt2 = 0
        bu_done = {}
        bu_n = [0]

        def emit_bu(J_, jj_, tc_):
            bks = (5, 6) if bu_n[0] % 2 == 0 else (4, 7)
            bu_n[0] += 1
            rows_ = slice(jj_ * 32, (jj_ + 1) * 32) if jj_ < 3 else slice(64, 128)
            LBu_ = LBv if jj_ < 3 else LBzv
            for ri_, bk_ in ((0, bks[0]), (1, bks[1])):
                fw.op(pe, lambda e, ri_=ri_, bk_=bk_: e.matmul(
                    self.bank(bk_), lhsT=LBu_[ri_][rows_, J_, :], rhs=bigv[rows_, J_, tc_ * 512:(tc_ + 1) * 512],
                    start=True, stop=True), reads=[lbb, uTb_buf], writes=[self.pbank[bk_]])
            bu_done[(J_, jj_, tc_)] = bks

        for J in range(16):
            js = J % 2
            fw.dma(out=ufl[js], in_=U[J], reads=[Ub], writes=[uflb[js]])
            for ri in range(2):
                cm = clm[ri].rearrange("p (jj x) -> p jj x", jj=4)
                fw.op(pool, lambda e, ri=ri: e.memset(clm[ri], 0.0), writes=[clmb])
                for jj in range(4):
                    fw.op(pool, lambda e, ri=ri, jj=jj, cm=cm, J=J: e.tensor_copy(
                        out=cm[:, jj, jj * 32:(jj + 1) * 32], in_=CLv[ri][:, J, jj * 32:(jj + 1) * 32]),
                        reads=[clb, clmb], writes=[clmb])
            for jj in range(4):
                fw.op(dve, lambda e, jj=jj: e.memset(st8[jj], 0.0), writes=[st8b[jj]])
            for jj in range(4):
                j = J * 4 + jj
                w = cnt % NW
                cnt += 1
                fw.op(dve, lambda e, w=w, j=j: e.tensor_scalar(out=tang[w], in0=tau[:, 0:513], scalar1=THR[:, j:j + 1],
                                                               scalar2=None, op0=ALU.mult),
                      reads=[taub, pqb], writes=[tabb[w]])
                self.sin_rr(dve, tabs_f[w][:, 0:513], tang[w], tabb[w], tk[w], tr_[w])
                self.sin_rr(dve, tabc_f[w][:, 0:513], tang[w], tabb[w], tk[w], tr_[w], shift=math.pi / 2)
                C512 = tabc_f[w][:, 512:513]
                S512 = tabs_f[w][:, 512:513]
                for tc in range(4):
                    ybk = tc
                    w2 = cnt2 % NW
                    cnt2 += 1
                    if (J, jj, tc) not in bu_done:
                        emit_bu(J, jj, tc)
                    b5, b6 = bu_done[(J, jj, tc)]
                    nxt = (J, jj, tc + 1) if tc < 3 else ((J, jj + 1, 0) if jj < 3 else ((J + 1, 0, 0) if J < 15 else None))
                    if nxt is not None:
                        emit_bu(*nxt)
                    btr, bti, rre, rim, ta, tb = wk[w2]
                    B = wkb[w2]
                    fw.op(dve, lambda e, w=w, btr=btr: e.tensor_tensor(out=btr, in0=self.bank(b5), in1=tabc[w], op=ALU.mult),
                          reads=[self.pbank[b5], tabb[w]], writes=[B])
                    fw.op(dve, lambda e, w=w, ta=ta: e.tensor_tensor(out=ta, in0=self.bank(b6), in1=tabs[w], op=ALU.mult),
                          reads=[self.pbank[b6], tabb[w]], writes=[B])
                    fw.op(dve, lambda e, btr=btr, ta=ta: e.tensor_tensor(out=btr, in0=btr, in1=ta, op=ALU.add),
                          reads=[B], writes=[B])
                    fw.op(dve, lambda e, w=w, bti=bti: e.tensor_tensor(out=bti, in0=self.bank(b6), in1=tabc[w], op=ALU.mult),
                          reads=[self.pbank[b6], tabb[w]], writes=[B])
                    fw.op(dve, lambda e, w=w, tb=tb: e.tensor_tensor(out=tb, in0=self.bank(b5), in1=tabs[w], op=ALU.mult),
                          reads=[self.pbank[b5], tabb[w]], writes=[B])
                    fw.op(dve, lambda e, bti=bti, tb=tb: e.tensor_tensor(out=bti, in0=bti, in1=tb, op=ALU.subtract),
                          reads=[B], writes=[B])
                    c8 = st8[jj]
                    cb8 = st8b[jj]
                    fw.op(dve, lambda e, c8=c8, j=j: e.tensor_scalar(out=c8[:, 2:3], in0=c8[:, 0:1],
                                                                     scalar1=C512, scalar2=None, op0=ALU.mult),
                          reads=[cb8, tabb[w]], writes=[cb8])
                    fw.op(dve, lambda e, c8=c8, j=j: e.tensor_scalar(out=c8[:, 4:5], in0=c8[:, 1:2],
                                                                     scalar1=S512, scalar2=None, op0=ALU.mult),
                          reads=[cb8, tabb[w]], writes=[cb8])
                    fw.op(dve, lambda e, c8=c8: e.tensor_tensor(out=c8[:, 2:3], in0=c8[:, 2:3], in1=c8[:, 4:5],
                                                                op=ALU.subtract), reads=[cb8], writes=[cb8])
                    fw.op(dve, lambda e, c8=c8, j=j: e.tensor_scalar(out=c8[:, 3:4], in0=c8[:, 0:1],
                                                                     scalar1=S512, scalar2=None, op0=ALU.mult),
                          reads=[cb8, tabb[w]], writes=[cb8])
                    fw.op(dve, lambda e, c8=c8, j=j: e.tensor_scalar(out=c8[:, 4:5], in0=c8[:, 1:2],
                                                                     scalar1=C512, scalar2=None, op0=ALU.mult),
                          reads=[cb8, tabb[w]], writes=[cb8])
                    fw.op(dve, lambda e, c8=c8: e.tensor_tensor(out=c8[:, 3:4], in0=c8[:, 3:4], in1=c8[:, 4:5],
                                                                op=ALU.add), reads=[cb8], writes=[cb8])
                    rho_b = RHO[:, j:j + 1].to_broadcast([128, 512])
                    fw.op(dve, lambda e, rre=rre, btr=btr, c8=c8, rho_b=rho_b: e.tensor_tensor_scan(
                        out=rre, data0=rho_b, data1=btr, initial=c8[:, 2:3], op0=ALU.mult, op1=ALU.add),
                        reads=[B, cb8, pqb], writes=[B])
                    fw.op(dve, lambda e, rim=rim, bti=bti, c8=c8, rho_b=rho_b: e.tensor_tensor_scan(
                        out=rim, data0=rho_b, data1=bti, initial=c8[:, 3:4], op0=ALU.mult, op1=ALU.add),
                        reads=[B, cb8, pqb], writes=[B])
                    fw.op(dve, lambda e, c8=c8, rre=rre: e.tensor_copy(out=c8[:, 0:1], in_=rre[:, 511:512]),
                          reads=[B, cb8], writes=[cb8])
                    fw.op(dve, lambda e, c8=c8, rim=rim: e.tensor_copy(out=c8[:, 1:2], in_=rim[:, 511:512]),
                          reads=[B, cb8], writes=[cb8])
                    fw.op(pool, lambda e, w=w, ta=ta, rre=rre: e.tensor_tensor(out=ta, in0=rre, in1=tabc[w], op=ALU.mult),
                          reads=[B, tabb[w]], writes=[B])
                    fw.op(pool, lambda e, w=w, tb=tb, rim=rim: e.tensor_tensor(out=tb, in0=rim, in1=tabs[w], op=ALU.mult),
                          reads=[B, tabb[w]], writes=[B])
                    fw.op(pool, lambda e, w=w, ta=ta, tb=tb: e.tensor_tensor(out=ta, in0=ta, in1=tb, op=ALU.subtract),
                          reads=[B], writes=[B])
                    fw.op(pool, lambda e, w=w, tb=tb, rre=rre: e.tensor_tensor(out=tb, in0=rre, in1=tabs[w], op=ALU.mult),
                          reads=[B, tabb[w]], writes=[B])
                    fw.op(pool, lambda e, w=w, rre=rre, rim=rim: e.tensor_tensor(out=rre, in0=rim, in1=tabc[w], op=ALU.mult),
                          reads=[B, tabb[w]], writes=[B])
                    fw.op(pool, lambda e, tb=tb, rre=rre: e.tensor_tensor(out=tb, in0=tb, in1=rre, op=ALU.add),
                          reads=[B], writes=[B])
                    cm0 = clm[0].rearrange("p (jj x) -> p jj x", jj=4)
                    cm1 = clm[1].rearrange("p (jj x) -> p jj x", jj=4)
                    fw.op(pe, lambda e, ta=ta, jj=jj, cm0=cm0: e.matmul(self.bank(ybk), lhsT=cm0[:, jj, :], rhs=ta,
                                                                        start=(jj == 0), stop=False),
                          reads=[clmb, B], writes=[self.pbank[ybk]])
                    fw.op(pe, lambda e, tb=tb, jj=jj, cm1=cm1: e.matmul(self.bank(ybk), lhsT=cm1[:, jj, :], rhs=tb,
                                                                        start=False, stop=(jj == 3)),
                          reads=[clmb, B], writes=[self.pbank[ybk]])
            for tc in range(4):
                ybk = tc
                ys = (J * 4 + tc) % 2
                fw.op(dve, lambda e, ys=ys, js=js, J=J, tc=tc: e.scalar_tensor_tensor(
                    out=yo[ys], in0=ufl[js][:, tc * 512:(tc + 1) * 512], scalar=dsk[:, J:J + 1], in1=self.bank(ybk),
                    op0=ALU.mult, op1=ALU.add), reads=[uflb[js], dskb, self.pbank[ybk]], writes=[yob[ys]])
                fw.op(act, lambda e, ys=ys, js=js, tc=tc: e.activation(out=yab[js][:, tc * 512:(tc + 1) * 512],
                                                                     in_=yo[ys], func=AF.Gelu),
                      reads=[yob[ys]], writes=[yabb[js]])
            fw.dma(out=YA[J], in_=yab[js], reads=[yabb[js]], writes=[YAb])
        self.release(mP)
        for q in range(4):
            fw.dma(out=bigv[:, q * 4:(q + 1) * 4, :], in_=YA.rearrange("j p t -> p j t")[:, q * 4:(q + 1) * 4, :],
                   reads=[YAb], writes=[xTb])
        mG = self.mark()
        wt = [self.sb(16 * 128, BF16) for _ in range(2)]
        wtb = [Buf(), Buf()]
        bgl = self.sb(16)
        bglb = Buf()
        b16 = self.sb(128)
        fw.dma(out=b16[0:16, :], in_=W["ssm_b_glu"][0].rearrange("(J p) -> J p", p=128), writes=[bglb])
        fw.op(pe, lambda e: e.transpose(out=self.bank(5)[:, 0:16], in_=b16[0:16, :], identity=self.ident[0:16, 0:16]),
              reads=[bglb, self.cb], writes=[self.pbank[5]])
        fw.op(dve, lambda e: e.tensor_copy(out=bgl, in_=self.bank(5)[:, 0:16]), reads=[self.pbank[5]], writes=[bglb])
        sg = [self.sb(512) for _ in range(2)]
        sgb = [Buf(), Buf()]
        y2 = [self.sb(T, BF16) for _ in range(2)]
        y2b = [Buf(), Buf()]
        for mo in range(16):
            s = mo % 2
            self.proj_fm(bigv, xTb, W["ssm_w_glu"][0][:, mo * 128:(mo + 1) * 128], wt[s], wtb[s])
            for tc in range(4):
                s2 = tc % 2
                fw.op(act, lambda e, s2=s2, tc=tc, mo=mo: e.activation(out=sg[s2], in_=self.bank(tc), func=AF.Sigmoid,
                                                                     bias=bgl[:, mo:mo + 1], scale=1.0),
                      reads=[self.pbank[tc], bglb], writes=[sgb[s2]])
                fw.op(dve, lambda e, s=s, s2=s2, tc=tc, mo=mo: e.tensor_tensor(
                    out=y2[s][:, tc * 512:(tc + 1) * 512], in0=sg[s2], in1=bigv[:, mo, tc * 512:(tc + 1) * 512],
                    op=ALU.mult), reads=[sgb[s2], xTb], writes=[y2b[s]])
            fw.dma(out=OT[mo], in_=y2[s], reads=[y2b[s]], writes=[OTb])
        self.release(mG)
        self.release(m0)
        self.outproj_ln(OT, OTb, W["ssm_w_out"][0], src, src_buf, W["ln_g"][L, 0], W["ln_b"][L, 0], dst, dst_buf)

    def dsa(self, L, src, src_buf, W, dst, dst_buf):
        fw = self.fw
        dve, act, pool, pe = fw.dve, fw.act, fw.pool, fw.pe
        QT, QTb = self.dram["QT"], self.dbuf["QT"]
        QI, QIb = self.dram["QI"], self.dbuf["QI"]
        MT, MTb = self.dram["MASKT"], self.dbuf["MASKT"]
        OT, OTb = self.dram["OT"], self.dbuf["OT"]
        w_in = W["dsa_w_in"][0]
        m0 = self.mark()
        kT = self.sb(T, BF16)
        kiT = self.sb(T, BF16)
        vtok = self.sb(16 * 132, BF16)
        vtv = vtok.rearrange("p (a b) -> p a b", a=16)
        wi = self.sb(16 * 16)
        wiv = wi.rearrange("p (a b) -> p a b", a=16)
        resb = Buf("dsa_res")
        fw.op(pool, lambda e: e.memset(vtok, 1.0), writes=[resb])
        m1 = self.mark()
        big = self.sb(16 * T, BF16)
        bigv = big.rearrange("p (a b) -> p a b", a=16)
        xTb = Buf("xT")
        self.build_xT(src, src_buf, bigv, xTb)
        wt = [self.sb(16 * 128, BF16) for _ in range(2)]
        wtb = [Buf(), Buf()]
        ob = [self.sb(T, BF16) for _ in range(2)]
        obb = [Buf(), Buf()]
        QSC = 128.0 ** -0.5
        n = 0
        for (col0, cnt_, dstD, dstDb, scale) in ((0, 16, QT, QTb, QSC), (2304, 16, QI, QIb, 1.0)):
            for hh in range(cnt_):
                s = n % 2
                n += 1
                self.proj_fm(bigv, xTb, w_in[:, col0 + hh * 128: col0 + (hh + 1) * 128], wt[s], wtb[s])
                for tc in range(4):
                    dstp = ob[s][:, tc * 512:(tc + 1) * 512]
                    if tc % 2 == 0:
                        fw.op(dve, lambda e, dstp=dstp, tc=tc, scale=scale: e.tensor_scalar(
                            out=dstp, in0=self.bank(tc), scalar1=scale, scalar2=None, op0=ALU.mult),
                            reads=[self.pbank[tc]], writes=[obb[s]])
                    else:
                        fw.op(act, lambda e, dstp=dstp, tc=tc, scale=scale: e.activation(
                            out=dstp, in_=self.bank(tc), func=AF.Copy, scale=scale),
                            reads=[self.pbank[tc]], writes=[obb[s]])
                fw.dma(out=dstD[hh], in_=ob[s], reads=[obb[s]], writes=[dstDb])
        for (col0, dstT) in ((2048, kT), (4352, kiT)):
            s = n % 2
            n += 1
            self.proj_fm(bigv, xTb, w_in[:, col0: col0 + 128], wt[s], wtb[s])
            for tc in range(4):
                dstp = dstT[:, tc * 512:(tc + 1) * 512]
                fw.op(act, lambda e, dstp=dstp, tc=tc: e.activation(out=dstp, in_=self.bank(tc), func=AF.Copy),
                      reads=[self.pbank[tc]], writes=[resb])
        wv = wt[0]
        wvv = wv.rearrange("p (a b) -> p a b", a=16)
        fw.dma(out=wvv, in_=w_in[:, 2176:2304].rearrange("(kc p) f -> p kc f", p=128), writes=[wtb[0]], q=pool)
        ww = wt[1][:, 0:256]
        wwv = ww.rearrange("p (a b) -> p a b", a=16)
        fw.dma(out=wwv, in_=w_in[:, 4480:4496].rearrange("(kc p) f -> p kc f", p=128), writes=[wtb[1]], q=pool)
        WSC = (16.0 ** -0.5) * (128.0 ** -0.5)
        for i in range(NT):
            bk = 4 + (i % 2)
            for kc in range(16):
                fw.op(pe, lambda e, i=i, kc=kc, bk=bk: e.matmul(self.bank(bk)[:, 0:128], lhsT=bigv[:, kc, i * 128:(i + 1) * 128],
                                                               rhs=wvv[:, kc, :], start=(kc == 0), stop=(kc == 15)),
                      reads=[xTb, wtb[0]], writes=[self.pbank[bk]])
            fw.op(act, lambda e, i=i, bk=bk: e.activation(out=vtv[:, i, 0:128], in_=self.bank(bk)[:, 0:128], func=AF.Copy),
                  reads=[self.pbank[bk]], writes=[resb])
            bk2 = 6 + (i % 2)
            for kc in range(16):
                fw.op(pe, lambda e, i=i, kc=kc, bk2=bk2: e.matmul(self.bank(bk2)[:, 0:16], lhsT=bigv[:, kc, i * 128:(i + 1) * 128],
                                                                 rhs=wwv[:, kc, :], start=(kc == 0), stop=(kc == 15)),
                      reads=[xTb, wtb[1]], writes=[self.pbank[bk2]])
            fw.op(dve, lambda e, i=i, bk2=bk2: e.tensor_scalar(out=wiv[:, i, :], in0=self.bank(bk2)[:, 0:16], scalar1=WSC,
                                                               scalar2=None, op0=ALU.mult),
                  reads=[self.pbank[bk2]], writes=[resb])
        self.release(m1)
        m2 = self.mark()
        qit = [self.sb(16 * 128, BF16) for _ in range(2)]
        qitb = [Buf(), Buf()]
        acc = self.sb(T)
        accb = Buf("acc")
        work = self.sb(T)
        workb = Buf("work")
        rl = [self.sb(512, BF16) for _ in range(2)]
        rlb = [Buf(), Buf()]
        m8 = self.sb(8)
        m8b = Buf()
        mk = self.sb(T, BF16)
        mkb = Buf()
        mT = [self.sb(16 * 128, BF16) for _ in range(2)]
        mTb = [Buf(), Buf()]
        QIv = QI.rearrange("h p t -> p h t")
        MTv = MT.rearrange("b p t -> p b t")
        nr = 0
        for i in range(NT):
            s = i % 2
            S_ = 128 * (i + 1)
            fw.dma(out=qit[s].rearrange("p (a b) -> p a b", a=16), in_=QIv[:, :, i * 128:(i + 1) * 128], reads=[QIb],
                   writes=[qitb[s]])
            qv = qit[s].rearrange("p (a b) -> p a b", a=16)
            nsc = (S_ + 511) // 512
            for hh in range(16):
                for sc in range(nsc):
                    c0 = sc * 512
                    c1 = min(S_, c0 + 512)
                    bk = nr % 4
                    r2 = nr % 2
                    nr += 1
                    fw.op(pe, lambda e, hh=hh, c0=c0, c1=c1, bk=bk, qv=qv: e.matmul(
                        self.bank(bk)[:, 0:c1 - c0], lhsT=qv[:, hh, :], rhs=kiT[:, c0:c1], start=True, stop=True),
                        reads=[qitb[s], resb], writes=[self.pbank[bk]])
                    fw.op(act, lambda e, c0=c0, c1=c1, bk=bk, r2=r2: e.activation(
                        out=rl[r2][:, 0:c1 - c0], in_=self.bank(bk)[:, 0:c1 - c0], func=AF.Relu),
                        reads=[self.pbank[bk]], writes=[rlb[r2]])
                    if hh == 0:
                        fw.op(dve, lambda e, c0=c0, c1=c1, r2=r2, i=i: e.tensor_scalar(
                            out=acc[:, c0:c1], in0=rl[r2][:, 0:c1 - c0], scalar1=wiv[:, i, 0:1], scalar2=None,
                            op0=ALU.mult), reads=[rlb[r2], resb], writes=[accb])
                    else:
                        fw.op(dve, lambda e, c0=c0, c1=c1, r2=r2, i=i, hh=hh: e.scalar_tensor_tensor(
                            out=acc[:, c0:c1], in0=rl[r2][:, 0:c1 - c0], scalar=wiv[:, i, hh:hh + 1], in1=acc[:, c0:c1],
                            op0=ALU.mult, op1=ALU.add), reads=[rlb[r2], resb, accb], writes=[accb])
            fw.op(pool, lambda e, S_=S_: e.affine_select(out=acc[:, S_ - 128:S_], in_=acc[:, S_ - 128:S_],
                                                         pattern=[[-1, 128]], compare_op=ALU.is_ge, fill=-1e30, base=0,
                                                         channel_multiplier=1), reads=[accb], writes=[accb])
            if i >= 2:
                cur = acc
                curb = accb
                for rnd in range(32):
                    fw.op(dve, lambda e, cur=cur, S_=S_: e.max(out=m8, in_=cur[:, 0:S_]), reads=[curb], writes=[m8b])
                    if rnd < 31:
                        fw.op(dve, lambda e, cur=cur, S_=S_: e.match_replace(out=work[:, 0:S_], in_to_replace=m8,
                                                                             in_values=cur[:, 0:S_], imm_value=-3e38),
                              reads=[curb, m8b], writes=[workb])
                        cur = work
                        curb = workb
                fw.op(dve, lambda e, S_=S_: e.tensor_scalar(out=mk[:, 0:S_], in0=acc[:, 0:S_], scalar1=m8[:, 7:8],
                                                            scalar2=None, op0=ALU.is_ge), reads=[accb, m8b], writes=[mkb])
            else:
                fw.op(dve, lambda e, S_=S_: e.tensor_scalar(out=mk[:, 0:S_], in0=acc[:, 0:S_], scalar1=-1e29,
                                                            scalar2=None, op0=ALU.is_ge), reads=[accb], writes=[mkb])
            mTv = mT[s].rearrange("p (a b) -> p a b", a=16)
            for g in range((i + 8) // 8):
                bk = 4 + (g + i) % 2
                bkb = self.bank(bk, BF16)
                nb_ = min(8, i + 1 - g * 8)
                for j in range(nb_):
                    b = g * 8 + j
                    fw.op(pe, lambda e, b=b, j=j, bkb=bkb: e.transpose(out=bkb[:, j * 128:(j + 1) * 128],
                                                                      in_=mk[:, b * 128:(b + 1) * 128], identity=self.identb),
                          reads=[mkb, self.cb], writes=[self.pbank[bk]])
                fw.op(act, lambda e, g=g, nb_=nb_, bkb=bkb, mTv=mTv: e.activation(
                    out=mTv[:, g * 8:g * 8 + nb_, :], in_=bkb[:, 0:nb_ * 128].rearrange("p (a b) -> p a b", a=nb_),
                    func=AF.Copy), reads=[self.pbank[bk]], writes=[mTb[s]])
            fw.dma(out=MTv[:, 0:i + 1, i * 128:(i + 1) * 128], in_=mTv[:, 0:i + 1, :], reads=[mTb[s]], writes=[MTb])
        self.release(m2)
        m3 = self.mark()
        A1 = self.sb(2432)
        a1b = Buf("A1")
        fw.op(pool, lambda e: e.iota(out=A1, pattern=[[-1, 2432]], base=384, channel_multiplier=1,
                                     allow_small_or_imprecise_dtypes=True), writes=[a1b])
        fw.op(pool, lambda e: e.tensor_scalar(out=A1, in0=A1, scalar1=0.0, scalar2=None, op0=ALU.min), reads=[a1b],
              writes=[a1b])
        mres = self.sb(16 * T, BF16)
        mrv = mres.rearrange("p (a b) -> p a b", a=16)
        mrb = Buf("maskres")
        fw.op(pool, lambda e: e.memset(mres, 0.0), writes=[mrb])
        for b in range(16):
            fw.dma(out=mrv[:, b, b * 128:T], in_=MT[b][:, b * 128:T], reads=[MTb], writes=[mrb])
        qh = [self.sb(T, BF16) for _ in range(2)]
        qhb = [Buf(), Buf()]
        oth = [self.sb(T, BF16) for _ in range(2)]
        othb = [Buf(), Buf()]
        NU = 3
        LGB = (0, 1, 7)
        tmp = [self.sb(512) for _ in range(NU)]
        tmpb = [Buf() for _ in range(NU)]
        pp = [self.sb(512, BF16) for _ in range(NU)]
        ppb = [Buf() for _ in range(NU)]
        pm = [self.sb(512, BF16) for _ in range(NU)]
        pmb = [Buf() for _ in range(NU)]
        rden = self.sb(4)
        rdb = Buf()
        on = self.sb(512, BF16)
        onb = Buf()
        units = [(hh, c, b) for hh in range(16) for c in range(4) for b in range(4 * (c + 1))]
        lg_done = [0]

        def emit_lg(upto):
            while lg_done[0] <= min(upto, len(units) - 1):
                idx = lg_done[0]
                hh_, c_, b_ = units[idx]
                s_ = hh_ % 2
                if c_ == 0 and b_ == 0:
                    fw.dma(out=qh[s_], in_=QT[hh_], reads=[QTb], writes=[qhb[s_]])
                lbk_ = LGB[idx % NU]
                fw.op(pe, lambda e, b_=b_, c_=c_, lbk_=lbk_, s_=s_: e.matmul(
                    self.bank(lbk_), lhsT=kT[:, b_ * 128:(b_ + 1) * 128], rhs=qh[s_][:, c_ * 512:(c_ + 1) * 512],
                    start=True, stop=True), reads=[resb, qhb[s_]], writes=[self.pbank[lbk_]])
                lg_done[0] += 1

        idx = -1
        for hh in range(16):
            s = hh % 2
            slope = 2.0 ** (-(hh + 1) / 2.0)
            for c in range(4):
                tbk = 6
                nb = 4 * (c + 1)
                for b in range(nb):
                    idx += 1
                    emit_lg(idx + 2)
                    u = idx % NU
                    lbk = LGB[u]
                    off = 512 * c - 128 * b + 384
                    fw.op(dve, lambda e, u=u, off=off, lbk=lbk, slope=slope: e.scalar_tensor_tensor(
                        out=tmp[u], in0=A1[:, off:off + 512], scalar=slope, in1=self.bank(lbk), op0=ALU.mult, op1=ALU.add),
                        reads=[a1b, self.pbank[lbk]], writes=[tmpb[u]])
                    fw.op(act, lambda e, u=u: e.activation(out=pp[u], in_=tmp[u], func=AF.Exp), reads=[tmpb[u]],
                          writes=[ppb[u]])
                    fw.op(pool, lambda e, u=u, b=b, c=c: e.tensor_tensor(out=pm[u], in0=pp[u],
                                                                        in1=mrv[:, b, c * 512:(c + 1) * 512], op=ALU.mult),
                          reads=[ppb[u], mrb], writes=[pmb[u]])
                    for sub in range(4):
                        tt_ = 4 * c + sub
                        if b > tt_:
                            continue
                        obk = 2 + sub
                        fw.op(pe, lambda e, u=u, sub=sub, b=b, tt_=tt_, obk=obk: e.matmul(
                            self.bank(obk)[:, 0:129], lhsT=pm[u][:, sub * 128:(sub + 1) * 128],
                            rhs=vtv[:, b, 0:129], start=(b == 0), stop=(b == tt_)),
                            reads=[pmb[u], resb], writes=[self.pbank[obk]])
                for sub in range(4):
                    obk = 2 + sub
                    fw.op(dve, lambda e, obk=obk, sub=sub: e.reciprocal(out=rden[:, sub:sub + 1], in_=self.bank(obk)[:, 128:129]),
                          reads=[self.pbank[obk]], writes=[rdb])
                    fw.op(dve, lambda e, sub=sub, obk=obk: e.tensor_scalar(
                        out=on[:, sub * 128:(sub + 1) * 128], in0=self.bank(obk)[:, 0:128],
                        scalar1=rden[:, sub:sub + 1], scalar2=None, op0=ALU.mult),
                        reads=[self.pbank[obk], rdb], writes=[onb])
                tbb = self.bank(tbk, BF16)
                for sub in range(4):
                    fw.op(pe, lambda e, sub=sub, tbb=tbb: e.transpose(out=tbb[:, sub * 128:(sub + 1) * 128],
                                                                      in_=on[:, sub * 128:(sub + 1) * 128], identity=self.identb),
                          reads=[onb, self.cb], writes=[self.pbank[tbk]])
                fw.op(act, lambda e, s=s, c=c, tbb=tbb: e.activation(out=oth[s][:, c * 512:(c + 1) * 512], in_=tbb[:, 0:512],
                                                                      func=AF.Copy), reads=[self.pbank[tbk]], writes=[othb[s]])
            fw.dma(out=OT[hh], in_=oth[s], reads=[othb[s]], writes=[OTb])
        self.release(m3)
        self.release(m0)
        self.outproj_ln(OT, OTb, W["dsa_w_out"][0], src, src_buf, W["ln_g"][L, 0], W["ln_b"][L, 0], dst, dst_buf)

    def gdn(self, li, L, src, src_buf, W, dst, dst_buf):
        fw = self.fw
        dve, act, pool, pe = fw.dve, fw.act, fw.pool, fw.pe
        OT, OTb = self.dram["OT"], self.dbuf["OT"]
        w_in = W["gdn_w_in"][li]
        m0 = self.mark()
        A128 = lambda: self.sb(128)
        U2, B2, NEGM4 = A128(), A128(), self.sb(512)
        gcb = Buf("gdnconst")
        fw.op(pool, lambda e: e.memset(U2, 1.0), writes=[gcb])
        fw.op(pool, lambda e: e.affine_select(out=U2, in_=U2, pattern=[[1, 128]], compare_op=ALU.is_ge, fill=0.0,
                                              base=0, channel_multiplier=-1), reads=[gcb], writes=[gcb])
        fw.op(pool, lambda e: e.memset(U2[0:64, 64:128], 0.0), reads=[gcb], writes=[gcb])
        fw.op(pool, lambda e: e.memset(B2, 0.0), writes=[gcb])
        fw.op(pool, lambda e: e.memset(B2[0:64, 0:64], 1.0), reads=[gcb], writes=[gcb])
        fw.op(pool, lambda e: e.memset(B2[64:128, 64:128], 1.0), reads=[gcb], writes=[gcb])
        N4 = NEGM4.rearrange("p (u i) -> p u i", u=4)
        fw.op(pool, lambda e: e.memset(NEGM4, 0.0), writes=[gcb])
        fw.op(pool, lambda e: e.affine_select(out=N4, in_=N4, pattern=[[0, 4], [1, 128]], compare_op=ALU.is_ge, fill=NEG,
                                              base=0, channel_multiplier=-1), reads=[gcb], writes=[gcb])
        fw.op(pool, lambda e: e.memset(N4[0:64, :, 64:128], NEG), reads=[gcb], writes=[gcb])
        id4 = self.ident.unsqueeze(1).to_broadcast([128, 4, 128])
        ABt = self.sb(16 * 32)
        ABv = ABt.rearrange("p (a b) -> p a b", a=16)
        GT, GC, EGC, EKD, NGC, BT, NB = [self.sb(256) for _ in range(7)]
        v16 = lambda a: a.rearrange("p (a b) -> p a b", a=16)
        CW = self.sb(48 * 4)
        CWv = CW.rearrange("p (c j) -> p c j", j=4)
        NGrow = self.sb(128)
        gb = Buf("gdn_g")
        self.load_row(NGrow, W["gdn_norm_g"][li], gb, 128)
        arow = self.sb(32)
        self.load_row(arow[:, 0:16], W["gdn_a_log"][li], gb, 16)
        self.load_row(arow[:, 16:32], W["gdn_dt_bias"][li], gb, 16)
        cwrow = self.sb(6144)
        fw.dma(out=cwrow[0:4, :], in_=W["gdn_conv_w"][li], writes=[gb])
        for c in range(48):
            fw.op(pe, lambda e, c=c: e.transpose(out=self.bank(4)[:, c * 4:(c + 1) * 4], in_=cwrow[0:4, c * 128:(c + 1) * 128],
                                                 identity=self.ident[0:4, 0:4]), reads=[gb, self.cb], writes=[self.pbank[4]])
        fw.op(dve, lambda e: e.tensor_copy(out=CW, in_=self.bank(4)[:, 0:192]), reads=[self.pbank[4]], writes=[gb])
        self.off -= 6144 + 0
        fw.barrier()
        big = self.sb(16 * T, BF16)
        bigv = big.rearrange("p (a b) -> p a b", a=16)
        xTb = Buf("xT")
        self.build_xT(src, src_buf, bigv, xTb)
        mg = self.mark()
        wab = self.sb(16 * 32, BF16)
        wabv = wab.rearrange("p (a b) -> p a b", a=16)
        wabb = Buf()
        fw.dma(out=wabv, in_=w_in[:, 8192:8224].rearrange("(kc p) f -> p kc f", p=128), writes=[wabb], q=pool)
        for i in range(NT):
            bk = 4 + i % 4
            for kc in range(16):
                fw.op(pe, lambda e, i=i, kc=kc, bk=bk: e.matmul(self.bank(bk)[:, 0:32], lhsT=bigv[:, kc, i * 128:(i + 1) * 128],
                                                               rhs=wabv[:, kc, :], start=(kc == 0), stop=(kc == 15)),
                      reads=[xTb, wabb], writes=[self.pbank[bk]])
            fw.op(act, lambda e, i=i, bk=bk: e.activation(out=ABv[:, i, :], in_=self.bank(bk)[:, 0:32], func=AF.Copy),
                  reads=[self.pbank[bk]], writes=[gb])
        t1, t2, t3 = self.sb(256), self.sb(256), self.sb(256)
        nea = self.sb(16)
        dtb_b = arow[:, 16:32].unsqueeze(1).to_broadcast([128, 16, 16])
        fw.op(dve, lambda e: e.tensor_tensor(out=v16(t1), in0=ABv[:, :, 0:16], in1=dtb_b, op=ALU.add), reads=[gb], writes=[gb])
        fw.op(dve, lambda e: e.tensor_scalar(out=t2, in0=t1, scalar1=-1.0, scalar2=None, op0=ALU.mult), reads=[gb], writes=[gb])
        fw.op(dve, lambda e: e.tensor_tensor(out=t2, in0=t2, in1=t1, op=ALU.max), reads=[gb], writes=[gb])
        fw.op(act, lambda e: e.activation(out=t2, in_=t2, func=AF.Exp, scale=-1.0), reads=[gb], writes=[gb])
        fw.op(dve, lambda e: e.tensor_scalar(out=t2, in0=t2, scalar1=1.0, scalar2=None, op0=ALU.add), reads=[gb], writes=[gb])
        fw.op(act, lambda e: e.activation(out=t2, in_=t2, func=AF.Ln), reads=[gb], writes=[gb])
        fw.op(dve, lambda e: e.scalar_tensor_tensor(out=t3, in0=t1, scalar=0.0, in1=t2, op0=ALU.max, op1=ALU.add),
              reads=[gb], writes=[gb])
        fw.op(act, lambda e: e.activation(out=nea, in_=arow[:, 0:16], func=AF.Exp), reads=[gb], writes=[gb])
        fw.op(dve, lambda e: e.tensor_scalar(out=nea, in0=nea, scalar1=-1.0, scalar2=None, op0=ALU.mult), reads=[gb], writes=[gb])
        fw.op(dve, lambda e: e.tensor_tensor(out=v16(GT), in0=v16(t3), in1=nea.unsqueeze(1).to_broadcast([128, 16, 16]),
                                             op=ALU.mult), reads=[gb], writes=[gb])
        fw.op(act, lambda e: e.activation(out=v16(BT), in_=ABv[:, :, 16:32], func=AF.Sigmoid), reads=[gb], writes=[gb])
        fw.op(dve, lambda e: e.tensor_scalar(out=NB, in0=BT, scalar1=-1.0, scalar2=None, op0=ALU.mult), reads=[gb], writes=[gb])
        fw.op(pe, lambda e: e.matmul(self.bank(4)[:, 0:256], lhsT=U2, rhs=GT, start=True, stop=True), reads=[gb, gcb],
              writes=[self.pbank[4]])
        fw.op(pe, lambda e: e.matmul(self.bank(5)[:, 0:256], lhsT=B2, rhs=GT, start=True, stop=True), reads=[gb, gcb],
              writes=[self.pbank[5]])
        fw.op(dve, lambda e: e.tensor_copy(out=GC, in_=self.bank(4)[:, 0:256]), reads=[self.pbank[4]], writes=[gb])
        fw.op(act, lambda e: e.activation(out=EGC, in_=self.bank(4)[:, 0:256], func=AF.Exp), reads=[self.pbank[4]], writes=[gb])
        fw.op(dve, lambda e: e.tensor_tensor(out=t1, in0=self.bank(5)[:, 0:256], in1=GC, op=ALU.subtract),
              reads=[self.pbank[5], gb], writes=[gb])
        fw.op(act, lambda e: e.activation(out=EKD, in_=t1, func=AF.Exp), reads=[gb], writes=[gb])
        fw.op(dve, lambda e: e.tensor_scalar(out=NGC, in0=GC, scalar1=-1.0, scalar2=None, op0=ALU.mult), reads=[gb], writes=[gb])
        self.release(mg)
        GCv, EGCv, EKDv, NGCv, BTv, NBv = [v16(a) for a in (GC, EGC, EKD, NGC, BT, NB)]
        wt = [self.sb(16 * 128, BF16) for _ in range(2)]
        wtb = [Buf(), Buf()]
        qT, kT, vT = [self.sb(T).bitcast(BF16)[:, 0:T] for _ in range(3)]
        qkvb = Buf("qkv")
        ktok_f, vtok_f, otok = self.sb(T), self.sb(T), self.sb(T)
        ktok, vtok = ktok_f.bitcast(BF16)[:, 0:T], vtok_f.bitcast(BF16)[:, 0:T]
        ktv, vtv, otv = [a.rearrange("p (a b) -> p a b", a=16) for a in (ktok, vtok, otok)]
        tokb = Buf("tok")
        otb = Buf("otok")
        regA = self.sb(8192)
        raw = regA[:, 0:2056]
        cacc = regA[:, 2056:2056 + 2048]
        tmpq = regA[:, 4104:4104 + 2048]
        rb = Buf("raw")
        names = ["DG4", "EGR4", "Dt4", "Mt4", "Nn4", "Qa", "Qta", "Qb", "Qtb", "X4", "AT4", "KG4", "KD4", "WT4", "BU4", "QD4"]
        F32T = ("DG4", "EGR4", "Dt4", "BU4")
        QTL = {n_: (regA[:, i_ * 512:(i_ + 1) * 512] if n_ in F32T else regA[:, i_ * 512:(i_ + 1) * 512].bitcast(BF16)[:, 0:512])
               for i_, n_ in enumerate(names)}
        QB = {n_: Buf(n_) for n_ in names}
        q3 = lambda a: a.rearrange("p (u i) -> p u i", u=4)
        VN = self.sb(128, BF16)
        vnb = Buf("VN")
        S = self.sb(128)
        Sb = Buf("S")
        Sbf = self.sb(128, BF16)
        Sbfb = Buf("Sbf")
        oth = [self.sb(T, BF16) for _ in range(2)]
        othb = [Buf(), Buf()]
        sm = self.sb(64)
        smb = Buf()
        fw.op(dve, lambda e: e.memset(raw[:, 0:3], 0.0), writes=[rb])
        nbk = [0]

        def nb_():
            nbk[0] += 1
            return 4 + nbk[0] % 4

        for h in range(16):
            fw.barrier()
            for kind, col0, dstT in (("q", 0, qT), ("k", 2048, kT), ("v", 4096, vT)):
                s = nbk[0] % 2
                nbk[0] += 1
                self.proj_fm(bigv, xTb, w_in[:, col0 + h * 128: col0 + (h + 1) * 128], wt[s], wtb[s])
                for tc in range(4):
                    dstp = raw[:, 3 + tc * 512: 3 + (tc + 1) * 512]
                    if tc % 2 == 0:
                        fw.op(dve, lambda e, dstp=dstp, tc=tc: e.tensor_copy(out=dstp, in_=self.bank(tc)),
                              reads=[self.pbank[tc]], writes=[rb])
                    else:
                        fw.op(act, lambda e, dstp=dstp, tc=tc: e.activation(out=dstp, in_=self.bank(tc), func=AF.Copy),
                              reads=[self.pbank[tc]], writes=[rb])
                ch = (col0 // 128) + h
                fw.op(dve, lambda e, ch=ch: e.tensor_scalar(out=cacc, in0=raw[:, 0:T], scalar1=CWv[:, ch, 0:1], scalar2=None,
                                                            op0=ALU.mult), reads=[rb, gb], writes=[rb])
                for j in range(1, 4):
                    fw.op(dve, lambda e, ch=ch, j=j: e.scalar_tensor_tensor(
                        out=cacc, in0=raw[:, j:j + T], scalar=CWv[:, ch, j:j + 1], in1=cacc, op0=ALU.mult, op1=ALU.add),
                        reads=[rb, gb], writes=[rb])
                if kind == "v":
                    fw.op(act, lambda e: e.activation(out=vT, in_=cacc, func=AF.Silu), reads=[rb], writes=[qkvb])
                    continue
                fw.op(act, lambda e: e.activation(out=tmpq, in_=cacc, func=AF.Silu), reads=[rb], writes=[rb])
                sq16 = raw[:, 8:8 + T // 2].bitcast(BF16)
                fw.op(act, lambda e: e.activation(out=sq16, in_=tmpq, func=AF.Square), reads=[rb], writes=[rb])
                for tc in range(4):
                    fw.op(pe, lambda e, tc=tc: e.matmul(self.bank(tc), lhsT=self.onesb, rhs=sq16[:, tc * 512:(tc + 1) * 512],
                                                        start=True, stop=True), reads=[rb, self.cb], writes=[self.pbank[tc]])
                    cs_ = cacc[:, tc * 512:(tc + 1) * 512]
                    fw.op(dve, lambda e, tc=tc, cs_=cs_: e.tensor_scalar(out=cs_, in0=self.bank(tc), scalar1=RMS_EPS,
                                                                         scalar2=None, op0=ALU.add),
                          reads=[self.pbank[tc], rb], writes=[rb])
                fw.op(act, lambda e: e.activation(out=cacc, in_=cacc, func=AF.Sqrt), reads=[rb], writes=[rb])
                fw.op(dve, lambda e: e.reciprocal(out=cacc, in_=cacc), reads=[rb], writes=[rb])
                sc_ = (128.0 ** -0.5) if kind == "q" else 1.0
                fw.op(dve, lambda e, dstT=dstT, sc_=sc_: e.scalar_tensor_tensor(out=dstT, in0=tmpq, scalar=sc_, in1=cacc,
                                                                                op0=ALU.mult, op1=ALU.mult),
                      reads=[rb], writes=[qkvb])
            for srcT, dv_ in ((kT, ktv), (vT, vtv)):
                for g in range(4):
                    bk = nb_()
                    bkb = self.bank(bk, BF16)
                    for j in range(4):
                        P_ = g * 4 + j
                        fw.op(pe, lambda e, srcT=srcT, P_=P_, j=j, bkb=bkb: e.transpose(
                            out=bkb[:, j * 128:(j + 1) * 128], in_=srcT[:, P_ * 128:(P_ + 1) * 128],
                            identity=self.identb), reads=[qkvb, self.cb], writes=[self.pbank[bk]])
                    fw.op(act, lambda e, dv_=dv_, g=g, bkb=bkb: e.activation(
                        out=dv_[:, g * 4:(g + 1) * 4, :], in_=bkb[:, 0:512].rearrange("p (a b) -> p a b", a=4), func=AF.Copy),
                        reads=[self.pbank[bk]], writes=[tokb])
            fw.barrier()
            fw.op(dve, lambda e: e.memset(S, 0.0), writes=[Sb])
            fw.op(dve, lambda e: e.memset(Sbf, 0.0), writes=[Sbfb])
            for Q in range(4):
                cols4 = slice(Q * 512, (Q + 1) * 512)
                P0 = Q * 4
                bc4 = lambda a: a[:, P0:P0 + 4, h].unsqueeze(2).to_broadcast([128, 4, 128])
                L_ = QTL
                fw.op(pool, lambda e: e.tensor_tensor(out=q3(L_["DG4"]), in0=id4, in1=bc4(GCv), op=ALU.mult),
                      reads=[gb, self.cb], writes=[QB["DG4"]])
                bA, bB = nb_(), nb_()
                fw.op(pe, lambda e: e.matmul(self.bank(bA), lhsT=self.ones, rhs=L_["DG4"], start=True, stop=True),
                      reads=[QB["DG4"], self.cb], writes=[self.pbank[bA]])
                fw.op(pe, lambda e: e.matmul(self.bank(bB), lhsT=self.ones, rhs=L_["DG4"], start=True, stop=False),
                      reads=[QB["DG4"], self.cb], writes=[self.pbank[bB]])
                fw.op(pe, lambda e: e.matmul(self.bank(bB), lhsT=self.ident, rhs=NEGM4, start=False, stop=True),
                      reads=[gcb, self.cb], writes=[self.pbank[bB]])
                fw.op(act, lambda e: e.activation(out=L_["EGR4"], in_=self.bank(bA), func=AF.Exp), reads=[self.pbank[bA]],
                      writes=[QB["EGR4"]])
                for u in range(4):
                    fw.op(act, lambda e, u=u: e.activation(out=L_["Dt4"][:, u * 128:(u + 1) * 128],
                                                           in_=self.bank(bB)[:, u * 128:(u + 1) * 128], func=AF.Exp,
                                                           bias=NGCv[:, P0 + u, h:h + 1], scale=1.0),
                          reads=[self.pbank[bB], gb], writes=[QB["Dt4"]])
                bK, bQ = nb_(), nb_()
                for u in range(4):
                    cu = slice((P0 + u) * 128, (P0 + u + 1) * 128)
                    fw.op(pe, lambda e, u=u, cu=cu: e.matmul(self.bank(bK)[:, u * 128:(u + 1) * 128], lhsT=kT[:, cu], rhs=kT[:, cu],
                                                             start=True, stop=True), reads=[qkvb], writes=[self.pbank[bK]])
                for u in range(4):
                    cu = slice((P0 + u) * 128, (P0 + u + 1) * 128)
                    fw.op(pe, lambda e, u=u, cu=cu: e.matmul(self.bank(bQ)[:, u * 128:(u + 1) * 128], lhsT=kT[:, cu], rhs=qT[:, cu],
                                                             start=True, stop=True), reads=[qkvb], writes=[self.pbank[bQ]])
                fw.op(dve, lambda e: e.tensor_tensor(out=q3(L_["Mt4"]), in0=self.bank(bK).rearrange("p (u i) -> p u i", u=4),
                                                     in1=bc4(BTv), op=ALU.mult), reads=[self.pbank[bK], gb], writes=[QB["Mt4"]])
                fw.op(dve, lambda e: e.tensor_tensor(out=L_["Mt4"], in0=L_["Mt4"], in1=L_["Dt4"], op=ALU.mult),
                      reads=[QB["Dt4"], QB["Mt4"]], writes=[QB["Mt4"]])
                fw.op(pool, lambda e: e.affine_select(out=q3(L_["Mt4"]), in_=q3(L_["Mt4"]), pattern=[[0, 4], [1, 128]],
                                                      compare_op=ALU.not_equal, fill=0.0, base=0, channel_multiplier=-1),
                      reads=[QB["Mt4"]], writes=[QB["Mt4"]])
                fw.op(dve, lambda e: e.tensor_tensor(out=L_["AT4"], in0=self.bank(bQ), in1=L_["Dt4"], op=ALU.mult),
                      reads=[self.pbank[bQ], QB["Dt4"]], writes=[QB["AT4"]])
                bN = nb_()
                bNb = self.bank(bN, BF16)
                for u in range(4):
                    fw.op(pe, lambda e, u=u: e.transpose(out=bNb[:, u * 128:(u + 1) * 128],
                                                         in_=L_["Mt4"][:, u * 128:(u + 1) * 128], identity=self.identb),
                          reads=[QB["Mt4"], self.cb], writes=[self.pbank[bN]])
                fw.op(act, lambda e: e.activation(out=L_["Nn4"], in_=bNb[:, 0:512], func=AF.Copy), reads=[self.pbank[bN]],
                      writes=[QB["Nn4"]])
                fw.op(pool, lambda e: e.tensor_tensor(out=q3(L_["X4"]), in0=id4, in1=q3(L_["Mt4"]), op=ALU.subtract),
                      reads=[QB["Mt4"], self.cb], writes=[QB["X4"]])
                Qn, Qtn = "Mt4", "Nn4"
                pp_ = [("Qa", "Qta"), ("Qb", "Qtb")]
                for lvl in range(1, 6):
                    Qo, Qto = pp_[lvl % 2]
                    bt = nb_()
                    for u in range(4):
                        us = slice(u * 128, (u + 1) * 128)
                        fw.op(pe, lambda e, us=us, Qn=Qn, Qtn=Qtn, bt=bt: e.matmul(self.bank(bt)[:, us], lhsT=L_[Qn][:, us],
                                                                                   rhs=L_[Qtn][:, us], start=True, stop=True),
                              reads=[QB[Qn], QB[Qtn]], writes=[self.pbank[bt]])
                    fw.op(act, lambda e, Qto=Qto, bt=bt: e.activation(out=L_[Qto], in_=self.bank(bt), func=AF.Copy),
                          reads=[self.pbank[bt]], writes=[QB[Qto]])
                    if lvl < 5:
                        bq = nb_()
                        for u in range(4):
                            us = slice(u * 128, (u + 1) * 128)
                            fw.op(pe, lambda e, us=us, Qn=Qn, Qtn=Qtn, bq=bq: e.matmul(self.bank(bq)[:, us], lhsT=L_[Qtn][:, us],
                                                                                       rhs=L_[Qn][:, us], start=True, stop=True),
                                  reads=[QB[Qn], QB[Qtn]], writes=[self.pbank[bq]])
                        fw.op(dve, lambda e, Qo=Qo, bq=bq: e.tensor_copy(out=L_[Qo], in_=self.bank(bq)),
                              reads=[self.pbank[bq]], writes=[QB[Qo]])
                    bx = nb_()
                    for u in range(4):
                        us = slice(u * 128, (u + 1) * 128)
                        fw.op(pe, lambda e, us=us, Qto=Qto, bx=bx: e.matmul(self.bank(bx)[:, us], lhsT=L_[Qto][:, us],
                                                                            rhs=L_["X4"][:, us], start=True, stop=True),
                              reads=[QB[Qto], QB["X4"]], writes=[self.pbank[bx]])
                    fw.op(dve, lambda e, bx=bx: e.tensor_tensor(out=L_["X4"], in0=L_["X4"], in1=self.bank(bx), op=ALU.add),
                          reads=[self.pbank[bx], QB["X4"]], writes=[QB["X4"]])
                    Qn, Qtn = Qo, Qto
                fw.op(pool, lambda e: e.tensor_tensor(out=q3(L_["KG4"]), in0=ktv[:, P0:P0 + 4, :], in1=bc4(EGCv), op=ALU.mult),
                      reads=[tokb, gb], writes=[QB["KG4"]])
                fw.op(pool, lambda e: e.tensor_tensor(out=q3(L_["KD4"]), in0=ktv[:, P0:P0 + 4, :], in1=bc4(EKDv), op=ALU.mult),
                      reads=[tokb, gb], writes=[QB["KD4"]])
                bW, bU = nb_(), nb_()
                for u in range(4):
                    us = slice(u * 128, (u + 1) * 128)
                    fw.op(pe, lambda e, us=us: e.matmul(self.bank(bW)[:, us], lhsT=L_["KG4"][:, us], rhs=L_["X4"][:, us],
                                                        start=True, stop=True), reads=[QB["KG4"], QB["X4"]], writes=[self.pbank[bW]])
                for u in range(4):
                    us = slice(u * 128, (u + 1) * 128)
                    fw.op(pe, lambda e, us=us, u=u: e.matmul(self.bank(bU)[:, us], lhsT=L_["X4"][:, us], rhs=vtv[:, P0 + u, :],
                                                             start=True, stop=True), reads=[QB["X4"], tokb], writes=[self.pbank[bU]])
                fw.op(act, lambda e: e.activation(out=L_["WT4"], in_=self.bank(bW), func=AF.Copy), reads=[self.pbank[bW]],
                      writes=[QB["WT4"]])
                fw.op(dve, lambda e: e.tensor_tensor(out=q3(L_["BU4"]), in0=self.bank(bU).rearrange("p (u i) -> p u i", u=4),
                                                     in1=bc4(BTv), op=ALU.mult), reads=[self.pbank[bU], gb], writes=[QB["BU4"]])
                fw.op(dve, lambda e: e.tensor_tensor(out=L_["QD4"], in0=qT[:, cols4], in1=L_["EGR4"], op=ALU.mult),
                      reads=[qkvb, QB["EGR4"]], writes=[QB["QD4"]])
                for u in range(4):
                    us = slice(u * 128, (u + 1) * 128)
                    P_ = P0 + u
                    for c in range(2):
                        rows = slice(c * 64, (c + 1) * 64)
                        bw = nb_()
                        fw.op(pe, lambda e, us=us, bw=bw: e.matmul(self.bank(bw)[:, 0:128], lhsT=L_["WT4"][:, us], rhs=Sbf,
                                                                   start=True, stop=True), reads=[QB["WT4"], Sbfb],
                              writes=[self.pbank[bw]])
                        fw.op(dve, lambda e, rows=rows, bw=bw, P_=P_, us=us: e.scalar_tensor_tensor(
                            out=VN[rows, :], in0=self.bank(bw)[rows, 0:128], scalar=NBv[rows, P_, h:h + 1],
                            in1=L_["BU4"][rows, us], op0=ALU.mult, op1=ALU.add),
                            reads=[self.pbank[bw], gb, QB["BU4"]], writes=[vnb])
                        bo = nb_()
                        fw.op(pe, lambda e, us=us, bo=bo: e.matmul(self.bank(bo)[:, 0:128], lhsT=L_["QD4"][:, us], rhs=Sbf,
                                                                   start=True, stop=False), reads=[QB["QD4"], Sbfb],
                              writes=[self.pbank[bo]])
                        fw.op(pe, lambda e, us=us, bo=bo, rows=rows: e.matmul(self.bank(bo)[:, 0:128], lhsT=L_["AT4"][rows, us],
                                                                              rhs=VN[rows, :], start=False, stop=True),
                              reads=[QB["AT4"], vnb], writes=[self.pbank[bo]])
                        fw.op(act, lambda e, rows=rows, bo=bo, P_=P_: e.activation(out=otv[rows, P_, :], in_=self.bank(bo)[rows, 0:128],
                                                                                   func=AF.Copy), reads=[self.pbank[bo]], writes=[otb])
                        bs = nb_()
                        fw.op(pe, lambda e, us=us, bs=bs, rows=rows: e.matmul(self.bank(bs)[:, 0:128], lhsT=L_["KD4"][rows, us],
                                                                              rhs=VN[rows, :], start=True, stop=True),
                              reads=[QB["KD4"], vnb], writes=[self.pbank[bs]])
                        cdc = u * 128 + c * 64 + 63
                        fw.op(dve, lambda e, bs=bs, cdc=cdc: e.scalar_tensor_tensor(
                            out=S, in0=S, scalar=L_["EGR4"][:, cdc:cdc + 1], in1=self.bank(bs)[:, 0:128], op0=ALU.mult, op1=ALU.add),
                            reads=[self.pbank[bs], QB["EGR4"], Sb], writes=[Sb])
                        fw.op(act, lambda e: e.activation(out=Sbf, in_=S, func=AF.Copy), reads=[Sb], writes=[Sbfb])
            fw.barrier()
            zs = ktok_f
            zsv = zs.rearrange("p (a b) -> p a b", a=16)
            zb = Buf("zs")
            s = nbk[0] % 2
            nbk[0] += 1
            wzv = wt[s].rearrange("p (a b) -> p a b", a=16)
            fw.dma(out=wzv, in_=w_in[:, 6144 + h * 128: 6144 + (h + 1) * 128].rearrange("(kc p) f -> p kc f", p=128),
                   writes=[wtb[s]], q=pool)
            for g in range(4):
                bk = g
                for j in range(4):
                    i = g * 4 + j
                    for kc in range(16):
                        fw.op(pe, lambda e, i=i, j=j, kc=kc, bk=bk: e.matmul(
                            self.bank(bk)[:, j * 128:(j + 1) * 128], lhsT=bigv[:, kc, i * 128:(i + 1) * 128], rhs=wzv[:, kc, :],
                            start=(kc == 0), stop=(kc == 15)), reads=[xTb, wtb[s]], writes=[self.pbank[bk]])
                fw.op(act, lambda e, g=g, bk=bk: e.activation(out=zsv[:, g * 4:(g + 1) * 4, :],
                                                              in_=self.bank(bk).rearrange("p (a b) -> p a b", a=4), func=AF.Silu),
                      reads=[self.pbank[bk]], writes=[zb])
            sq = regA[:, 0:T]
            sqb = Buf("sq")
            fw.op(pool, lambda e: e.tensor_tensor(out=sq, in0=otok, in1=otok, op=ALU.mult), reads=[otb], writes=[sqb])
            ms = sm[:, 0:16]
            fw.op(dve, lambda e: e.tensor_reduce(out=ms, in_=sq.rearrange("p (a b) -> p a b", a=16), axis=AX.X, op=ALU.add),
                  reads=[sqb], writes=[smb])
            fw.op(dve, lambda e: e.tensor_scalar(out=ms, in0=ms, scalar1=1.0 / 128.0, scalar2=RMS_EPS, op0=ALU.mult, op1=ALU.add),
                  reads=[smb], writes=[smb])
            fw.op(act, lambda e: e.activation(out=ms, in_=ms, func=AF.Sqrt), reads=[smb], writes=[smb])
            fw.op(dve, lambda e: e.reciprocal(out=ms, in_=ms), reads=[smb], writes=[smb])
            fw.op(dve, lambda e: e.tensor_tensor(out=otv, in0=otv, in1=ms.unsqueeze(2).to_broadcast([128, 16, 128]), op=ALU.mult),
                  reads=[smb, otb], writes=[otb])
            fw.op(pool, lambda e: e.tensor_tensor(out=otv, in0=otv, in1=NGrow.unsqueeze(1).to_broadcast([128, 16, 128]),
                                                  op=ALU.mult), reads=[gb, otb], writes=[otb])
            fw.op(dve, lambda e: e.tensor_tensor(out=otok, in0=otok, in1=zs, op=ALU.mult), reads=[zb, otb], writes=[otb])
            s2 = h % 2
            for g in range(4):
                bk = 4 + g
                for j in range(4):
                    P_ = g * 4 + j
                    fw.op(pe, lambda e, P_=P_, j=j, bk=bk: e.transpose(out=self.bank(bk)[:, j * 128:(j + 1) * 128], in_=otv[:, P_, :],
                                                                      identity=self.ident), reads=[otb, self.cb], writes=[self.pbank[bk]])
                fw.op(act, lambda e, g=g, bk=bk, s2=s2: e.activation(out=oth[s2][:, g * 512:(g + 1) * 512], in_=self.bank(bk),
                                                                    func=AF.Copy), reads=[self.pbank[bk]], writes=[othb[s2]])
            fw.dma(out=OT[h], in_=oth[s2], reads=[othb[s2]], writes=[OTb])
        self.release(m0)
        self.outproj_ln(OT, OTb, W["gdn_w_out"][li], src, src_buf, W["ln_g"][L, 0], W["ln_b"][L, 0], dst, dst_buf)

    def init_yg(self):
        fw = self.fw
        m = self.mark()
        z = self.sb(D)
        zb = Buf()
        fw.op(fw.dve, lambda e: e.memset(z, 0.0), writes=[zb])
        fw.dma(out=self.dram["YG"][NSLOT:NSLOT + 128, :], in_=z, reads=[zb], writes=[self.dbuf["YG"]])
        self.release(m)


WEIGHT_SPECS = [
    ("ln_g", (4, 2, 2048)), ("ln_b", (4, 2, 2048)), ("moe_rg_w", (4, 2048, 4)), ("moe_rg_b", (4, 4)),
    ("moe_re_w", (4, 2048, 32)), ("moe_re_b", (4, 32)), ("moe_w_gate", (4, 32, 2048, 512)),
    ("moe_w_up", (4, 32, 2048, 512)), ("moe_w_down", (4, 32, 512, 2048)), ("gdn_w_in", (2, 2048, 8224)),
    ("gdn_conv_w", (2, 4, 6144)), ("gdn_a_log", (2, 16)), ("gdn_dt_bias", (2, 16)), ("gdn_norm_g", (2, 128)),
    ("gdn_w_out", (2, 2048, 2048)), ("ssm_w_in", (1, 2048, 2048)), ("ssm_b_re", (1, 128, 64, 16)),
    ("ssm_b_im", (1, 128, 64, 16)), ("ssm_c_re", (1, 128, 16, 64)), ("ssm_c_im", (1, 128, 16, 64)),
    ("ssm_a_re", (1, 128, 64)), ("ssm_a_im", (1, 128, 64)), ("ssm_log_dt", (1, 128)), ("ssm_d", (1, 2048)),
    ("ssm_w_glu", (1, 2048, 2048)), ("ssm_b_glu", (1, 2048)), ("ssm_w_out", (1, 2048, 2048)),
    ("dsa_w_in", (1, 2048, 4496)), ("dsa_w_out", (1, 2048, 2048)),
]


def build(stages, ext_in=(), ext_out=(), weights=None):
    nc = bass.Bass("TRN2", target_bir_lowering=False)
    st = contextlib.ExitStack()
    with st:
        k = K(nc, st, set(ext_in), set(ext_out))
        fw = k.fw
        used = weights if weights is not None else [n for n, _ in WEIGHT_SPECS]
        W = {}
        for n, shp in WEIGHT_SPECS:
            if n in used:
                W[n] = nc.dram_tensor(n, list(shp), F32, kind="ExternalInput").ap()
        k.W = W
        names = set()
        for stg in stages:
            names.update(stg[2:])
        if "x" in names:
            k.dram["x"] = nc.dram_tensor("x", [T, D], F32, kind="ExternalInput").ap()
            k.dbuf["x"] = Buf("x")
        k.dram["out"] = nc.dram_tensor("out", [T, D], F32, kind="ExternalOutput").ap()
        k.dbuf["out"] = Buf("out")
        k.dt("XA", [T, D], F32)
        k.dt("XM", [T, D], F32)
        k.dt("OT", [16, 128, T], BF16)
        k.dt("XG", [NSLOT + 128, D], BF16)
        k.dt("YG", [NSLOT + 128, D], F32)
        k.dt("U", [16, 128, T], F32)
        k.dt("YA", [16, 128, T], BF16)
        k.dt("QT", [16, 128, T], BF16)
        k.dt("QI", [16, 128, T], BF16)
        k.dt("MASKT", [16, 128, T], BF16)
        k.init_yg()
        for stg in stages:
            kind, L, src, dst = stg
            S, Sb, Dd, Db = k.dram[src], k.dbuf[src], k.dram[dst], k.dbuf[dst]
            if kind == "moe":
                k.moe(L, S, Sb, W, Dd, Db)
            elif kind == "gdn":
                k.gdn(L // 3, L, S, Sb, W, Dd, Db)
            elif kind == "s5":
                k.s5(L, S, Sb, W, Dd, Db)
            elif kind == "dsa":
                k.dsa(L, S, Sb, W, Dd, Db)
            else:
                raise ValueError(kind)
        fw.finish()
        k.stats = {e.name: e.n_instr for e in fw.engs}
    return nc, k


_MIXERS = ("gdn", "s5", "dsa")


def _stages():
    st = []
    for L in range(DEPTH):
        src = "x" if L == 0 else "XA"
        st.append((_MIXERS[L % 3], L, src, "XM"))
        st.append(("moe", L, "XM", "out" if L == DEPTH - 1 else "XA"))
    return st


def kernel(**inputs):
    x = np.ascontiguousarray(np.asarray(inputs["x"], dtype=np.float32))
    nc, _k = build(_stages())
    wts = {n: np.ascontiguousarray(np.asarray(inputs[n], dtype=np.float32)) for n, _ in WEIGHT_SPECS}
    n_cores = 8
    in_maps = []
    for c in range(n_cores):
        m = dict(wts)
        m["x"] = x[c]
        in_maps.append(m)
    res = run_bass_kernel_spmd(nc, in_maps, core_ids=list(range(n_cores)))
    return np.stack([np.asarray(r["out"], dtype=np.float32) for r in res.results], axis=0)
```

```python
import contextlib
import math
import numpy as np
import concourse.bass as bass
import concourse.mybir as mybir
from concourse.bass_utils import run_bass_kernel_spmd

F32 = mybir.dt.float32
BF16 = mybir.dt.bfloat16
I32 = mybir.dt.int32
AF = mybir.ActivationFunctionType
ALU = mybir.AluOpType
AX = mybir.AxisListType

T = 2048
D = 2048
NT = 16
DEPTH = 4
DN_ALPHA = (2.0 * DEPTH) ** 0.25
LN_EPS = 1e-5
RMS_EPS = 1e-6
NEXP = 32
FF = 512
CAP = 256
NSLOT = NEXP * CAP
GDN_IN = 8224
DSA_IN = 4496
NEG = -30000.0


class Buf:
    __slots__ = ("name", "writer", "readers")

    def __init__(self, name=""):
        self.name = name
        self.writer = None
        self.readers = []


class Eng:
    def __init__(self, fw, name, hw, is_pe=False):
        self.fw = fw
        self.name = name
        self.hw = hw
        self.is_pe = is_pe
        self.sems = []
        self.cnt = 0
        self.waited = {}
        self.n_instr = 0

    def cur_sem(self):
        if not self.sems or self.cnt >= self.fw.EPOCH:
            self.sems.append(self.fw.new_sem(f"{self.name}_p{len(self.sems)}"))
            self.cnt = 0
        return self.sems[-1]


class FW:
    EPOCH = 60000
    NP = 24

    def __init__(self, nc, stack):
        self.nc = nc
        self.stack = stack
        self.sem_handles = {}
        self.nsem = 0
        self.pe = Eng(self, "pe", nc.tensor, is_pe=True)
        self.dve = Eng(self, "dve", nc.vector)
        self.act = Eng(self, "act", nc.scalar)
        self.pool = Eng(self, "pool", nc.gpsimd)
        self.sp = Eng(self, "sp", nc.sync)
        self.engs = [self.pe, self.dve, self.act, self.pool, self.sp]
        self.dma_pool = {}
        self.dma_rr = {}
        self.all_dma_tokens = []

    def new_sem(self, name):
        h = self.stack.enter_context(self.nc.semaphore(name))
        self.nsem += 1
        self.sem_handles[self.nsem] = h
        return self.nsem

    def _wait(self, eng, tok):
        if tok is None:
            return
        key, val, src = tok
        if eng.is_pe and src == "pe":
            return
        if eng.waited.get(key, 0) >= val:
            return
        eng.waited[key] = val
        eng.hw.wait_ge(self.sem_handles[key], val)

    def _note_read(self, b, tok):
        b.readers.append(tok)
        if len(b.readers) > 16:
            last = {}
            for r in b.readers:
                k2 = (r[2], r[0])
                if k2 not in last or last[k2][1] < r[1]:
                    last[k2] = r
            b.readers = list(last.values())

    def op(self, eng, fn, reads=(), writes=()):
        for b in reads:
            self._wait(eng, b.writer)
        for b in writes:
            self._wait(eng, b.writer)
            for r in b.readers:
                if r[2] == eng.name:
                    continue
                self._wait(eng, r)
        ins = fn(eng.hw)
        key = eng.cur_sem()
        eng.cnt += 1
        ins.then_inc(self.sem_handles[key], 1)
        tok = (key, eng.cnt, eng.name)
        for b in reads:
            self._note_read(b, tok)
        for b in writes:
            b.writer = tok
            b.readers = []
        eng.n_instr += 1
        return tok

    def dma(self, out, in_, reads=(), writes=(), q=None, indirect=None, **kw):
        eng = q or self.sp
        for b in reads:
            self._wait(eng, b.writer)
        for b in writes:
            self._wait(eng, b.writer)
            for r in b.readers:
                self._wait(eng, r)
        pool = self.dma_pool.setdefault(eng.name, [])
        if len(pool) < self.NP:
            pool.append([self.new_sem(f"dma_{eng.name}_{len(pool)}"), 0])
            slot = pool[-1]
        else:
            i = self.dma_rr.get(eng.name, 0)
            slot = pool[i % self.NP]
            self.dma_rr[eng.name] = i + 1
        key, uses = slot
        if uses > 0:
            self._wait(eng, (key, 16 * uses, "dma"))
        if indirect is None:
            ins = eng.hw.dma_start(out=out, in_=in_, **kw)
        else:
            ins = eng.hw.indirect_dma_start(out=out, in_=in_, **indirect)
        slot[1] = uses + 1
        ins.then_inc(self.sem_handles[key], 16)
        tok = (key, 16 * (uses + 1), "dma")
        for b in reads:
            self._note_read(b, tok)
        for b in writes:
            b.writer = tok
            b.readers = []
        self.all_dma_tokens.append(tok)
        if len(self.all_dma_tokens) > 400:
            self._compact_dma()
        eng.n_instr += 1
        return tok

    def _compact_dma(self):
        last = {}
        for t in self.all_dma_tokens:
            if t[0] not in last or last[t[0]][1] < t[1]:
                last[t[0]] = t
        self.all_dma_tokens = list(last.values())

    def barrier(self, engs=None):
        toks = []
        for e in self.engs:
            if e.sems and e.cnt > 0:
                toks.append((e.sems[-1], e.cnt, e.name))
        self._compact_dma()
        toks += self.all_dma_tokens
        for e in (engs or self.engs):
            for key, val, src in toks:
                if src == e.name:
                    continue
                if e.waited.get(key, 0) >= val:
                    continue
                e.waited[key] = val
                e.hw.wait_ge(self.sem_handles[key], val)

    def finish(self):
        self.barrier(engs=[self.sp])


class K:
    ARENA = 46000

    def __init__(self, nc, st, ext_in, ext_out):
        self.nc = nc
        self.st = st
        self.fw = FW(nc, st)
        self.ext_in = ext_in
        self.ext_out = ext_out
        self.arena = st.enter_context(nc.sbuf_tensor("arena", [128, self.ARENA], F32))
        self.psum = st.enter_context(nc.psum_tensor("psum", [128, 4096], F32))
        self.off = 0
        self.pbank = [Buf(f"bank{i}") for i in range(8)]
        self.dram = {}
        self.dbuf = {}
        self._consts()

    def sb(self, n, dt=F32):
        words = n if dt != BF16 else (n + 1) // 2
        words = (words + 7) // 8 * 8
        assert self.off + words <= self.ARENA, (self.off, words)
        ap = self.arena[:, self.off:self.off + words]
        self.off += words
        if dt == BF16:
            ap = ap.bitcast(BF16)[:, 0:n]
        elif dt == I32:
            ap = ap.bitcast(I32)[:, 0:n]
        else:
            ap = ap[:, 0:n]
        return ap

    def mark(self):
        return self.off

    def release(self, m):
        self.fw.barrier()
        self.off = m

    def bank(self, i, dt=F32):
        ap = self.psum[:, i * 512:(i + 1) * 512]
        if dt == BF16:
            ap = ap.bitcast(BF16)
        return ap

    def dt(self, name, shape, dtype):
        kind = "Internal"
        if name in self.ext_in:
            kind = "ExternalInput"
        elif name in self.ext_out:
            kind = "ExternalOutput"
        t = self.nc.dram_tensor(name, list(shape), dtype, kind=kind).ap()
        self.dram[name] = t
        self.dbuf[name] = Buf(name)
        return t

    def _consts(self):
        fw = self.fw
        P = fw.pool
        self.cb = Buf("consts")
        cb = self.cb
        self.ident = self.sb(128)
        self.ones = self.sb(128)
        self.identb = self.sb(128, BF16)
        self.onesb = self.sb(128, BF16)
        self.sltb = self.sb(128, BF16)
        slt = self.sb(128)
        fw.op(P, lambda e: e.memset(self.ident, 0.0), writes=[cb])
        fw.op(P, lambda e: e.affine_select(out=self.ident, in_=self.ident, pattern=[[-1, 128]],
                                           compare_op=ALU.not_equal, fill=1.0, base=0, channel_multiplier=1),
              reads=[cb], writes=[cb])
        fw.op(P, lambda e: e.memset(self.ones, 1.0), writes=[cb])
        fw.op(P, lambda e: e.memset(slt, 1.0), writes=[cb])
        fw.op(P, lambda e: e.affine_select(out=slt, in_=slt, pattern=[[1, 128]], compare_op=ALU.is_gt,
                                           fill=0.0, base=0, channel_multiplier=-1), reads=[cb], writes=[cb])
        fw.op(P, lambda e: e.tensor_copy(out=self.identb, in_=self.ident), reads=[cb], writes=[cb])
        fw.op(P, lambda e: e.tensor_copy(out=self.onesb, in_=self.ones), reads=[cb], writes=[cb])
        fw.op(P, lambda e: e.tensor_copy(out=self.sltb, in_=slt), reads=[cb], writes=[cb])
        self.ebase = self.sb(NEXP)
        fw.op(P, lambda e: e.iota(out=self.ebase, pattern=[[CAP, NEXP]], base=0, channel_multiplier=0,
                                  allow_small_or_imprecise_dtypes=True), writes=[cb])
        self.trash = self.sb(1)
        fw.op(P, lambda e: e.iota(out=self.trash, pattern=[[0, 1]], base=NSLOT, channel_multiplier=1,
                                  allow_small_or_imprecise_dtypes=True), writes=[cb])
        self.const_mark = self.off

    def load_row(self, dst, src_row, buf, n):
        src = src_row.rearrange("(o n) -> o n", o=1).to_broadcast([128, n]) if len(src_row.shape) == 1 \
            else src_row.to_broadcast([128, n])
        self.fw.dma(out=dst, in_=src, writes=[buf])

    def build_xT(self, src, src_buf, xT, xT_buf):
        fw = self.fw
        m = self.mark()
        xin = [self.sb(D) for _ in range(2)]
        xb = [Buf("xin0"), Buf("xin1")]
        for i in range(NT):
            s = i % 2
            fw.dma(out=xin[s], in_=src[i * 128:(i + 1) * 128, :], reads=[src_buf], writes=[xb[s]])
            for g in range(4):
                bk = (i * 4 + g) % 8
                pb = self.pbank[bk]
                for j in range(4):
                    fc = g * 4 + j
                    fw.op(fw.pe, lambda e, fc=fc, j=j, bk=bk, s=s: e.transpose(
                        out=self.bank(bk)[:, j * 128:(j + 1) * 128], in_=xin[s][:, fc * 128:(fc + 1) * 128],
                        identity=self.ident), reads=[xb[s], self.cb], writes=[pb])
                dst = xT[:, g * 4:(g + 1) * 4, i * 128:(i + 1) * 128]
                srcp = self.bank(bk).rearrange("p (a b) -> p a b", a=4)
                if g % 2 == 0:
                    fw.op(fw.dve, lambda e, dst=dst, srcp=srcp: e.tensor_copy(out=dst, in_=srcp),
                          reads=[pb], writes=[xT_buf])
                else:
                    fw.op(fw.act, lambda e, dst=dst, srcp=srcp: e.activation(out=dst, in_=srcp, func=AF.Copy),
                          reads=[pb], writes=[xT_buf])
        self.release(m)

    def ln_tail(self, z, zb, grow, brow, gbuf, tmp, tmpb, stat, statb, out, outb):
        fw = self.fw
        mean = stat[:, 0:1]
        ssq = stat[:, 1:2]
        rstd = stat[:, 2:3]
        nmean = stat[:, 3:4]
        fw.op(fw.dve, lambda e: e.tensor_reduce(out=mean, in_=z, axis=AX.X, op=ALU.add), reads=[zb], writes=[statb])
        fw.op(fw.dve, lambda e: e.tensor_scalar(out=nmean, in0=mean, scalar1=-1.0 / D, scalar2=None, op0=ALU.mult),
              reads=[statb], writes=[statb])
        fw.op(fw.act, lambda e: e.activation(out=tmp, in_=z, func=AF.Square, bias=nmean, scale=1.0, accum_out=ssq),
              reads=[zb, statb], writes=[tmpb, statb])
        fw.op(fw.dve, lambda e: e.tensor_scalar(out=rstd, in0=ssq, scalar1=1.0 / D, scalar2=LN_EPS, op0=ALU.mult,
                                                op1=ALU.add), reads=[statb], writes=[statb])
        fw.op(fw.act, lambda e: e.activation(out=rstd, in_=rstd, func=AF.Sqrt), reads=[statb], writes=[statb])
        fw.op(fw.dve, lambda e: e.reciprocal(out=rstd, in_=rstd), reads=[statb], writes=[statb])
        fw.op(fw.dve, lambda e: e.tensor_scalar(out=tmp, in0=z, scalar1=nmean, scalar2=rstd, op0=ALU.add,
                                                op1=ALU.mult), reads=[zb, statb, tmpb], writes=[tmpb])
        fw.op(fw.pool, lambda e: e.tensor_tensor(out=tmp, in0=tmp, in1=grow, op=ALU.mult), reads=[tmpb, gbuf],
              writes=[tmpb])
        fw.op(fw.dve, lambda e: e.tensor_tensor(out=out, in0=tmp, in1=brow, op=ALU.add), reads=[tmpb, gbuf],
              writes=[outb])

    def outproj_ln(self, OT, OTb, w_out, resid, resid_buf, ln_g, ln_b, dst, dst_buf):
        fw = self.fw
        m = self.mark()
        W = self.sb(16 * D, BF16)
        Wv = W.rearrange("p (a b) -> p a b", a=16)
        Wb = Buf("wout")
        wsrc = w_out.rearrange("(fc p) m -> p fc m", p=128)
        for q in range(4):
            fw.dma(out=Wv[:, q * 4:(q + 1) * 4, :], in_=wsrc[:, q * 4:(q + 1) * 4, :], writes=[Wb], q=fw.pool)
        grow = self.sb(D)
        brow = self.sb(D)
        gbuf = Buf("lnrows")
        self.load_row(grow, ln_g, gbuf, D)
        self.load_row(brow, ln_b, gbuf, D)
        oT = [self.sb(16 * 128, BF16) for _ in range(2)]
        oTb = [Buf(), Buf()]
        rz = [self.sb(D) for _ in range(2)]
        rzb = [Buf(), Buf()]
        tmp = self.sb(D)
        tmpb = Buf()
        stat = [self.sb(4) for _ in range(2)]
        statb = [Buf(), Buf()]
        OTv = OT.rearrange("fc p t -> p fc t")
        for i in range(NT):
            s = i % 2
            fw.dma(out=oT[s].rearrange("p (a b) -> p a b", a=16), in_=OTv[:, :, i * 128:(i + 1) * 128],
                   reads=[OTb], writes=[oTb[s]])
            fw.dma(out=rz[s], in_=resid[i * 128:(i + 1) * 128, :], reads=[resid_buf], writes=[rzb[s]])
            for mc in range(4):
                bk = (i * 4 + mc) % 8
                pb = self.pbank[bk]
                for fc in range(16):
                    fw.op(fw.pe, lambda e, fc=fc, mc=mc, bk=bk, s=s: e.matmul(
                        self.bank(bk), lhsT=oT[s][:, fc * 128:(fc + 1) * 128], rhs=Wv[:, fc, mc * 512:(mc + 1) * 512],
                        start=(fc == 0), stop=(fc == 15)), reads=[oTb[s], Wb], writes=[pb])
                zs = rz[s][:, mc * 512:(mc + 1) * 512]
                fw.op(fw.dve, lambda e, zs=zs, bk=bk: e.scalar_tensor_tensor(
                    out=zs, in0=zs, scalar=DN_ALPHA, in1=self.bank(bk), op0=ALU.mult, op1=ALU.add),
                    reads=[pb, rzb[s]], writes=[rzb[s]])
            self.ln_tail(rz[s], rzb[s], grow, brow, gbuf, tmp, tmpb, stat[s], statb[s], rz[s], rzb[s])
            fw.dma(out=dst[i * 128:(i + 1) * 128, :], in_=rz[s], reads=[rzb[s]], writes=[dst_buf])
        self.release(m)

    def moe(self, L, xm, xm_buf, W, dst, dst_buf):
        fw = self.fw
        XG, YG = self.dram["XG"], self.dram["YG"]
        XGb, YGb = self.dbuf["XG"], self.dbuf["YG"]
        m0 = self.mark()
        dest = self.sb(NT * 2, I32)
        gate = self.sb(NT * 2)
        routeb = Buf("route")
        acum = self.sb(NEXP)
        acumb = self.sb(NEXP, BF16)
        acb = Buf("acum")
        fw.op(fw.dve, lambda e: e.memset(acum, 0.0), writes=[acb])
        fw.op(fw.dve, lambda e: e.memset(acumb, 0.0), writes=[acb])
        m1 = self.mark()
        wr = self.sb(16 * 36)
        wrv = wr.rearrange("p (a b) -> p a b", a=16)
        wrb = Buf("wr")
        fw.dma(out=wrv[:, :, 0:4], in_=W["moe_rg_w"][L].rearrange("(fc p) g -> p fc g", p=128), writes=[wrb])
        fw.dma(out=wrv[:, :, 4:36], in_=W["moe_re_w"][L].rearrange("(fc p) g -> p fc g", p=128), writes=[wrb])
        brow = self.sb(36)
        self.load_row(brow[:, 0:4], W["moe_rg_b"][L], wrb, 4)
        self.load_row(brow[:, 4:36], W["moe_re_b"][L], wrb, 32)
        xin = [self.sb(D) for _ in range(2)]
        xinb = [Buf(), Buf()]
        xTf = [self.sb(16 * 128) for _ in range(2)]
        xTfb = [Buf(), Buf()]
        xbf = [self.sb(D, BF16) for _ in range(2)]
        xbfb = [Buf(), Buf()]
        sm = [self.sb(256) for _ in range(2)]
        smb = [Buf(), Buf()]
        for i in range(NT):
            s = i % 2
            fw.dma(out=xin[s], in_=xm[i * 128:(i + 1) * 128, :], reads=[xm_buf], writes=[xinb[s]])
            for g in range(4):
                bk = g
                pb = self.pbank[bk]
                for j in range(4):
                    fc = g * 4 + j
                    fw.op(fw.pe, lambda e, fc=fc, j=j, bk=bk, s=s: e.transpose(
                        out=self.bank(bk)[:, j * 128:(j + 1) * 128], in_=xin[s][:, fc * 128:(fc + 1) * 128],
                        identity=self.ident), reads=[xinb[s], self.cb], writes=[pb])
                dstp = xTf[s][:, g * 512:(g + 1) * 512]
                if g % 2 == 0:
                    fw.op(fw.dve, lambda e, dstp=dstp, bk=bk: e.tensor_copy(out=dstp, in_=self.bank(bk)),
                          reads=[pb], writes=[xTfb[s]])
                else:
                    fw.op(fw.act, lambda e, dstp=dstp, bk=bk: e.activation(out=dstp, in_=self.bank(bk), func=AF.Copy),
                          reads=[pb], writes=[xTfb[s]])
            fw.op(fw.pool, lambda e, s=s: e.tensor_copy(out=xbf[s], in_=xin[s]), reads=[xinb[s]], writes=[xbfb[s]])
            pl = self.pbank[4]
            lg_ps = self.bank(4)[:, 0:36]
            for fc in range(16):
                fw.op(fw.pe, lambda e, fc=fc, s=s: e.matmul(lg_ps, lhsT=xTf[s][:, fc * 128:(fc + 1) * 128],
                                                           rhs=wrv[:, fc, :], start=(fc == 0), stop=(fc == 15)),
                      reads=[xTfb[s], wrb], writes=[pl])
            S = sm[s]
            Sb = smb[s]
            lg = S[:, 0:36]
            fw.op(fw.dve, lambda e, lg=lg: e.tensor_tensor(out=lg, in0=lg_ps, in1=brow, op=ALU.add),
                  reads=[pl, wrb], writes=[Sb])
            gmax = S[:, 36:37]
            ngmax = S[:, 37:38]
            gsum = S[:, 38:39]
            pg = S[:, 39:40]
            ohg = S[:, 40:44]
            ex4 = S[:, 44:48]
            fw.op(fw.dve, lambda e: e.tensor_reduce(out=gmax, in_=lg[:, 0:4], axis=AX.X, op=ALU.max),
                  reads=[Sb], writes=[Sb])
            fw.op(fw.dve, lambda e: e.tensor_scalar(out=ngmax, in0=gmax, scalar1=-1.0, scalar2=None, op0=ALU.mult),
                  reads=[Sb], writes=[Sb])
            fw.op(fw.act, lambda e: e.activation(out=ex4, in_=lg[:, 0:4], func=AF.Exp, bias=ngmax, scale=1.0,
                                                 accum_out=gsum), reads=[Sb], writes=[Sb])
            fw.op(fw.dve, lambda e: e.reciprocal(out=pg, in_=gsum), reads=[Sb], writes=[Sb])
            fw.op(fw.dve, lambda e: e.tensor_scalar(out=ohg, in0=lg[:, 0:4], scalar1=gmax, scalar2=None,
                                                    op0=ALU.is_ge), reads=[Sb], writes=[Sb])
            esel = S[:, 48:56]
            le = lg[:, 4:36]
            fw.op(fw.dve, lambda e: e.tensor_scalar(out=esel, in0=le[:, 0:8], scalar1=ohg[:, 0:1], scalar2=None,
                                                    op0=ALU.mult), reads=[Sb], writes=[Sb])
            for g in range(1, 4):
                fw.op(fw.dve, lambda e, g=g: e.scalar_tensor_tensor(
                    out=esel, in0=le[:, g * 8:(g + 1) * 8], scalar=ohg[:, g:g + 1], in1=esel, op0=ALU.mult,
                    op1=ALU.add), reads=[Sb], writes=[Sb])
            top8 = S[:, 56:64]
            fw.op(fw.dve, lambda e: e.max(out=top8, in_=esel), reads=[Sb], writes=[Sb])
            oh1 = S[:, 64:72]
            oh2 = S[:, 72:80]
            fw.op(fw.dve, lambda e: e.tensor_scalar(out=oh1, in0=esel, scalar1=top8[:, 0:1], scalar2=None,
                                                    op0=ALU.is_equal), reads=[Sb], writes=[Sb])
            fw.op(fw.dve, lambda e: e.tensor_scalar(out=oh2, in0=esel, scalar1=top8[:, 1:2], scalar2=None,
                                                    op0=ALU.is_equal), reads=[Sb], writes=[Sb])
            dv = S[:, 80:82]
            fw.op(fw.dve, lambda e: e.tensor_tensor(out=dv[:, 0:1], in0=top8[:, 0:1], in1=top8[:, 1:2],
                                                    op=ALU.subtract), reads=[Sb], writes=[Sb])
            fw.op(fw.dve, lambda e: e.tensor_tensor(out=dv[:, 1:2], in0=top8[:, 1:2], in1=top8[:, 0:1],
                                                    op=ALU.subtract), reads=[Sb], writes=[Sb])
            p12 = S[:, 82:84]
            fw.op(fw.act, lambda e: e.activation(out=p12, in_=dv, func=AF.Sigmoid), reads=[Sb], writes=[Sb])
            fw.op(fw.dve, lambda e: e.tensor_scalar(out=p12, in0=p12, scalar1=pg, scalar2=None, op0=ALU.mult),
                  reads=[Sb], writes=[Sb])
            A1 = S[:, 96:128]
            A2 = S[:, 128:160]
            for g in range(4):
                fw.op(fw.dve, lambda e, g=g: e.tensor_scalar(out=A1[:, g * 8:(g + 1) * 8], in0=oh1,
                                                              scalar1=ohg[:, g:g + 1], scalar2=None, op0=ALU.mult),
                      reads=[Sb], writes=[Sb])
                fw.op(fw.dve, lambda e, g=g: e.tensor_scalar(out=A2[:, g * 8:(g + 1) * 8], in0=oh2,
                                                              scalar1=ohg[:, g:g + 1], scalar2=None, op0=ALU.mult),
                      reads=[Sb], writes=[Sb])
            A12 = S[:, 160:192]
            A12b = S[:, 192:208].bitcast(BF16)
            fw.op(fw.dve, lambda e: e.tensor_tensor(out=A12, in0=A1, in1=A2, op=ALU.add), reads=[Sb], writes=[Sb])
            fw.op(fw.dve, lambda e: e.tensor_copy(out=A12b, in_=A12), reads=[Sb], writes=[Sb])
            pp = self.pbank[5]
            pos_ps = self.bank(5)[:, 0:32]
            fw.op(fw.pe, lambda e: e.matmul(pos_ps, lhsT=self.onesb, rhs=acumb, start=True, stop=False),
                  reads=[acb, self.cb], writes=[pp])
            fw.op(fw.pe, lambda e: e.matmul(pos_ps, lhsT=self.sltb, rhs=A12b, start=False, stop=True),
                  reads=[Sb, self.cb], writes=[pp])
            slot = S[:, 208:240]
            fw.op(fw.dve, lambda e: e.tensor_tensor(out=slot, in0=pos_ps, in1=self.ebase, op=ALU.add),
                  reads=[pp, self.cb], writes=[Sb])
            fw.op(fw.pool, lambda e: e.tensor_tensor(out=acum, in0=acum, in1=A12, op=ALU.add), reads=[Sb, acb],
                  writes=[acb])
            fw.op(fw.pool, lambda e: e.tensor_copy(out=acumb, in_=acum), reads=[acb], writes=[acb])
            tmp32 = S[:, 240:256]
            dr = S[:, 84:86]
            pr = S[:, 86:88]
            for r, A in ((0, A1), (1, A2)):
                tt = S[:, 224:256]
            scr = xTf[s][:, 0:32]
            for r, A in ((0, A1), (1, A2)):
                fw.op(fw.dve, lambda e, r=r, A=A: e.scalar_tensor_tensor(
                    out=scr, in0=A, scalar=1.0, in1=slot, op0=ALU.mult, op1=ALU.mult, accum_out=dr[:, r:r + 1]),
                    reads=[Sb, xTfb[s], pl], writes=[Sb, xTfb[s]])
                fw.op(fw.dve, lambda e, r=r, A=A: e.scalar_tensor_tensor(
                    out=scr, in0=A, scalar=1.0, in1=pos_ps, op0=ALU.mult, op1=ALU.mult, accum_out=pr[:, r:r + 1]),
                    reads=[Sb, xTfb[s], pp], writes=[Sb, xTfb[s]])
            valid = S[:, 88:90]
            fw.op(fw.dve, lambda e: e.tensor_scalar(out=valid, in0=pr, scalar1=float(CAP) - 0.5, scalar2=None,
                                                    op0=ALU.is_lt), reads=[Sb], writes=[Sb])
            fw.op(fw.dve, lambda e: e.tensor_tensor(out=p12, in0=p12, in1=valid, op=ALU.mult), reads=[Sb],
                  writes=[Sb])
            fw.op(fw.dve, lambda e: e.tensor_scalar(out=dr, in0=dr, scalar1=self.trash, scalar2=None,
                                                    op0=ALU.subtract), reads=[Sb, self.cb], writes=[Sb])
            fw.op(fw.dve, lambda e: e.tensor_tensor(out=dr, in0=dr, in1=valid, op=ALU.mult), reads=[Sb], writes=[Sb])
            fw.op(fw.dve, lambda e: e.tensor_scalar(out=dr, in0=dr, scalar1=self.trash, scalar2=None, op0=ALU.add),
                  reads=[Sb, self.cb], writes=[Sb])
            fw.op(fw.dve, lambda e, i=i: e.tensor_copy(out=dest[:, 2 * i:2 * i + 2], in_=dr), reads=[Sb],
                  writes=[routeb])
            fw.op(fw.dve, lambda e, i=i: e.tensor_copy(out=gate[:, 2 * i:2 * i + 2], in_=p12), reads=[Sb],
                  writes=[routeb])
            for r in range(2):
                fw.dma(out=XG, in_=xbf[s], reads=[xbfb[s], routeb], writes=[XGb], q=fw.pool,
                       indirect=dict(out_offset=bass.IndirectOffsetOnAxis(ap=dest[:, 2 * i + r:2 * i + r + 1], axis=0),
                                     in_offset=None))
        self.release(m1)
        m2 = self.mark()
        NB = 2
        wg = [self.sb(16 * FF, BF16) for _ in range(NB)]
        wu = [self.sb(16 * FF, BF16) for _ in range(NB)]
        wd = [self.sb(4 * D, BF16) for _ in range(NB)]
        wbuf = [Buf() for _ in range(NB)]
        xg = [self.sb(D, BF16) for _ in range(2)]
        xgb = [Buf(), Buf()]
        xgT = self.sb(16 * CAP, BF16)
        xgTv = xgT.rearrange("p (a b) -> p a b", a=16)
        xgTb = Buf()
        hT = self.sb(4 * CAP, BF16)
        hTv = hT.rearrange("p (a b) -> p a b", a=4)
        hTb = Buf()
        sg = self.sb(CAP)
        sgb = Buf()
        yt = [self.sb(D) for _ in range(2)]
        ytb = [Buf(), Buf()]
        pbk = 0
        for ex in range(NEXP):
            s = ex % NB
            fw.dma(out=wg[s].rearrange("p (a b) -> p a b", a=16),
                   in_=W["moe_w_gate"][L, ex].rearrange("(kc p) f -> p kc f", p=128), writes=[wbuf[s]], q=fw.pool)
            fw.dma(out=wu[s].rearrange("p (a b) -> p a b", a=16),
                   in_=W["moe_w_up"][L, ex].rearrange("(kc p) f -> p kc f", p=128), writes=[wbuf[s]], q=fw.pool)
            fw.dma(out=wd[s].rearrange("p (a b) -> p a b", a=4),
                   in_=W["moe_w_down"][L, ex].rearrange("(fc p) m -> p fc m", p=128), writes=[wbuf[s]], q=fw.pool)
            wgv = wg[s].rearrange("p (a b) -> p a b", a=16)
            wuv = wu[s].rearrange("p (a b) -> p a b", a=16)
            wdv = wd[s].rearrange("p (a b) -> p a b", a=4)
            for stl in range(CAP // 128):
                xs = stl % 2
                fw.dma(out=xg[xs], in_=XG[ex * CAP + stl * 128: ex * CAP + (stl + 1) * 128, :], reads=[XGb],
                       writes=[xgb[xs]])
                for g in range(2):
                    bk = pbk % 8
                    pbk += 1
                    pb = self.pbank[bk]
                    bkb = self.bank(bk, BF16)
                    for j in range(8):
                        kc = g * 8 + j
                        fw.op(fw.pe, lambda e, kc=kc, j=j, bkb=bkb, xs=xs: e.transpose(
                            out=bkb[:, j * 128:(j + 1) * 128], in_=xg[xs][:, kc * 128:(kc + 1) * 128],
                            identity=self.identb), reads=[xgb[xs], self.cb], writes=[pb])
                    dstp = xgTv[:, g * 8:(g + 1) * 8, stl * 128:(stl + 1) * 128]
                    srcp = bkb.rearrange("p (a b) -> p a b", a=8)
                    if g == 0:
                        fw.op(fw.dve, lambda e, dstp=dstp, srcp=srcp: e.tensor_copy(out=dstp, in_=srcp),
                              reads=[pb], writes=[xgTb])
                    else:
                        fw.op(fw.act, lambda e, dstp=dstp, srcp=srcp: e.activation(out=dstp, in_=srcp, func=AF.Copy),
                              reads=[pb], writes=[xgTb])
            for fc in range(4):
                bkg = pbk % 8
                bku = (pbk + 1) % 8
                pbk += 2
                for (bk, wv) in ((bkg, wgv), (bku, wuv)):
                    for kc in range(16):
                        fw.op(fw.pe, lambda e, bk=bk, wv=wv, kc=kc, fc=fc: e.matmul(
                            self.bank(bk)[:, 0:CAP], lhsT=wv[:, kc, fc * 128:(fc + 1) * 128], rhs=xgTv[:, kc, :],
                            start=(kc == 0), stop=(kc == 15)), reads=[wbuf[s], xgTb], writes=[self.pbank[bk]])
                fw.op(fw.act, lambda e, bkg=bkg: e.activation(out=sg, in_=self.bank(bkg)[:, 0:CAP], func=AF.Silu),
                      reads=[self.pbank[bkg]], writes=[sgb])
                fw.op(fw.dve, lambda e, bku=bku, fc=fc: e.tensor_tensor(out=hTv[:, fc, :], in0=sg,
                                                                        in1=self.bank(bku)[:, 0:CAP], op=ALU.mult),
                      reads=[self.pbank[bku], sgb], writes=[hTb])
            for stl in range(CAP // 128):
                ys = (ex * 2 + stl) % 2
                for mc in range(4):
                    bk = pbk % 8
                    pbk += 1
                    for fc in range(4):
                        fw.op(fw.pe, lambda e, bk=bk, fc=fc, mc=mc, stl=stl: e.matmul(
                            self.bank(bk), lhsT=hTv[:, fc, stl * 128:(stl + 1) * 128],
                            rhs=wdv[:, fc, mc * 512:(mc + 1) * 512], start=(fc == 0), stop=(fc == 3)),
                            reads=[hTb, wbuf[s]], writes=[self.pbank[bk]])
                    dstp = yt[ys][:, mc * 512:(mc + 1) * 512]
                    if mc % 2 == 0:
                        fw.op(fw.dve, lambda e, dstp=dstp, bk=bk: e.tensor_copy(out=dstp, in_=self.bank(bk)),
                              reads=[self.pbank[bk]], writes=[ytb[ys]])
                    else:
                        fw.op(fw.act, lambda e, dstp=dstp, bk=bk: e.activation(out=dstp, in_=self.bank(bk),
                                                                               func=AF.Copy),
                              reads=[self.pbank[bk]], writes=[ytb[ys]])
                fw.dma(out=YG[ex * CAP + stl * 128: ex * CAP + (stl + 1) * 128, :], in_=yt[ys], reads=[ytb[ys]],
                       writes=[YGb])
        self.release(m2)
        m3 = self.mark()
        grow = self.sb(D)
        brow2 = self.sb(D)
        gbuf = Buf()
        self.load_row(grow, W["ln_g"][L, 1], gbuf, D)
        self.load_row(brow2, W["ln_b"][L, 1], gbuf, D)
        xr = [self.sb(D) for _ in range(2)]
        xrb = [Buf(), Buf()]
        y1 = [self.sb(D) for _ in range(2)]
        y1b = [Buf(), Buf()]
        y2 = [self.sb(D) for _ in range(2)]
        y2b = [Buf(), Buf()]
        tmp = self.sb(D)
        tmpb = Buf()
        stat = [self.sb(4) for _ in range(2)]
        statb = [Buf(), Buf()]
        for i in range(NT):
            s = i % 2
            fw.dma(out=xr[s], in_=xm[i * 128:(i + 1) * 128, :], reads=[xm_buf], writes=[xrb[s]])
            for (yy, yb, r) in ((y1[s], y1b[s], 0), (y2[s], y2b[s], 1)):
                fw.dma(out=yy, in_=YG, reads=[YGb, routeb], writes=[yb], q=fw.pool,
                       indirect=dict(out_offset=None,
                                     in_offset=bass.IndirectOffsetOnAxis(ap=dest[:, 2 * i + r:2 * i + r + 1], axis=0)))
            fw.op(fw.act, lambda e, s=s, i=i: e.activation(out=y1[s], in_=y1[s], func=AF.Identity,
                                                           scale=gate[:, 2 * i:2 * i + 1]),
                  reads=[y1b[s], routeb], writes=[y1b[s]])
            fw.op(fw.dve, lambda e, s=s, i=i: e.scalar_tensor_tensor(
                out=y2[s], in0=y2[s], scalar=gate[:, 2 * i + 1:2 * i + 2], in1=y1[s], op0=ALU.mult, op1=ALU.add),
                reads=[y1b[s], y2b[s], routeb], writes=[y2b[s]])
            fw.op(fw.dve, lambda e, s=s: e.scalar_tensor_tensor(
                out=xr[s], in0=xr[s], scalar=DN_ALPHA, in1=y2[s], op0=ALU.mult, op1=ALU.add),
                reads=[xrb[s], y2b[s]], writes=[xrb[s]])
            self.ln_tail(xr[s], xrb[s], grow, brow2, gbuf, tmp, tmpb, stat[s], statb[s], xr[s], xrb[s])
            fw.dma(out=dst[i * 128:(i + 1) * 128, :], in_=xr[s], reads=[xrb[s]], writes=[dst_buf])
        self.release(m3)
        self.release(m0)

    def proj_fm(self, xTv, xTb, w_cols, wt, wtb, banks=(0, 1, 2, 3)):
        fw = self.fw
        fw.dma(out=wt.rearrange("p (a b) -> p a b", a=16), in_=w_cols.rearrange("(kc p) f -> p kc f", p=128),
               writes=[wtb], q=fw.pool)
        wv = wt.rearrange("p (a b) -> p a b", a=16)
        for tc in range(4):
            bk = banks[tc]
            for kc in range(16):
                fw.op(fw.pe, lambda e, bk=bk, kc=kc, tc=tc: e.matmul(
                    self.bank(bk), lhsT=wv[:, kc, :], rhs=xTv[:, kc, tc * 512:(tc + 1) * 512],
                    start=(kc == 0), stop=(kc == 15)), reads=[wtb, xTb], writes=[self.pbank[bk]])

    def sin_rr(self, eng, out, ang, buf, k, r, n_part=128, shift=0.0):
        fw = self.fw
        MAG = 12582912.0
        C1 = 6.28125
        C2 = 2.0 * math.pi - 6.28125
        fw.op(eng, lambda e: e.tensor_scalar(out=k, in0=ang, scalar1=1.0 / (2.0 * math.pi),
                                             scalar2=shift / (2.0 * math.pi), op0=ALU.mult, op1=ALU.add),
              reads=[buf], writes=[buf])
        fw.op(eng, lambda e: e.tensor_scalar(out=k, in0=k, scalar1=MAG, scalar2=None, op0=ALU.add),
              reads=[buf], writes=[buf])
        fw.op(eng, lambda e: e.tensor_scalar(out=k, in0=k, scalar1=-MAG, scalar2=None, op0=ALU.add),
              reads=[buf], writes=[buf])
        fw.op(eng, lambda e: e.scalar_tensor_tensor(out=r, in0=k, scalar=-C1, in1=ang, op0=ALU.mult, op1=ALU.add),
              reads=[buf], writes=[buf]) if eng is fw.dve else None
        if eng is not fw.dve:
            raise ValueError
        fw.op(eng, lambda e: e.scalar_tensor_tensor(out=r, in0=k, scalar=-C2, in1=r, op0=ALU.mult, op1=ALU.add),
              reads=[buf], writes=[buf])
        fw.op(eng, lambda e: e.tensor_scalar(out=r, in0=r, scalar1=shift, scalar2=math.pi, op0=ALU.add, op1=ALU.min),
              reads=[buf], writes=[buf])
        fw.op(eng, lambda e: e.tensor_scalar(out=r, in0=r, scalar1=-math.pi, scalar2=None, op0=ALU.max),
              reads=[buf], writes=[buf])
        fw.op(fw.act, lambda e: e.activation(out=out, in_=r, func=AF.Sin), reads=[buf], writes=[buf])

    def s5(self, L, src, src_buf, W, dst, dst_buf):
        fw = self.fw
        dve, act, pool, pe = fw.dve, fw.act, fw.pool, fw.pe
        U, Ub = self.dram["U"], self.dbuf["U"]
        YA, YAb = self.dram["YA"], self.dbuf["YA"]
        OT, OTb = self.dram["OT"], self.dbuf["OT"]
        m0 = self.mark()
        big = self.sb(16 * T, BF16)
        bigv = big.rearrange("p (a b) -> p a b", a=16)
        xTb = Buf("xT")
        self.build_xT(src, src_buf, bigv, xTb)
        mU = self.mark()
        wt = [self.sb(16 * 128, BF16) for _ in range(2)]
        wtb = [Buf(), Buf()]
        uf = [self.sb(T) for _ in range(2)]
        ufb = [Buf(), Buf()]
        for J in range(16):
            s = J % 2
            self.proj_fm(bigv, xTb, W["ssm_w_in"][0][:, J * 128:(J + 1) * 128], wt[s], wtb[s])
            for tc in range(4):
                eng = dve if tc % 2 == 0 else act
                dstp = uf[s][:, tc * 512:(tc + 1) * 512]
                if tc % 2 == 0:
                    fw.op(dve, lambda e, dstp=dstp, tc=tc: e.tensor_copy(out=dstp, in_=self.bank(tc)),
                          reads=[self.pbank[tc]], writes=[ufb[s]])
                else:
                    fw.op(act, lambda e, dstp=dstp, tc=tc: e.activation(out=dstp, in_=self.bank(tc), func=AF.Copy),
                          reads=[self.pbank[tc]], writes=[ufb[s]])
            fw.dma(out=U[J], in_=uf[s], reads=[ufb[s]], writes=[Ub])
        self.release(mU)
        uTb_buf = xTb
        for q in range(4):
            fw.dma(out=bigv[:, q * 4:(q + 1) * 4, :], in_=U.rearrange("j p t -> p j t")[:, q * 4:(q + 1) * 4, :],
                   reads=[Ub], writes=[uTb_buf], q=pool)
        mP = self.mark()
        PQ = self.sb(6 * 64)
        LB = [self.sb(16 * 128, BF16) for _ in range(2)]
        CL = [self.sb(16 * 128) for _ in range(2)]
        LBz = [self.sb(16 * 128, BF16) for _ in range(2)]
        LBzv = [a.rearrange("p (J q) -> p J q", J=16) for a in LBz]
        dsk = self.sb(16)
        tau = self.sb(520)
        m96 = self.sb(1)
        mT = self.mark()
        pb_ = Buf("s5par")
        A = lambda: self.sb(128)
        are, aim, dtb, mag, ang, kk, rr, sn, cs, den, fre, fim, t1, t2 = [A() for _ in range(14)]
        ldt = self.sb(2)
        fw.dma(out=are[0:64, :], in_=W["ssm_a_re"][0].rearrange("(j two) p -> j (two p)", two=2), writes=[pb_])
        fw.dma(out=aim[0:64, :], in_=W["ssm_a_im"][0].rearrange("(j two) p -> j (two p)", two=2), writes=[pb_])
        fw.dma(out=ldt[0:64, :], in_=W["ssm_log_dt"][0].rearrange("(j two) -> j two", two=2), writes=[pb_])
        h = slice(0, 64)
        fw.op(act, lambda e: e.activation(out=ldt[h, :], in_=ldt[h, :], func=AF.Exp), reads=[pb_], writes=[pb_])
        for two in range(2):
            fw.op(dve, lambda e, two=two: e.tensor_scalar(out=dtb[h, two * 64:(two + 1) * 64], in0=self.ones[h, 0:64],
                                                         scalar1=ldt[h, two:two + 1], scalar2=None, op0=ALU.mult),
                  reads=[pb_, self.cb], writes=[pb_])
        tt = lambda o, a, b, op: fw.op(dve, lambda e: e.tensor_tensor(out=o[h, :], in0=a[h, :], in1=b[h, :], op=op),
                                       reads=[pb_], writes=[pb_])
        tt(mag, are, dtb, ALU.mult)
        fw.op(act, lambda e: e.activation(out=mag[h, :], in_=mag[h, :], func=AF.Exp), reads=[pb_], writes=[pb_])
        tt(ang, aim, dtb, ALU.mult)
        self.sin_rr(dve, sn[h, :], ang[h, :], pb_, kk[h, :], rr[h, :])
        thr = A()
        fw.op(dve, lambda e: e.tensor_copy(out=thr[h, :], in_=rr[h, :]), reads=[pb_], writes=[pb_])
        self.sin_rr(dve, cs[h, :], ang[h, :], pb_, kk[h, :], rr[h, :], shift=math.pi / 2)
        lre, lim = A(), A()
        tt(lre, mag, cs, ALU.mult)
        tt(lim, mag, sn, ALU.mult)
        tt(t1, are, are, ALU.mult)
        tt(t2, aim, aim, ALU.mult)
        tt(den, t1, t2, ALU.add)
        fw.op(dve, lambda e: e.reciprocal(out=den[h, :], in_=den[h, :]), reads=[pb_], writes=[pb_])
        lm1 = A()
        fw.op(dve, lambda e: e.tensor_scalar(out=lm1[h, :], in0=lre[h, :], scalar1=-1.0, scalar2=None, op0=ALU.add),
              reads=[pb_], writes=[pb_])
        tt(t1, lm1, are, ALU.mult)
        tt(t2, lim, aim, ALU.mult)
        tt(fre, t1, t2, ALU.add)
        tt(fre, fre, den, ALU.mult)
        tt(t1, lim, are, ALU.mult)
        tt(t2, lm1, aim, ALU.mult)
        tt(fim, t1, t2, ALU.subtract)
        tt(fim, fim, den, ALU.mult)
        PQv = PQ.rearrange("p (a b) -> p a b", a=6)
        pqb = Buf("pq")
        for n_, srcp in enumerate((mag, thr, cs, sn, fre, fim)):
            fw.op(pe, lambda e, n_=n_, srcp=srcp: e.transpose(out=self.bank(0)[:, n_ * 64:(n_ + 1) * 64],
                                                              in_=srcp[0:64, :], identity=self.ident[0:64, 0:64]),
                  reads=[pb_, self.cb], writes=[self.pbank[0]])
        fw.op(dve, lambda e: e.tensor_copy(out=PQ, in_=self.bank(0)[:, 0:384]), reads=[self.pbank[0]], writes=[pqb])
        RHO, THR, COS1, SIN1, FRE, FIM = [PQv[:, n_, :] for n_ in range(6)]
        bre = self.sb(64 * 16)
        bim = self.sb(64 * 16)
        bbr = self.sb(64 * 16)
        bbi = self.sb(64 * 16)
        tb1 = self.sb(64 * 16)
        bb_ = Buf("bb")
        v3 = lambda a: a.rearrange("p (j c) -> p j c", c=16)
        fw.dma(out=v3(bre), in_=W["ssm_b_re"][0].rearrange("(j two) p c -> (two p) j c", two=2), writes=[bb_])
        fw.dma(out=v3(bim), in_=W["ssm_b_im"][0].rearrange("(j two) p c -> (two p) j c", two=2), writes=[bb_])
        bc = lambda a: a.unsqueeze(2).to_broadcast([128, 64, 16])
        t3 = lambda o, a, b, op: fw.op(dve, lambda e: e.tensor_tensor(out=v3(o), in0=v3(a), in1=b, op=op),
                                       reads=[bb_, pqb], writes=[bb_])
        t3(bbr, bre, bc(FRE), ALU.mult)
        t3(tb1, bim, bc(FIM), ALU.mult)
        t3(bbr, bbr, v3(tb1), ALU.subtract)
        t3(bbi, bim, bc(FRE), ALU.mult)
        t3(tb1, bre, bc(FIM), ALU.mult)
        t3(bbi, bbi, v3(tb1), ALU.add)
        LBv = [a.rearrange("p (J q) -> p J q", J=16) for a in LB]
        lbb = Buf("LB")
        arr = self.sb(16 * 128)
        arrb = Buf("arr")
        arr5 = arr.rearrange("p (J jj two c) -> p J jj two c", J=16, jj=4, two=2)
        for ri, bbx in enumerate((bbr, bbi)):
            fw.op(pool, lambda e: e.memset(arr, 0.0), writes=[arrb])
            b4 = bbx.rearrange("p (J jj c) -> p J jj c", J=16, jj=4)
            for two in range(2):
                ps = slice(two * 64, (two + 1) * 64)
                fw.op(pool, lambda e, two=two, ps=ps, b4=b4: e.tensor_copy(out=arr5[ps, :, :, two, :], in_=b4[ps]),
                      reads=[bb_, arrb], writes=[arrb])
            av = arr.rearrange("p (J x) -> p J x", J=16)
            for g in range(4):
                bk = 1 + g
                for j4 in range(4):
                    J = g * 4 + j4
                    fw.op(pe, lambda e, J=J, j4=j4, bk=bk: e.transpose(out=self.bank(bk)[:, j4 * 128:(j4 + 1) * 128],
                                                                       in_=av[:, J, :], identity=self.ident),
                          reads=[arrb, self.cb], writes=[self.pbank[bk]])
                fw.op(act, lambda e, g=g, bk=bk, ri=ri: e.activation(
                    out=LBv[ri][:, g * 4:(g + 1) * 4, :], in_=self.bank(bk).rearrange("p (a b) -> p a b", a=4),
                    func=AF.Copy), reads=[self.pbank[bk]], writes=[lbb])
        fw.op(pool, lambda e: e.memset(m96, 1.0), writes=[lbb])
        fw.op(pool, lambda e: e.affine_select(out=m96, in_=m96, pattern=[[0, 1]], compare_op=ALU.is_ge, fill=0.0,
                                              base=-96, channel_multiplier=1), reads=[lbb], writes=[lbb])
        for ri in range(2):
            fw.op(dve, lambda e, ri=ri: e.tensor_scalar(out=LBz[ri][64:128, :], in0=LB[ri][64:128, :],
                                                       scalar1=m96[64:128, :], scalar2=None, op0=ALU.mult),
                  reads=[lbb], writes=[lbb])
        CLv = [a.rearrange("p (J q) -> p J q", J=16) for a in CL]
        clb = Buf("CL")
        for ri, cname in enumerate(("ssm_c_re", "ssm_c_im")):
            fw.op(pool, lambda e: e.memset(arr, 0.0), writes=[arrb])
            a4 = arr.rearrange("p (J two q) -> p J two q", J=16, two=2)
            csrc = W[cname][0].rearrange("(J jj two) c p -> jj two c J p", jj=4, two=2)
            for jj in range(4):
                for two in range(2):
                    r0 = jj * 32 + two * 16
                    fw.dma(out=a4[r0:r0 + 16, :, two, :], in_=csrc[jj, two], writes=[arrb])
            av = arr.rearrange("p (J x) -> p J x", J=16)
            for g in range(4):
                bk = 1 + g
                for j4 in range(4):
                    J = g * 4 + j4
                    fw.op(pe, lambda e, J=J, j4=j4, bk=bk: e.transpose(out=self.bank(bk)[:, j4 * 128:(j4 + 1) * 128],
                                                                       in_=av[:, J, :], identity=self.ident),
                          reads=[arrb, self.cb], writes=[self.pbank[bk]])
                fw.op(act, lambda e, g=g, bk=bk, ri=ri: e.activation(
                    out=CLv[ri][:, g * 4:(g + 1) * 4, :], in_=self.bank(bk).rearrange("p (a b) -> p a b", a=4),
                    func=AF.Copy, scale=(1.0 if ri == 0 else -1.0)), reads=[self.pbank[bk]], writes=[clb])
        dskb = Buf("dsk")
        d16 = self.sb(128)
        fw.dma(out=d16[0:16, :], in_=W["ssm_d"][0].rearrange("(J p) -> J p", p=128), writes=[dskb])
        fw.op(pe, lambda e: e.transpose(out=self.bank(5)[:, 0:16], in_=d16[0:16, :], identity=self.ident[0:16, 0:16]),
              reads=[dskb, self.cb], writes=[self.pbank[5]])
        fw.op(dve, lambda e: e.tensor_copy(out=dsk, in_=self.bank(5)[:, 0:16]), reads=[self.pbank[5]], writes=[dskb])
        taub = Buf("tau")
        fw.op(pool, lambda e: e.iota(out=tau[:, 0:513], pattern=[[1, 513]], base=0, channel_multiplier=0,
                                     allow_small_or_imprecise_dtypes=True), writes=[taub])
        self.release(mT)
        NW = 2
        tabc_f = [self.sb(520) for _ in range(NW)]
        tabs_f = [self.sb(520) for _ in range(NW)]
        tk = [self.sb(520)[:, 0:513] for _ in range(NW)]
        tr_ = [self.sb(520)[:, 0:513] for _ in range(NW)]
        tang = [self.sb(520)[:, 0:513] for _ in range(NW)]
        tabc = [a[:, 0:512] for a in tabc_f]
        tabs = [a[:, 0:512] for a in tabs_f]
        tabb = [Buf() for _ in range(NW)]
        wk = [[self.sb(512) for _ in range(6)] for _ in range(NW)]
        wkb = [Buf() for _ in range(NW)]
        st8 = [self.sb(8) for _ in range(4)]
        st8b = [Buf() for _ in range(4)]
        clm = [self.sb(4 * 128) for _ in range(2)]
        clmb = Buf("clm")
        ufl0 = self.sb(T)
        ufl = [ufl0, ufl0]
        uflb0 = Buf()
        uflb = [uflb0, uflb0]
        yo = [self.sb(512) for _ in range(2)]
        yob = [Buf(), Buf()]
        yab0 = self.sb(T, BF16)
        yab = [yab0, yab0]
        yabb0 = Buf()
        yabb = [yabb0, yabb0]
        cnt = 0
        cnt2 = 0
        bu_done = {}
        bu_n = [0]

        def emit_bu(J_, jj_, tc_):
            bks = (5, 6) if bu_n[0] % 2 == 0 else (4, 7)
            bu_n[0] += 1
            rows_ = slice(jj_ * 32, (jj_ + 1) * 32) if jj_ < 3 else slice(64, 128)
            LBu_ = LBv if jj_ < 3 else LBzv
            for ri_, bk_ in ((0, bks[0]), (1, bks[1])):
                fw.op(pe, lambda e, ri_=ri_, bk_=bk_: e.matmul(
                    self.bank(bk_), lhsT=LBu_[ri_][rows_, J_, :], rhs=bigv[rows_, J_, tc_ * 512:(tc_ + 1) * 512],
                    start=True, stop=True), reads=[lbb, uTb_buf], writes=[self.pbank[bk_]])
            bu_done[(J_, jj_, tc_)] = bks

        for J in range(16):
            js = J % 2
            fw.dma(out=ufl[js], in_=U[J], reads=[Ub], writes=[uflb[js]])
            for ri in range(2):
                cm = clm[ri].rearrange("p (jj x) -> p jj x", jj=4)
                fw.op(pool, lambda e, ri=ri: e.memset(clm[ri], 0.0), writes=[clmb])
                for jj in range(4):
                    fw.op(pool, lambda e, ri=ri, jj=jj, cm=cm, J=J: e.tensor_copy(
                        out=cm[:, jj, jj * 32:(jj + 1) * 32], in_=CLv[ri][:, J, jj * 32:(jj + 1) * 32]),
                        reads=[clb, clmb], writes=[clmb])
            for jj in range(4):
                fw.op(dve, lambda e, jj=jj: e.memset(st8[jj], 0.0), writes=[st8b[jj]])
            for jj in range(4):
                j = J * 4 + jj
                w = cnt % NW
                cnt += 1
                fw.op(dve, lambda e, w=w, j=j: e.tensor_scalar(out=tang[w], in0=tau[:, 0:513], scalar1=THR[:, j:j + 1],
                                                               scalar2=None, op0=ALU.mult),
                      reads=[taub, pqb], writes=[tabb[w]])
                self.sin_rr(dve, tabs_f[w][:, 0:513], tang[w], tabb[w], tk[w], tr_[w])
                self.sin_rr(dve, tabc_f[w][:, 0:513], tang[w], tabb[w], tk[w], tr_[w], shift=math.pi / 2)
                C512 = tabc_f[w][:, 512:513]
                S512 = tabs_f[w][:, 512:513]
                for tc in range(4):
                    ybk = tc
                    w2 = cnt2 % NW
                    cnt2 += 1
                    if (J, jj, tc) not in bu_done:
                        emit_bu(J, jj, tc)
                    b5, b6 = bu_done[(J, jj, tc)]
                    nxt = (J, jj, tc + 1) if tc < 3 else ((J, jj + 1, 0) if jj < 3 else ((J + 1, 0, 0) if J < 15 else None))
                    if nxt is not None:
                        emit_bu(*nxt)
                    btr, bti, rre, rim, ta, tb = wk[w2]
                    B = wkb[w2]
                    fw.op(dve, lambda e, w=w, btr=btr: e.tensor_tensor(out=btr, in0=self.bank(b5), in1=tabc[w], op=ALU.mult),
                          reads=[self.pbank[b5], tabb[w]], writes=[B])
                    fw.op(dve, lambda e, w=w, ta=ta: e.tensor_tensor(out=ta, in0=self.bank(b6), in1=tabs[w], op=ALU.mult),
                          reads=[self.pbank[b6], tabb[w]], writes=[B])
                    fw.op(dve, lambda e, btr=btr, ta=ta: e.tensor_tensor(out=btr, in0=btr, in1=ta, op=ALU.add),
                          reads=[B], writes=[B])
                    fw.op(dve, lambda e, w=w, bti=bti: e.tensor_tensor(out=bti, in0=self.bank(b6), in1=tabc[w], op=ALU.mult),
                          reads=[self.pbank[b6], tabb[w]], writes=[B])
                    fw.op(dve, lambda e, w=w, tb=tb: e.tensor_tensor(out=tb, in0=self.bank(b5), in1=tabs[w], op=ALU.mult),
                          reads=[self.pbank[b5], tabb[w]], writes=[B])
                    fw.op(dve, lambda e, bti=bti, tb=tb: e.tensor_tensor(out=bti, in0=bti, in1=tb, op=ALU.subtract),
                          reads=[B], writes=[B])
                    c8 = st8[jj]
                    cb8 = st8b[jj]
                    fw.op(dve, lambda e, c8=c8, j=j: e.tensor_scalar(out=c8[:, 2:3], in0=c8[:, 0:1],
                                                                     scalar1=C512, scalar2=None, op0=ALU.mult),
                          reads=[cb8, tabb[w]], writes=[cb8])
                    fw.op(dve, lambda e, c8=c8, j=j: e.tensor_scalar(out=c8[:, 4:5], in0=c8[:, 1:2],
                                                                     scalar1=S512, scalar2=None, op0=ALU.mult),
                          reads=[cb8, tabb[w]], writes=[cb8])
                    fw.op(dve, lambda e, c8=c8: e.tensor_tensor(out=c8[:, 2:3], in0=c8[:, 2:3], in1=c8[:, 4:5],
                                                                op=ALU.subtract), reads=[cb8], writes=[cb8])
                    fw.op(dve, lambda e, c8=c8, j=j: e.tensor_scalar(out=c8[:, 3:4], in0=c8[:, 0:1],
                                                                     scalar1=S512, scalar2=None, op0=ALU.mult),
                          reads=[cb8, tabb[w]], writes=[cb8])
                    fw.op(dve, lambda e, c8=c8, j=j: e.tensor_scalar(out=c8[:, 4:5], in0=c8[:, 1:2],
                                                                     scalar1=C512, scalar2=None, op0=ALU.mult),
                          reads=[cb8, tabb[w]], writes=[cb8])
                    fw.op(dve, lambda e, c8=c8: e.tensor_tensor(out=c8[:, 3:4], in0=c8[:, 3:4], in1=c8[:, 4:5],
                                                                op=ALU.add), reads=[cb8], writes=[cb8])
                    rho_b = RHO[:, j:j + 1].to_broadcast([128, 512])
                    fw.op(dve, lambda e, rre=rre, btr=btr, c8=c8, rho_b=rho_b: e.tensor_tensor_scan(
                        out=rre, data0=rho_b, data1=btr, initial=c8[:, 2:3], op0=ALU.mult, op1=ALU.add),
                        reads=[B, cb8, pqb], writes=[B])
                    fw.op(dve, lambda e, rim=rim, bti=bti, c8=c8, rho_b=rho_b: e.tensor_tensor_scan(
                        out=rim, data0=rho_b, data1=bti, initial=c8[:, 3:4], op0=ALU.mult, op1=ALU.add),
                        reads=[B, cb8, pqb], writes=[B])
                    fw.op(dve, lambda e, c8=c8, rre=rre: e.tensor_copy(out=c8[:, 0:1], in_=rre[:, 511:512]),
                          reads=[B, cb8], writes=[cb8])
                    fw.op(dve, lambda e, c8=c8, rim=rim: e.tensor_copy(out=c8[:, 1:2], in_=rim[:, 511:512]),
                          reads=[B, cb8], writes=[cb8])
                    fw.op(pool, lambda e, w=w, ta=ta, rre=rre: e.tensor_tensor(out=ta, in0=rre, in1=tabc[w], op=ALU.mult),
                          reads=[B, tabb[w]], writes=[B])
                    fw.op(pool, lambda e, w=w, tb=tb, rim=rim: e.tensor_tensor(out=tb, in0=rim, in1=tabs[w], op=ALU.mult),
                          reads=[B, tabb[w]], writes=[B])
                    fw.op(pool, lambda e, w=w, ta=ta, tb=tb: e.tensor_tensor(out=ta, in0=ta, in1=tb, op=ALU.subtract),
                          reads=[B], writes=[B])
                    fw.op(pool, lambda e, w=w, tb=tb, rre=rre: e.tensor_tensor(out=tb, in0=rre, in1=tabs[w], op=ALU.mult),
                          reads=[B, tabb[w]], writes=[B])
                    fw.op(pool, lambda e, w=w, rre=rre, rim=rim: e.tensor_tensor(out=rre, in0=rim, in1=tabc[w], op=ALU.mult),
                          reads=[B, tabb[w]], writes=[B])
                    fw.op(pool, lambda e, tb=tb, rre=rre: e.tensor_tensor(out=tb, in0=tb, in1=rre, op=ALU.add),
                          reads=[B], writes=[B])
                    cm0 = clm[0].rearrange("p (jj x) -> p jj x", jj=4)
                    cm1 = clm[1].rearrange("p (jj x) -> p jj x", jj=4)
                    fw.op(pe, lambda e, ta=ta, jj=jj, cm0=cm0: e.matmul(self.bank(ybk), lhsT=cm0[:, jj, :], rhs=ta,
                                                                        start=(jj == 0), stop=False),
                          reads=[clmb, B], writes=[self.pbank[ybk]])
                    fw.op(pe, lambda e, tb=tb, jj=jj, cm1=cm1: e.matmul(self.bank(ybk), lhsT=cm1[:, jj, :], rhs=tb,
                                                                        start=False, stop=(jj == 3)),
                          reads=[clmb, B], writes=[self.pbank[ybk]])
            for tc in range(4):
                ybk = tc
                ys = (J * 4 + tc) % 2
                fw.op(dve, lambda e, ys=ys, js=js, J=J, tc=tc: e.scalar_tensor_tensor(
                    out=yo[ys], in0=ufl[js][:, tc * 512:(tc + 1) * 512], scalar=dsk[:, J:J + 1], in1=self.bank(ybk),
                    op0=ALU.mult, op1=ALU.add), reads=[uflb[js], dskb, self.pbank[ybk]], writes=[yob[ys]])
                fw.op(act, lambda e, ys=ys, js=js, tc=tc: e.activation(out=yab[js][:, tc * 512:(tc + 1) * 512],
                                                                     in_=yo[ys], func=AF.Gelu),
                      reads=[yob[ys]], writes=[yabb[js]])
            fw.dma(out=YA[J], in_=yab[js], reads=[yabb[js]], writes=[YAb])
        self.release(mP)
        for q in range(4):
            fw.dma(out=bigv[:, q * 4:(q + 1) * 4, :], in_=YA.rearrange("j p t -> p j t")[:, q * 4:(q + 1) * 4, :],
                   reads=[YAb], writes=[xTb])
        mG = self.mark()
        wt = [self.sb(16 * 128, BF16) for _ in range(2)]
        wtb = [Buf(), Buf()]
        bgl = self.sb(16)
        bglb = Buf()
        b16 = self.sb(128)
        fw.dma(out=b16[0:16, :], in_=W["ssm_b_glu"][0].rearrange("(J p) -> J p", p=128), writes=[bglb])
        fw.op(pe, lambda e: e.transpose(out=self.bank(5)[:, 0:16], in_=b16[0:16, :], identity=self.ident[0:16, 0:16]),
              reads=[bglb, self.cb], writes=[self.pbank[5]])
        fw.op(dve, lambda e: e.tensor_copy(out=bgl, in_=self.bank(5)[:, 0:16]), reads=[self.pbank[5]], writes=[bglb])
        sg = [self.sb(512) for _ in range(2)]
        sgb = [Buf(), Buf()]
        y2 = [self.sb(T, BF16) for _ in range(2)]
        y2b = [Buf(), Buf()]
        for mo in range(16):
            s = mo % 2
            self.proj_fm(bigv, xTb, W["ssm_w_glu"][0][:, mo * 128:(mo + 1) * 128], wt[s], wtb[s])
            for tc in range(4):
                s2 = tc % 2
                fw.op(act, lambda e, s2=s2, tc=tc, mo=mo: e.activation(out=sg[s2], in_=self.bank(tc), func=AF.Sigmoid,
                                                                     bias=bgl[:, mo:mo + 1], scale=1.0),
                      reads=[self.pbank[tc], bglb], writes=[sgb[s2]])
                fw.op(dve, lambda e, s=s, s2=s2, tc=tc, mo=mo: e.tensor_tensor(
                    out=y2[s][:, tc * 512:(tc + 1) * 512], in0=sg[s2], in1=bigv[:, mo, tc * 512:(tc + 1) * 512],
                    op=ALU.mult), reads=[sgb[s2], xTb], writes=[y2b[s]])
            fw.dma(out=OT[mo], in_=y2[s], reads=[y2b[s]], writes=[OTb])
        self.release(mG)
        self.release(m0)
        self.outproj_ln(OT, OTb, W["ssm_w_out"][0], src, src_buf, W["ln_g"][L, 0], W["ln_b"][L, 0], dst, dst_buf)

    def dsa(self, L, src, src_buf, W, dst, dst_buf):
        fw = self.fw
        dve, act, pool, pe = fw.dve, fw.act, fw.pool, fw.pe
        QT, QTb = self.dram["QT"], self.dbuf["QT"]
        QI, QIb = self.dram["QI"], self.dbuf["QI"]
        MT, MTb = self.dram["MASKT"], self.dbuf["MASKT"]
        OT, OTb = self.dram["OT"], self.dbuf["OT"]
        w_in = W["dsa_w_in"][0]
        m0 = self.mark()
        kT = self.sb(T, BF16)
        kiT = self.sb(T, BF16)
        vtok = self.sb(16 * 132, BF16)
        vtv = vtok.rearrange("p (a b) -> p a b", a=16)
        wi = self.sb(16 * 16)
        wiv = wi.rearrange("p (a b) -> p a b", a=16)
        resb = Buf("dsa_res")
        fw.op(pool, lambda e: e.memset(vtok, 1.0), writes=[resb])
        m1 = self.mark()
        big = self.sb(16 * T, BF16)
        bigv = big.rearrange("p (a b) -> p a b", a=16)
        xTb = Buf("xT")
        self.build_xT(src, src_buf, bigv, xTb)
        wt = [self.sb(16 * 128, BF16) for _ in range(2)]
        wtb = [Buf(), Buf()]
        ob = [self.sb(T, BF16) for _ in range(2)]
        obb = [Buf(), Buf()]
        QSC = 128.0 ** -0.5
        n = 0
        for (col0, cnt_, dstD, dstDb, scale) in ((0, 16, QT, QTb, QSC), (2304, 16, QI, QIb, 1.0)):
            for hh in range(cnt_):
                s = n % 2
                n += 1
                self.proj_fm(bigv, xTb, w_in[:, col0 + hh * 128: col0 + (hh + 1) * 128], wt[s], wtb[s])
                for tc in range(4):
                    dstp = ob[s][:, tc * 512:(tc + 1) * 512]
                    if tc % 2 == 0:
                        fw.op(dve, lambda e, dstp=dstp, tc=tc, scale=scale: e.tensor_scalar(
                            out=dstp, in0=self.bank(tc), scalar1=scale, scalar2=None, op0=ALU.mult),
                            reads=[self.pbank[tc]], writes=[obb[s]])
                    else:
                        fw.op(act, lambda e, dstp=dstp, tc=tc, scale=scale: e.activation(
                            out=dstp, in_=self.bank(tc), func=AF.Copy, scale=scale),
                            reads=[self.pbank[tc]], writes=[obb[s]])
                fw.dma(out=dstD[hh], in_=ob[s], reads=[obb[s]], writes=[dstDb])
        for (col0, dstT) in ((2048, kT), (4352, kiT)):
            s = n % 2
            n += 1
            self.proj_fm(bigv, xTb, w_in[:, col0: col0 + 128], wt[s], wtb[s])
            for tc in range(4):
                dstp = dstT[:, tc * 512:(tc + 1) * 512]
                fw.op(act, lambda e, dstp=dstp, tc=tc: e.activation(out=dstp, in_=self.bank(tc), func=AF.Copy),
                      reads=[self.pbank[tc]], writes=[resb])
        wv = wt[0]
        wvv = wv.rearrange("p (a b) -> p a b", a=16)
        fw.dma(out=wvv, in_=w_in[:, 2176:2304].rearrange("(kc p) f -> p kc f", p=128), writes=[wtb[0]], q=pool)
        ww = wt[1][:, 0:256]
        wwv = ww.rearrange("p (a b) -> p a b", a=16)
        fw.dma(out=wwv, in_=w_in[:, 4480:4496].rearrange("(kc p) f -> p kc f", p=128), writes=[wtb[1]], q=pool)
        WSC = (16.0 ** -0.5) * (128.0 ** -0.5)
        for i in range(NT):
            bk = 4 + (i % 2)
            for kc in range(16):
                fw.op(pe, lambda e, i=i, kc=kc, bk=bk: e.matmul(self.bank(bk)[:, 0:128], lhsT=bigv[:, kc, i * 128:(i + 1) * 128],
                                                               rhs=wvv[:, kc, :], start=(kc == 0), stop=(kc == 15)),
                      reads=[xTb, wtb[0]], writes=[self.pbank[bk]])
            fw.op(act, lambda e, i=i, bk=bk: e.activation(out=vtv[:, i, 0:128], in_=self.bank(bk)[:, 0:128], func=AF.Copy),
                  reads=[self.pbank[bk]], writes=[resb])
            bk2 = 6 + (i % 2)
            for kc in range(16):
                fw.op(pe, lambda e, i=i, kc=kc, bk2=bk2: e.matmul(self.bank(bk2)[:, 0:16], lhsT=bigv[:, kc, i * 128:(i + 1) * 128],
                                                                 rhs=wwv[:, kc, :], start=(kc == 0), stop=(kc == 15)),
                      reads=[xTb, wtb[1]], writes=[self.pbank[bk2]])
            fw.op(dve, lambda e, i=i, bk2=bk2: e.tensor_scalar(out=wiv[:, i, :], in0=self.bank(bk2)[:, 0:16], scalar1=WSC,
                                                               scalar2=None, op0=ALU.mult),
                  reads=[self.pbank[bk2]], writes=[resb])
        self.release(m1)
        m2 = self.mark()
        qit = [self.sb(16 * 128, BF16) for _ in range(2)]
        qitb = [Buf(), Buf()]
        acc = self.sb(T)
        accb = Buf("acc")
        work = self.sb(T)
        workb = Buf("work")
        rl = [self.sb(512, BF16) for _ in range(2)]
        rlb = [Buf(), Buf()]
        m8 = self.sb(8)
        m8b = Buf()
        mk = self.sb(T, BF16)
        mkb = Buf()
        mT = [self.sb(16 * 128, BF16) for _ in range(2)]
        mTb = [Buf(), Buf()]
        QIv = QI.rearrange("h p t -> p h t")
        MTv = MT.rearrange("b p t -> p b t")
        nr = 0
        for i in range(NT):
            s = i % 2
            S_ = 128 * (i + 1)
            fw.dma(out=qit[s].rearrange("p (a b) -> p a b", a=16), in_=QIv[:, :, i * 128:(i + 1) * 128], reads=[QIb],
                   writes=[qitb[s]])
            qv = qit[s].rearrange("p (a b) -> p a b", a=16)
            nsc = (S_ + 511) // 512
            for hh in range(16):
                for sc in range(nsc):
                    c0 = sc * 512
                    c1 = min(S_, c0 + 512)
                    bk = nr % 4
                    r2 = nr % 2
                    nr += 1
                    fw.op(pe, lambda e, hh=hh, c0=c0, c1=c1, bk=bk, qv=qv: e.matmul(
                        self.bank(bk)[:, 0:c1 - c0], lhsT=qv[:, hh, :], rhs=kiT[:, c0:c1], start=True, stop=True),
                        reads=[qitb[s], resb], writes=[self.pbank[bk]])
                    fw.op(act, lambda e, c0=c0, c1=c1, bk=bk, r2=r2: e.activation(
                        out=rl[r2][:, 0:c1 - c0], in_=self.bank(bk)[:, 0:c1 - c0], func=AF.Relu),
                        reads=[self.pbank[bk]], writes=[rlb[r2]])
                    if hh == 0:
                        fw.op(dve, lambda e, c0=c0, c1=c1, r2=r2, i=i: e.tensor_scalar(
                            out=acc[:, c0:c1], in0=rl[r2][:, 0:c1 - c0], scalar1=wiv[:, i, 0:1], scalar2=None,
                            op0=ALU.mult), reads=[rlb[r2], resb], writes=[accb])
                    else:
                        fw.op(dve, lambda e, c0=c0, c1=c1, r2=r2, i=i, hh=hh: e.scalar_tensor_tensor(
                            out=acc[:, c0:c1], in0=rl[r2][:, 0:c1 - c0], scalar=wiv[:, i, hh:hh + 1], in1=acc[:, c0:c1],
                            op0=ALU.mult, op1=ALU.add), reads=[rlb[r2], resb, accb], writes=[accb])
            fw.op(pool, lambda e, S_=S_: e.affine_select(out=acc[:, S_ - 128:S_], in_=acc[:, S_ - 128:S_],
                                                         pattern=[[-1, 128]], compare_op=ALU.is_ge, fill=-1e30, base=0,
                                                         channel_multiplier=1), reads=[accb], writes=[accb])
            if i >= 2:
                cur = acc
                curb = accb
                for rnd in range(32):
                    fw.op(dve, lambda e, cur=cur, S_=S_: e.max(out=m8, in_=cur[:, 0:S_]), reads=[curb], writes=[m8b])
                    if rnd < 31:
                        fw.op(dve, lambda e, cur=cur, S_=S_: e.match_replace(out=work[:, 0:S_], in_to_replace=m8,
                                                                             in_values=cur[:, 0:S_], imm_value=-3e38),
                              reads=[curb, m8b], writes=[workb])
                        cur = work
                        curb = workb
                fw.op(dve, lambda e, S_=S_: e.tensor_scalar(out=mk[:, 0:S_], in0=acc[:, 0:S_], scalar1=m8[:, 7:8],
                                                            scalar2=None, op0=ALU.is_ge), reads=[accb, m8b], writes=[mkb])
            else:
                fw.op(dve, lambda e, S_=S_: e.tensor_scalar(out=mk[:, 0:S_], in0=acc[:, 0:S_], scalar1=-1e29,
                                                            scalar2=None, op0=ALU.is_ge), reads=[accb], writes=[mkb])
            mTv = mT[s].rearrange("p (a b) -> p a b", a=16)
            for g in range((i + 8) // 8):
                bk = 4 + (g + i) % 2
                bkb = self.bank(bk, BF16)
                nb_ = min(8, i + 1 - g * 8)
                for j in range(nb_):
                    b = g * 8 + j
                    fw.op(pe, lambda e, b=b, j=j, bkb=bkb: e.transpose(out=bkb[:, j * 128:(j + 1) * 128],
                                                                      in_=mk[:, b * 128:(b + 1) * 128], identity=self.identb),
                          reads=[mkb, self.cb], writes=[self.pbank[bk]])
                fw.op(act, lambda e, g=g, nb_=nb_, bkb=bkb, mTv=mTv: e.activation(
                    out=mTv[:, g * 8:g * 8 + nb_, :], in_=bkb[:, 0:nb_ * 128].rearrange("p (a b) -> p a b", a=nb_),
                    func=AF.Copy), reads=[self.pbank[bk]], writes=[mTb[s]])
            fw.dma(out=MTv[:, 0:i + 1, i * 128:(i + 1) * 128], in_=mTv[:, 0:i + 1, :], reads=[mTb[s]], writes=[MTb])
        self.release(m2)
        m3 = self.mark()
        A1 = self.sb(2432)
        a1b = Buf("A1")
        fw.op(pool, lambda e: e.iota(out=A1, pattern=[[-1, 2432]], base=384, channel_multiplier=1,
                                     allow_small_or_imprecise_dtypes=True), writes=[a1b])
        fw.op(pool, lambda e: e.tensor_scalar(out=A1, in0=A1, scalar1=0.0, scalar2=None, op0=ALU.min), reads=[a1b],
              writes=[a1b])
        mres = self.sb(16 * T, BF16)
        mrv = mres.rearrange("p (a b) -> p a b", a=16)
        mrb = Buf("maskres")
        fw.op(pool, lambda e: e.memset(mres, 0.0), writes=[mrb])
        for b in range(16):
            fw.dma(out=mrv[:, b, b * 128:T], in_=MT[b][:, b * 128:T], reads=[MTb], writes=[mrb])
        qh = [self.sb(T, BF16) for _ in range(2)]
        qhb = [Buf(), Buf()]
        oth = [self.sb(T, BF16) for _ in range(2)]
        othb = [Buf(), Buf()]
        NU = 3
        LGB = (0, 1, 7)
        tmp = [self.sb(512) for _ in range(NU)]
        tmpb = [Buf() for _ in range(NU)]
        pp = [self.sb(512, BF16) for _ in range(NU)]
        ppb = [Buf() for _ in range(NU)]
        pm = [self.sb(512, BF16) for _ in range(NU)]
        pmb = [Buf() for _ in range(NU)]
        rden = self.sb(4)
        rdb = Buf()
        on = self.sb(512, BF16)
        onb = Buf()
        units = [(hh, c, b) for hh in range(16) for c in range(4) for b in range(4 * (c + 1))]
        lg_done = [0]

        def emit_lg(upto):
            while lg_done[0] <= min(upto, len(units) - 1):
                idx = lg_done[0]
                hh_, c_, b_ = units[idx]
                s_ = hh_ % 2
                if c_ == 0 and b_ == 0:
                    fw.dma(out=qh[s_], in_=QT[hh_], reads=[QTb], writes=[qhb[s_]])
                lbk_ = LGB[idx % NU]
                fw.op(pe, lambda e, b_=b_, c_=c_, lbk_=lbk_, s_=s_: e.matmul(
                    self.bank(lbk_), lhsT=kT[:, b_ * 128:(b_ + 1) * 128], rhs=qh[s_][:, c_ * 512:(c_ + 1) * 512],
                    start=True, stop=True), reads=[resb, qhb[s_]], writes=[self.pbank[lbk_]])
                lg_done[0] += 1

        idx = -1
        for hh in range(16):
            s = hh % 2
            slope = 2.0 ** (-(hh + 1) / 2.0)
            for c in range(4):
                tbk = 6
                nb = 4 * (c + 1)
                for b in range(nb):
                    idx += 1
                    emit_lg(idx + 2)
                    u = idx % NU
                    lbk = LGB[u]
                    off = 512 * c - 128 * b + 384
                    fw.op(dve, lambda e, u=u, off=off, lbk=lbk, slope=slope: e.scalar_tensor_tensor(
                        out=tmp[u], in0=A1[:, off:off + 512], scalar=slope, in1=self.bank(lbk), op0=ALU.mult, op1=ALU.add),
                        reads=[a1b, self.pbank[lbk]], writes=[tmpb[u]])
                    fw.op(act, lambda e, u=u: e.activation(out=pp[u], in_=tmp[u], func=AF.Exp), reads=[tmpb[u]],
                          writes=[ppb[u]])
                    fw.op(pool, lambda e, u=u, b=b, c=c: e.tensor_tensor(out=pm[u], in0=pp[u],
                                                                        in1=mrv[:, b, c * 512:(c + 1) * 512], op=ALU.mult),
                          reads=[ppb[u], mrb], writes=[pmb[u]])
                    for sub in range(4):
                        tt_ = 4 * c + sub
                        if b > tt_:
                            continue
                        obk = 2 + sub
                        fw.op(pe, lambda e, u=u, sub=sub, b=b, tt_=tt_, obk=obk: e.matmul(
                            self.bank(obk)[:, 0:129], lhsT=pm[u][:, sub * 128:(sub + 1) * 128],
                            rhs=vtv[:, b, 0:129], start=(b == 0), stop=(b == tt_)),
                            reads=[pmb[u], resb], writes=[self.pbank[obk]])
                for sub in range(4):
                    obk = 2 + sub
                    fw.op(dve, lambda e, obk=obk, sub=sub: e.reciprocal(out=rden[:, sub:sub + 1], in_=self.bank(obk)[:, 128:129]),
                          reads=[self.pbank[obk]], writes=[rdb])
                    fw.op(dve, lambda e, sub=sub, obk=obk: e.tensor_scalar(
                        out=on[:, sub * 128:(sub + 1) * 128], in0=self.bank(obk)[:, 0:128],
                        scalar1=rden[:, sub:sub + 1], scalar2=None, op0=ALU.mult),
                        reads=[self.pbank[obk], rdb], writes=[onb])
                tbb = self.bank(tbk, BF16)
                for sub in range(4):
                    fw.op(pe, lambda e, sub=sub, tbb=tbb: e.transpose(out=tbb[:, sub * 128:(sub + 1) * 128],
                                                                      in_=on[:, sub * 128:(sub + 1) * 128], identity=self.identb),
                          reads=[onb, self.cb], writes=[self.pbank[tbk]])
                fw.op(act, lambda e, s=s, c=c, tbb=tbb: e.activation(out=oth[s][:, c * 512:(c + 1) * 512], in_=tbb[:, 0:512],
                                                                      func=AF.Copy), reads=[self.pbank[tbk]], writes=[othb[s]])
            fw.dma(out=OT[hh], in_=oth[s], reads=[othb[s]], writes=[OTb])
        self.release(m3)
        self.release(m0)
        self.outproj_ln(OT, OTb, W["dsa_w_out"][0], src, src_buf, W["ln_g"][L, 0], W["ln_b"][L, 0], dst, dst_buf)

    def gdn(self, li, L, src, src_buf, W, dst, dst_buf):
        fw = self.fw
        dve, act, pool, pe = fw.dve, fw.act, fw.pool, fw.pe
        OT, OTb = self.dram["OT"], self.dbuf["OT"]
        w_in = W["gdn_w_in"][li]
        m0 = self.mark()
        A128 = lambda: self.sb(128)
        U2, B2, NEGM4 = A128(), A128(), self.sb(512)
        gcb = Buf("gdnconst")
        fw.op(pool, lambda e: e.memset(U2, 1.0), writes=[gcb])
        fw.op(pool, lambda e: e.affine_select(out=U2, in_=U2, pattern=[[1, 128]], compare_op=ALU.is_ge, fill=0.0,
                                              base=0, channel_multiplier=-1), reads=[gcb], writes=[gcb])
        fw.op(pool, lambda e: e.memset(U2[0:64, 64:128], 0.0), reads=[gcb], writes=[gcb])
        fw.op(pool, lambda e: e.memset(B2, 0.0), writes=[gcb])
        fw.op(pool, lambda e: e.memset(B2[0:64, 0:64], 1.0), reads=[gcb], writes=[gcb])
        fw.op(pool, lambda e: e.memset(B2[64:128, 64:128], 1.0), reads=[gcb], writes=[gcb])
        N4 = NEGM4.rearrange("p (u i) -> p u i", u=4)
        fw.op(pool, lambda e: e.memset(NEGM4, 0.0), writes=[gcb])
        fw.op(pool, lambda e: e.affine_select(out=N4, in_=N4, pattern=[[0, 4], [1, 128]], compare_op=ALU.is_ge, fill=NEG,
                                              base=0, channel_multiplier=-1), reads=[gcb], writes=[gcb])
        fw.op(pool, lambda e: e.memset(N4[0:64, :, 64:128], NEG), reads=[gcb], writes=[gcb])
        id4 = self.ident.unsqueeze(1).to_broadcast([128, 4, 128])
        ABt = self.sb(16 * 32)
        ABv = ABt.rearrange("p (a b) -> p a b", a=16)
        GT, GC, EGC, EKD, NGC, BT, NB = [self.sb(256) for _ in range(7)]
        v16 = lambda a: a.rearrange("p (a b) -> p a b", a=16)
        CW = self.sb(48 * 4)
        CWv = CW.rearrange("p (c j) -> p c j", j=4)
        NGrow = self.sb(128)
        gb = Buf("gdn_g")
        self.load_row(NGrow, W["gdn_norm_g"][li], gb, 128)
        arow = self.sb(32)
        self.load_row(arow[:, 0:16], W["gdn_a_log"][li], gb, 16)
        self.load_row(arow[:, 16:32], W["gdn_dt_bias"][li], gb, 16)
        cwrow = self.sb(6144)
        fw.dma(out=cwrow[0:4, :], in_=W["gdn_conv_w"][li], writes=[gb])
        for c in range(48):
            fw.op(pe, lambda e, c=c: e.transpose(out=self.bank(4)[:, c * 4:(c + 1) * 4], in_=cwrow[0:4, c * 128:(c + 1) * 128],
                                                 identity=self.ident[0:4, 0:4]), reads=[gb, self.cb], writes=[self.pbank[4]])
        fw.op(dve, lambda e: e.tensor_copy(out=CW, in_=self.bank(4)[:, 0:192]), reads=[self.pbank[4]], writes=[gb])
        self.off -= 6144 + 0
        fw.barrier()
        big = self.sb(16 * T, BF16)
        bigv = big.rearrange("p (a b) -> p a b", a=16)
        xTb = Buf("xT")
        self.build_xT(src, src_buf, bigv, xTb)
        mg = self.mark()
        wab = self.sb(16 * 32, BF16)
        wabv = wab.rearrange("p (a b) -> p a b", a=16)
        wabb = Buf()
        fw.dma(out=wabv, in_=w_in[:, 8192:8224].rearrange("(kc p) f -> p kc f", p=128), writes=[wabb], q=pool)
        for i in range(NT):
            bk = 4 + i % 4
            for kc in range(16):
                fw.op(pe, lambda e, i=i, kc=kc, bk=bk: e.matmul(self.bank(bk)[:, 0:32], lhsT=bigv[:, kc, i * 128:(i + 1) * 128],
                                                               rhs=wabv[:, kc, :], start=(kc == 0), stop=(kc == 15)),
                      reads=[xTb, wabb], writes=[self.pbank[bk]])
            fw.op(act, lambda e, i=i, bk=bk: e.activation(out=ABv[:, i, :], in_=self.bank(bk)[:, 0:32], func=AF.Copy),
                  reads=[self.pbank[bk]], writes=[gb])
        t1, t2, t3 = self.sb(256), self.sb(256), self.sb(256)
        nea = self.sb(16)
        dtb_b = arow[:, 16:32].unsqueeze(1).to_broadcast([128, 16, 16])
        fw.op(dve, lambda e: e.tensor_tensor(out=v16(t1), in0=ABv[:, :, 0:16], in1=dtb_b, op=ALU.add), reads=[gb], writes=[gb])
        fw.op(dve, lambda e: e.tensor_scalar(out=t2, in0=t1, scalar1=-1.0, scalar2=None, op0=ALU.mult), reads=[gb], writes=[gb])
        fw.op(dve, lambda e: e.tensor_tensor(out=t2, in0=t2, in1=t1, op=ALU.max), reads=[gb], writes=[gb])
        fw.op(act, lambda e: e.activation(out=t2, in_=t2, func=AF.Exp, scale=-1.0), reads=[gb], writes=[gb])
        fw.op(dve, lambda e: e.tensor_scalar(out=t2, in0=t2, scalar1=1.0, scalar2=None, op0=ALU.add), reads=[gb], writes=[gb])
        fw.op(act, lambda e: e.activation(out=t2, in_=t2, func=AF.Ln), reads=[gb], writes=[gb])
        fw.op(dve, lambda e: e.scalar_tensor_tensor(out=t3, in0=t1, scalar=0.0, in1=t2, op0=ALU.max, op1=ALU.add),
              reads=[gb], writes=[gb])
        fw.op(act, lambda e: e.activation(out=nea, in_=arow[:, 0:16], func=AF.Exp), reads=[gb], writes=[gb])
        fw.op(dve, lambda e: e.tensor_scalar(out=nea, in0=nea, scalar1=-1.0, scalar2=None, op0=ALU.mult), reads=[gb], writes=[gb])
        fw.op(dve, lambda e: e.tensor_tensor(out=v16(GT), in0=v16(t3), in1=nea.unsqueeze(1).to_broadcast([128, 16, 16]),
                                             op=ALU.mult), reads=[gb], writes=[gb])
        fw.op(act, lambda e: e.activation(out=v16(BT), in_=ABv[:, :, 16:32], func=AF.Sigmoid), reads=[gb], writes=[gb])
        fw.op(dve, lambda e: e.tensor_scalar(out=NB, in0=BT, scalar1=-1.0, scalar2=None, op0=ALU.mult), reads=[gb], writes=[gb])
        fw.op(pe, lambda e: e.matmul(self.bank(4)[:, 0:256], lhsT=U2, rhs=GT, start=True, stop=True), reads=[gb, gcb],
              writes=[self.pbank[4]])
        fw.op(pe, lambda e: e.matmul(self.bank(5)[:, 0:256], lhsT=B2, rhs=GT, start=True, stop=True), reads=[gb, gcb],
              writes=[self.pbank[5]])
        fw.op(dve, lambda e: e.tensor_copy(out=GC, in_=self.bank(4)[:, 0:256]), reads=[self.pbank[4]], writes=[gb])
        fw.op(act, lambda e: e.activation(out=EGC, in_=self.bank(4)[:, 0:256], func=AF.Exp), reads=[self.pbank[4]], writes=[gb])
        fw.op(dve, lambda e: e.tensor_tensor(out=t1, in0=self.bank(5)[:, 0:256], in1=GC, op=ALU.subtract),
              reads=[self.pbank[5], gb], writes=[gb])
        fw.op(act, lambda e: e.activation(out=EKD, in_=t1, func=AF.Exp), reads=[gb], writes=[gb])
        fw.op(dve, lambda e: e.tensor_scalar(out=NGC, in0=GC, scalar1=-1.0, scalar2=None, op0=ALU.mult), reads=[gb], writes=[gb])
        self.release(mg)
        GCv, EGCv, EKDv, NGCv, BTv, NBv = [v16(a) for a in (GC, EGC, EKD, NGC, BT, NB)]
        wt = [self.sb(16 * 128, BF16) for _ in range(2)]
        wtb = [Buf(), Buf()]
        qT, kT, vT = [self.sb(T).bitcast(BF16)[:, 0:T] for _ in range(3)]
        qkvb = Buf("qkv")
        ktok_f, vtok_f, otok = self.sb(T), self.sb(T), self.sb(T)
        ktok, vtok = ktok_f.bitcast(BF16)[:, 0:T], vtok_f.bitcast(BF16)[:, 0:T]
        ktv, vtv, otv = [a.rearrange("p (a b) -> p a b", a=16) for a in (ktok, vtok, otok)]
        tokb = Buf("tok")
        otb = Buf("otok")
        regA = self.sb(8192)
        raw = regA[:, 0:2056]
        cacc = regA[:, 2056:2056 + 2048]
        tmpq = regA[:, 4104:4104 + 2048]
        rb = Buf("raw")
        _f = lambda w0: regA[:, w0:w0 + 512]
        _h = lambda w0: regA[:, w0:w0 + 256].bitcast(BF16)[:, 0:512]
        shared = {"DG4": _f(0), "Dt4": _f(512)}
        for i_, n_ in enumerate(("Mt4", "Nn4", "Qa", "Qta", "Qb", "Qtb", "X4", "KG4")):
            shared[n_] = _h(3072 + 256 * i_)
        sharedB = {n_: Buf(n_) for n_ in shared}
        QSET = []
        for st_ in range(2):
            d_ = dict(shared)
            b_ = dict(sharedB)
            d_["EGR4"] = _f(1024 + 512 * st_)
            d_["BU4"] = _f(2048 + 512 * st_)
            for i_, n_ in enumerate(("AT4", "KD4", "WT4", "QD4")):
                d_[n_] = _h(5120 + 512 * i_ + 256 * st_)
            for n_ in ("EGR4", "BU4", "AT4", "KD4", "WT4", "QD4"):
                b_[n_] = Buf(n_ + str(st_))
            QSET.append((d_, b_))
        q3 = lambda a: a.rearrange("p (u i) -> p u i", u=4)
        VN = self.sb(128, BF16)
        vnb = Buf("VN")
        S = self.sb(128)
        Sb = Buf("S")
        Sbf = self.sb(128, BF16)
        Sbfb = Buf("Sbf")
        oth = [self.sb(T, BF16) for _ in range(2)]
        othb = [Buf(), Buf()]
        sm = self.sb(64)
        smb = Buf()
        fw.op(dve, lambda e: e.memset(raw[:, 0:3], 0.0), writes=[rb])
        nbk = [0]

        def nb_():
            nbk[0] += 1
            return nbk[0] % 8

        for h in range(16):
            fw.barrier()
            kinds = (("q", 0, qT), ("k", 2048, kT), ("v", 4096, vT))

            def emit_proj(n_):
                s_ = nbk[0] % 2
                nbk[0] += 1
                c0_ = kinds[n_][1]
                self.proj_fm(bigv, xTb, w_in[:, c0_ + h * 128: c0_ + (h + 1) * 128], wt[s_], wtb[s_])

            emit_proj(0)
            for kn, (kind, col0, dstT) in enumerate(kinds):
                for tc in range(4):
                    dstp = raw[:, 3 + tc * 512: 3 + (tc + 1) * 512]
                    if tc % 2 == 0:
                        fw.op(dve, lambda e, dstp=dstp, tc=tc: e.tensor_copy(out=dstp, in_=self.bank(tc)),
                              reads=[self.pbank[tc]], writes=[rb])
                    else:
                        fw.op(act, lambda e, dstp=dstp, tc=tc: e.activation(out=dstp, in_=self.bank(tc), func=AF.Copy),
                              reads=[self.pbank[tc]], writes=[rb])
                if kn + 1 < 3:
                    emit_proj(kn + 1)
                ch = (col0 // 128) + h
                fw.op(dve, lambda e, ch=ch: e.tensor_scalar(out=cacc, in0=raw[:, 0:T], scalar1=CWv[:, ch, 0:1], scalar2=None,
                                                            op0=ALU.mult), reads=[rb, gb], writes=[rb])
                for j in range(1, 4):
                    fw.op(dve, lambda e, ch=ch, j=j: e.scalar_tensor_tensor(
                        out=cacc, in0=raw[:, j:j + T], scalar=CWv[:, ch, j:j + 1], in1=cacc, op0=ALU.mult, op1=ALU.add),
                        reads=[rb, gb], writes=[rb])
                if kind == "v":
                    fw.op(act, lambda e: e.activation(out=vT, in_=cacc, func=AF.Silu), reads=[rb], writes=[qkvb])
                    continue
                fw.op(act, lambda e: e.activation(out=tmpq, in_=cacc, func=AF.Silu), reads=[rb], writes=[rb])
                sq16 = raw[:, 8:8 + T // 2].bitcast(BF16)
                fw.op(act, lambda e: e.activation(out=sq16, in_=tmpq, func=AF.Square), reads=[rb], writes=[rb])
                for tc in range(4):
                    fw.op(pe, lambda e, tc=tc: e.matmul(self.bank(4 + tc), lhsT=self.onesb, rhs=sq16[:, tc * 512:(tc + 1) * 512],
                                                        start=True, stop=True), reads=[rb, self.cb], writes=[self.pbank[4 + tc]])
                    cs_ = cacc[:, tc * 512:(tc + 1) * 512]
                    fw.op(dve, lambda e, tc=tc, cs_=cs_: e.tensor_scalar(out=cs_, in0=self.bank(4 + tc), scalar1=RMS_EPS,
                                                                         scalar2=None, op0=ALU.add),
                          reads=[self.pbank[4 + tc], rb], writes=[rb])
                fw.op(act, lambda e: e.activation(out=cacc, in_=cacc, func=AF.Sqrt), reads=[rb], writes=[rb])
                fw.op(dve, lambda e: e.reciprocal(out=cacc, in_=cacc), reads=[rb], writes=[rb])
                sc_ = (128.0 ** -0.5) if kind == "q" else 1.0
                fw.op(dve, lambda e, dstT=dstT, sc_=sc_: e.scalar_tensor_tensor(out=dstT, in0=tmpq, scalar=sc_, in1=cacc,
                                                                                op0=ALU.mult, op1=ALU.mult),
                      reads=[rb], writes=[qkvb])
            for srcT, dv_ in ((kT, ktv), (vT, vtv)):
                for g in range(4):
                    bk = nb_()
                    bkb = self.bank(bk, BF16)
                    for j in range(4):
                        P_ = g * 4 + j
                        fw.op(pe, lambda e, srcT=srcT, P_=P_, j=j, bkb=bkb: e.transpose(
                            out=bkb[:, j * 128:(j + 1) * 128], in_=srcT[:, P_ * 128:(P_ + 1) * 128],
                            identity=self.identb), reads=[qkvb, self.cb], writes=[self.pbank[bk]])
                    fw.op(act, lambda e, dv_=dv_, g=g, bkb=bkb: e.activation(
                        out=dv_[:, g * 4:(g + 1) * 4, :], in_=bkb[:, 0:512].rearrange("p (a b) -> p a b", a=4), func=AF.Copy),
                        reads=[self.pbank[bk]], writes=[tokb])
            fw.barrier()
            fw.op(dve, lambda e: e.memset(S, 0.0), writes=[Sb])
            fw.op(dve, lambda e: e.memset(Sbf, 0.0), writes=[Sbfb])
            def prep_gen(Q, L_, QB):
                    cols4 = slice(Q * 512, (Q + 1) * 512)
                    P0 = Q * 4
                    bc4 = lambda a: a[:, P0:P0 + 4, h].unsqueeze(2).to_broadcast([128, 4, 128])
                    fw.op(pool, lambda e: e.tensor_tensor(out=q3(L_["DG4"]), in0=id4, in1=bc4(GCv), op=ALU.mult),
                          reads=[gb, self.cb], writes=[QB["DG4"]])
                    yield
                    bA, bB = nb_(), nb_()
                    fw.op(pe, lambda e: e.matmul(self.bank(bA), lhsT=self.ones, rhs=L_["DG4"], start=True, stop=True),
                          reads=[QB["DG4"], self.cb], writes=[self.pbank[bA]])
                    fw.op(pe, lambda e: e.matmul(self.bank(bB), lhsT=self.ones, rhs=L_["DG4"], start=True, stop=False),
                          reads=[QB["DG4"], self.cb], writes=[self.pbank[bB]])
                    fw.op(pe, lambda e: e.matmul(self.bank(bB), lhsT=self.ident, rhs=NEGM4, start=False, stop=True),
                          reads=[gcb, self.cb], writes=[self.pbank[bB]])
                    fw.op(act, lambda e: e.activation(out=L_["EGR4"], in_=self.bank(bA), func=AF.Exp), reads=[self.pbank[bA]],
                          writes=[QB["EGR4"]])
                    yield
                    for u in range(4):
                        fw.op(act, lambda e, u=u: e.activation(out=L_["Dt4"][:, u * 128:(u + 1) * 128],
                                                               in_=self.bank(bB)[:, u * 128:(u + 1) * 128], func=AF.Exp,
                                                               bias=NGCv[:, P0 + u, h:h + 1], scale=1.0),
                              reads=[self.pbank[bB], gb], writes=[QB["Dt4"]])
                        yield
                    bK, bQ = nb_(), nb_()
                    for u in range(4):
                        cu = slice((P0 + u) * 128, (P0 + u + 1) * 128)
                        fw.op(pe, lambda e, u=u, cu=cu: e.matmul(self.bank(bK)[:, u * 128:(u + 1) * 128], lhsT=kT[:, cu], rhs=kT[:, cu],
                                                                 start=True, stop=True), reads=[qkvb], writes=[self.pbank[bK]])
                    for u in range(4):
                        cu = slice((P0 + u) * 128, (P0 + u + 1) * 128)
                        fw.op(pe, lambda e, u=u, cu=cu: e.matmul(self.bank(bQ)[:, u * 128:(u + 1) * 128], lhsT=kT[:, cu], rhs=qT[:, cu],
                                                                 start=True, stop=True), reads=[qkvb], writes=[self.pbank[bQ]])
                    fw.op(dve, lambda e: e.tensor_tensor(out=q3(L_["Mt4"]), in0=self.bank(bK).rearrange("p (u i) -> p u i", u=4),
                                                         in1=bc4(BTv), op=ALU.mult), reads=[self.pbank[bK], gb], writes=[QB["Mt4"]])
                    yield
                    fw.op(dve, lambda e: e.tensor_tensor(out=L_["Mt4"], in0=L_["Mt4"], in1=L_["Dt4"], op=ALU.mult),
                          reads=[QB["Dt4"], QB["Mt4"]], writes=[QB["Mt4"]])
                    yield
                    fw.op(pool, lambda e: e.affine_select(out=q3(L_["Mt4"]), in_=q3(L_["Mt4"]), pattern=[[0, 4], [1, 128]],
                                                          compare_op=ALU.not_equal, fill=0.0, base=0, channel_multiplier=-1),
                          reads=[QB["Mt4"]], writes=[QB["Mt4"]])
                    yield
                    fw.op(dve, lambda e: e.tensor_tensor(out=L_["AT4"], in0=self.bank(bQ), in1=L_["Dt4"], op=ALU.mult),
                          reads=[self.pbank[bQ], QB["Dt4"]], writes=[QB["AT4"]])
                    yield
                    bN = nb_()
                    bNb = self.bank(bN, BF16)
                    for u in range(4):
                        fw.op(pe, lambda e, u=u: e.transpose(out=bNb[:, u * 128:(u + 1) * 128],
                                                             in_=L_["Mt4"][:, u * 128:(u + 1) * 128], identity=self.identb),
                              reads=[QB["Mt4"], self.cb], writes=[self.pbank[bN]])
                    fw.op(act, lambda e: e.activation(out=L_["Nn4"], in_=bNb[:, 0:512], func=AF.Copy), reads=[self.pbank[bN]],
                          writes=[QB["Nn4"]])
                    yield
                    fw.op(pool, lambda e: e.tensor_tensor(out=q3(L_["X4"]), in0=id4, in1=q3(L_["Mt4"]), op=ALU.subtract),
                          reads=[QB["Mt4"], self.cb], writes=[QB["X4"]])
                    yield
                    Qn, Qtn = "Mt4", "Nn4"
                    pp_ = [("Qa", "Qta"), ("Qb", "Qtb")]
                    for lvl in range(1, 6):
                        Qo, Qto = pp_[lvl % 2]
                        bt = nb_()
                        for u in range(4):
                            us = slice(u * 128, (u + 1) * 128)
                            fw.op(pe, lambda e, us=us, Qn=Qn, Qtn=Qtn, bt=bt: e.matmul(self.bank(bt)[:, us], lhsT=L_[Qn][:, us],
                                                                                       rhs=L_[Qtn][:, us], start=True, stop=True),
                                  reads=[QB[Qn], QB[Qtn]], writes=[self.pbank[bt]])
                        fw.op(act, lambda e, Qto=Qto, bt=bt: e.activation(out=L_[Qto], in_=self.bank(bt), func=AF.Copy),
                              reads=[self.pbank[bt]], writes=[QB[Qto]])
                        yield
                        if lvl < 5:
                            bq = nb_()
                            for u in range(4):
                                us = slice(u * 128, (u + 1) * 128)
                                fw.op(pe, lambda e, us=us, Qn=Qn, Qtn=Qtn, bq=bq: e.matmul(self.bank(bq)[:, us], lhsT=L_[Qtn][:, us],
                                                                                           rhs=L_[Qn][:, us], start=True, stop=True),
                                      reads=[QB[Qn], QB[Qtn]], writes=[self.pbank[bq]])
                            fw.op(dve, lambda e, Qo=Qo, bq=bq: e.tensor_copy(out=L_[Qo], in_=self.bank(bq)),
                                  reads=[self.pbank[bq]], writes=[QB[Qo]])
                            yield
                        bx = nb_()
                        for u in range(4):
                            us = slice(u * 128, (u + 1) * 128)
                            fw.op(pe, lambda e, us=us, Qto=Qto, bx=bx: e.matmul(self.bank(bx)[:, us], lhsT=L_[Qto][:, us],
                                                                                rhs=L_["X4"][:, us], start=True, stop=True),
                                  reads=[QB[Qto], QB["X4"]], writes=[self.pbank[bx]])
                        fw.op(dve, lambda e, bx=bx: e.tensor_tensor(out=L_["X4"], in0=L_["X4"], in1=self.bank(bx), op=ALU.add),
                              reads=[self.pbank[bx], QB["X4"]], writes=[QB["X4"]])
                        yield
                        Qn, Qtn = Qo, Qto
                    fw.op(pool, lambda e: e.tensor_tensor(out=q3(L_["KG4"]), in0=ktv[:, P0:P0 + 4, :], in1=bc4(EGCv), op=ALU.mult),
                          reads=[tokb, gb], writes=[QB["KG4"]])
                    yield
                    fw.op(pool, lambda e: e.tensor_tensor(out=q3(L_["KD4"]), in0=ktv[:, P0:P0 + 4, :], in1=bc4(EKDv), op=ALU.mult),
                          reads=[tokb, gb], writes=[QB["KD4"]])
                    yield
                    bW, bU = nb_(), nb_()
                    for u in range(4):
                        us = slice(u * 128, (u + 1) * 128)
                        fw.op(pe, lambda e, us=us: e.matmul(self.bank(bW)[:, us], lhsT=L_["KG4"][:, us], rhs=L_["X4"][:, us],
                                                            start=True, stop=True), reads=[QB["KG4"], QB["X4"]], writes=[self.pbank[bW]])
                    for u in range(4):
                        us = slice(u * 128, (u + 1) * 128)
                        fw.op(pe, lambda e, us=us, u=u: e.matmul(self.bank(bU)[:, us], lhsT=L_["X4"][:, us], rhs=vtv[:, P0 + u, :],
                                                                 start=True, stop=True), reads=[QB["X4"], tokb], writes=[self.pbank[bU]])
                    fw.op(act, lambda e: e.activation(out=L_["WT4"], in_=self.bank(bW), func=AF.Copy), reads=[self.pbank[bW]],
                          writes=[QB["WT4"]])
                    yield
                    fw.op(dve, lambda e: e.tensor_tensor(out=q3(L_["BU4"]), in0=self.bank(bU).rearrange("p (u i) -> p u i", u=4),
                                                         in1=bc4(BTv), op=ALU.mult), reads=[self.pbank[bU], gb], writes=[QB["BU4"]])
                    yield
                    fw.op(dve, lambda e: e.tensor_tensor(out=L_["QD4"], in0=qT[:, cols4], in1=L_["EGR4"], op=ALU.mult),
                          reads=[qkvb, QB["EGR4"]], writes=[QB["QD4"]])
                    yield
                    yield

            def chunk_gen(Q, L_, QB):
                    P0 = Q * 4
                    for u in range(4):
                        us = slice(u * 128, (u + 1) * 128)
                        P_ = P0 + u
                        for c in range(2):
                            rows = slice(c * 64, (c + 1) * 64)
                            bw = nb_()
                            fw.op(pe, lambda e, us=us, bw=bw: e.matmul(self.bank(bw)[:, 0:128], lhsT=L_["WT4"][:, us], rhs=Sbf,
                                                                       start=True, stop=True), reads=[QB["WT4"], Sbfb],
                                  writes=[self.pbank[bw]])
                            fw.op(dve, lambda e, rows=rows, bw=bw, P_=P_, us=us: e.scalar_tensor_tensor(
                                out=VN[rows, :], in0=self.bank(bw)[rows, 0:128], scalar=NBv[rows, P_, h:h + 1],
                                in1=L_["BU4"][rows, us], op0=ALU.mult, op1=ALU.add),
                                reads=[self.pbank[bw], gb, QB["BU4"]], writes=[vnb])
                            yield
                            bo = nb_()
                            fw.op(pe, lambda e, us=us, bo=bo: e.matmul(self.bank(bo)[:, 0:128], lhsT=L_["QD4"][:, us], rhs=Sbf,
                                                                       start=True, stop=False), reads=[QB["QD4"], Sbfb],
                                  writes=[self.pbank[bo]])
                            fw.op(pe, lambda e, us=us, bo=bo, rows=rows: e.matmul(self.bank(bo)[:, 0:128], lhsT=L_["AT4"][rows, us],
                                                                                  rhs=VN[rows, :], start=False, stop=True),
                                  reads=[QB["AT4"], vnb], writes=[self.pbank[bo]])
                            fw.op(act, lambda e, rows=rows, bo=bo, P_=P_: e.activation(out=otv[rows, P_, :], in_=self.bank(bo)[rows, 0:128],
                                                                                       func=AF.Copy), reads=[self.pbank[bo]], writes=[otb])
                            yield
                            bs = nb_()
                            fw.op(pe, lambda e, us=us, bs=bs, rows=rows: e.matmul(self.bank(bs)[:, 0:128], lhsT=L_["KD4"][rows, us],
                                                                                  rhs=VN[rows, :], start=True, stop=True),
                                  reads=[QB["KD4"], vnb], writes=[self.pbank[bs]])
                            cdc = u * 128 + c * 64 + 63
                            fw.op(dve, lambda e, bs=bs, cdc=cdc: e.scalar_tensor_tensor(
                                out=S, in0=S, scalar=L_["EGR4"][:, cdc:cdc + 1], in1=self.bank(bs)[:, 0:128], op0=ALU.mult, op1=ALU.add),
                                reads=[self.pbank[bs], QB["EGR4"], Sb], writes=[Sb])
                            yield
                            fw.op(act, lambda e: e.activation(out=Sbf, in_=S, func=AF.Copy), reads=[Sb], writes=[Sbfb])
                            yield
                    yield

            for _ in prep_gen(0, QSET[0][0], QSET[0][1]):
                pass
            for Q in range(4):
                gc_ = chunk_gen(Q, QSET[Q % 2][0], QSET[Q % 2][1])
                gp_ = prep_gen(Q + 1, QSET[(Q + 1) % 2][0], QSET[(Q + 1) % 2][1]) if Q < 3 else iter(())
                alive_c = alive_p = True
                while alive_c or alive_p:
                    if alive_c:
                        try:
                            next(gc_)
                        except StopIteration:
                            alive_c = False
                    if alive_p:
                        try:
                            next(gp_)
                        except StopIteration:
                            alive_p = False
            fw.barrier()
            zs = ktok_f
            zsv = zs.rearrange("p (a b) -> p a b", a=16)
            zb = Buf("zs")
            s = nbk[0] % 2
            nbk[0] += 1
            wzv = wt[s].rearrange("p (a b) -> p a b", a=16)
            fw.dma(out=wzv, in_=w_in[:, 6144 + h * 128: 6144 + (h + 1) * 128].rearrange("(kc p) f -> p kc f", p=128),
                   writes=[wtb[s]], q=pool)
            for g in range(4):
                bk = g
                for j in range(4):
                    i = g * 4 + j
                    for kc in range(16):
                        fw.op(pe, lambda e, i=i, j=j, kc=kc, bk=bk: e.matmul(
                            self.bank(bk)[:, j * 128:(j + 1) * 128], lhsT=bigv[:, kc, i * 128:(i + 1) * 128], rhs=wzv[:, kc, :],
                            start=(kc == 0), stop=(kc == 15)), reads=[xTb, wtb[s]], writes=[self.pbank[bk]])
                fw.op(act, lambda e, g=g, bk=bk: e.activation(out=zsv[:, g * 4:(g + 1) * 4, :],
                                                              in_=self.bank(bk).rearrange("p (a b) -> p a b", a=4), func=AF.Silu),
                      reads=[self.pbank[bk]], writes=[zb])
            sq = regA[:, 0:T]
            sqb = Buf("sq")
            fw.op(pool, lambda e: e.tensor_tensor(out=sq, in0=otok, in1=otok, op=ALU.mult), reads=[otb], writes=[sqb])
            ms = sm[:, 0:16]
            fw.op(dve, lambda e: e.tensor_reduce(out=ms, in_=sq.rearrange("p (a b) -> p a b", a=16), axis=AX.X, op=ALU.add),
                  reads=[sqb], writes=[smb])
            fw.op(dve, lambda e: e.tensor_scalar(out=ms, in0=ms, scalar1=1.0 / 128.0, scalar2=RMS_EPS, op0=ALU.mult, op1=ALU.add),
                  reads=[smb], writes=[smb])
            fw.op(act, lambda e: e.activation(out=ms, in_=ms, func=AF.Sqrt), reads=[smb], writes=[smb])
            fw.op(dve, lambda e: e.reciprocal(out=ms, in_=ms), reads=[smb], writes=[smb])
            fw.op(dve, lambda e: e.tensor_tensor(out=otv, in0=otv, in1=ms.unsqueeze(2).to_broadcast([128, 16, 128]), op=ALU.mult),
                  reads=[smb, otb], writes=[otb])
            fw.op(pool, lambda e: e.tensor_tensor(out=otv, in0=otv, in1=NGrow.unsqueeze(1).to_broadcast([128, 16, 128]),
                                                  op=ALU.mult), reads=[gb, otb], writes=[otb])
            fw.op(dve, lambda e: e.tensor_tensor(out=otok, in0=otok, in1=zs, op=ALU.mult), reads=[zb, otb], writes=[otb])
            s2 = h % 2
            for g in range(4):
                bk = 4 + g
                for j in range(4):
                    P_ = g * 4 + j
                    fw.op(pe, lambda e, P_=P_, j=j, bk=bk: e.transpose(out=self.bank(bk)[:, j * 128:(j + 1) * 128], in_=otv[:, P_, :],
                                                                      identity=self.ident), reads=[otb, self.cb], writes=[self.pbank[bk]])
                fw.op(act, lambda e, g=g, bk=bk, s2=s2: e.activation(out=oth[s2][:, g * 512:(g + 1) * 512], in_=self.bank(bk),
                                                                    func=AF.Copy), reads=[self.pbank[bk]], writes=[othb[s2]])
            fw.dma(out=OT[h], in_=oth[s2], reads=[othb[s2]], writes=[OTb])
        self.release(m0)
        self.outproj_ln(OT, OTb, W["gdn_w_out"][li], src, src_buf, W["ln_g"][L, 0], W["ln_b"][L, 0], dst, dst_buf)

    def init_yg(self):
        fw = self.fw
        m = self.mark()
        z = self.sb(D)
        zb = Buf()
        fw.op(fw.dve, lambda e: e.memset(z, 0.0), writes=[zb])
        fw.dma(out=self.dram["YG"][NSLOT:NSLOT + 128, :], in_=z, reads=[zb], writes=[self.dbuf["YG"]])
        self.release(m)


WEIGHT_SPECS = [
    ("ln_g", (4, 2, 2048)), ("ln_b", (4, 2, 2048)), ("moe_rg_w", (4, 2048, 4)), ("moe_rg_b", (4, 4)),
    ("moe_re_w", (4, 2048, 32)), ("moe_re_b", (4, 32)), ("moe_w_gate", (4, 32, 2048, 512)),
    ("moe_w_up", (4, 32, 2048, 512)), ("moe_w_down", (4, 32, 512, 2048)), ("gdn_w_in", (2, 2048, 8224)),
    ("gdn_conv_w", (2, 4, 6144)), ("gdn_a_log", (2, 16)), ("gdn_dt_bias", (2, 16)), ("gdn_norm_g", (2, 128)),
    ("gdn_w_out", (2, 2048, 2048)), ("ssm_w_in", (1, 2048, 2048)), ("ssm_b_re", (1, 128, 64, 16)),
    ("ssm_b_im", (1, 128, 64, 16)), ("ssm_c_re", (1, 128, 16, 64)), ("ssm_c_im", (1, 128, 16, 64)),
    ("ssm_a_re", (1, 128, 64)), ("ssm_a_im", (1, 128, 64)), ("ssm_log_dt", (1, 128)), ("ssm_d", (1, 2048)),
    ("ssm_w_glu", (1, 2048, 2048)), ("ssm_b_glu", (1, 2048)), ("ssm_w_out", (1, 2048, 2048)),
    ("dsa_w_in", (1, 2048, 4496)), ("dsa_w_out", (1, 2048, 2048)),
]


def build(stages, ext_in=(), ext_out=(), weights=None):
    nc = bass.Bass("TRN2", target_bir_lowering=False)
    st = contextlib.ExitStack()
    with st:
        k = K(nc, st, set(ext_in), set(ext_out))
        fw = k.fw
        used = weights if weights is not None else [n for n, _ in WEIGHT_SPECS]
        W = {}
        for n, shp in WEIGHT_SPECS:
            if n in used:
                W[n] = nc.dram_tensor(n, list(shp), F32, kind="ExternalInput").ap()
        k.W = W
        names = set()
        for stg in stages:
            names.update(stg[2:])
        if "x" in names:
            k.dram["x"] = nc.dram_tensor("x", [T, D], F32, kind="ExternalInput").ap()
            k.dbuf["x"] = Buf("x")
        k.dram["out"] = nc.dram_tensor("out", [T, D], F32, kind="ExternalOutput").ap()
        k.dbuf["out"] = Buf("out")
        k.dt("XA", [T, D], F32)
        k.dt("XM", [T, D], F32)
        k.dt("OT", [16, 128, T], BF16)
        k.dt("XG", [NSLOT + 128, D], BF16)
        k.dt("YG", [NSLOT + 128, D], F32)
        k.dt("U", [16, 128, T], F32)
        k.dt("YA", [16, 128, T], BF16)
        k.dt("QT", [16, 128, T], BF16)
        k.dt("QI", [16, 128, T], BF16)
        k.dt("MASKT", [16, 128, T], BF16)
        k.init_yg()
        for stg in stages:
            kind, L, src, dst = stg
            S, Sb, Dd, Db = k.dram[src], k.dbuf[src], k.dram[dst], k.dbuf[dst]
            if kind == "moe":
                k.moe(L, S, Sb, W, Dd, Db)
            elif kind == "gdn":
                k.gdn(L // 3, L, S, Sb, W, Dd, Db)
            elif kind == "s5":
                k.s5(L, S, Sb, W, Dd, Db)
            elif kind == "dsa":
                k.dsa(L, S, Sb, W, Dd, Db)
            else:
                raise ValueError(kind)
        fw.finish()
        k.stats = {e.name: e.n_instr for e in fw.engs}
    return nc, k


_MIXERS = ("gdn", "s5", "dsa")


def _stages():
    st = []
    for L in range(DEPTH):
        src = "x" if L == 0 else "XA"
        st.append((_MIXERS[L % 3], L, src, "XM"))
        st.append(("moe", L, "XM", "out" if L == DEPTH - 1 else "XA"))
    return st


def kernel(**inputs):
    x = np.ascontiguousarray(np.asarray(inputs["x"], dtype=np.float32))
    nc, _k = build(_stages())
    wts = {n: np.ascontiguousarray(np.asarray(inputs[n], dtype=np.float32)) for n, _ in WEIGHT_SPECS}
    n_cores = 8
    in_maps = []
    for c in range(n_cores):
        m = dict(wts)
        m["x"] = x[c]
        in_maps.append(m)
    res = run_bass_kernel_spmd(nc, in_maps, core_ids=list(range(n_cores)))
    return np.stack([np.asarray(r["out"], dtype=np.float32) for r in res.results], axis=0)
```

```python
import contextlib
import math
import numpy as np
import concourse.bass as bass
import concourse.mybir as mybir
from concourse.bass_utils import run_bass_kernel_spmd

F32 = mybir.dt.float32
BF16 = mybir.dt.bfloat16
I32 = mybir.dt.int32
AF = mybir.ActivationFunctionType
ALU = mybir.AluOpType
AX = mybir.AxisListType

T = 2048
D = 2048
NT = 16
DEPTH = 4
DN_ALPHA = (2.0 * DEPTH) ** 0.25
LN_EPS = 1e-5
RMS_EPS = 1e-6
NEXP = 32
FF = 512
CAP = 256
NSLOT = NEXP * CAP
GDN_IN = 8224
DSA_IN = 4496
NEG = -30000.0


class Buf:
    __slots__ = ("name", "writer", "readers")

    def __init__(self, name=""):
        self.name = name
        self.writer = None
        self.readers = []


class Eng:
    def __init__(self, fw, name, hw, is_pe=False):
        self.fw = fw
        self.name = name
        self.hw = hw
        self.is_pe = is_pe
        self.sems = []
        self.cnt = 0
        self.waited = {}
        self.n_instr = 0

    def cur_sem(self):
        if not self.sems or self.cnt >= self.fw.EPOCH:
            self.sems.append(self.fw.new_sem(f"{self.name}_p{len(self.sems)}"))
            self.cnt = 0
        return self.sems[-1]


class FW:
    EPOCH = 60000
    NP = 24

    def __init__(self, nc, stack):
        self.nc = nc
        self.stack = stack
        self.sem_handles = {}
        self.nsem = 0
        self.pe = Eng(self, "pe", nc.tensor, is_pe=True)
        self.dve = Eng(self, "dve", nc.vector)
        self.act = Eng(self, "act", nc.scalar)
        self.pool = Eng(self, "pool", nc.gpsimd)
        self.sp = Eng(self, "sp", nc.sync)
        self.engs = [self.pe, self.dve, self.act, self.pool, self.sp]
        self.dma_pool = {}
        self.dma_rr = {}
        self.all_dma_tokens = []

    def new_sem(self, name):
        h = self.stack.enter_context(self.nc.semaphore(name))
        self.nsem += 1
        self.sem_handles[self.nsem] = h
        return self.nsem

    def _wait(self, eng, tok):
        if tok is None:
            return
        key, val, src = tok
        if eng.is_pe and src == "pe":
            return
        if eng.waited.get(key, 0) >= val:
            return
        eng.waited[key] = val
        eng.hw.wait_ge(self.sem_handles[key], val)

    def _note_read(self, b, tok):
        b.readers.append(tok)
        if len(b.readers) > 16:
            last = {}
            for r in b.readers:
                k2 = (r[2], r[0])
                if k2 not in last or last[k2][1] < r[1]:
                    last[k2] = r
            b.readers = list(last.values())

    def op(self, eng, fn, reads=(), writes=()):
        for b in reads:
            self._wait(eng, b.writer)
        for b in writes:
            self._wait(eng, b.writer)
            for r in b.readers:
                if r[2] == eng.name:
                    continue
                self._wait(eng, r)
        ins = fn(eng.hw)
        key = eng.cur_sem()
        eng.cnt += 1
        ins.then_inc(self.sem_handles[key], 1)
        tok = (key, eng.cnt, eng.name)
        for b in reads:
            self._note_read(b, tok)
        for b in writes:
            b.writer = tok
            b.readers = []
        eng.n_instr += 1
        return tok

    def dma(self, out, in_, reads=(), writes=(), q=None, indirect=None, **kw):
        eng = q or self.sp
        for b in reads:
            self._wait(eng, b.writer)
        for b in writes:
            self._wait(eng, b.writer)
            for r in b.readers:
                self._wait(eng, r)
        pool = self.dma_pool.setdefault(eng.name, [])
        if len(pool) < self.NP:
            pool.append([self.new_sem(f"dma_{eng.name}_{len(pool)}"), 0])
            slot = pool[-1]
        else:
            i = self.dma_rr.get(eng.name, 0)
            slot = pool[i % self.NP]
            self.dma_rr[eng.name] = i + 1
        key, uses = slot
        if uses > 0:
            self._wait(eng, (key, 16 * uses, "dma"))
        if indirect is None:
            ins = eng.hw.dma_start(out=out, in_=in_, **kw)
        else:
            ins = eng.hw.indirect_dma_start(out=out, in_=in_, **indirect)
        slot[1] = uses + 1
        ins.then_inc(self.sem_handles[key], 16)
        tok = (key, 16 * (uses + 1), "dma")
        for b in reads:
            self._note_read(b, tok)
        for b in writes:
            b.writer = tok
            b.readers = []
        self.all_dma_tokens.append(tok)
        if len(self.all_dma_tokens) > 400:
            self._compact_dma()
        eng.n_instr += 1
        return tok

    def _compact_dma(self):
        last = {}
        for t in self.all_dma_tokens:
            if t[0] not in last or last[t[0]][1] < t[1]:
                last[t[0]] = t
        self.all_dma_tokens = list(last.values())

    def barrier(self, engs=None):
        toks = []
        for e in self.engs:
            if e.sems and e.cnt > 0:
                toks.append((e.sems[-1], e.cnt, e.name))
        self._compact_dma()
        toks += self.all_dma_tokens
        for e in (engs or self.engs):
            for key, val, src in toks:
                if src == e.name:
                    continue
                if e.waited.get(key, 0) >= val:
                    continue
                e.waited[key] = val
                e.hw.wait_ge(self.sem_handles[key], val)

    def finish(self):
        self.barrier(engs=[self.sp])


class K:
    ARENA = 46000

    def __init__(self, nc, st, ext_in, ext_out):
        self.nc = nc
        self.st = st
        self.fw = FW(nc, st)
        self.ext_in = ext_in
        self.ext_out = ext_out
        self.arena = st.enter_context(nc.sbuf_tensor("arena", [128, self.ARENA], F32))
        self.psum = st.enter_context(nc.psum_tensor("psum", [128, 4096], F32))
        self.off = 0
        self.pbank = [Buf(f"bank{i}") for i in range(8)]
        self.dram = {}
        self.dbuf = {}
        self._consts()

    def sb(self, n, dt=F32):
        words = n if dt != BF16 else (n + 1) // 2
        words = (words + 7) // 8 * 8
        assert self.off + words <= self.ARENA, (self.off, words)
        ap = self.arena[:, self.off:self.off + words]
        self.off += words
        if dt == BF16:
            ap = ap.bitcast(BF16)[:, 0:n]
        elif dt == I32:
            ap = ap.bitcast(I32)[:, 0:n]
        else:
            ap = ap[:, 0:n]
        return ap

    def mark(self):
        return self.off

    def release(self, m):
        self.fw.barrier()
        self.off = m

    def bank(self, i, dt=F32):
        ap = self.psum[:, i * 512:(i + 1) * 512]
        if dt == BF16:
            ap = ap.bitcast(BF16)
        return ap

    def dt(self, name, shape, dtype):
        kind = "Internal"
        if name in self.ext_in:
            kind = "ExternalInput"
        elif name in self.ext_out:
            kind = "ExternalOutput"
        t = self.nc.dram_tensor(name, list(shape), dtype, kind=kind).ap()
        self.dram[name] = t
        self.dbuf[name] = Buf(name)
        return t

    def _consts(self):
        fw = self.fw
        P = fw.pool
        self.cb = Buf("consts")
        cb = self.cb
        self.ident = self.sb(128)
        self.ones = self.sb(128)
        self.identb = self.sb(128, BF16)
        self.onesb = self.sb(128, BF16)
        self.sltb = self.sb(128, BF16)
        slt = self.sb(128)
        fw.op(P, lambda e: e.memset(self.ident, 0.0), writes=[cb])
        fw.op(P, lambda e: e.affine_select(out=self.ident, in_=self.ident, pattern=[[-1, 128]],
                                           compare_op=ALU.not_equal, fill=1.0, base=0, channel_multiplier=1),
              reads=[cb], writes=[cb])
        fw.op(P, lambda e: e.memset(self.ones, 1.0), writes=[cb])
        fw.op(P, lambda e: e.memset(slt, 1.0), writes=[cb])
        fw.op(P, lambda e: e.affine_select(out=slt, in_=slt, pattern=[[1, 128]], compare_op=ALU.is_gt,
                                           fill=0.0, base=0, channel_multiplier=-1), reads=[cb], writes=[cb])
        fw.op(P, lambda e: e.tensor_copy(out=self.identb, in_=self.ident), reads=[cb], writes=[cb])
        fw.op(P, lambda e: e.tensor_copy(out=self.onesb, in_=self.ones), reads=[cb], writes=[cb])
        fw.op(P, lambda e: e.tensor_copy(out=self.sltb, in_=slt), reads=[cb], writes=[cb])
        self.ebase = self.sb(NEXP)
        fw.op(P, lambda e: e.iota(out=self.ebase, pattern=[[CAP, NEXP]], base=0, channel_multiplier=0,
                                  allow_small_or_imprecise_dtypes=True), writes=[cb])
        self.trash = self.sb(1)
        fw.op(P, lambda e: e.iota(out=self.trash, pattern=[[0, 1]], base=NSLOT, channel_multiplier=1,
                                  allow_small_or_imprecise_dtypes=True), writes=[cb])
        self.const_mark = self.off

    def load_row(self, dst, src_row, buf, n):
        src = src_row.rearrange("(o n) -> o n", o=1).to_broadcast([128, n]) if len(src_row.shape) == 1 \
            else src_row.to_broadcast([128, n])
        self.fw.dma(out=dst, in_=src, writes=[buf])

    def build_xT(self, src, src_buf, xT, xT_buf):
        fw = self.fw
        m = self.mark()
        xin = [self.sb(D) for _ in range(2)]
        xb = [Buf("xin0"), Buf("xin1")]
        for i in range(NT):
            s = i % 2
            fw.dma(out=xin[s], in_=src[i * 128:(i + 1) * 128, :], reads=[src_buf], writes=[xb[s]])
            for g in range(4):
                bk = (i * 4 + g) % 8
                pb = self.pbank[bk]
                for j in range(4):
                    fc = g * 4 + j
                    fw.op(fw.pe, lambda e, fc=fc, j=j, bk=bk, s=s: e.transpose(
                        out=self.bank(bk)[:, j * 128:(j + 1) * 128], in_=xin[s][:, fc * 128:(fc + 1) * 128],
                        identity=self.ident), reads=[xb[s], self.cb], writes=[pb])
                dst = xT[:, g * 4:(g + 1) * 4, i * 128:(i + 1) * 128]
                srcp = self.bank(bk).rearrange("p (a b) -> p a b", a=4)
                if g % 2 == 0:
                    fw.op(fw.dve, lambda e, dst=dst, srcp=srcp: e.tensor_copy(out=dst, in_=srcp),
                          reads=[pb], writes=[xT_buf])
                else:
                    fw.op(fw.act, lambda e, dst=dst, srcp=srcp: e.activation(out=dst, in_=srcp, func=AF.Copy),
                          reads=[pb], writes=[xT_buf])
        self.release(m)

    def ln_tail(self, z, zb, grow, brow, gbuf, tmp, tmpb, stat, statb, out, outb):
        fw = self.fw
        mean = stat[:, 0:1]
        ssq = stat[:, 1:2]
        rstd = stat[:, 2:3]
        nmean = stat[:, 3:4]
        fw.op(fw.dve, lambda e: e.tensor_reduce(out=mean, in_=z, axis=AX.X, op=ALU.add), reads=[zb], writes=[statb])
        fw.op(fw.dve, lambda e: e.tensor_scalar(out=nmean, in0=mean, scalar1=-1.0 / D, scalar2=None, op0=ALU.mult),
              reads=[statb], writes=[statb])
        fw.op(fw.act, lambda e: e.activation(out=tmp, in_=z, func=AF.Square, bias=nmean, scale=1.0, accum_out=ssq),
              reads=[zb, statb], writes=[tmpb, statb])
        fw.op(fw.dve, lambda e: e.tensor_scalar(out=rstd, in0=ssq, scalar1=1.0 / D, scalar2=LN_EPS, op0=ALU.mult,
                                                op1=ALU.add), reads=[statb], writes=[statb])
        fw.op(fw.act, lambda e: e.activation(out=rstd, in_=rstd, func=AF.Sqrt), reads=[statb], writes=[statb])
        fw.op(fw.dve, lambda e: e.reciprocal(out=rstd, in_=rstd), reads=[statb], writes=[statb])
        fw.op(fw.dve, lambda e: e.tensor_scalar(out=tmp, in0=z, scalar1=nmean, scalar2=rstd, op0=ALU.add,
                                                op1=ALU.mult), reads=[zb, statb, tmpb], writes=[tmpb])
        fw.op(fw.dve, lambda e: e.tensor_tensor(out=tmp, in0=tmp, in1=grow, op=ALU.mult), reads=[tmpb, gbuf],
              writes=[tmpb])
        fw.op(fw.dve, lambda e: e.tensor_tensor(out=out, in0=tmp, in1=brow, op=ALU.add), reads=[tmpb, gbuf],
              writes=[outb])

    def outproj_ln(self, OT, OTb, w_out, resid, resid_buf, ln_g, ln_b, dst, dst_buf):
        fw = self.fw
        m = self.mark()
        W = self.sb(16 * D, BF16)
        Wv = W.rearrange("p (a b) -> p a b", a=16)
        Wb = Buf("wout")
        wsrc = w_out.rearrange("(fc p) m -> p fc m", p=128)
        for q in range(4):
            fw.dma(out=Wv[:, q * 4:(q + 1) * 4, :], in_=wsrc[:, q * 4:(q + 1) * 4, :], writes=[Wb], q=fw.pool)
        grow = self.sb(D)
        brow = self.sb(D)
        gbuf = Buf("lnrows")
        self.load_row(grow, ln_g, gbuf, D)
        self.load_row(brow, ln_b, gbuf, D)
        oT = [self.sb(16 * 128, BF16) for _ in range(2)]
        oTb = [Buf(), Buf()]
        rz = [self.sb(D) for _ in range(2)]
        rzb = [Buf(), Buf()]
        tmp = self.sb(D)
        tmpb = Buf()
        stat = [self.sb(4) for _ in range(2)]
        statb = [Buf(), Buf()]
        OTv = OT.rearrange("fc p t -> p fc t")
        for i in range(NT):
            s = i % 2
            fw.dma(out=oT[s].rearrange("p (a b) -> p a b", a=16), in_=OTv[:, :, i * 128:(i + 1) * 128],
                   reads=[OTb], writes=[oTb[s]])
            fw.dma(out=rz[s], in_=resid[i * 128:(i + 1) * 128, :], reads=[resid_buf], writes=[rzb[s]])
            for mc in range(4):
                bk = (i * 4 + mc) % 8
                pb = self.pbank[bk]
                for fc in range(16):
                    fw.op(fw.pe, lambda e, fc=fc, mc=mc, bk=bk, s=s: e.matmul(
                        self.bank(bk), lhsT=oT[s][:, fc * 128:(fc + 1) * 128], rhs=Wv[:, fc, mc * 512:(mc + 1) * 512],
                        start=(fc == 0), stop=(fc == 15)), reads=[oTb[s], Wb], writes=[pb])
                zs = rz[s][:, mc * 512:(mc + 1) * 512]
                fw.op(fw.dve, lambda e, zs=zs, bk=bk: e.scalar_tensor_tensor(
                    out=zs, in0=zs, scalar=DN_ALPHA, in1=self.bank(bk), op0=ALU.mult, op1=ALU.add),
                    reads=[pb, rzb[s]], writes=[rzb[s]])
            self.ln_tail(rz[s], rzb[s], grow, brow, gbuf, tmp, tmpb, stat[s], statb[s], rz[s], rzb[s])
            fw.dma(out=dst[i * 128:(i + 1) * 128, :], in_=rz[s], reads=[rzb[s]], writes=[dst_buf])
        self.release(m)

    def moe(self, L, xm, xm_buf, W, dst, dst_buf):
        fw = self.fw
        XG, YG = self.dram["XG"], self.dram["YG"]
        XGb, YGb = self.dbuf["XG"], self.dbuf["YG"]
        m0 = self.mark()
        dest = self.sb(NT * 2, I32)
        gate = self.sb(NT * 2)
        routeb = Buf("route")
        acum = self.sb(NEXP)
        acumb = self.sb(NEXP, BF16)
        acb = Buf("acum")
        fw.op(fw.dve, lambda e: e.memset(acum, 0.0), writes=[acb])
        fw.op(fw.dve, lambda e: e.memset(acumb, 0.0), writes=[acb])
        m1 = self.mark()
        wr = self.sb(16 * 36)
        wrv = wr.rearrange("p (a b) -> p a b", a=16)
        wrb = Buf("wr")
        fw.dma(out=wrv[:, :, 0:4], in_=W["moe_rg_w"][L].rearrange("(fc p) g -> p fc g", p=128), writes=[wrb])
        fw.dma(out=wrv[:, :, 4:36], in_=W["moe_re_w"][L].rearrange("(fc p) g -> p fc g", p=128), writes=[wrb])
        brow = self.sb(36)
        self.load_row(brow[:, 0:4], W["moe_rg_b"][L], wrb, 4)
        self.load_row(brow[:, 4:36], W["moe_re_b"][L], wrb, 32)
        xin = [self.sb(D) for _ in range(2)]
        xinb = [Buf(), Buf()]
        xTf = [self.sb(16 * 128) for _ in range(2)]
        xTfb = [Buf(), Buf()]
        xbf = [self.sb(D, BF16) for _ in range(2)]
        xbfb = [Buf(), Buf()]
        sm = [self.sb(256) for _ in range(2)]
        smb = [Buf(), Buf()]
        for i in range(NT):
            s = i % 2
            fw.dma(out=xin[s], in_=xm[i * 128:(i + 1) * 128, :], reads=[xm_buf], writes=[xinb[s]])
            for g in range(4):
                bk = g
                pb = self.pbank[bk]
                for j in range(4):
                    fc = g * 4 + j
                    fw.op(fw.pe, lambda e, fc=fc, j=j, bk=bk, s=s: e.transpose(
                        out=self.bank(bk)[:, j * 128:(j + 1) * 128], in_=xin[s][:, fc * 128:(fc + 1) * 128],
                        identity=self.ident), reads=[xinb[s], self.cb], writes=[pb])
                dstp = xTf[s][:, g * 512:(g + 1) * 512]
                if g % 2 == 0:
                    fw.op(fw.dve, lambda e, dstp=dstp, bk=bk: e.tensor_copy(out=dstp, in_=self.bank(bk)),
                          reads=[pb], writes=[xTfb[s]])
                else:
                    fw.op(fw.act, lambda e, dstp=dstp, bk=bk: e.activation(out=dstp, in_=self.bank(bk), func=AF.Copy),
                          reads=[pb], writes=[xTfb[s]])
            fw.op(fw.act, lambda e, s=s: e.activation(out=xbf[s], in_=xin[s], func=AF.Copy), reads=[xinb[s]], writes=[xbfb[s]])
            pl = self.pbank[4]
            lg_ps = self.bank(4)[:, 0:36]
            for fc in range(16):
                fw.op(fw.pe, lambda e, fc=fc, s=s: e.matmul(lg_ps, lhsT=xTf[s][:, fc * 128:(fc + 1) * 128],
                                                           rhs=wrv[:, fc, :], start=(fc == 0), stop=(fc == 15)),
                      reads=[xTfb[s], wrb], writes=[pl])
            S = sm[s]
            Sb = smb[s]
            lg = S[:, 0:36]
            fw.op(fw.dve, lambda e, lg=lg: e.tensor_tensor(out=lg, in0=lg_ps, in1=brow, op=ALU.add),
                  reads=[pl, wrb], writes=[Sb])
            gmax = S[:, 36:37]
            ngmax = S[:, 37:38]
            gsum = S[:, 38:39]
            pg = S[:, 39:40]
            ohg = S[:, 40:44]
            ex4 = S[:, 44:48]
            fw.op(fw.dve, lambda e: e.tensor_reduce(out=gmax, in_=lg[:, 0:4], axis=AX.X, op=ALU.max),
                  reads=[Sb], writes=[Sb])
            fw.op(fw.dve, lambda e: e.tensor_scalar(out=ngmax, in0=gmax, scalar1=-1.0, scalar2=None, op0=ALU.mult),
                  reads=[Sb], writes=[Sb])
            fw.op(fw.act, lambda e: e.activation(out=ex4, in_=lg[:, 0:4], func=AF.Exp, bias=ngmax, scale=1.0,
                                                 accum_out=gsum), reads=[Sb], writes=[Sb])
            fw.op(fw.dve, lambda e: e.reciprocal(out=pg, in_=gsum), reads=[Sb], writes=[Sb])
            fw.op(fw.dve, lambda e: e.tensor_scalar(out=ohg, in0=lg[:, 0:4], scalar1=gmax, scalar2=None,
                                                    op0=ALU.is_ge), reads=[Sb], writes=[Sb])
            esel = S[:, 48:56]
            le = lg[:, 4:36]
            fw.op(fw.dve, lambda e: e.tensor_scalar(out=esel, in0=le[:, 0:8], scalar1=ohg[:, 0:1], scalar2=None,
                                                    op0=ALU.mult), reads=[Sb], writes=[Sb])
            for g in range(1, 4):
                fw.op(fw.dve, lambda e, g=g: e.scalar_tensor_tensor(
                    out=esel, in0=le[:, g * 8:(g + 1) * 8], scalar=ohg[:, g:g + 1], in1=esel, op0=ALU.mult,
                    op1=ALU.add), reads=[Sb], writes=[Sb])
            top8 = S[:, 56:64]
            fw.op(fw.dve, lambda e: e.max(out=top8, in_=esel), reads=[Sb], writes=[Sb])
            oh1 = S[:, 64:72]
            oh2 = S[:, 72:80]
            fw.op(fw.dve, lambda e: e.tensor_scalar(out=oh1, in0=esel, scalar1=top8[:, 0:1], scalar2=None,
                                                    op0=ALU.is_equal), reads=[Sb], writes=[Sb])
            fw.op(fw.dve, lambda e: e.tensor_scalar(out=oh2, in0=esel, scalar1=top8[:, 1:2], scalar2=None,
                                                    op0=ALU.is_equal), reads=[Sb], writes=[Sb])
            dv = S[:, 80:82]
            fw.op(fw.dve, lambda e: e.tensor_tensor(out=dv[:, 0:1], in0=top8[:, 0:1], in1=top8[:, 1:2],
                                                    op=ALU.subtract), reads=[Sb], writes=[Sb])
            fw.op(fw.dve, lambda e: e.tensor_tensor(out=dv[:, 1:2], in0=top8[:, 1:2], in1=top8[:, 0:1],
                                                    op=ALU.subtract), reads=[Sb], writes=[Sb])
            p12 = S[:, 82:84]
            fw.op(fw.act, lambda e: e.activation(out=p12, in_=dv, func=AF.Sigmoid), reads=[Sb], writes=[Sb])
            fw.op(fw.dve, lambda e: e.tensor_scalar(out=p12, in0=p12, scalar1=pg, scalar2=None, op0=ALU.mult),
                  reads=[Sb], writes=[Sb])
            A1 = S[:, 96:128]
            A2 = S[:, 128:160]
            for g in range(4):
                fw.op(fw.dve, lambda e, g=g: e.tensor_scalar(out=A1[:, g * 8:(g + 1) * 8], in0=oh1,
                                                              scalar1=ohg[:, g:g + 1], scalar2=None, op0=ALU.mult),
                      reads=[Sb], writes=[Sb])
                fw.op(fw.dve, lambda e, g=g: e.tensor_scalar(out=A2[:, g * 8:(g + 1) * 8], in0=oh2,
                                                              scalar1=ohg[:, g:g + 1], scalar2=None, op0=ALU.mult),
                      reads=[Sb], writes=[Sb])
            A12 = S[:, 160:192]
            A12b = S[:, 192:208].bitcast(BF16)
            fw.op(fw.dve, lambda e: e.tensor_tensor(out=A12, in0=A1, in1=A2, op=ALU.add), reads=[Sb], writes=[Sb])
            fw.op(fw.dve, lambda e: e.tensor_copy(out=A12b, in_=A12), reads=[Sb], writes=[Sb])
            pp = self.pbank[5]
            pos_ps = self.bank(5)[:, 0:32]
            fw.op(fw.pe, lambda e: e.matmul(pos_ps, lhsT=self.onesb, rhs=acumb, start=True, stop=False),
                  reads=[acb, self.cb], writes=[pp])
            fw.op(fw.pe, lambda e: e.matmul(pos_ps, lhsT=self.sltb, rhs=A12b, start=False, stop=True),
                  reads=[Sb, self.cb], writes=[pp])
            slot = S[:, 208:240]
            fw.op(fw.dve, lambda e: e.tensor_tensor(out=slot, in0=pos_ps, in1=self.ebase, op=ALU.add),
                  reads=[pp, self.cb], writes=[Sb])
            fw.op(fw.pool, lambda e: e.tensor_tensor(out=acum, in0=acum, in1=A12, op=ALU.add), reads=[Sb, acb],
                  writes=[acb])
            fw.op(fw.pool, lambda e: e.tensor_copy(out=acumb, in_=acum), reads=[acb], writes=[acb])
            tmp32 = S[:, 240:256]
            dr = S[:, 84:86]
            pr = S[:, 86:88]
            for r, A in ((0, A1), (1, A2)):
                tt = S[:, 224:256]
            scr = xTf[s][:, 0:32]
            for r, A in ((0, A1), (1, A2)):
                fw.op(fw.dve, lambda e, r=r, A=A: e.scalar_tensor_tensor(
                    out=scr, in0=A, scalar=1.0, in1=slot, op0=ALU.mult, op1=ALU.mult, accum_out=dr[:, r:r + 1]),
                    reads=[Sb, xTfb[s], pl], writes=[Sb, xTfb[s]])
                fw.op(fw.dve, lambda e, r=r, A=A: e.scalar_tensor_tensor(
                    out=scr, in0=A, scalar=1.0, in1=pos_ps, op0=ALU.mult, op1=ALU.mult, accum_out=pr[:, r:r + 1]),
                    reads=[Sb, xTfb[s], pp], writes=[Sb, xTfb[s]])
            valid = S[:, 88:90]
            fw.op(fw.dve, lambda e: e.tensor_scalar(out=valid, in0=pr, scalar1=float(CAP) - 0.5, scalar2=None,
                                                    op0=ALU.is_lt), reads=[Sb], writes=[Sb])
            fw.op(fw.dve, lambda e: e.tensor_tensor(out=p12, in0=p12, in1=valid, op=ALU.mult), reads=[Sb],
                  writes=[Sb])
            fw.op(fw.dve, lambda e: e.tensor_scalar(out=dr, in0=dr, scalar1=self.trash, scalar2=None,
                                                    op0=ALU.subtract), reads=[Sb, self.cb], writes=[Sb])
            fw.op(fw.dve, lambda e: e.tensor_tensor(out=dr, in0=dr, in1=valid, op=ALU.mult), reads=[Sb], writes=[Sb])
            fw.op(fw.dve, lambda e: e.tensor_scalar(out=dr, in0=dr, scalar1=self.trash, scalar2=None, op0=ALU.add),
                  reads=[Sb, self.cb], writes=[Sb])
            fw.op(fw.dve, lambda e, i=i: e.tensor_copy(out=dest[:, 2 * i:2 * i + 2], in_=dr), reads=[Sb],
                  writes=[routeb])
            fw.op(fw.dve, lambda e, i=i: e.tensor_copy(out=gate[:, 2 * i:2 * i + 2], in_=p12), reads=[Sb],
                  writes=[routeb])
            for r in range(2):
                fw.dma(out=XG, in_=xbf[s], reads=[xbfb[s], routeb], writes=[XGb], q=fw.pool,
                       indirect=dict(out_offset=bass.IndirectOffsetOnAxis(ap=dest[:, 2 * i + r:2 * i + r + 1], axis=0),
                                     in_offset=None))
        self.release(m1)
        m2 = self.mark()
        NB = 2
        wg = [self.sb(16 * FF, BF16) for _ in range(NB)]
        wu = [self.sb(16 * FF, BF16) for _ in range(NB)]
        wd = [self.sb(4 * D, BF16) for _ in range(NB)]
        wbuf = [Buf() for _ in range(NB)]
        xg = [self.sb(D, BF16) for _ in range(2)]
        xgb = [Buf(), Buf()]
        xgT = self.sb(16 * CAP, BF16)
        xgTv = xgT.rearrange("p (a b) -> p a b", a=16)
        xgTb = Buf()
        hT = self.sb(4 * CAP, BF16)
        hTv = hT.rearrange("p (a b) -> p a b", a=4)
        hTb = Buf()
        sg = self.sb(CAP)
        sgb = Buf()
        yt = [self.sb(D) for _ in range(2)]
        ytb = [Buf(), Buf()]
        pbk = 0
        for ex in range(NEXP):
            s = ex % NB
            fw.dma(out=wg[s].rearrange("p (a b) -> p a b", a=16),
                   in_=W["moe_w_gate"][L, ex].rearrange("(kc p) f -> p kc f", p=128), writes=[wbuf[s]], q=fw.pool)
            fw.dma(out=wu[s].rearrange("p (a b) -> p a b", a=16),
                   in_=W["moe_w_up"][L, ex].rearrange("(kc p) f -> p kc f", p=128), writes=[wbuf[s]], q=fw.pool)
            fw.dma(out=wd[s].rearrange("p (a b) -> p a b", a=4),
                   in_=W["moe_w_down"][L, ex].rearrange("(fc p) m -> p fc m", p=128), writes=[wbuf[s]], q=fw.pool)
            wgv = wg[s].rearrange("p (a b) -> p a b", a=16)
            wuv = wu[s].rearrange("p (a b) -> p a b", a=16)
            wdv = wd[s].rearrange("p (a b) -> p a b", a=4)
            for stl in range(CAP // 128):
                xs = stl % 2
                fw.dma(out=xg[xs], in_=XG[ex * CAP + stl * 128: ex * CAP + (stl + 1) * 128, :], reads=[XGb],
                       writes=[xgb[xs]])
                for g in range(2):
                    bk = pbk % 8
                    pbk += 1
                    pb = self.pbank[bk]
                    bkb = self.bank(bk, BF16)
                    for j in range(8):
                        kc = g * 8 + j
                        fw.op(fw.pe, lambda e, kc=kc, j=j, bkb=bkb, xs=xs: e.transpose(
                            out=bkb[:, j * 128:(j + 1) * 128], in_=xg[xs][:, kc * 128:(kc + 1) * 128],
                            identity=self.identb), reads=[xgb[xs], self.cb], writes=[pb])
                    dstp = xgTv[:, g * 8:(g + 1) * 8, stl * 128:(stl + 1) * 128]
                    srcp = bkb.rearrange("p (a b) -> p a b", a=8)
                    if g == 0:
                        fw.op(fw.dve, lambda e, dstp=dstp, srcp=srcp: e.tensor_copy(out=dstp, in_=srcp),
                              reads=[pb], writes=[xgTb])
                    else:
                        fw.op(fw.act, lambda e, dstp=dstp, srcp=srcp: e.activation(out=dstp, in_=srcp, func=AF.Copy),
                              reads=[pb], writes=[xgTb])
            for fc in range(4):
                bkg = pbk % 8
                bku = (pbk + 1) % 8
                pbk += 2
                for (bk, wv) in ((bkg, wgv), (bku, wuv)):
                    for kc in range(16):
                        fw.op(fw.pe, lambda e, bk=bk, wv=wv, kc=kc, fc=fc: e.matmul(
                            self.bank(bk)[:, 0:CAP], lhsT=wv[:, kc, fc * 128:(fc + 1) * 128], rhs=xgTv[:, kc, :],
                            start=(kc == 0), stop=(kc == 15)), reads=[wbuf[s], xgTb], writes=[self.pbank[bk]])
                fw.op(fw.act, lambda e, bkg=bkg: e.activation(out=sg, in_=self.bank(bkg)[:, 0:CAP], func=AF.Silu),
                      reads=[self.pbank[bkg]], writes=[sgb])
                fw.op(fw.dve, lambda e, bku=bku, fc=fc: e.tensor_tensor(out=hTv[:, fc, :], in0=sg,
                                                                        in1=self.bank(bku)[:, 0:CAP], op=ALU.mult),
                      reads=[self.pbank[bku], sgb], writes=[hTb])
            for stl in range(CAP // 128):
                ys = (ex * 2 + stl) % 2
                for mc in range(4):
                    bk = pbk % 8
                    pbk += 1
                    for fc in range(4):
                        fw.op(fw.pe, lambda e, bk=bk, fc=fc, mc=mc, stl=stl: e.matmul(
                            self.bank(bk), lhsT=hTv[:, fc, stl * 128:(stl + 1) * 128],
                            rhs=wdv[:, fc, mc * 512:(mc + 1) * 512], start=(fc == 0), stop=(fc == 3)),
                            reads=[hTb, wbuf[s]], writes=[self.pbank[bk]])
                    dstp = yt[ys][:, mc * 512:(mc + 1) * 512]
                    if mc % 2 == 0:
                        fw.op(fw.dve, lambda e, dstp=dstp, bk=bk: e.tensor_copy(out=dstp, in_=self.bank(bk)),
                              reads=[self.pbank[bk]], writes=[ytb[ys]])
                    else:
                        fw.op(fw.act, lambda e, dstp=dstp, bk=bk: e.activation(out=dstp, in_=self.bank(bk),
                                                                               func=AF.Copy),
                              reads=[self.pbank[bk]], writes=[ytb[ys]])
                fw.dma(out=YG[ex * CAP + stl * 128: ex * CAP + (stl + 1) * 128, :], in_=yt[ys], reads=[ytb[ys]],
                       writes=[YGb])
        self.release(m2)
        m3 = self.mark()
        grow = self.sb(D)
        brow2 = self.sb(D)
        gbuf = Buf()
        self.load_row(grow, W["ln_g"][L, 1], gbuf, D)
        self.load_row(brow2, W["ln_b"][L, 1], gbuf, D)
        xr = [self.sb(D) for _ in range(2)]
        xrb = [Buf(), Buf()]
        y1 = [self.sb(D) for _ in range(2)]
        y1b = [Buf(), Buf()]
        y2 = [self.sb(D) for _ in range(2)]
        y2b = [Buf(), Buf()]
        tmp = self.sb(D)
        tmpb = Buf()
        stat = [self.sb(4) for _ in range(2)]
        statb = [Buf(), Buf()]
        for i in range(NT):
            s = i % 2
            fw.dma(out=xr[s], in_=xm[i * 128:(i + 1) * 128, :], reads=[xm_buf], writes=[xrb[s]])
            for (yy, yb, r) in ((y1[s], y1b[s], 0), (y2[s], y2b[s], 1)):
                fw.dma(out=yy, in_=YG, reads=[YGb, routeb], writes=[yb], q=fw.pool,
                       indirect=dict(out_offset=None,
                                     in_offset=bass.IndirectOffsetOnAxis(ap=dest[:, 2 * i + r:2 * i + r + 1], axis=0)))
            fw.op(fw.act, lambda e, s=s, i=i: e.activation(out=y1[s], in_=y1[s], func=AF.Identity,
                                                           scale=gate[:, 2 * i:2 * i + 1]),
                  reads=[y1b[s], routeb], writes=[y1b[s]])
            fw.op(fw.dve, lambda e, s=s, i=i: e.scalar_tensor_tensor(
                out=y2[s], in0=y2[s], scalar=gate[:, 2 * i + 1:2 * i + 2], in1=y1[s], op0=ALU.mult, op1=ALU.add),
                reads=[y1b[s], y2b[s], routeb], writes=[y2b[s]])
            fw.op(fw.dve, lambda e, s=s: e.scalar_tensor_tensor(
                out=xr[s], in0=xr[s], scalar=DN_ALPHA, in1=y2[s], op0=ALU.mult, op1=ALU.add),
                reads=[xrb[s], y2b[s]], writes=[xrb[s]])
            self.ln_tail(xr[s], xrb[s], grow, brow2, gbuf, tmp, tmpb, stat[s], statb[s], xr[s], xrb[s])
            fw.dma(out=dst[i * 128:(i + 1) * 128, :], in_=xr[s], reads=[xrb[s]], writes=[dst_buf])
        self.release(m3)
        self.release(m0)

    def proj_fm(self, xTv, xTb, w_cols, wt, wtb, banks=(0, 1, 2, 3)):
        fw = self.fw
        fw.dma(out=wt.rearrange("p (a b) -> p a b", a=16), in_=w_cols.rearrange("(kc p) f -> p kc f", p=128),
               writes=[wtb], q=fw.pool)
        wv = wt.rearrange("p (a b) -> p a b", a=16)
        for tc in range(4):
            bk = banks[tc]
            for kc in range(16):
                fw.op(fw.pe, lambda e, bk=bk, kc=kc, tc=tc: e.matmul(
                    self.bank(bk), lhsT=wv[:, kc, :], rhs=xTv[:, kc, tc * 512:(tc + 1) * 512],
                    start=(kc == 0), stop=(kc == 15)), reads=[wtb, xTb], writes=[self.pbank[bk]])

    def sin_rr(self, eng, out, ang, buf, k, r, n_part=128, shift=0.0):
        fw = self.fw
        MAG = 12582912.0
        C1 = 6.28125
        C2 = 2.0 * math.pi - 6.28125
        fw.op(eng, lambda e: e.tensor_scalar(out=k, in0=ang, scalar1=1.0 / (2.0 * math.pi),
                                             scalar2=shift / (2.0 * math.pi), op0=ALU.mult, op1=ALU.add),
              reads=[buf], writes=[buf])
        fw.op(eng, lambda e: e.tensor_scalar(out=k, in0=k, scalar1=MAG, scalar2=None, op0=ALU.add),
              reads=[buf], writes=[buf])
        fw.op(eng, lambda e: e.tensor_scalar(out=k, in0=k, scalar1=-MAG, scalar2=None, op0=ALU.add),
              reads=[buf], writes=[buf])
        fw.op(eng, lambda e: e.scalar_tensor_tensor(out=r, in0=k, scalar=-C1, in1=ang, op0=ALU.mult, op1=ALU.add),
              reads=[buf], writes=[buf]) if eng is fw.dve else None
        if eng is not fw.dve:
            raise ValueError
        fw.op(eng, lambda e: e.scalar_tensor_tensor(out=r, in0=k, scalar=-C2, in1=r, op0=ALU.mult, op1=ALU.add),
              reads=[buf], writes=[buf])
        fw.op(eng, lambda e: e.tensor_scalar(out=r, in0=r, scalar1=shift, scalar2=math.pi, op0=ALU.add, op1=ALU.min),
              reads=[buf], writes=[buf])
        fw.op(eng, lambda e: e.tensor_scalar(out=r, in0=r, scalar1=-math.pi, scalar2=None, op0=ALU.max),
              reads=[buf], writes=[buf])
        fw.op(fw.act, lambda e: e.activation(out=out, in_=r, func=AF.Sin), reads=[buf], writes=[buf])

    def s5(self, L, src, src_buf, W, dst, dst_buf):
        fw = self.fw
        dve, act, pool, pe = fw.dve, fw.act, fw.pool, fw.pe
        U, Ub = self.dram["U"], self.dbuf["U"]
        YA, YAb = self.dram["YA"], self.dbuf["YA"]
        OT, OTb = self.dram["OT"], self.dbuf["OT"]
        m0 = self.mark()
        big = self.sb(16 * T, BF16)
        bigv = big.rearrange("p (a b) -> p a b", a=16)
        xTb = Buf("xT")
        self.build_xT(src, src_buf, bigv, xTb)
        mU = self.mark()
        wt = [self.sb(16 * 128, BF16) for _ in range(2)]
        wtb = [Buf(), Buf()]
        uf = [self.sb(T) for _ in range(2)]
        ufb = [Buf(), Buf()]
        for J in range(16):
            s = J % 2
            self.proj_fm(bigv, xTb, W["ssm_w_in"][0][:, J * 128:(J + 1) * 128], wt[s], wtb[s])
            for tc in range(4):
                eng = dve if tc % 2 == 0 else act
                dstp = uf[s][:, tc * 512:(tc + 1) * 512]
                if tc % 2 == 0:
                    fw.op(dve, lambda e, dstp=dstp, tc=tc: e.tensor_copy(out=dstp, in_=self.bank(tc)),
                          reads=[self.pbank[tc]], writes=[ufb[s]])
                else:
                    fw.op(act, lambda e, dstp=dstp, tc=tc: e.activation(out=dstp, in_=self.bank(tc), func=AF.Copy),
                          reads=[self.pbank[tc]], writes=[ufb[s]])
            fw.dma(out=U[J], in_=uf[s], reads=[ufb[s]], writes=[Ub])
        self.release(mU)
        uTb_buf = xTb
        for q in range(4):
            fw.dma(out=bigv[:, q * 4:(q + 1) * 4, :], in_=U.rearrange("j p t -> p j t")[:, q * 4:(q + 1) * 4, :],
                   reads=[Ub], writes=[uTb_buf], q=pool)
        mP = self.mark()
        PQ = self.sb(6 * 64)
        LB = [self.sb(16 * 128, BF16) for _ in range(2)]
        CL = [self.sb(16 * 128) for _ in range(2)]
        LBz = [self.sb(16 * 128, BF16) for _ in range(2)]
        LBzv = [a.rearrange("p (J q) -> p J q", J=16) for a in LBz]
        dsk = self.sb(16)
        tau = self.sb(520)
        m96 = self.sb(1)
        mT = self.mark()
        pb_ = Buf("s5par")
        A = lambda: self.sb(128)
        are, aim, dtb, mag, ang, kk, rr, sn, cs, den, fre, fim, t1, t2 = [A() for _ in range(14)]
        ldt = self.sb(2)
        fw.dma(out=are[0:64, :], in_=W["ssm_a_re"][0].rearrange("(j two) p -> j (two p)", two=2), writes=[pb_])
        fw.dma(out=aim[0:64, :], in_=W["ssm_a_im"][0].rearrange("(j two) p -> j (two p)", two=2), writes=[pb_])
        fw.dma(out=ldt[0:64, :], in_=W["ssm_log_dt"][0].rearrange("(j two) -> j two", two=2), writes=[pb_])
        h = slice(0, 64)
        fw.op(act, lambda e: e.activation(out=ldt[h, :], in_=ldt[h, :], func=AF.Exp), reads=[pb_], writes=[pb_])
        for two in range(2):
            fw.op(dve, lambda e, two=two: e.tensor_scalar(out=dtb[h, two * 64:(two + 1) * 64], in0=self.ones[h, 0:64],
                                                         scalar1=ldt[h, two:two + 1], scalar2=None, op0=ALU.mult),
                  reads=[pb_, self.cb], writes=[pb_])
        tt = lambda o, a, b, op: fw.op(dve, lambda e: e.tensor_tensor(out=o[h, :], in0=a[h, :], in1=b[h, :], op=op),
                                       reads=[pb_], writes=[pb_])
        tt(mag, are, dtb, ALU.mult)
        fw.op(act, lambda e: e.activation(out=mag[h, :], in_=mag[h, :], func=AF.Exp), reads=[pb_], writes=[pb_])
        tt(ang, aim, dtb, ALU.mult)
        self.sin_rr(dve, sn[h, :], ang[h, :], pb_, kk[h, :], rr[h, :])
        thr = A()
        fw.op(dve, lambda e: e.tensor_copy(out=thr[h, :], in_=rr[h, :]), reads=[pb_], writes=[pb_])
        self.sin_rr(dve, cs[h, :], ang[h, :], pb_, kk[h, :], rr[h, :], shift=math.pi / 2)
        lre, lim = A(), A()
        tt(lre, mag, cs, ALU.mult)
        tt(lim, mag, sn, ALU.mult)
        tt(t1, are, are, ALU.mult)
        tt(t2, aim, aim, ALU.mult)
        tt(den, t1, t2, ALU.add)
        fw.op(dve, lambda e: e.reciprocal(out=den[h, :], in_=den[h, :]), reads=[pb_], writes=[pb_])
        lm1 = A()
        fw.op(dve, lambda e: e.tensor_scalar(out=lm1[h, :], in0=lre[h, :], scalar1=-1.0, scalar2=None, op0=ALU.add),
              reads=[pb_], writes=[pb_])
        tt(t1, lm1, are, ALU.mult)
        tt(t2, lim, aim, ALU.mult)
        tt(fre, t1, t2, ALU.add)
        tt(fre, fre, den, ALU.mult)
        tt(t1, lim, are, ALU.mult)
        tt(t2, lm1, aim, ALU.mult)
        tt(fim, t1, t2, ALU.subtract)
        tt(fim, fim, den, ALU.mult)
        PQv = PQ.rearrange("p (a b) -> p a b", a=6)
        pqb = Buf("pq")
        for n_, srcp in enumerate((mag, thr, cs, sn, fre, fim)):
            fw.op(pe, lambda e, n_=n_, srcp=srcp: e.transpose(out=self.bank(0)[:, n_ * 64:(n_ + 1) * 64],
                                                              in_=srcp[0:64, :], identity=self.ident[0:64, 0:64]),
                  reads=[pb_, self.cb], writes=[self.pbank[0]])
        fw.op(dve, lambda e: e.tensor_copy(out=PQ, in_=self.bank(0)[:, 0:384]), reads=[self.pbank[0]], writes=[pqb])
        RHO, THR, COS1, SIN1, FRE, FIM = [PQv[:, n_, :] for n_ in range(6)]
        bre = self.sb(64 * 16)
        bim = self.sb(64 * 16)
        bbr = self.sb(64 * 16)
        bbi = self.sb(64 * 16)
        tb1 = self.sb(64 * 16)
        bb_ = Buf("bb")
        v3 = lambda a: a.rearrange("p (j c) -> p j c", c=16)
        fw.dma(out=v3(bre), in_=W["ssm_b_re"][0].rearrange("(j two) p c -> (two p) j c", two=2), writes=[bb_])
        fw.dma(out=v3(bim), in_=W["ssm_b_im"][0].rearrange("(j two) p c -> (two p) j c", two=2), writes=[bb_])
        bc = lambda a: a.unsqueeze(2).to_broadcast([128, 64, 16])
        t3 = lambda o, a, b, op: fw.op(dve, lambda e: e.tensor_tensor(out=v3(o), in0=v3(a), in1=b, op=op),
                                       reads=[bb_, pqb], writes=[bb_])
        t3(bbr, bre, bc(FRE), ALU.mult)
        t3(tb1, bim, bc(FIM), ALU.mult)
        t3(bbr, bbr, v3(tb1), ALU.subtract)
        t3(bbi, bim, bc(FRE), ALU.mult)
        t3(tb1, bre, bc(FIM), ALU.mult)
        t3(bbi, bbi, v3(tb1), ALU.add)
        LBv = [a.rearrange("p (J q) -> p J q", J=16) for a in LB]
        lbb = Buf("LB")
        arr = self.sb(16 * 128)
        arrb = Buf("arr")
        arr5 = arr.rearrange("p (J jj two c) -> p J jj two c", J=16, jj=4, two=2)
        for ri, bbx in enumerate((bbr, bbi)):
            fw.op(pool, lambda e: e.memset(arr, 0.0), writes=[arrb])
            b4 = bbx.rearrange("p (J jj c) -> p J jj c", J=16, jj=4)
            for two in range(2):
                ps = slice(two * 64, (two + 1) * 64)
                fw.op(pool, lambda e, two=two, ps=ps, b4=b4: e.tensor_copy(out=arr5[ps, :, :, two, :], in_=b4[ps]),
                      reads=[bb_, arrb], writes=[arrb])
            av = arr.rearrange("p (J x) -> p J x", J=16)
            for g in range(4):
                bk = 1 + g
                for j4 in range(4):
                    J = g * 4 + j4
                    fw.op(pe, lambda e, J=J, j4=j4, bk=bk: e.transpose(out=self.bank(bk)[:, j4 * 128:(j4 + 1) * 128],
                                                                       in_=av[:, J, :], identity=self.ident),
                          reads=[arrb, self.cb], writes=[self.pbank[bk]])
                fw.op(act, lambda e, g=g, bk=bk, ri=ri: e.activation(
                    out=LBv[ri][:, g * 4:(g + 1) * 4, :], in_=self.bank(bk).rearrange("p (a b) -> p a b", a=4),
                    func=AF.Copy), reads=[self.pbank[bk]], writes=[lbb])
        fw.op(pool, lambda e: e.memset(m96, 1.0), writes=[lbb])
        fw.op(pool, lambda e: e.affine_select(out=m96, in_=m96, pattern=[[0, 1]], compare_op=ALU.is_ge, fill=0.0,
                                              base=-96, channel_multiplier=1), reads=[lbb], writes=[lbb])
        for ri in range(2):
            fw.op(dve, lambda e, ri=ri: e.tensor_scalar(out=LBz[ri][64:128, :], in0=LB[ri][64:128, :],
                                                       scalar1=m96[64:128, :], scalar2=None, op0=ALU.mult),
                  reads=[lbb], writes=[lbb])
        CLv = [a.rearrange("p (J q) -> p J q", J=16) for a in CL]
        clb = Buf("CL")
        for ri, cname in enumerate(("ssm_c_re", "ssm_c_im")):
            fw.op(pool, lambda e: e.memset(arr, 0.0), writes=[arrb])
            a4 = arr.rearrange("p (J two q) -> p J two q", J=16, two=2)
            csrc = W[cname][0].rearrange("(J jj two) c p -> jj two c J p", jj=4, two=2)
            for jj in range(4):
                for two in range(2):
                    r0 = jj * 32 + two * 16
                    fw.dma(out=a4[r0:r0 + 16, :, two, :], in_=csrc[jj, two], writes=[arrb])
            av = arr.rearrange("p (J x) -> p J x", J=16)
            for g in range(4):
                bk = 1 + g
                for j4 in range(4):
                    J = g * 4 + j4
                    fw.op(pe, lambda e, J=J, j4=j4, bk=bk: e.transpose(out=self.bank(bk)[:, j4 * 128:(j4 + 1) * 128],
                                                                       in_=av[:, J, :], identity=self.ident),
                          reads=[arrb, self.cb], writes=[self.pbank[bk]])
                fw.op(act, lambda e, g=g, bk=bk, ri=ri: e.activation(
                    out=CLv[ri][:, g * 4:(g + 1) * 4, :], in_=self.bank(bk).rearrange("p (a b) -> p a b", a=4),
                    func=AF.Copy, scale=(1.0 if ri == 0 else -1.0)), reads=[self.pbank[bk]], writes=[clb])
        dskb = Buf("dsk")
        d16 = self.sb(128)
        fw.dma(out=d16[0:16, :], in_=W["ssm_d"][0].rearrange("(J p) -> J p", p=128), writes=[dskb])
        fw.op(pe, lambda e: e.transpose(out=self.bank(5)[:, 0:16], in_=d16[0:16, :], identity=self.ident[0:16, 0:16]),
              reads=[dskb, self.cb], writes=[self.pbank[5]])
        fw.op(dve, lambda e: e.tensor_copy(out=dsk, in_=self.bank(5)[:, 0:16]), reads=[self.pbank[5]], writes=[dskb])
        taub = Buf("tau")
        fw.op(pool, lambda e: e.iota(out=tau[:, 0:513], pattern=[[1, 513]], base=0, channel_multiplier=0,
                                     allow_small_or_imprecise_dtypes=True), writes=[taub])
        self.release(mT)
        NW = 2
        tabc_f = [self.sb(520) for _ in range(NW)]
        tabs_f = [self.sb(520) for _ in range(NW)]
        tk = [self.sb(520)[:, 0:513] for _ in range(NW)]
        tr_ = [self.sb(520)[:, 0:513] for _ in range(NW)]
        tang = [self.sb(520)[:, 0:513] for _ in range(NW)]
        tabc = [a[:, 0:512] for a in tabc_f]
        tabs = [a[:, 0:512] for a in tabs_f]
        tabb = [Buf() for _ in range(NW)]
        wk = [[self.sb(512) for _ in range(6)] for _ in range(NW)]
        wkb = [Buf() for _ in range(NW)]
        st8 = [self.sb(8) for _ in range(4)]
        st8b = [Buf() for _ in range(4)]
        clm = [self.sb(4 * 128) for _ in range(2)]
        clmb = Buf("clm")
        ufl0 = self.sb(T)
        ufl = [ufl0, ufl0]
        uflb0 = Buf()
        uflb = [uflb0, uflb0]
        yo = [self.sb(512) for _ in range(2)]
        yob = [Buf(), Buf()]
        yab0 = self.sb(T, BF16)
        yab = [yab0, yab0]
        yabb0 = Buf()
        yabb = [yabb0, yabb0]
        cnt = 0
        cnt2 = 0
        bu_done = {}
        bu_n = [0]

        def emit_bu(J_, jj_, tc_):
            bks = (5, 6) if bu_n[0] % 2 == 0 else (4, 7)
            bu_n[0] += 1
            rows_ = slice(jj_ * 32, (jj_ + 1) * 32) if jj_ < 3 else slice(64, 128)
            LBu_ = LBv if jj_ < 3 else LBzv
            for ri_, bk_ in ((0, bks[0]), (1, bks[1])):
                fw.op(pe, lambda e, ri_=ri_, bk_=bk_: e.matmul(
                    self.bank(bk_), lhsT=LBu_[ri_][rows_, J_, :], rhs=bigv[rows_, J_, tc_ * 512:(tc_ + 1) * 512],
                    start=True, stop=True), reads=[lbb, uTb_buf], writes=[self.pbank[bk_]])
            bu_done[(J_, jj_, tc_)] = bks

        for J in range(16):
            js = J % 2
            fw.dma(out=ufl[js], in_=U[J], reads=[Ub], writes=[uflb[js]])
            for ri in range(2):
                cm = clm[ri].rearrange("p (jj x) -> p jj x", jj=4)
                fw.op(pool, lambda e, ri=ri: e.memset(clm[ri], 0.0), writes=[clmb])
                for jj in range(4):
                    fw.op(pool, lambda e, ri=ri, jj=jj, cm=cm, J=J: e.tensor_copy(
                        out=cm[:, jj, jj * 32:(jj + 1) * 32], in_=CLv[ri][:, J, jj * 32:(jj + 1) * 32]),
                        reads=[clb, clmb], writes=[clmb])
            for jj in range(4):
                fw.op(dve, lambda e, jj=jj: e.memset(st8[jj], 0.0), writes=[st8b[jj]])
            for jj in range(4):
                j = J * 4 + jj
                w = cnt % NW
                cnt += 1
                fw.op(dve, lambda e, w=w, j=j: e.tensor_scalar(out=tang[w], in0=tau[:, 0:513], scalar1=THR[:, j:j + 1],
                                                               scalar2=None, op0=ALU.mult),
                      reads=[taub, pqb], writes=[tabb[w]])
                self.sin_rr(dve, tabs_f[w][:, 0:513], tang[w], tabb[w], tk[w], tr_[w])
                self.sin_rr(dve, tabc_f[w][:, 0:513], tang[w], tabb[w], tk[w], tr_[w], shift=math.pi / 2)
                C512 = tabc_f[w][:, 512:513]
                S512 = tabs_f[w][:, 512:513]
                for tc in range(4):
                    ybk = tc
                    w2 = cnt2 % NW
                    cnt2 += 1
                    if (J, jj, tc) not in bu_done:
                        emit_bu(J, jj, tc)
                    b5, b6 = bu_done[(J, jj, tc)]
                    nxt = (J, jj, tc + 1) if tc < 3 else ((J, jj + 1, 0) if jj < 3 else ((J + 1, 0, 0) if J < 15 else None))
                    if nxt is not None:
                        emit_bu(*nxt)
                    btr, bti, rre, rim, ta, tb = wk[w2]
                    B = wkb[w2]
                    fw.op(dve, lambda e, w=w, btr=btr: e.tensor_tensor(out=btr, in0=self.bank(b5), in1=tabc[w], op=ALU.mult),
                          reads=[self.pbank[b5], tabb[w]], writes=[B])
                    fw.op(dve, lambda e, w=w, ta=ta: e.tensor_tensor(out=ta, in0=self.bank(b6), in1=tabs[w], op=ALU.mult),
                          reads=[self.pbank[b6], tabb[w]], writes=[B])
                    fw.op(dve, lambda e, btr=btr, ta=ta: e.tensor_tensor(out=btr, in0=btr, in1=ta, op=ALU.add),
                          reads=[B], writes=[B])
                    fw.op(dve, lambda e, w=w, bti=bti: e.tensor_tensor(out=bti, in0=self.bank(b6), in1=tabc[w], op=ALU.mult),
                          reads=[self.pbank[b6], tabb[w]], writes=[B])
                    fw.op(dve, lambda e, w=w, tb=tb: e.tensor_tensor(out=tb, in0=self.bank(b5), in1=tabs[w], op=ALU.mult),
                          reads=[self.pbank[b5], tabb[w]], writes=[B])
                    fw.op(dve, lambda e, bti=bti, tb=tb: e.tensor_tensor(out=bti, in0=bti, in1=tb, op=ALU.subtract),
                          reads=[B], writes=[B])
                    c8 = st8[jj]
                    cb8 = st8b[jj]
                    fw.op(dve, lambda e, c8=c8, j=j: e.tensor_scalar(out=c8[:, 2:3], in0=c8[:, 0:1],
                                                                     scalar1=C512, scalar2=None, op0=ALU.mult),
                          reads=[cb8, tabb[w]], writes=[cb8])
                    fw.op(dve, lambda e, c8=c8, j=j: e.tensor_scalar(out=c8[:, 4:5], in0=c8[:, 1:2],
                                                                     scalar1=S512, scalar2=None, op0=ALU.mult),
                          reads=[cb8, tabb[w]], writes=[cb8])
                    fw.op(dve, lambda e, c8=c8: e.tensor_tensor(out=c8[:, 2:3], in0=c8[:, 2:3], in1=c8[:, 4:5],
                                                                op=ALU.subtract), reads=[cb8], writes=[cb8])
                    fw.op(dve, lambda e, c8=c8, j=j: e.tensor_scalar(out=c8[:, 3:4], in0=c8[:, 0:1],
                                                                     scalar1=S512, scalar2=None, op0=ALU.mult),
                          reads=[cb8, tabb[w]], writes=[cb8])
                    fw.op(dve, lambda e, c8=c8, j=j: e.tensor_scalar(out=c8[:, 4:5], in0=c8[:, 1:2],
                                                                     scalar1=C512, scalar2=None, op0=ALU.mult),
                          reads=[cb8, tabb[w]], writes=[cb8])
                    fw.op(dve, lambda e, c8=c8: e.tensor_tensor(out=c8[:, 3:4], in0=c8[:, 3:4], in1=c8[:, 4:5],
                                                                op=ALU.add), reads=[cb8], writes=[cb8])
                    rho_b = RHO[:, j:j + 1].to_broadcast([128, 512])
                    fw.op(dve, lambda e, rre=rre, btr=btr, c8=c8, rho_b=rho_b: e.tensor_tensor_scan(
                        out=rre, data0=rho_b, data1=btr, initial=c8[:, 2:3], op0=ALU.mult, op1=ALU.add),
                        reads=[B, cb8, pqb], writes=[B])
                    fw.op(dve, lambda e, rim=rim, bti=bti, c8=c8, rho_b=rho_b: e.tensor_tensor_scan(
                        out=rim, data0=rho_b, data1=bti, initial=c8[:, 3:4], op0=ALU.mult, op1=ALU.add),
                        reads=[B, cb8, pqb], writes=[B])
                    fw.op(dve, lambda e, c8=c8, rre=rre: e.tensor_copy(out=c8[:, 0:1], in_=rre[:, 511:512]),
                          reads=[B, cb8], writes=[cb8])
                    fw.op(dve, lambda e, c8=c8, rim=rim: e.tensor_copy(out=c8[:, 1:2], in_=rim[:, 511:512]),
                          reads=[B, cb8], writes=[cb8])
                    fw.op(pool, lambda e, w=w, ta=ta, rre=rre: e.tensor_tensor(out=ta, in0=rre, in1=tabc[w], op=ALU.mult),
                          reads=[B, tabb[w]], writes=[B])
                    fw.op(pool, lambda e, w=w, tb=tb, rim=rim: e.tensor_tensor(out=tb, in0=rim, in1=tabs[w], op=ALU.mult),
                          reads=[B, tabb[w]], writes=[B])
                    fw.op(pool, lambda e, w=w, ta=ta, tb=tb: e.tensor_tensor(out=ta, in0=ta, in1=tb, op=ALU.subtract),
                          reads=[B], writes=[B])
                    fw.op(pool, lambda e, w=w, tb=tb, rre=rre: e.tensor_tensor(out=tb, in0=rre, in1=tabs[w], op=ALU.mult),
                          reads=[B, tabb[w]], writes=[B])
                    fw.op(pool, lambda e, w=w, rre=rre, rim=rim: e.tensor_tensor(out=rre, in0=rim, in1=tabc[w], op=ALU.mult),
                          reads=[B, tabb[w]], writes=[B])
                    fw.op(pool, lambda e, tb=tb, rre=rre: e.tensor_tensor(out=tb, in0=tb, in1=rre, op=ALU.add),
                          reads=[B], writes=[B])
                    cm0 = clm[0].rearrange("p (jj x) -> p jj x", jj=4)
                    cm1 = clm[1].rearrange("p (jj x) -> p jj x", jj=4)
                    fw.op(pe, lambda e, ta=ta, jj=jj, cm0=cm0: e.matmul(self.bank(ybk), lhsT=cm0[:, jj, :], rhs=ta,
                                                                        start=(jj == 0), stop=False),
                          reads=[clmb, B], writes=[self.pbank[ybk]])
                    fw.op(pe, lambda e, tb=tb, jj=jj, cm1=cm1: e.matmul(self.bank(ybk), lhsT=cm1[:, jj, :], rhs=tb,
                                                                        start=False, stop=(jj == 3)),
                          reads=[clmb, B], writes=[self.pbank[ybk]])
            for tc in range(4):
                ybk = tc
                ys = (J * 4 + tc) % 2
                fw.op(dve, lambda e, ys=ys, js=js, J=J, tc=tc: e.scalar_tensor_tensor(
                    out=yo[ys], in0=ufl[js][:, tc * 512:(tc + 1) * 512], scalar=dsk[:, J:J + 1], in1=self.bank(ybk),
                    op0=ALU.mult, op1=ALU.add), reads=[uflb[js], dskb, self.pbank[ybk]], writes=[yob[ys]])
                fw.op(act, lambda e, ys=ys, js=js, tc=tc: e.activation(out=yab[js][:, tc * 512:(tc + 1) * 512],
                                                                     in_=yo[ys], func=AF.Gelu),
                      reads=[yob[ys]], writes=[yabb[js]])
            fw.dma(out=YA[J], in_=yab[js], reads=[yabb[js]], writes=[YAb])
        self.release(mP)
        for q in range(4):
            fw.dma(out=bigv[:, q * 4:(q + 1) * 4, :], in_=YA.rearrange("j p t -> p j t")[:, q * 4:(q + 1) * 4, :],
                   reads=[YAb], writes=[xTb])
        mG = self.mark()
        wt = [self.sb(16 * 128, BF16) for _ in range(2)]
        wtb = [Buf(), Buf()]
        bgl = self.sb(16)
        bglb = Buf()
        b16 = self.sb(128)
        fw.dma(out=b16[0:16, :], in_=W["ssm_b_glu"][0].rearrange("(J p) -> J p", p=128), writes=[bglb])
        fw.op(pe, lambda e: e.transpose(out=self.bank(5)[:, 0:16], in_=b16[0:16, :], identity=self.ident[0:16, 0:16]),
              reads=[bglb, self.cb], writes=[self.pbank[5]])
        fw.op(dve, lambda e: e.tensor_copy(out=bgl, in_=self.bank(5)[:, 0:16]), reads=[self.pbank[5]], writes=[bglb])
        sg = [self.sb(512) for _ in range(2)]
        sgb = [Buf(), Buf()]
        y2 = [self.sb(T, BF16) for _ in range(2)]
        y2b = [Buf(), Buf()]
        for mo in range(16):
            s = mo % 2
            self.proj_fm(bigv, xTb, W["ssm_w_glu"][0][:, mo * 128:(mo + 1) * 128], wt[s], wtb[s])
            for tc in range(4):
                s2 = tc % 2
                fw.op(act, lambda e, s2=s2, tc=tc, mo=mo: e.activation(out=sg[s2], in_=self.bank(tc), func=AF.Sigmoid,
                                                                     bias=bgl[:, mo:mo + 1], scale=1.0),
                      reads=[self.pbank[tc], bglb], writes=[sgb[s2]])
                fw.op(dve, lambda e, s=s, s2=s2, tc=tc, mo=mo: e.tensor_tensor(
                    out=y2[s][:, tc * 512:(tc + 1) * 512], in0=sg[s2], in1=bigv[:, mo, tc * 512:(tc + 1) * 512],
                    op=ALU.mult), reads=[sgb[s2], xTb], writes=[y2b[s]])
            fw.dma(out=OT[mo], in_=y2[s], reads=[y2b[s]], writes=[OTb])
        self.release(mG)
        self.release(m0)
        self.outproj_ln(OT, OTb, W["ssm_w_out"][0], src, src_buf, W["ln_g"][L, 0], W["ln_b"][L, 0], dst, dst_buf)

    def dsa(self, L, src, src_buf, W, dst, dst_buf):
        fw = self.fw
        dve, act, pool, pe = fw.dve, fw.act, fw.pool, fw.pe
        QT, QTb = self.dram["QT"], self.dbuf["QT"]
        QI, QIb = self.dram["QI"], self.dbuf["QI"]
        MT, MTb = self.dram["MASKT"], self.dbuf["MASKT"]
        OT, OTb = self.dram["OT"], self.dbuf["OT"]
        w_in = W["dsa_w_in"][0]
        m0 = self.mark()
        kT = self.sb(T, BF16)
        kiT = self.sb(T, BF16)
        vtok = self.sb(16 * 132, BF16)
        vtv = vtok.rearrange("p (a b) -> p a b", a=16)
        wi = self.sb(16 * 16)
        wiv = wi.rearrange("p (a b) -> p a b", a=16)
        resb = Buf("dsa_res")
        fw.op(pool, lambda e: e.memset(vtok, 1.0), writes=[resb])
        m1 = self.mark()
        big = self.sb(16 * T, BF16)
        bigv = big.rearrange("p (a b) -> p a b", a=16)
        xTb = Buf("xT")
        self.build_xT(src, src_buf, bigv, xTb)
        wt = [self.sb(16 * 128, BF16) for _ in range(2)]
        wtb = [Buf(), Buf()]
        ob = [self.sb(T, BF16) for _ in range(2)]
        obb = [Buf(), Buf()]
        QSC = 128.0 ** -0.5
        n = 0
        for (col0, cnt_, dstD, dstDb, scale) in ((0, 16, QT, QTb, QSC), (2304, 16, QI, QIb, 1.0)):
            for hh in range(cnt_):
                s = n % 2
                n += 1
                self.proj_fm(bigv, xTb, w_in[:, col0 + hh * 128: col0 + (hh + 1) * 128], wt[s], wtb[s])
                for tc in range(4):
                    dstp = ob[s][:, tc * 512:(tc + 1) * 512]
                    if tc % 2 == 0:
                        fw.op(dve, lambda e, dstp=dstp, tc=tc, scale=scale: e.tensor_scalar(
                            out=dstp, in0=self.bank(tc), scalar1=scale, scalar2=None, op0=ALU.mult),
                            reads=[self.pbank[tc]], writes=[obb[s]])
                    else:
                        fw.op(act, lambda e, dstp=dstp, tc=tc, scale=scale: e.activation(
                            out=dstp, in_=self.bank(tc), func=AF.Copy, scale=scale),
                            reads=[self.pbank[tc]], writes=[obb[s]])
                fw.dma(out=dstD[hh], in_=ob[s], reads=[obb[s]], writes=[dstDb])
        for (col0, dstT) in ((2048, kT), (4352, kiT)):
            s = n % 2
            n += 1
            self.proj_fm(bigv, xTb, w_in[:, col0: col0 + 128], wt[s], wtb[s])
            for tc in range(4):
                dstp = dstT[:, tc * 512:(tc + 1) * 512]
                fw.op(act, lambda e, dstp=dstp, tc=tc: e.activation(out=dstp, in_=self.bank(tc), func=AF.Copy),
                      reads=[self.pbank[tc]], writes=[resb])
        wv = wt[0]
        wvv = wv.rearrange("p (a b) -> p a b", a=16)
        fw.dma(out=wvv, in_=w_in[:, 2176:2304].rearrange("(kc p) f -> p kc f", p=128), writes=[wtb[0]], q=pool)
        ww = wt[1][:, 0:256]
        wwv = ww.rearrange("p (a b) -> p a b", a=16)
        fw.dma(out=wwv, in_=w_in[:, 4480:4496].rearrange("(kc p) f -> p kc f", p=128), writes=[wtb[1]], q=pool)
        WSC = (16.0 ** -0.5) * (128.0 ** -0.5)
        for i in range(NT):
            bk = 4 + (i % 2)
            for kc in range(16):
                fw.op(pe, lambda e, i=i, kc=kc, bk=bk: e.matmul(self.bank(bk)[:, 0:128], lhsT=bigv[:, kc, i * 128:(i + 1) * 128],
                                                               rhs=wvv[:, kc, :], start=(kc == 0), stop=(kc == 15)),
                      reads=[xTb, wtb[0]], writes=[self.pbank[bk]])
            fw.op(act, lambda e, i=i, bk=bk: e.activation(out=vtv[:, i, 0:128], in_=self.bank(bk)[:, 0:128], func=AF.Copy),
                  reads=[self.pbank[bk]], writes=[resb])
            bk2 = 6 + (i % 2)
            for kc in range(16):
                fw.op(pe, lambda e, i=i, kc=kc, bk2=bk2: e.matmul(self.bank(bk2)[:, 0:16], lhsT=bigv[:, kc, i * 128:(i + 1) * 128],
                                                                 rhs=wwv[:, kc, :], start=(kc == 0), stop=(kc == 15)),
                      reads=[xTb, wtb[1]], writes=[self.pbank[bk2]])
            fw.op(dve, lambda e, i=i, bk2=bk2: e.tensor_scalar(out=wiv[:, i, :], in0=self.bank(bk2)[:, 0:16], scalar1=WSC,
                                                               scalar2=None, op0=ALU.mult),
                  reads=[self.pbank[bk2]], writes=[resb])
        self.release(m1)
        m2 = self.mark()
        qit = [self.sb(16 * 128, BF16) for _ in range(2)]
        qitb = [Buf(), Buf()]
        acc = self.sb(T)
        accb = Buf("acc")
        work = self.sb(T)
        workb = Buf("work")
        rl = [self.sb(512, BF16) for _ in range(2)]
        rlb = [Buf(), Buf()]
        m8 = self.sb(8)
        m8b = Buf()
        mk = self.sb(T, BF16)
        mkb = Buf()
        mT = [self.sb(16 * 128, BF16) for _ in range(2)]
        mTb = [Buf(), Buf()]
        QIv = QI.rearrange("h p t -> p h t")
        MTv = MT.rearrange("b p t -> p b t")
        nr = 0
        for i in range(NT):
            s = i % 2
            S_ = 128 * (i + 1)
            fw.dma(out=qit[s].rearrange("p (a b) -> p a b", a=16), in_=QIv[:, :, i * 128:(i + 1) * 128], reads=[QIb],
                   writes=[qitb[s]])
            qv = qit[s].rearrange("p (a b) -> p a b", a=16)
            nsc = (S_ + 511) // 512
            for hh in range(16):
                for sc in range(nsc):
                    c0 = sc * 512
                    c1 = min(S_, c0 + 512)
                    bk = nr % 4
                    r2 = nr % 2
                    nr += 1
                    fw.op(pe, lambda e, hh=hh, c0=c0, c1=c1, bk=bk, qv=qv: e.matmul(
                        self.bank(bk)[:, 0:c1 - c0], lhsT=qv[:, hh, :], rhs=kiT[:, c0:c1], start=True, stop=True),
                        reads=[qitb[s], resb], writes=[self.pbank[bk]])
                    fw.op(act, lambda e, c0=c0, c1=c1, bk=bk, r2=r2: e.activation(
                        out=rl[r2][:, 0:c1 - c0], in_=self.bank(bk)[:, 0:c1 - c0], func=AF.Relu),
                        reads=[self.pbank[bk]], writes=[rlb[r2]])
                    if hh == 0:
                        fw.op(dve, lambda e, c0=c0, c1=c1, r2=r2, i=i: e.tensor_scalar(
                            out=acc[:, c0:c1], in0=rl[r2][:, 0:c1 - c0], scalar1=wiv[:, i, 0:1], scalar2=None,
                            op0=ALU.mult), reads=[rlb[r2], resb], writes=[accb])
                    else:
                        fw.op(dve, lambda e, c0=c0, c1=c1, r2=r2, i=i, hh=hh: e.scalar_tensor_tensor(
                            out=acc[:, c0:c1], in0=rl[r2][:, 0:c1 - c0], scalar=wiv[:, i, hh:hh + 1], in1=acc[:, c0:c1],
                            op0=ALU.mult, op1=ALU.add), reads=[rlb[r2], resb, accb], writes=[accb])
            fw.op(pool, lambda e, S_=S_: e.affine_select(out=acc[:, S_ - 128:S_], in_=acc[:, S_ - 128:S_],
                                                         pattern=[[-1, 128]], compare_op=ALU.is_ge, fill=-1e30, base=0,
                                                         channel_multiplier=1), reads=[accb], writes=[accb])
            if i >= 2:
                cur = acc
                curb = accb
                for rnd in range(32):
                    fw.op(dve, lambda e, cur=cur, S_=S_: e.max(out=m8, in_=cur[:, 0:S_]), reads=[curb], writes=[m8b])
                    if rnd < 31:
                        fw.op(dve, lambda e, cur=cur, S_=S_: e.match_replace(out=work[:, 0:S_], in_to_replace=m8,
                                                                             in_values=cur[:, 0:S_], imm_value=-3e38),
                              reads=[curb, m8b], writes=[workb])
                        cur = work
                        curb = workb
                fw.op(dve, lambda e, S_=S_: e.tensor_scalar(out=mk[:, 0:S_], in0=acc[:, 0:S_], scalar1=m8[:, 7:8],
                                                            scalar2=None, op0=ALU.is_ge), reads=[accb, m8b], writes=[mkb])
            else:
                fw.op(dve, lambda e, S_=S_: e.tensor_scalar(out=mk[:, 0:S_], in0=acc[:, 0:S_], scalar1=-1e29,
                                                            scalar2=None, op0=ALU.is_ge), reads=[accb], writes=[mkb])
            mTv = mT[s].rearrange("p (a b) -> p a b", a=16)
            for g in range((i + 8) // 8):
                bk = 4 + (g + i) % 2
                bkb = self.bank(bk, BF16)
                nb_ = min(8, i + 1 - g * 8)
                for j in range(nb_):
                    b = g * 8 + j
                    fw.op(pe, lambda e, b=b, j=j, bkb=bkb: e.transpose(out=bkb[:, j * 128:(j + 1) * 128],
                                                                      in_=mk[:, b * 128:(b + 1) * 128], identity=self.identb),
                          reads=[mkb, self.cb], writes=[self.pbank[bk]])
                fw.op(act, lambda e, g=g, nb_=nb_, bkb=bkb, mTv=mTv: e.activation(
                    out=mTv[:, g * 8:g * 8 + nb_, :], in_=bkb[:, 0:nb_ * 128].rearrange("p (a b) -> p a b", a=nb_),
                    func=AF.Copy), reads=[self.pbank[bk]], writes=[mTb[s]])
            fw.dma(out=MTv[:, 0:i + 1, i * 128:(i + 1) * 128], in_=mTv[:, 0:i + 1, :], reads=[mTb[s]], writes=[MTb])
        self.release(m2)
        m3 = self.mark()
        A1 = self.sb(2432)
        a1b = Buf("A1")
        fw.op(pool, lambda e: e.iota(out=A1, pattern=[[-1, 2432]], base=384, channel_multiplier=1,
                                     allow_small_or_imprecise_dtypes=True), writes=[a1b])
        fw.op(pool, lambda e: e.tensor_scalar(out=A1, in0=A1, scalar1=0.0, scalar2=None, op0=ALU.min), reads=[a1b],
              writes=[a1b])
        mres = self.sb(16 * T, BF16)
        mrv = mres.rearrange("p (a b) -> p a b", a=16)
        mrb = Buf("maskres")
        fw.op(pool, lambda e: e.memset(mres, 0.0), writes=[mrb])
        for b in range(16):
            fw.dma(out=mrv[:, b, b * 128:T], in_=MT[b][:, b * 128:T], reads=[MTb], writes=[mrb])
        qh = [self.sb(T, BF16) for _ in range(2)]
        qhb = [Buf(), Buf()]
        oth = [self.sb(T, BF16) for _ in range(2)]
        othb = [Buf(), Buf()]
        NU = 3
        LGB = (0, 1, 7)
        tmp = [self.sb(512) for _ in range(NU)]
        tmpb = [Buf() for _ in range(NU)]
        pp = [self.sb(512, BF16) for _ in range(NU)]
        ppb = [Buf() for _ in range(NU)]
        pm = [self.sb(512, BF16) for _ in range(NU)]
        pmb = [Buf() for _ in range(NU)]
        rden = self.sb(4)
        rdb = Buf()
        on = self.sb(512, BF16)
        onb = Buf()
        units = [(hh, c, b) for hh in range(16) for c in range(4) for b in range(4 * (c + 1))]
        lg_done = [0]

        def emit_lg(upto):
            while lg_done[0] <= min(upto, len(units) - 1):
                idx = lg_done[0]
                hh_, c_, b_ = units[idx]
                s_ = hh_ % 2
                if c_ == 0 and b_ == 0:
                    fw.dma(out=qh[s_], in_=QT[hh_], reads=[QTb], writes=[qhb[s_]])
                lbk_ = LGB[idx % NU]
                fw.op(pe, lambda e, b_=b_, c_=c_, lbk_=lbk_, s_=s_: e.matmul(
                    self.bank(lbk_), lhsT=kT[:, b_ * 128:(b_ + 1) * 128], rhs=qh[s_][:, c_ * 512:(c_ + 1) * 512],
                    start=True, stop=True), reads=[resb, qhb[s_]], writes=[self.pbank[lbk_]])
                lg_done[0] += 1

        idx = -1
        for hh in range(16):
            s = hh % 2
            slope = 2.0 ** (-(hh + 1) / 2.0)
            for c in range(4):
                tbk = 6
                nb = 4 * (c + 1)
                for b in range(nb):
                    idx += 1
                    emit_lg(idx + 2)
                    u = idx % NU
                    lbk = LGB[u]
                    off = 512 * c - 128 * b + 384
                    fw.op(dve, lambda e, u=u, off=off, lbk=lbk, slope=slope: e.scalar_tensor_tensor(
                        out=tmp[u], in0=A1[:, off:off + 512], scalar=slope, in1=self.bank(lbk), op0=ALU.mult, op1=ALU.add),
                        reads=[a1b, self.pbank[lbk]], writes=[tmpb[u]])
                    fw.op(act, lambda e, u=u: e.activation(out=pp[u], in_=tmp[u], func=AF.Exp), reads=[tmpb[u]],
                          writes=[ppb[u]])
                    fw.op(pool, lambda e, u=u, b=b, c=c: e.tensor_tensor(out=pm[u], in0=pp[u],
                                                                        in1=mrv[:, b, c * 512:(c + 1) * 512], op=ALU.mult),
                          reads=[ppb[u], mrb], writes=[pmb[u]])
                    for sub in range(4):
                        tt_ = 4 * c + sub
                        if b > tt_:
                            continue
                        obk = 2 + sub
                        fw.op(pe, lambda e, u=u, sub=sub, b=b, tt_=tt_, obk=obk: e.matmul(
                            self.bank(obk)[:, 0:129], lhsT=pm[u][:, sub * 128:(sub + 1) * 128],
                            rhs=vtv[:, b, 0:129], start=(b == 0), stop=(b == tt_)),
                            reads=[pmb[u], resb], writes=[self.pbank[obk]])
                for sub in range(4):
                    obk = 2 + sub
                    fw.op(dve, lambda e, obk=obk, sub=sub: e.reciprocal(out=rden[:, sub:sub + 1], in_=self.bank(obk)[:, 128:129]),
                          reads=[self.pbank[obk]], writes=[rdb])
                    fw.op(dve, lambda e, sub=sub, obk=obk: e.tensor_scalar(
                        out=on[:, sub * 128:(sub + 1) * 128], in0=self.bank(obk)[:, 0:128],
                        scalar1=rden[:, sub:sub + 1], scalar2=None, op0=ALU.mult),
                        reads=[self.pbank[obk], rdb], writes=[onb])
                tbb = self.bank(tbk, BF16)
                for sub in range(4):
                    fw.op(pe, lambda e, sub=sub, tbb=tbb: e.transpose(out=tbb[:, sub * 128:(sub + 1) * 128],
                                                                      in_=on[:, sub * 128:(sub + 1) * 128], identity=self.identb),
                          reads=[onb, self.cb], writes=[self.pbank[tbk]])
                fw.op(act, lambda e, s=s, c=c, tbb=tbb: e.activation(out=oth[s][:, c * 512:(c + 1) * 512], in_=tbb[:, 0:512],
                                                                      func=AF.Copy), reads=[self.pbank[tbk]], writes=[othb[s]])
            fw.dma(out=OT[hh], in_=oth[s], reads=[othb[s]], writes=[OTb])
        self.release(m3)
        self.release(m0)
        self.outproj_ln(OT, OTb, W["dsa_w_out"][0], src, src_buf, W["ln_g"][L, 0], W["ln_b"][L, 0], dst, dst_buf)

    def gdn(self, li, L, src, src_buf, W, dst, dst_buf):
        fw = self.fw
        dve, act, pool, pe = fw.dve, fw.act, fw.pool, fw.pe
        OT, OTb = self.dram["OT"], self.dbuf["OT"]
        w_in = W["gdn_w_in"][li]
        m0 = self.mark()
        A128 = lambda: self.sb(128)
        U2, B2, NEGM4 = A128(), A128(), self.sb(512)
        gcb = Buf("gdnconst")
        fw.op(pool, lambda e: e.memset(U2, 1.0), writes=[gcb])
        fw.op(pool, lambda e: e.affine_select(out=U2, in_=U2, pattern=[[1, 128]], compare_op=ALU.is_ge, fill=0.0,
                                              base=0, channel_multiplier=-1), reads=[gcb], writes=[gcb])
        fw.op(pool, lambda e: e.memset(U2[0:64, 64:128], 0.0), reads=[gcb], writes=[gcb])
        fw.op(pool, lambda e: e.memset(B2, 0.0), writes=[gcb])
        fw.op(pool, lambda e: e.memset(B2[0:64, 0:64], 1.0), reads=[gcb], writes=[gcb])
        fw.op(pool, lambda e: e.memset(B2[64:128, 64:128], 1.0), reads=[gcb], writes=[gcb])
        N4 = NEGM4.rearrange("p (u i) -> p u i", u=4)
        fw.op(pool, lambda e: e.memset(NEGM4, 0.0), writes=[gcb])
        fw.op(pool, lambda e: e.affine_select(out=N4, in_=N4, pattern=[[0, 4], [1, 128]], compare_op=ALU.is_ge, fill=NEG,
                                              base=0, channel_multiplier=-1), reads=[gcb], writes=[gcb])
        fw.op(pool, lambda e: e.memset(N4[0:64, :, 64:128], NEG), reads=[gcb], writes=[gcb])
        id4 = self.ident.unsqueeze(1).to_broadcast([128, 4, 128])
        ABt = self.sb(16 * 32)
        ABv = ABt.rearrange("p (a b) -> p a b", a=16)
        GT, GC, EGC, EKD, NGC, BT, NB = [self.sb(256) for _ in range(7)]
        v16 = lambda a: a.rearrange("p (a b) -> p a b", a=16)
        CW = self.sb(48 * 4)
        CWv = CW.rearrange("p (c j) -> p c j", j=4)
        NGrow = self.sb(128)
        gb = Buf("gdn_g")
        self.load_row(NGrow, W["gdn_norm_g"][li], gb, 128)
        arow = self.sb(32)
        self.load_row(arow[:, 0:16], W["gdn_a_log"][li], gb, 16)
        self.load_row(arow[:, 16:32], W["gdn_dt_bias"][li], gb, 16)
        cwrow = self.sb(6144)
        fw.dma(out=cwrow[0:4, :], in_=W["gdn_conv_w"][li], writes=[gb])
        for c in range(48):
            fw.op(pe, lambda e, c=c: e.transpose(out=self.bank(4)[:, c * 4:(c + 1) * 4], in_=cwrow[0:4, c * 128:(c + 1) * 128],
                                                 identity=self.ident[0:4, 0:4]), reads=[gb, self.cb], writes=[self.pbank[4]])
        fw.op(dve, lambda e: e.tensor_copy(out=CW, in_=self.bank(4)[:, 0:192]), reads=[self.pbank[4]], writes=[gb])
        self.off -= 6144 + 0
        fw.barrier()
        big = self.sb(16 * T, BF16)
        bigv = big.rearrange("p (a b) -> p a b", a=16)
        xTb = Buf("xT")
        self.build_xT(src, src_buf, bigv, xTb)
        mg = self.mark()
        wab = self.sb(16 * 32, BF16)
        wabv = wab.rearrange("p (a b) -> p a b", a=16)
        wabb = Buf()
        fw.dma(out=wabv, in_=w_in[:, 8192:8224].rearrange("(kc p) f -> p kc f", p=128), writes=[wabb], q=pool)
        for i in range(NT):
            bk = 4 + i % 4
            for kc in range(16):
                fw.op(pe, lambda e, i=i, kc=kc, bk=bk: e.matmul(self.bank(bk)[:, 0:32], lhsT=bigv[:, kc, i * 128:(i + 1) * 128],
                                                               rhs=wabv[:, kc, :], start=(kc == 0), stop=(kc == 15)),
                      reads=[xTb, wabb], writes=[self.pbank[bk]])
            fw.op(act, lambda e, i=i, bk=bk: e.activation(out=ABv[:, i, :], in_=self.bank(bk)[:, 0:32], func=AF.Copy),
                  reads=[self.pbank[bk]], writes=[gb])
        t1, t2, t3 = self.sb(256), self.sb(256), self.sb(256)
        nea = self.sb(16)
        dtb_b = arow[:, 16:32].unsqueeze(1).to_broadcast([128, 16, 16])
        fw.op(dve, lambda e: e.tensor_tensor(out=v16(t1), in0=ABv[:, :, 0:16], in1=dtb_b, op=ALU.add), reads=[gb], writes=[gb])
        fw.op(dve, lambda e: e.tensor_scalar(out=t2, in0=t1, scalar1=-1.0, scalar2=None, op0=ALU.mult), reads=[gb], writes=[gb])
        fw.op(dve, lambda e: e.tensor_tensor(out=t2, in0=t2, in1=t1, op=ALU.max), reads=[gb], writes=[gb])
        fw.op(act, lambda e: e.activation(out=t2, in_=t2, func=AF.Exp, scale=-1.0), reads=[gb], writes=[gb])
        fw.op(dve, lambda e: e.tensor_scalar(out=t2, in0=t2, scalar1=1.0, scalar2=None, op0=ALU.add), reads=[gb], writes=[gb])
        fw.op(act, lambda e: e.activation(out=t2, in_=t2, func=AF.Ln), reads=[gb], writes=[gb])
        fw.op(dve, lambda e: e.scalar_tensor_tensor(out=t3, in0=t1, scalar=0.0, in1=t2, op0=ALU.max, op1=ALU.add),
              reads=[gb], writes=[gb])
        fw.op(act, lambda e: e.activation(out=nea, in_=arow[:, 0:16], func=AF.Exp), reads=[gb], writes=[gb])
        fw.op(dve, lambda e: e.tensor_scalar(out=nea, in0=nea, scalar1=-1.0, scalar2=None, op0=ALU.mult), reads=[gb], writes=[gb])
        fw.op(dve, lambda e: e.tensor_tensor(out=v16(GT), in0=v16(t3), in1=nea.unsqueeze(1).to_broadcast([128, 16, 16]),
                                             op=ALU.mult), reads=[gb], writes=[gb])
        fw.op(act, lambda e: e.activation(out=v16(BT), in_=ABv[:, :, 16:32], func=AF.Sigmoid), reads=[gb], writes=[gb])
        fw.op(dve, lambda e: e.tensor_scalar(out=NB, in0=BT, scalar1=-1.0, scalar2=None, op0=ALU.mult), reads=[gb], writes=[gb])
        fw.op(pe, lambda e: e.matmul(self.bank(4)[:, 0:256], lhsT=U2, rhs=GT, start=True, stop=True), reads=[gb, gcb],
              writes=[self.pbank[4]])
        fw.op(pe, lambda e: e.matmul(self.bank(5)[:, 0:256], lhsT=B2, rhs=GT, start=True, stop=True), reads=[gb, gcb],
              writes=[self.pbank[5]])
        fw.op(dve, lambda e: e.tensor_copy(out=GC, in_=self.bank(4)[:, 0:256]), reads=[self.pbank[4]], writes=[gb])
        fw.op(act, lambda e: e.activation(out=EGC, in_=self.bank(4)[:, 0:256], func=AF.Exp), reads=[self.pbank[4]], writes=[gb])
        fw.op(dve, lambda e: e.tensor_tensor(out=t1, in0=self.bank(5)[:, 0:256], in1=GC, op=ALU.subtract),
              reads=[self.pbank[5], gb], writes=[gb])
        fw.op(act, lambda e: e.activation(out=EKD, in_=t1, func=AF.Exp), reads=[gb], writes=[gb])
        fw.op(dve, lambda e: e.tensor_scalar(out=NGC, in0=GC, scalar1=-1.0, scalar2=None, op0=ALU.mult), reads=[gb], writes=[gb])
        self.release(mg)
        GCv, EGCv, EKDv, NGCv, BTv, NBv = [v16(a) for a in (GC, EGC, EKD, NGC, BT, NB)]
        wt = [self.sb(16 * 128, BF16) for _ in range(2)]
        wtb = [Buf(), Buf()]
        qT, kT, vT = [self.sb(T).bitcast(BF16)[:, 0:T] for _ in range(3)]
        qkvb = Buf("qkv")
        ktok_f, vtok_f, otok = self.sb(T), self.sb(T), self.sb(T)
        ktok, vtok = ktok_f.bitcast(BF16)[:, 0:T], vtok_f.bitcast(BF16)[:, 0:T]
        ktv, vtv, otv = [a.rearrange("p (a b) -> p a b", a=16) for a in (ktok, vtok, otok)]
        tokb = Buf("tok")
        otb = Buf("otok")
        regA = self.sb(8192)
        raw = regA[:, 0:2056]
        cacc = regA[:, 2056:2056 + 2048]
        tmpq = regA[:, 4104:4104 + 2048]
        rb = Buf("raw")
        _f = lambda w0: regA[:, w0:w0 + 512]
        _h = lambda w0: regA[:, w0:w0 + 256].bitcast(BF16)[:, 0:512]
        shared = {"DG4": _f(0), "Dt4": _f(512)}
        for i_, n_ in enumerate(("Mt4", "Nn4", "Qa", "Qta", "Qb", "Qtb", "X4", "KG4")):
            shared[n_] = _h(3072 + 256 * i_)
        sharedB = {n_: Buf(n_) for n_ in shared}
        QSET = []
        for st_ in range(2):
            d_ = dict(shared)
            b_ = dict(sharedB)
            d_["EGR4"] = _f(1024 + 512 * st_)
            d_["BU4"] = _f(2048 + 512 * st_)
            for i_, n_ in enumerate(("AT4", "KD4", "WT4", "QD4")):
                d_[n_] = _h(5120 + 512 * i_ + 256 * st_)
            for n_ in ("EGR4", "BU4", "AT4", "KD4", "WT4", "QD4"):
                b_[n_] = Buf(n_ + str(st_))
            QSET.append((d_, b_))
        q3 = lambda a: a.rearrange("p (u i) -> p u i", u=4)
        VN = self.sb(128, BF16)
        vnb = Buf("VN")
        S = self.sb(128)
        Sb = Buf("S")
        Sbf = self.sb(128, BF16)
        Sbfb = Buf("Sbf")
        oth = [self.sb(T, BF16) for _ in range(2)]
        othb = [Buf(), Buf()]
        sm = self.sb(64)
        smb = Buf()
        fw.op(dve, lambda e: e.memset(raw[:, 0:3], 0.0), writes=[rb])
        nbk = [0]

        def nb_():
            nbk[0] += 1
            return nbk[0] % 8

        for h in range(16):
            fw.barrier()
            kinds = (("q", 0, qT), ("k", 2048, kT), ("v", 4096, vT))

            def emit_proj(n_):
                s_ = nbk[0] % 2
                nbk[0] += 1
                c0_ = kinds[n_][1]
                self.proj_fm(bigv, xTb, w_in[:, c0_ + h * 128: c0_ + (h + 1) * 128], wt[s_], wtb[s_])

            emit_proj(0)
            for kn, (kind, col0, dstT) in enumerate(kinds):
                for tc in range(4):
                    dstp = raw[:, 3 + tc * 512: 3 + (tc + 1) * 512]
                    if tc % 2 == 0:
                        fw.op(dve, lambda e, dstp=dstp, tc=tc: e.tensor_copy(out=dstp, in_=self.bank(tc)),
                              reads=[self.pbank[tc]], writes=[rb])
                    else:
                        fw.op(act, lambda e, dstp=dstp, tc=tc: e.activation(out=dstp, in_=self.bank(tc), func=AF.Copy),
                              reads=[self.pbank[tc]], writes=[rb])
                if kn + 1 < 3:
                    emit_proj(kn + 1)
                ch = (col0 // 128) + h
                fw.op(dve, lambda e, ch=ch: e.tensor_scalar(out=cacc, in0=raw[:, 0:T], scalar1=CWv[:, ch, 0:1], scalar2=None,
                                                            op0=ALU.mult), reads=[rb, gb], writes=[rb])
                for j in range(1, 4):
                    fw.op(dve, lambda e, ch=ch, j=j: e.scalar_tensor_tensor(
                        out=cacc, in0=raw[:, j:j + T], scalar=CWv[:, ch, j:j + 1], in1=cacc, op0=ALU.mult, op1=ALU.add),
                        reads=[rb, gb], writes=[rb])
                if kind == "v":
                    fw.op(act, lambda e: e.activation(out=vT, in_=cacc, func=AF.Silu), reads=[rb], writes=[qkvb])
                    continue
                fw.op(act, lambda e: e.activation(out=tmpq, in_=cacc, func=AF.Silu), reads=[rb], writes=[rb])
                sq16 = raw[:, 8:8 + T // 2].bitcast(BF16)
                fw.op(act, lambda e: e.activation(out=sq16, in_=tmpq, func=AF.Square), reads=[rb], writes=[rb])
                for tc in range(4):
                    fw.op(pe, lambda e, tc=tc: e.matmul(self.bank(4 + tc), lhsT=self.onesb, rhs=sq16[:, tc * 512:(tc + 1) * 512],
                                                        start=True, stop=True), reads=[rb, self.cb], writes=[self.pbank[4 + tc]])
                    cs_ = cacc[:, tc * 512:(tc + 1) * 512]
                    fw.op(dve, lambda e, tc=tc, cs_=cs_: e.tensor_scalar(out=cs_, in0=self.bank(4 + tc), scalar1=RMS_EPS,
                                                                         scalar2=None, op0=ALU.add),
                          reads=[self.pbank[4 + tc], rb], writes=[rb])
                fw.op(act, lambda e: e.activation(out=cacc, in_=cacc, func=AF.Sqrt), reads=[rb], writes=[rb])
                fw.op(dve, lambda e: e.reciprocal(out=cacc, in_=cacc), reads=[rb], writes=[rb])
                sc_ = (128.0 ** -0.5) if kind == "q" else 1.0
                fw.op(dve, lambda e, dstT=dstT, sc_=sc_: e.scalar_tensor_tensor(out=dstT, in0=tmpq, scalar=sc_, in1=cacc,
                                                                                op0=ALU.mult, op1=ALU.mult),
                      reads=[rb], writes=[qkvb])
            for srcT, dv_ in ((kT, ktv), (vT, vtv)):
                for g in range(4):
                    bk = nb_()
                    bkb = self.bank(bk, BF16)
                    for j in range(4):
                        P_ = g * 4 + j
                        fw.op(pe, lambda e, srcT=srcT, P_=P_, j=j, bkb=bkb: e.transpose(
                            out=bkb[:, j * 128:(j + 1) * 128], in_=srcT[:, P_ * 128:(P_ + 1) * 128],
                            identity=self.identb), reads=[qkvb, self.cb], writes=[self.pbank[bk]])
                    fw.op(act, lambda e, dv_=dv_, g=g, bkb=bkb: e.activation(
                        out=dv_[:, g * 4:(g + 1) * 4, :], in_=bkb[:, 0:512].rearrange("p (a b) -> p a b", a=4), func=AF.Copy),
                        reads=[self.pbank[bk]], writes=[tokb])
            fw.barrier()
            fw.op(dve, lambda e: e.memset(S, 0.0), writes=[Sb])
            fw.op(dve, lambda e: e.memset(Sbf, 0.0), writes=[Sbfb])
            def prep_gen(Q, L_, QB):
                    cols4 = slice(Q * 512, (Q + 1) * 512)
                    P0 = Q * 4
                    bc4 = lambda a: a[:, P0:P0 + 4, h].unsqueeze(2).to_broadcast([128, 4, 128])
                    fw.op(pool, lambda e: e.tensor_tensor(out=q3(L_["DG4"]), in0=id4, in1=bc4(GCv), op=ALU.mult),
                          reads=[gb, self.cb], writes=[QB["DG4"]])
                    yield
                    bA, bB = nb_(), nb_()
                    fw.op(pe, lambda e: e.matmul(self.bank(bA), lhsT=self.ones, rhs=L_["DG4"], start=True, stop=True),
                          reads=[QB["DG4"], self.cb], writes=[self.pbank[bA]])
                    fw.op(pe, lambda e: e.matmul(self.bank(bB), lhsT=self.ones, rhs=L_["DG4"], start=True, stop=False),
                          reads=[QB["DG4"], self.cb], writes=[self.pbank[bB]])
                    fw.op(pe, lambda e: e.matmul(self.bank(bB), lhsT=self.ident, rhs=NEGM4, start=False, stop=True),
                          reads=[gcb, self.cb], writes=[self.pbank[bB]])
                    fw.op(act, lambda e: e.activation(out=L_["EGR4"], in_=self.bank(bA), func=AF.Exp), reads=[self.pbank[bA]],
                          writes=[QB["EGR4"]])
                    yield
                    for u in range(4):
                        fw.op(act, lambda e, u=u: e.activation(out=L_["Dt4"][:, u * 128:(u + 1) * 128],
                                                               in_=self.bank(bB)[:, u * 128:(u + 1) * 128], func=AF.Exp,
                                                               bias=NGCv[:, P0 + u, h:h + 1], scale=1.0),
                              reads=[self.pbank[bB], gb], writes=[QB["Dt4"]])
                        yield
                    bK, bQ = nb_(), nb_()
                    for u in range(4):
                        cu = slice((P0 + u) * 128, (P0 + u + 1) * 128)
                        fw.op(pe, lambda e, u=u, cu=cu: e.matmul(self.bank(bK)[:, u * 128:(u + 1) * 128], lhsT=kT[:, cu], rhs=kT[:, cu],
                                                                 start=True, stop=True), reads=[qkvb], writes=[self.pbank[bK]])
                    for u in range(4):
                        cu = slice((P0 + u) * 128, (P0 + u + 1) * 128)
                        fw.op(pe, lambda e, u=u, cu=cu: e.matmul(self.bank(bQ)[:, u * 128:(u + 1) * 128], lhsT=kT[:, cu], rhs=qT[:, cu],
                                                                 start=True, stop=True), reads=[qkvb], writes=[self.pbank[bQ]])
                    fw.op(dve, lambda e: e.tensor_tensor(out=q3(L_["Mt4"]), in0=self.bank(bK).rearrange("p (u i) -> p u i", u=4),
                                                         in1=bc4(BTv), op=ALU.mult), reads=[self.pbank[bK], gb], writes=[QB["Mt4"]])
                    yield
                    fw.op(dve, lambda e: e.tensor_tensor(out=L_["Mt4"], in0=L_["Mt4"], in1=L_["Dt4"], op=ALU.mult),
                          reads=[QB["Dt4"], QB["Mt4"]], writes=[QB["Mt4"]])
                    yield
                    fw.op(pool, lambda e: e.affine_select(out=q3(L_["Mt4"]), in_=q3(L_["Mt4"]), pattern=[[0, 4], [1, 128]],
                                                          compare_op=ALU.not_equal, fill=0.0, base=0, channel_multiplier=-1),
                          reads=[QB["Mt4"]], writes=[QB["Mt4"]])
                    yield
                    fw.op(dve, lambda e: e.tensor_tensor(out=L_["AT4"], in0=self.bank(bQ), in1=L_["Dt4"], op=ALU.mult),
                          reads=[self.pbank[bQ], QB["Dt4"]], writes=[QB["AT4"]])
                    yield
                    bN = nb_()
                    bNb = self.bank(bN, BF16)
                    for u in range(4):
                        fw.op(pe, lambda e, u=u: e.transpose(out=bNb[:, u * 128:(u + 1) * 128],
                                                             in_=L_["Mt4"][:, u * 128:(u + 1) * 128], identity=self.identb),
                              reads=[QB["Mt4"], self.cb], writes=[self.pbank[bN]])
                    fw.op(act, lambda e: e.activation(out=L_["Nn4"], in_=bNb[:, 0:512], func=AF.Copy), reads=[self.pbank[bN]],
                          writes=[QB["Nn4"]])
                    yield
                    fw.op(pool, lambda e: e.tensor_tensor(out=q3(L_["X4"]), in0=id4, in1=q3(L_["Mt4"]), op=ALU.subtract),
                          reads=[QB["Mt4"], self.cb], writes=[QB["X4"]])
                    yield
                    Qn, Qtn = "Mt4", "Nn4"
                    pp_ = [("Qa", "Qta"), ("Qb", "Qtb")]
                    for lvl in range(1, 6):
                        Qo, Qto = pp_[lvl % 2]
                        bt = nb_()
                        for u in range(4):
                            us = slice(u * 128, (u + 1) * 128)
                            fw.op(pe, lambda e, us=us, Qn=Qn, Qtn=Qtn, bt=bt: e.matmul(self.bank(bt)[:, us], lhsT=L_[Qn][:, us],
                                                                                       rhs=L_[Qtn][:, us], start=True, stop=True),
                                  reads=[QB[Qn], QB[Qtn]], writes=[self.pbank[bt]])
                        fw.op(act, lambda e, Qto=Qto, bt=bt: e.activation(out=L_[Qto], in_=self.bank(bt), func=AF.Copy),
                              reads=[self.pbank[bt]], writes=[QB[Qto]])
                        yield
                        if lvl < 5:
                            bq = nb_()
                            for u in range(4):
                                us = slice(u * 128, (u + 1) * 128)
                                fw.op(pe, lambda e, us=us, Qn=Qn, Qtn=Qtn, bq=bq: e.matmul(self.bank(bq)[:, us], lhsT=L_[Qtn][:, us],
                                                                                           rhs=L_[Qn][:, us], start=True, stop=True),
                                      reads=[QB[Qn], QB[Qtn]], writes=[self.pbank[bq]])
                            fw.op(dve, lambda e, Qo=Qo, bq=bq: e.tensor_copy(out=L_[Qo], in_=self.bank(bq)),
                                  reads=[self.pbank[bq]], writes=[QB[Qo]])
                            yield
                        bx = nb_()
                        for u in range(4):
                            us = slice(u * 128, (u + 1) * 128)
                            fw.op(pe, lambda e, us=us, Qto=Qto, bx=bx: e.matmul(self.bank(bx)[:, us], lhsT=L_[Qto][:, us],
                                                                                rhs=L_["X4"][:, us], start=True, stop=True),
                                  reads=[QB[Qto], QB["X4"]], writes=[self.pbank[bx]])
                        fw.op(dve, lambda e, bx=bx: e.tensor_tensor(out=L_["X4"], in0=L_["X4"], in1=self.bank(bx), op=ALU.add),
                              reads=[self.pbank[bx], QB["X4"]], writes=[QB["X4"]])
                        yield
                        Qn, Qtn = Qo, Qto
                    fw.op(pool, lambda e: e.tensor_tensor(out=q3(L_["KG4"]), in0=ktv[:, P0:P0 + 4, :], in1=bc4(EGCv), op=ALU.mult),
                          reads=[tokb, gb], writes=[QB["KG4"]])
                    yield
                    fw.op(pool, lambda e: e.tensor_tensor(out=q3(L_["KD4"]), in0=ktv[:, P0:P0 + 4, :], in1=bc4(EKDv), op=ALU.mult),
                          reads=[tokb, gb], writes=[QB["KD4"]])
                    yield
                    bW, bU = nb_(), nb_()
                    for u in range(4):
                        us = slice(u * 128, (u + 1) * 128)
                        fw.op(pe, lambda e, us=us: e.matmul(self.bank(bW)[:, us], lhsT=L_["KG4"][:, us], rhs=L_["X4"][:, us],
                                                            start=True, stop=True), reads=[QB["KG4"], QB["X4"]], writes=[self.pbank[bW]])
                    for u in range(4):
                        us = slice(u * 128, (u + 1) * 128)
                        fw.op(pe, lambda e, us=us, u=u: e.matmul(self.bank(bU)[:, us], lhsT=L_["X4"][:, us], rhs=vtv[:, P0 + u, :],
                                                                 start=True, stop=True), reads=[QB["X4"], tokb], writes=[self.pbank[bU]])
                    fw.op(act, lambda e: e.activation(out=L_["WT4"], in_=self.bank(bW), func=AF.Copy), reads=[self.pbank[bW]],
                          writes=[QB["WT4"]])
                    yield
                    fw.op(dve, lambda e: e.tensor_tensor(out=q3(L_["BU4"]), in0=self.bank(bU).rearrange("p (u i) -> p u i", u=4),
                                                         in1=bc4(BTv), op=ALU.mult), reads=[self.pbank[bU], gb], writes=[QB["BU4"]])
                    yield
                    fw.op(dve, lambda e: e.tensor_tensor(out=L_["QD4"], in0=qT[:, cols4], in1=L_["EGR4"], op=ALU.mult),
                          reads=[qkvb, QB["EGR4"]], writes=[QB["QD4"]])
                    yield
                    yield

            def chunk_gen(Q, L_, QB):
                    P0 = Q * 4
                    for u in range(4):
                        us = slice(u * 128, (u + 1) * 128)
                        P_ = P0 + u
                        for c in range(2):
                            rows = slice(c * 64, (c + 1) * 64)
                            bw = nb_()
                            fw.op(pe, lambda e, us=us, bw=bw: e.matmul(self.bank(bw)[:, 0:128], lhsT=L_["WT4"][:, us], rhs=Sbf,
                                                                       start=True, stop=True), reads=[QB["WT4"], Sbfb],
                                  writes=[self.pbank[bw]])
                            fw.op(dve, lambda e, rows=rows, bw=bw, P_=P_, us=us: e.scalar_tensor_tensor(
                                out=VN[rows, :], in0=self.bank(bw)[rows, 0:128], scalar=NBv[rows, P_, h:h + 1],
                                in1=L_["BU4"][rows, us], op0=ALU.mult, op1=ALU.add),
                                reads=[self.pbank[bw], gb, QB["BU4"]], writes=[vnb])
                            yield
                            bo = nb_()
                            fw.op(pe, lambda e, us=us, bo=bo: e.matmul(self.bank(bo)[:, 0:128], lhsT=L_["QD4"][:, us], rhs=Sbf,
                                                                       start=True, stop=False), reads=[QB["QD4"], Sbfb],
                                  writes=[self.pbank[bo]])
                            fw.op(pe, lambda e, us=us, bo=bo, rows=rows: e.matmul(self.bank(bo)[:, 0:128], lhsT=L_["AT4"][rows, us],
                                                                                  rhs=VN[rows, :], start=False, stop=True),
                                  reads=[QB["AT4"], vnb], writes=[self.pbank[bo]])
                            fw.op(act, lambda e, rows=rows, bo=bo, P_=P_: e.activation(out=otv[rows, P_, :], in_=self.bank(bo)[rows, 0:128],
                                                                                       func=AF.Copy), reads=[self.pbank[bo]], writes=[otb])
                            yield
                            bs = nb_()
                            fw.op(pe, lambda e, us=us, bs=bs, rows=rows: e.matmul(self.bank(bs)[:, 0:128], lhsT=L_["KD4"][rows, us],
                                                                                  rhs=VN[rows, :], start=True, stop=True),
                                  reads=[QB["KD4"], vnb], writes=[self.pbank[bs]])
                            cdc = u * 128 + c * 64 + 63
                            fw.op(dve, lambda e, bs=bs, cdc=cdc: e.scalar_tensor_tensor(
                                out=S, in0=S, scalar=L_["EGR4"][:, cdc:cdc + 1], in1=self.bank(bs)[:, 0:128], op0=ALU.mult, op1=ALU.add),
                                reads=[self.pbank[bs], QB["EGR4"], Sb], writes=[Sb])
                            yield
                            fw.op(act, lambda e: e.activation(out=Sbf, in_=S, func=AF.Copy), reads=[Sb], writes=[Sbfb])
                            yield
                    yield

            for _ in prep_gen(0, QSET[0][0], QSET[0][1]):
                pass
            for Q in range(4):
                gc_ = chunk_gen(Q, QSET[Q % 2][0], QSET[Q % 2][1])
                gp_ = prep_gen(Q + 1, QSET[(Q + 1) % 2][0], QSET[(Q + 1) % 2][1]) if Q < 3 else iter(())
                alive_c = alive_p = True
                while alive_c or alive_p:
                    if alive_c:
                        try:
                            next(gc_)
                        except StopIteration:
                            alive_c = False
                    if alive_p:
                        try:
                            next(gp_)
                        except StopIteration:
                            alive_p = False
            fw.barrier()
            zs = ktok_f
            zsv = zs.rearrange("p (a b) -> p a b", a=16)
            zb = Buf("zs")
            s = nbk[0] % 2
            nbk[0] += 1
            wzv = wt[s].rearrange("p (a b) -> p a b", a=16)
            fw.dma(out=wzv, in_=w_in[:, 6144 + h * 128: 6144 + (h + 1) * 128].rearrange("(kc p) f -> p kc f", p=128),
                   writes=[wtb[s]], q=pool)
            for g in range(4):
                bk = g
                for j in range(4):
                    i = g * 4 + j
                    for kc in range(16):
                        fw.op(pe, lambda e, i=i, j=j, kc=kc, bk=bk: e.matmul(
                            self.bank(bk)[:, j * 128:(j + 1) * 128], lhsT=bigv[:, kc, i * 128:(i + 1) * 128], rhs=wzv[:, kc, :],
                            start=(kc == 0), stop=(kc == 15)), reads=[xTb, wtb[s]], writes=[self.pbank[bk]])
                fw.op(act, lambda e, g=g, bk=bk: e.activation(out=zsv[:, g * 4:(g + 1) * 4, :],
                                                              in_=self.bank(bk).rearrange("p (a b) -> p a b", a=4), func=AF.Silu),
                      reads=[self.pbank[bk]], writes=[zb])
            sq = regA[:, 0:T]
            sqb = Buf("sq")
            fw.op(pool, lambda e: e.tensor_tensor(out=sq, in0=otok, in1=otok, op=ALU.mult), reads=[otb], writes=[sqb])
            ms = sm[:, 0:16]
            fw.op(dve, lambda e: e.tensor_reduce(out=ms, in_=sq.rearrange("p (a b) -> p a b", a=16), axis=AX.X, op=ALU.add),
                  reads=[sqb], writes=[smb])
            fw.op(dve, lambda e: e.tensor_scalar(out=ms, in0=ms, scalar1=1.0 / 128.0, scalar2=RMS_EPS, op0=ALU.mult, op1=ALU.add),
                  reads=[smb], writes=[smb])
            fw.op(act, lambda e: e.activation(out=ms, in_=ms, func=AF.Sqrt), reads=[smb], writes=[smb])
            fw.op(dve, lambda e: e.reciprocal(out=ms, in_=ms), reads=[smb], writes=[smb])
            fw.op(dve, lambda e: e.tensor_tensor(out=otv, in0=otv, in1=ms.unsqueeze(2).to_broadcast([128, 16, 128]), op=ALU.mult),
                  reads=[smb, otb], writes=[otb])
            fw.op(pool, lambda e: e.tensor_tensor(out=otv, in0=otv, in1=NGrow.unsqueeze(1).to_broadcast([128, 16, 128]),
                                                  op=ALU.mult), reads=[gb, otb], writes=[otb])
            fw.op(dve, lambda e: e.tensor_tensor(out=otok, in0=otok, in1=zs, op=ALU.mult), reads=[zb, otb], writes=[otb])
            s2 = h % 2
            for g in range(4):
                bk = 4 + g
                for j in range(4):
                    P_ = g * 4 + j
                    fw.op(pe, lambda e, P_=P_, j=j, bk=bk: e.transpose(out=self.bank(bk)[:, j * 128:(j + 1) * 128], in_=otv[:, P_, :],
                                                                      identity=self.ident), reads=[otb, self.cb], writes=[self.pbank[bk]])
                fw.op(act, lambda e, g=g, bk=bk, s2=s2: e.activation(out=oth[s2][:, g * 512:(g + 1) * 512], in_=self.bank(bk),
                                                                    func=AF.Copy), reads=[self.pbank[bk]], writes=[othb[s2]])
            fw.dma(out=OT[h], in_=oth[s2], reads=[othb[s2]], writes=[OTb])
        self.release(m0)
        self.outproj_ln(OT, OTb, W["gdn_w_out"][li], src, src_buf, W["ln_g"][L, 0], W["ln_b"][L, 0], dst, dst_buf)

    def init_yg(self):
        fw = self.fw
        m = self.mark()
        z = self.sb(D)
        zb = Buf()
        fw.op(fw.dve, lambda e: e.memset(z, 0.0), writes=[zb])
        fw.dma(out=self.dram["YG"][NSLOT:NSLOT + 128, :], in_=z, reads=[zb], writes=[self.dbuf["YG"]])
        self.release(m)


WEIGHT_SPECS = [
    ("ln_g", (4, 2, 2048)), ("ln_b", (4, 2, 2048)), ("moe_rg_w", (4, 2048, 4)), ("moe_rg_b", (4, 4)),
    ("moe_re_w", (4, 2048, 32)), ("moe_re_b", (4, 32)), ("moe_w_gate", (4, 32, 2048, 512)),
    ("moe_w_up", (4, 32, 2048, 512)), ("moe_w_down", (4, 32, 512, 2048)), ("gdn_w_in", (2, 2048, 8224)),
    ("gdn_conv_w", (2, 4, 6144)), ("gdn_a_log", (2, 16)), ("gdn_dt_bias", (2, 16)), ("gdn_norm_g", (2, 128)),
    ("gdn_w_out", (2, 2048, 2048)), ("ssm_w_in", (1, 2048, 2048)), ("ssm_b_re", (1, 128, 64, 16)),
    ("ssm_b_im", (1, 128, 64, 16)), ("ssm_c_re", (1, 128, 16, 64)), ("ssm_c_im", (1, 128, 16, 64)),
    ("ssm_a_re", (1, 128, 64)), ("ssm_a_im", (1, 128, 64)), ("ssm_log_dt", (1, 128)), ("ssm_d", (1, 2048)),
    ("ssm_w_glu", (1, 2048, 2048)), ("ssm_b_glu", (1, 2048)), ("ssm_w_out", (1, 2048, 2048)),
    ("dsa_w_in", (1, 2048, 4496)), ("dsa_w_out", (1, 2048, 2048)),
]


def build(stages, ext_in=(), ext_out=(), weights=None):
    nc = bass.Bass("TRN2", target_bir_lowering=False)
    st = contextlib.ExitStack()
    with st:
        k = K(nc, st, set(ext_in), set(ext_out))
        fw = k.fw
        used = weights if weights is not None else [n for n, _ in WEIGHT_SPECS]
        W = {}
        for n, shp in WEIGHT_SPECS:
            if n in used:
                W[n] = nc.dram_tensor(n, list(shp), F32, kind="ExternalInput").ap()
        k.W = W
        names = set()
        for stg in stages:
            names.update(stg[2:])
        if "x" in names:
            k.dram["x"] = nc.dram_tensor("x", [T, D], F32, kind="ExternalInput").ap()
            k.dbuf["x"] = Buf("x")
        k.dram["out"] = nc.dram_tensor("out", [T, D], F32, kind="ExternalOutput").ap()
        k.dbuf["out"] = Buf("out")
        k.dt("XA", [T, D], F32)
        k.dt("XM", [T, D], F32)
        k.dt("OT", [16, 128, T], BF16)
        k.dt("XG", [NSLOT + 128, D], BF16)
        k.dt("YG", [NSLOT + 128, D], F32)
        k.dt("U", [16, 128, T], F32)
        k.dt("YA", [16, 128, T], BF16)
        k.dt("QT", [16, 128, T], BF16)
        k.dt("QI", [16, 128, T], BF16)
        k.dt("MASKT", [16, 128, T], BF16)
        k.init_yg()
        for stg in stages:
            kind, L, src, dst = stg
            S, Sb, Dd, Db = k.dram[src], k.dbuf[src], k.dram[dst], k.dbuf[dst]
            if kind == "moe":
                k.moe(L, S, Sb, W, Dd, Db)
            elif kind == "gdn":
                k.gdn(L // 3, L, S, Sb, W, Dd, Db)
            elif kind == "s5":
                k.s5(L, S, Sb, W, Dd, Db)
            elif kind == "dsa":
                k.dsa(L, S, Sb, W, Dd, Db)
            else:
                raise ValueError(kind)
        fw.finish()
        k.stats = {e.name: e.n_instr for e in fw.engs}
    return nc, k


_MIXERS = ("gdn", "s5", "dsa")


def _stages():
    st = []
    for L in range(DEPTH):
        src = "x" if L == 0 else "XA"
        st.append((_MIXERS[L % 3], L, src, "XM"))
        st.append(("moe", L, "XM", "out" if L == DEPTH - 1 else "XA"))
    return st


def kernel(**inputs):
    x = np.ascontiguousarray(np.asarray(inputs["x"], dtype=np.float32))
    nc, _k = build(_stages())
    wts = {n: np.ascontiguousarray(np.asarray(inputs[n], dtype=np.float32)) for n, _ in WEIGHT_SPECS}
    n_cores = 8
    in_maps = []
    for c in range(n_cores):
        m = dict(wts)
        m["x"] = x[c]
        in_maps.append(m)
    res = run_bass_kernel_spmd(nc, in_maps, core_ids=list(range(n_cores)))
    return np.stack([np.asarray(r["out"], dtype=np.float32) for r in res.results], axis=0)
```
